# Optimizing a Trainium2 kernel written in Bass

```python
import math
import jax, jax.numpy as jnp
from jax import lax
import numpy as np

D_MODEL = 1024
BATCH = 2
SEQ = 16384
DEPTH = 2

HEAD_DIM = 64
N_Q_HEADS = D_MODEL // HEAD_DIM
N_KV_HEADS = 4
GQA_REP = N_Q_HEADS // N_KV_HEADS
ROPE_DIM = HEAD_DIM // 4
ROPE_THETA = 500000.0
ATTN_SCALE = HEAD_DIM ** -0.5
Q_BLOCK = 128
KV_COLS = N_KV_HEADS * HEAD_DIM
N_BRANCH = 3
CMP_LEN = 32
CMP_STRIDE = 16
CMP_HIDDEN = 256
SEL_LEN = 64
N_SELECT = 16
WIN_LEN = 512
FORCE_SCORE = 1.0e4
NSA_COLS = D_MODEL + N_BRANCH * 2 * KV_COLS + N_Q_HEADS * N_BRANCH
DIL_PATTERNS = ((128, 1), (512, 4), (2048, 16))
DIL_BLOCK = 128
DIL_COLS = D_MODEL + len(DIL_PATTERNS) * 2 * KV_COLS
N_EXPERTS = 32
N_GROUPS = 4
EXPERTS_PER_GROUP = N_EXPERTS // N_GROUPS
TOP_K = 2
D_EXPERT = 512
MOE_BLOCK = 128
DN_ALPHA = (2.0 * DEPTH) ** 0.25
DN_BETA = (8.0 * DEPTH) ** -0.25
LN_EPS = 1e-5
N_A = (DEPTH + 1) // 2
N_B = DEPTH // 2

kernel_name = 'hybrid_nsa_dilated_grouped_moe_deepnorm'


def layer_norm(x, g, b):
    xf = x.astype(jnp.float32)
    mu = xf.mean(-1, keepdims=True)
    var = jnp.square(xf - mu).mean(-1, keepdims=True)
    return ((xf - mu) * lax.rsqrt(var + LN_EPS) * g.astype(jnp.float32) + b.astype(jnp.float32)).astype(x.dtype)


def partial_rope(t, pos):
    half = ROPE_DIM // 2
    inv = ROPE_THETA ** (-jnp.arange(half, dtype=jnp.float32) * (2.0 / ROPE_DIM))
    ang = pos.astype(jnp.float32)[:, :, None, None] * inv
    cos, sin = jnp.cos(ang), jnp.sin(ang)
    tf = t.astype(jnp.float32)
    t1, t2, rest = tf[..., :half], tf[..., half:ROPE_DIM], tf[..., ROPE_DIM:]
    return jnp.concatenate([t1 * cos - t2 * sin, t1 * sin + t2 * cos, rest], -1).astype(t.dtype)


def masked_softmax(s, mask):
    s = jnp.where(mask, s.astype(jnp.float32), -jnp.inf)
    m = jnp.max(s, axis=-1, keepdims=True)
    m = jnp.where(jnp.isfinite(m), m, 0.0)
    e = jnp.where(mask, jnp.exp(s - m), 0.0)
    return e / jnp.maximum(e.sum(-1, keepdims=True), 1e-30)


def nsa_mixer(h, positions, w_in, pos_k, w1_k, w2_k, pos_v, w1_v, w2_v, w_o):
    B, S, _ = h.shape
    G, R, E, H = N_KV_HEADS, GQA_REP, HEAD_DIM, N_Q_HEADS
    proj = h @ w_in
    q = partial_rope(proj[..., :D_MODEL].reshape(B, S, H, E), positions)
    kv = proj[..., D_MODEL:D_MODEL + N_BRANCH * 2 * KV_COLS].reshape(B, S, N_BRANCH, 2, G, E)
    gate = jax.nn.sigmoid(proj[..., D_MODEL + N_BRANCH * 2 * KV_COLS:].astype(jnp.float32)).reshape(B, S, H, N_BRANCH)

    n_cmp = (S - CMP_LEN) // CMP_STRIDE + 1
    blk_idx = jnp.arange(n_cmp)[:, None] * CMP_STRIDE + jnp.arange(CMP_LEN)[None, :]

    def compress(t, pos_emb, w1, w2):
        blocks = t[:, blk_idx] + pos_emb[:, None, :]
        flat = blocks.transpose(0, 1, 3, 2, 4).reshape(B, n_cmp, G, CMP_LEN * E)
        return jax.nn.gelu(flat @ w1) @ w2

    cmp_end = blk_idx[:, -1]
    kc = partial_rope(compress(kv[:, :, 0, 0], pos_k, w1_k, w2_k), positions[:, cmp_end])
    vc = compress(kv[:, :, 0, 1], pos_v, w1_v, w2_v)

    n_sel = S // SEL_LEN
    k_top = min(N_SELECT, n_sel)
    ksb = partial_rope(kv[:, :, 1, 0], positions).reshape(B, n_sel, SEL_LEN, G, E).transpose(0, 3, 1, 2, 4)
    vsb = kv[:, :, 1, 1].reshape(B, n_sel, SEL_LEN, G, E).transpose(0, 3, 1, 2, 4)
    cs = jnp.arange(n_cmp) * CMP_STRIDE
    ss = jnp.arange(n_sel) * SEL_LEN
    overlap = ((cs[:, None] < ss[None, :] + SEL_LEN) & (cs[:, None] + CMP_LEN > ss[None, :])).astype(jnp.float32)

    pad = ((0, 0), (WIN_LEN, 0), (0, 0), (0, 0))
    kwp = jnp.pad(partial_rope(kv[:, :, 2, 0], positions), pad)
    vwp = jnp.pad(kv[:, :, 2, 1], pad)

    nqb = S // Q_BLOCK
    q_blocks = q.reshape(B, nqb, Q_BLOCK, G, R, E).transpose(1, 0, 2, 3, 4, 5)
    g_blocks = gate.reshape(B, nqb, Q_BLOCK, H, N_BRANCH).transpose(1, 0, 2, 3, 4)
    bi = jnp.arange(B)[:, None, None, None]
    gi = jnp.arange(G)[None, :, None, None]
    cmp_last = cs + CMP_LEN - 1
    sel_ids = jnp.arange(n_sel)

    def block_fn(args):
        qb_idx, qb, gb = args
        t = qb_idx * Q_BLOCK + jnp.arange(Q_BLOCK)
        s_c = jnp.einsum('bqgre,bnge->bgrqn', qb, kc) * ATTN_SCALE
        p_c = masked_softmax(s_c, cmp_last[None, :] <= t[:, None])
        o_c = jnp.einsum('bgrqn,bnge->bqgre', p_c.astype(vc.dtype), vc)
        imp = jnp.einsum('bgrqn,ns->bgqs', p_c, overlap)
        cur = t // SEL_LEN
        forced = (sel_ids[None, :] == 0) | (sel_ids[None, :] == cur[:, None]) | (sel_ids[None, :] == cur[:, None] - 1)
        imp = jnp.where(forced, FORCE_SCORE, imp)
        imp = jnp.where(sel_ids[None, :] <= cur[:, None], imp, -jnp.inf)
        _, idx = lax.top_k(imp, k_top)
        ks = ksb[bi, gi, idx]
        vs = vsb[bi, gi, idx]
        s_s = jnp.einsum('bqgre,bgqjle->bgrqjl', qb, ks) * ATTN_SCALE
        tok = idx[..., None] * SEL_LEN + jnp.arange(SEL_LEN)
        valid_s = (tok <= t[None, None, :, None, None]).reshape(B, G, 1, Q_BLOCK, k_top * SEL_LEN)
        p_s = masked_softmax(s_s.reshape(B, G, R, Q_BLOCK, k_top * SEL_LEN), valid_s)
        p_s = p_s.reshape(B, G, R, Q_BLOCK, k_top, SEL_LEN)
        o_s = jnp.einsum('bgrqjl,bgqjle->bqgre', p_s.astype(vs.dtype), vs)
        kw = lax.dynamic_slice_in_dim(kwp, qb_idx * Q_BLOCK, WIN_LEN + Q_BLOCK, axis=1)
        vw = lax.dynamic_slice_in_dim(vwp, qb_idx * Q_BLOCK, WIN_LEN + Q_BLOCK, axis=1)
        kpos = qb_idx * Q_BLOCK - WIN_LEN + jnp.arange(WIN_LEN + Q_BLOCK)
        dist = t[:, None] - kpos[None, :]
        valid_w = (dist >= 0) & (dist < WIN_LEN) & (kpos[None, :] >= 0)
        s_w = jnp.einsum('bqgre,bkge->bgrqk', qb, kw) * ATTN_SCALE
        p_w = masked_softmax(s_w, valid_w)
        o_w = jnp.einsum('bgrqk,bkge->bqgre', p_w.astype(vw.dtype), vw)
        o = (o_c.reshape(B, Q_BLOCK, H, E) * gb[..., 0:1]
             + o_s.reshape(B, Q_BLOCK, H, E) * gb[..., 1:2]
             + o_w.reshape(B, Q_BLOCK, H, E) * gb[..., 2:3])
        return o.reshape(B, Q_BLOCK, D_MODEL).astype(h.dtype)

    outs = lax.map(block_fn, (jnp.arange(nqb), q_blocks, g_blocks))
    return outs.transpose(1, 0, 2, 3).reshape(B, S, D_MODEL) @ w_o


def dilated_band_attention(q, k, v, dil, steps):
    B, S, G, R, E = q.shape
    L = S // dil
    nb = -(-L // DIL_BLOCK)
    pad = nb * DIL_BLOCK - L
    qd = q.reshape(B, L, dil, G, R, E).transpose(0, 2, 3, 4, 1, 5)
    kd = k.reshape(B, L, dil, G, E).transpose(0, 2, 3, 1, 4)
    vd = v.reshape(B, L, dil, G, E).transpose(0, 2, 3, 1, 4)
    qd = jnp.pad(qd, ((0, 0),) * 4 + ((0, pad), (0, 0)))
    kd = jnp.pad(kd, ((0, 0),) * 3 + ((0, pad), (0, 0)))
    vd = jnp.pad(vd, ((0, 0),) * 3 + ((0, pad), (0, 0)))
    qb = qd.reshape(B, dil, G, R, nb, DIL_BLOCK, E)
    kb = kd.reshape(B, dil, G, nb, DIL_BLOCK, E)
    vb = vd.reshape(B, dil, G, nb, DIL_BLOCK, E)
    prev = ((0, 0), (0, 0), (0, 0), (1, 0), (0, 0), (0, 0))
    kk = jnp.concatenate([jnp.pad(kb, prev)[:, :, :, :-1], kb], axis=-2)
    vv = jnp.concatenate([jnp.pad(vb, prev)[:, :, :, :-1], vb], axis=-2)
    qi = jnp.arange(DIL_BLOCK)
    kj = jnp.arange(2 * DIL_BLOCK)
    dist = DIL_BLOCK + qi[:, None] - kj[None, :]
    key_sub = (jnp.arange(nb)[:, None, None] - 1) * DIL_BLOCK + kj[None, None, :]
    valid = (dist >= 0) & (dist <= steps) & (key_sub >= 0)
    s = jnp.einsum('bdgrnqe,bdgnke->bdgrnqk', qb, kk).astype(jnp.float32) * ATTN_SCALE
    s = jnp.where(valid, s, -jnp.inf)
    m = jnp.max(s, axis=-1)
    e = jnp.where(valid, jnp.exp(s - m[..., None]), 0.0)
    l = e.sum(-1)
    o = jnp.einsum('bdgrnqk,bdgnke->bdgrnqe', e, vv.astype(jnp.float32)) / l[..., None]
    o = o.reshape(B, dil, G, R, nb * DIL_BLOCK, E)[:, :, :, :, :L]
    o = o.transpose(0, 4, 1, 2, 3, 5).reshape(B, S, G, R, E)
    m = m.reshape(B, dil, G, R, nb * DIL_BLOCK)[..., :L].transpose(0, 4, 1, 2, 3).reshape(B, S, G, R)
    l = l.reshape(B, dil, G, R, nb * DIL_BLOCK)[..., :L].transpose(0, 4, 1, 2, 3).reshape(B, S, G, R)
    return o, m, l


def dilated_mixer(h, positions, w_in, w_o):
    B, S, _ = h.shape
    G, R, E = N_KV_HEADS, GQA_REP, HEAD_DIM
    proj = h @ w_in
    q = partial_rope(proj[..., :D_MODEL].reshape(B, S, N_Q_HEADS, E), positions).reshape(B, S, G, R, E)
    kv = proj[..., D_MODEL:].reshape(B, S, len(DIL_PATTERNS), 2, G, E)
    outs, maxes, dens = [], [], []
    for p, (window, dil) in enumerate(DIL_PATTERNS):
        o, m, l = dilated_band_attention(q, partial_rope(kv[:, :, p, 0], positions), kv[:, :, p, 1], dil, window // dil)
        outs.append(o)
        maxes.append(m)
        dens.append(l)
    m_all = jnp.stack(maxes, 0)
    w = jnp.stack(dens, 0) * jnp.exp(m_all - m_all.max(0))
    w = w / w.sum(0)
    o = jnp.einsum('pbsgr,pbsgre->bsgre', w, jnp.stack(outs, 0))
    return o.reshape(B, S, D_MODEL).astype(h.dtype) @ w_o


def moe_ffn(h, router_w, router_b, w_gate, w_up, w_down):
    B, S, D = h.shape
    N = B * S
    xt = h.reshape(N, D)
    scores = jax.nn.sigmoid((xt @ router_w).astype(jnp.float32))
    grp = (scores + router_b.astype(jnp.float32)).reshape(N, N_GROUPS, EXPERTS_PER_GROUP)
    best = jnp.argmax(lax.top_k(grp, TOP_K)[0].sum(-1), axis=-1)
    in_grp = jnp.arange(N_GROUPS)[None, :] == best[:, None]
    masked = jnp.where(in_grp[:, :, None], grp, -jnp.inf).reshape(N, N_EXPERTS)
    _, eidx = lax.top_k(masked, TOP_K)
    wt = jnp.take_along_axis(scores, eidx, -1)
    wt = wt / wt.sum(-1, keepdims=True)
    A = N * TOP_K
    e_flat = eidx.reshape(A)
    tok_flat = jnp.repeat(jnp.arange(N, dtype=jnp.int32), TOP_K)
    w_flat = wt.reshape(A)
    order = jnp.argsort(e_flat)
    e_sorted = e_flat[order]
    counts = jnp.bincount(e_flat, length=N_EXPERTS)
    starts = jnp.cumsum(counts) - counts
    padded = (counts + MOE_BLOCK - 1) // MOE_BLOCK * MOE_BLOCK
    pad_ends = jnp.cumsum(padded)
    pad_starts = pad_ends - padded
    dest = pad_starts[e_sorted] + jnp.arange(A) - starts[e_sorted]
    n_rows = A + N_EXPERTS * MOE_BLOCK
    n_blk = n_rows // MOE_BLOCK
    row_tok = jnp.zeros((n_rows,), jnp.int32).at[dest].set(tok_flat[order])
    row_w = jnp.zeros((n_rows,), jnp.float32).at[dest].set(w_flat[order])
    blk_exp = jnp.minimum(jnp.searchsorted(pad_ends, jnp.arange(n_blk) * MOE_BLOCK, side='right'), N_EXPERTS - 1)
    xs = xt[row_tok].reshape(n_blk, MOE_BLOCK, D)

    def expert_block(args):
        xb, e = args
        return (jax.nn.silu(xb @ w_gate[e]) * (xb @ w_up[e])) @ w_down[e]

    ys = lax.map(expert_block, (xs, blk_exp)).reshape(n_rows, D)
    out = jnp.zeros((N, D), h.dtype).at[row_tok].add(ys * row_w[:, None].astype(ys.dtype))
    return out.reshape(B, S, D)


def setup_inputs(seed: int = 0) -> dict:
    key = jax.random.key(seed)
    ks = jax.random.split(key, 24)
    nrm = jax.random.normal
    D, F = D_MODEL, D_EXPERT
    cmp_in = CMP_LEN * HEAD_DIM
    return {
        'x': nrm(ks[0], (BATCH, SEQ, D), jnp.float32),
        'c': nrm(ks[1], (BATCH, D), jnp.float32),
        'positions': (jnp.arange(SEQ, dtype=jnp.int32)[None, :]
                      + jax.random.randint(ks[2], (BATCH, 1), 0, 4096, dtype=jnp.int32)),
        'ada_w': nrm(ks[3], (DEPTH, 2, D, 3 * D), jnp.float32) * (0.5 * D ** -0.5),
        'ada_b': nrm(ks[4], (DEPTH, 2, 3 * D), jnp.float32) * 0.02,
        'ln_g': 1.0 + 0.02 * nrm(ks[5], (DEPTH, 2, D), jnp.float32),
        'ln_b': 0.02 * nrm(ks[6], (DEPTH, 2, D), jnp.float32),
        'nsa_w_in': nrm(ks[7], (N_A, D, NSA_COLS), jnp.float32) * D ** -0.5,
        'nsa_cmp_pos_k': 0.02 * nrm(ks[8], (N_A, CMP_LEN, HEAD_DIM), jnp.float32),
        'nsa_cmp_w1_k': nrm(ks[9], (N_A, cmp_in, CMP_HIDDEN), jnp.float32) * cmp_in ** -0.5,
        'nsa_cmp_w2_k': nrm(ks[10], (N_A, CMP_HIDDEN, HEAD_DIM), jnp.float32) * CMP_HIDDEN ** -0.5,
        'nsa_cmp_pos_v': 0.02 * nrm(ks[11], (N_A, CMP_LEN, HEAD_DIM), jnp.float32),
        'nsa_cmp_w1_v': nrm(ks[12], (N_A, cmp_in, CMP_HIDDEN), jnp.float32) * cmp_in ** -0.5,
        'nsa_cmp_w2_v': nrm(ks[13], (N_A, CMP_HIDDEN, HEAD_DIM), jnp.float32) * CMP_HIDDEN ** -0.5,
        'nsa_w_o': nrm(ks[14], (N_A, D, D), jnp.float32) * (D ** -0.5 * DN_BETA),
        'dil_w_in': nrm(ks[15], (N_B, D, DIL_COLS), jnp.float32) * D ** -0.5,
        'dil_w_o': nrm(ks[16], (N_B, D, D), jnp.float32) * (D ** -0.5 * DN_BETA),
        'router_w': nrm(ks[17], (D, N_EXPERTS), jnp.float32) * D ** -0.5,
        'router_b': 0.01 * nrm(ks[18], (N_EXPERTS,), jnp.float32),
        'moe_w_gate': nrm(ks[19], (DEPTH, N_EXPERTS, D, F), jnp.float32) * D ** -0.5,
        'moe_w_up': nrm(ks[20], (DEPTH, N_EXPERTS, D, F), jnp.float32) * D ** -0.5,
        'moe_w_down': nrm(ks[21], (DEPTH, N_EXPERTS, F, D), jnp.float32) * (F ** -0.5 * DN_BETA),
    }


def reference(x, c, positions, ada_w, ada_b, ln_g, ln_b,
              nsa_w_in, nsa_cmp_pos_k, nsa_cmp_w1_k, nsa_cmp_w2_k,
              nsa_cmp_pos_v, nsa_cmp_w1_v, nsa_cmp_w2_v, nsa_w_o,
              dil_w_in, dil_w_o,
              router_w, router_b, moe_w_gate, moe_w_up, moe_w_down):
    cond = jax.nn.silu(c)
    for i in range(DEPTH):
        mod = (cond @ ada_w[i, 0] + ada_b[i, 0])[:, None, :]
        shift, scale, gate = jnp.split(mod, 3, axis=-1)
        hmod = x * (1.0 + scale) + shift
        if i % 2 == 0:
            j = i // 2
            y = nsa_mixer(hmod, positions, nsa_w_in[j], nsa_cmp_pos_k[j], nsa_cmp_w1_k[j], nsa_cmp_w2_k[j],
                          nsa_cmp_pos_v[j], nsa_cmp_w1_v[j], nsa_cmp_w2_v[j], nsa_w_o[j])
        else:
            j = i // 2
            y = dilated_mixer(hmod, positions, dil_w_in[j], dil_w_o[j])
        x = layer_norm(DN_ALPHA * x + gate * y, ln_g[i, 0], ln_b[i, 0])
        mod = (cond @ ada_w[i, 1] + ada_b[i, 1])[:, None, :]
        shift, scale, gate = jnp.split(mod, 3, axis=-1)
        hmod = x * (1.0 + scale) + shift
        y = moe_ffn(hmod, router_w, router_b, moe_w_gate[i], moe_w_up[i], moe_w_down[i])
        x = layer_norm(DN_ALPHA * x + gate * y, ln_g[i, 1], ln_b[i, 1])
    return x
```

```python
import numpy as np
from contextlib import ExitStack
import concourse.bass as bass
import concourse.mybir as mybir
from concourse.bass_utils import run_bass_kernel_spmd

F32 = mybir.dt.float32
BF16 = mybir.dt.bfloat16
I32 = mybir.dt.int32
AF = mybir.ActivationFunctionType
ALU = mybir.AluOpType
AX = mybir.AxisListType

ENGS = ("pe", "act", "dve", "pool", "sp")

D = 1024
ALPHA = (2.0 * 2) ** 0.25
LN_EPS = 1e-5
NEG = -30000.0


_UID = [0]


class Buf:
    __slots__ = ("name", "t", "writer", "readers", "dma_sem", "dma_cnt", "uid")

    def __init__(self, name, t=None):
        _UID[0] += 1
        self.uid = _UID[0]
        self.name = name
        self.t = t
        self.writer = None
        self.readers = []
        self.dma_sem = None
        self.dma_cnt = 0


class Prog:
    def __init__(self, nc, stack):
        self.nc = nc
        self.stack = stack
        self.ops = {e: [] for e in ENGS}
        self.cnt = {e: 0 for e in ENGS}
        self.waited = {}
        self.sem = {}
        for e in ENGS:
            self.sem[e] = stack.enter_context(nc.semaphore("s_" + e))
        self.n_dma_sems = 0
        self.dma_bufs = []
        self.scopes = []

    def sbuf(self, name, shape, dt):
        st = self.scopes[-1] if self.scopes else self.stack
        t = st.enter_context(self.nc.sbuf_tensor("sb_" + name, list(shape), dt))
        return Buf(name, t)

    def push(self):
        self.scopes.append(ExitStack())

    def pop(self):
        self.barrier()
        self.scopes.pop().close()

    def barrier(self):
        for eng in ENGS:
            waits = []
            for e2 in ENGS:
                if self.cnt[e2] > 0:
                    self._need(eng, ("eng", e2, self.cnt[e2]), waits)
            if eng == "pe" and self.cnt["pe"] > 0:
                pass
            for b in self.dma_bufs:
                self._need(eng, ("dma", b, b.dma_cnt), waits)
            self.ops[eng].append((waits, None, None))

    def psum(self, name, shape, dt=F32):
        t = self.stack.enter_context(self.nc.psum_tensor("ps_" + name, list(shape), dt))
        return Buf(name, t)

    def _need(self, eng, dep, waits):
        if dep[0] == "eng":
            _, e, idx = dep
            if e == eng and e == "pe":
                return
            key = (eng, "E", e)
            val = idx
            sem = self.sem[e]
        else:
            _, b, c = dep
            key = (eng, "D", b.uid)
            val = 16 * c
            sem = b.dma_sem
        if self.waited.get(key, 0) >= val:
            return
        self.waited[key] = val
        waits.append((sem, val))

    def _deps(self, eng, reads, writes):
        waits = []
        for b in reads:
            if b.writer is not None:
                self._need(eng, b.writer, waits)
        for b in writes:
            if b.writer is not None:
                self._need(eng, b.writer, waits)
            for r in b.readers:
                self._need(eng, r, waits)
        return waits

    def op(self, eng, fn, reads=(), writes=()):
        waits = self._deps(eng, reads, writes)
        self.cnt[eng] += 1
        me = ("eng", eng, self.cnt[eng])
        for b in reads:
            b.readers.append(me)
        for b in writes:
            b.writer = me
            b.readers = []
        self.ops[eng].append((waits, fn, (self.sem[eng], 1)))

    def dma(self, eng, fn, reads=(), writes=()):
        waits = self._deps(eng, reads, writes)
        dst = writes[0]
        if dst.dma_sem is None:
            dst.dma_sem = self.stack.enter_context(self.nc.semaphore("d%d" % self.n_dma_sems))
            self.n_dma_sems += 1
            self.dma_bufs.append(dst)
        dst.dma_cnt += 1
        me = ("dma", dst, dst.dma_cnt)
        for b in reads:
            b.readers.append(me)
        for b in writes:
            b.writer = me
            b.readers = []
        self.ops[eng].append((waits, fn, (dst.dma_sem, 16)))

    def wait_all(self, eng, bufs):
        waits = []
        for b in bufs:
            if b.writer is not None:
                self._need(eng, b.writer, waits)
        self.ops[eng].append((waits, None, None))

    def emit(self):
        nc = self.nc
        ops = self.ops
        with nc.Block() as block:
            def run(e, lst):
                for waits, fn, inc in lst:
                    for sem, val in waits:
                        e.wait_ge(sem, val)
                    if fn is not None:
                        fn(e).then_inc(inc[0], inc[1])

            @block.tensor
            def _(e):
                run(e, ops["pe"])

            @block.scalar
            def _(e):
                run(e, ops["act"])

            @block.vector
            def _(e):
                run(e, ops["dve"])

            @block.gpsimd
            def _(e):
                run(e, ops["pool"])

            @block.sync
            def _(e):
                run(e, ops["sp"])


def ss(a0, n, d):
    return slice(a0, a0 + (n - 1) * d + 1, d) if d > 1 else slice(a0, a0 + n)


def bcast_row(ap_row, n):
    return ap_row.to_broadcast([128, n])


def emit_layernorm(P, z, stats, mv, rstd, xn):
    for h in range(2):
        P.op("dve", lambda e, h=h: e.bn_stats(stats.t[:, h, :], z.t[:, h * 512:(h + 1) * 512]), reads=[z], writes=[stats])
    P.op("dve", lambda e: e.bn_aggr(mv.t[:], stats.t[:]), reads=[stats], writes=[mv])
    P.op("dve", lambda e: e.tensor_scalar(rstd.t[:], mv.t[:, 1:2], LN_EPS, None, ALU.add), reads=[mv], writes=[rstd])
    P.op("act", lambda e: e.sqrt(rstd.t[:], rstd.t[:]), reads=[rstd], writes=[rstd])
    P.op("dve", lambda e: e.reciprocal(rstd.t[:], rstd.t[:]), reads=[rstd], writes=[rstd])
    P.op("dve", lambda e: e.tensor_scalar(xn.t[:], z.t[:], mv.t[:, 0:1], rstd.t[:, 0:1], ALU.subtract, ALU.mult),
         reads=[z, mv, rstd], writes=[xn])


NTOK = 4096
CAP = 1024
NEXP = 32


def build_ffn(dbg=False):
    nc = bass.Bass("TRN2", target_bir_lowering=False)
    NT = NTOK // 128
    dt_in = lambda name, shape, dt=F32: nc.dram_tensor(name, list(shape), dt, kind="ExternalInput").ap()
    x_d = dt_in("x", [NTOK, D])
    o_d = dt_in("o", [NTOK, D])
    cT_d = dt_in("cT", [128, 8])
    adaw_d = dt_in("adaw", [2, D, 3 * D])
    adab_d = dt_in("adab", [2, 3 * D])
    lng_d = dt_in("lng", [2, D])
    lnb_d = dt_in("lnb", [2, D])
    wo_d = dt_in("wo", [D, D])
    rw_d = dt_in("rw", [D, NEXP])
    rb_d = dt_in("rb", [1, NEXP])
    NE_ = (1 if dbg == 1 else 2) if dbg in (1, 2, 3) else NEXP
    if dbg == 4:
        dbg_a = nc.dram_tensor("dbg_a", [128, 4 * D], F32, kind="ExternalOutput").ap()
    if dbg in (3, 4):
        dbg_i = nc.dram_tensor("dbg_i", [128, NTOK // 128 * 2], I32, kind="ExternalOutput").ap()
    wg_d = dt_in("wg", [NE_, D, 512])
    wu_d = dt_in("wu", [NE_, D, 512])
    wd_d = dt_in("wd", [NE_, 512, D])
    idn_d = dt_in("idn", [128, 128])
    tri_d = dt_in("tri", [128, 128])
    offs_d = dt_in("offs", [1, NEXP])
    y_d = nc.dram_tensor("y", [NTOK, D], F32, kind="ExternalOutput").ap()
    XS = nc.dram_tensor("XS", [NEXP * CAP, D], BF16).ap()
    YS = nc.dram_tensor("YS", [NEXP * CAP, D], F32).ap()
    X1 = nc.dram_tensor("X1", [NTOK, D], F32, kind="ExternalOutput" if dbg else "Internal").ap()
    if dbg == 2:
        dbg_xs = nc.dram_tensor("dbg_xs", [2 * CAP, D], BF16, kind="ExternalOutput").ap()
        dbg_ys = nc.dram_tensor("dbg_ys", [2 * CAP, D], F32, kind="ExternalOutput").ap()
    if dbg in (1, 2):
        dbg_i = nc.dram_tensor("dbg_i", [128, NTOK // 128 * 2], I32, kind="ExternalOutput").ap()
        dbg_w = nc.dram_tensor("dbg_w", [128, NTOK // 128 * 2], F32, kind="ExternalOutput").ap()
        dbg_m = nc.dram_tensor("dbg_m", [128, 4 * D], F32, kind="ExternalOutput").ap()

    with ExitStack() as st:
        P = Prog(nc, st)
        sb = P.sbuf
        idn = sb("idn", [128, 128], F32)
        idnb = sb("idnb", [128, 128], BF16)
        tri = sb("tri", [128, 128], BF16)
        ones = sb("ones", [128, 128], BF16)
        offsB = sb("offsB", [128, NEXP], F32)
        rbB = sb("rbB", [128, NEXP], F32)
        rw = sb("rw", [128, 8, NEXP], F32)
        wo = sb("wo", [128, 8, D], BF16)
        g1B = sb("g1B", [128, D], F32)
        sh2B = sb("sh2B", [128, D], F32)
        sc2B = sb("sc2B", [128, D], F32)
        g2B = sb("g2B", [128, D], F32)
        lgB = [sb("lgB%d" % i, [128, D], F32) for i in range(2)]
        lbB = [sb("lbB%d" % i, [128, D], F32) for i in range(2)]
        cT = sb("cT", [128, 8], F32)
        cond = sb("cond", [128, 8], F32)
        condB = sb("condB", [128, 8, 128], F32)
        base = sb("base", [128, NEXP], F32)
        desti = sb("desti", [128, NT, 2], I32)
        wts = sb("wts", [128, NT, 2], F32)
        psA = P.psum("psA", [128, 1024], BF16)
        psY = P.psum("psY", [128, 1024], F32)
        psT = P.psum("psT", [128, 1024], F32)
        psU = P.psum("psU", [128, 1024], F32)
        psS = P.psum("psS", [128, 512], F32)

        dmaq = ["sp", "act"]
        P.dma("sp", lambda e: e.dma_start(out=idn.t[:], in_=idn_d), writes=[idn])
        P.dma("pool", lambda e: e.dma_start(out=idnb.t[:], in_=idn_d), writes=[idnb])
        P.dma("pool", lambda e: e.dma_start(out=tri.t[:], in_=tri_d), writes=[tri])
        P.dma("sp", lambda e: e.dma_start(out=offsB.t[:], in_=bcast_row(offs_d, NEXP)), writes=[offsB])
        P.dma("sp", lambda e: e.dma_start(out=rbB.t[:], in_=bcast_row(rb_d, NEXP)), writes=[rbB])
        P.dma("sp", lambda e: e.dma_start(out=rw.t[:], in_=rw_d.rearrange("(k p) n -> p k n", p=128)), writes=[rw])
        P.dma("pool", lambda e: e.dma_start(out=wo.t[:], in_=wo_d.rearrange("(k p) n -> p k n", p=128)), writes=[wo])
        for i in range(2):
            P.dma("sp", lambda e, i=i: e.dma_start(out=lgB[i].t[:], in_=bcast_row(lng_d[i:i + 1, :], D)), writes=[lgB[i]])
            P.dma("sp", lambda e, i=i: e.dma_start(out=lbB[i].t[:], in_=bcast_row(lnb_d[i:i + 1, :], D)), writes=[lbB[i]])
        P.dma("sp", lambda e: e.dma_start(out=cT.t[:], in_=cT_d), writes=[cT])
        P.op("pool", lambda e: e.memset(ones.t[:], 1.0), writes=[ones])
        P.op("pool", lambda e: e.memset(base.t[:], 0.0), writes=[base])
        P.op("act", lambda e: e.activation(cond.t[:], cT.t[:], AF.Silu), reads=[cT], writes=[cond])
        for k in range(8):
            P.op("dve", lambda e, k=k: e.tensor_copy(condB.t[:, k, :], cond.t[:, k:k + 1].to_broadcast([128, 128])),
                 reads=[cond], writes=[condB])

        P.push()
        awb = [sb("awb%d" % i, [128, 8, 512], F32) for i in range(2)]
        abb = [sb("abb%d" % i, [128, 512], F32) for i in range(2)]
        jobs = []
        for h in range(2):
            jobs.append((0, 2 * D + h * 512, g1B, h * 512, False))
        for h in range(2):
            jobs.append((1, 0 * D + h * 512, sh2B, h * 512, False))
        for h in range(2):
            jobs.append((1, 1 * D + h * 512, sc2B, h * 512, True))
        for h in range(2):
            jobs.append((1, 2 * D + h * 512, g2B, h * 512, False))
        psM = [Buf("psM0", psS.t), Buf("psM1", psU.t)]
        for j, (s, c0, dst, d0, plus1) in enumerate(jobs):
            wb = awb[j % 2]
            bb = abb[j % 2]
            pm = psM[j % 2]
            P.dma(dmaq[j % 2], lambda e, s=s, c0=c0, wb=wb: e.dma_start(
                out=wb.t[:], in_=adaw_d[s, :, c0:c0 + 512].rearrange("(k p) n -> p k n", p=128)), writes=[wb])
            P.dma("sp", lambda e, s=s, c0=c0, bb=bb: e.dma_start(
                out=bb.t[:], in_=bcast_row(adab_d[s:s + 1, c0:c0 + 512], 512)), writes=[bb])
            for k in range(8):
                P.op("pe", lambda e, k=k, wb=wb, pm=pm: e.matmul(pm.t[:, 0:512], condB.t[:, k, :], wb.t[:, k, :],
                                                                 start=(k == 0), stop=(k == 7)),
                     reads=[condB, wb], writes=[pm])
            P.op("dve", lambda e, pm=pm, bb=bb, dst=dst, d0=d0: e.tensor_tensor(
                dst.t[:, d0:d0 + 512], pm.t[:, 0:512], bb.t[:], ALU.add), reads=[pm, bb], writes=[dst])
            if plus1:
                P.op("dve", lambda e, dst=dst, d0=d0: e.tensor_scalar(
                    dst.t[:, d0:d0 + 512], dst.t[:, d0:d0 + 512], 1.0, None, ALU.add), reads=[dst], writes=[dst])

        P.pop()
        P.push()
        NB = 2
        xt = [sb("xt%d" % i, [128, D], F32) for i in range(NB)]
        ot = [sb("ot%d" % i, [128, D], F32) for i in range(NB)]
        ob = [sb("ob%d" % i, [128, D], BF16) for i in range(NB)]
        oT = [sb("oT%d" % i, [128, 8, 128], BF16) for i in range(NB)]
        t1 = sb("t1", [128, D], F32)
        z = sb("z", [128, D], F32)
        xn = sb("xn", [128, D], F32)
        x1 = [sb("x1_%d" % i, [128, D], F32) for i in range(NB)]
        h2 = [sb("h2_%d" % i, [128, D], F32) for i in range(NB)]
        hb = [sb("hb%d" % i, [128, D], BF16) for i in range(NB)]
        h2T = [sb("h2T%d" % i, [128, 8, 128], F32) for i in range(NB)]
        stats = sb("stats", [128, 2, 6], F32)
        mv = sb("mv", [128, 2], F32)
        rstd = sb("rstd", [128, 1], F32)
        sc = sb("sc", [128, NEXP], F32)
        grp = sb("grp", [128, NEXP], F32)
        m8 = sb("m8", [128, 4, 8], F32)
        gs = sb("gs", [128, 4], F32)
        gmax = sb("gmax", [128, 1], F32)
        oh = sb("oh", [128, 4], F32)
        tmp4 = sb("tmp4", [128, 4], F32)
        thr = sb("thr", [128, 1], F32)
        ge = sb("ge", [128, NEXP], F32)
        sel = sb("sel", [128, NEXP], F32)
        selb = sb("selb", [128, NEXP], BF16)
        ws = sb("ws", [128, NEXP], F32)
        wsum = sb("wsum", [128, 1], F32)
        wt = sb("wt", [128, NEXP], F32)
        dall = sb("dall", [128, NEXP], F32)
        dhi = sb("dhi", [128, 1], F32)
        dsum = sb("dsum", [128, 1], F32)
        dpair = sb("dpair", [128, 2], F32)
        eq = sb("eq", [128, NEXP], F32)
        XSb = Buf("XS")
        X1b = Buf("X1")
        psLog = Buf("psLog", psS.t)

        for t in range(NT):
            b = t % NB
            r0 = t * 128
            P.dma("sp", lambda e, b=b, r0=r0: e.dma_start(out=xt[b].t[:], in_=x_d[r0:r0 + 128, :]), writes=[xt[b]])
            P.dma("act", lambda e, b=b, r0=r0: e.dma_start(out=ot[b].t[:], in_=o_d[r0:r0 + 128, :]), writes=[ot[b]])
            P.op("pool", lambda e, b=b: e.tensor_copy(ob[b].t[:], ot[b].t[:]), reads=[ot[b]], writes=[ob[b]])
            for k in range(8):
                P.op("pe", lambda e, b=b, k=k: e.transpose(psA.t[:, k * 128:(k + 1) * 128], ob[b].t[:, k * 128:(k + 1) * 128], idnb.t[:]),
                     reads=[ob[b], idnb], writes=[psA])
            P.op("act", lambda e, b=b: e.copy(oT[b].t[:].rearrange("p k m -> p (k m)"), psA.t[:]), reads=[psA], writes=[oT[b]])
            for nh in range(2):
                for k in range(8):
                    P.op("pe", lambda e, b=b, k=k, nh=nh: e.matmul(psY.t[:, nh * 512:(nh + 1) * 512], oT[b].t[:, k, :],
                                                                   wo.t[:, k, nh * 512:(nh + 1) * 512], start=(k == 0), stop=(k == 7)),
                         reads=[oT[b], wo], writes=[psY])
            for nh in range(2):
                sl = slice(nh * 512, (nh + 1) * 512)
                P.op("dve", lambda e, sl=sl: e.tensor_tensor(t1.t[:, sl], psY.t[:, sl], g1B.t[:, sl], ALU.mult),
                     reads=[psY, g1B], writes=[t1])
            P.op("dve", lambda e, b=b: e.scalar_tensor_tensor(z.t[:], xt[b].t[:], ALPHA, t1.t[:], ALU.mult, ALU.add),
                 reads=[xt[b], t1], writes=[z])
            emit_layernorm(P, z, stats, mv, rstd, xn)
            P.op("pool", lambda e, b=b: e.tensor_tensor(x1[b].t[:], xn.t[:], lgB[0].t[:], ALU.mult), reads=[xn, lgB[0]], writes=[x1[b]])
            P.op("pool", lambda e, b=b: e.tensor_tensor(x1[b].t[:], x1[b].t[:], lbB[0].t[:], ALU.add), reads=[x1[b], lbB[0]], writes=[x1[b]])
            P.dma("sp", lambda e, b=b, r0=r0: e.dma_start(out=X1[r0:r0 + 128, :], in_=x1[b].t[:]), reads=[x1[b]], writes=[X1b])
            P.op("pool", lambda e, b=b: e.tensor_tensor(h2[b].t[:], x1[b].t[:], sc2B.t[:], ALU.mult), reads=[x1[b], sc2B], writes=[h2[b]])
            P.op("pool", lambda e, b=b: e.tensor_tensor(h2[b].t[:], h2[b].t[:], sh2B.t[:], ALU.add), reads=[h2[b], sh2B], writes=[h2[b]])
            P.op("pool", lambda e, b=b: e.tensor_copy(hb[b].t[:], h2[b].t[:]), reads=[h2[b]], writes=[hb[b]])
            for k in range(8):
                P.op("pe", lambda e, b=b, k=k: e.transpose(psT.t[:, k * 128:(k + 1) * 128], h2[b].t[:, k * 128:(k + 1) * 128], idn.t[:]),
                     reads=[h2[b], idn], writes=[psT])
            P.op("act", lambda e, b=b: e.copy(h2T[b].t[:].rearrange("p k m -> p (k m)"), psT.t[:]), reads=[psT], writes=[h2T[b]])
            for k in range(8):
                P.op("pe", lambda e, b=b, k=k: e.matmul(psS.t[:, 0:NEXP], h2T[b].t[:, k, :], rw.t[:, k, :], start=(k == 0), stop=(k == 7)),
                     reads=[h2T[b], rw], writes=[psLog])
            P.op("act", lambda e: e.activation(sc.t[:], psS.t[:, 0:NEXP], AF.Sigmoid), reads=[psLog], writes=[sc])
            P.op("dve", lambda e: e.tensor_tensor(grp.t[:], sc.t[:], rbB.t[:], ALU.add), reads=[sc, rbB], writes=[grp])
            for g in range(4):
                P.op("dve", lambda e, g=g: e.max(out=m8.t[:, g, :], in_=grp.t[:, g * 8:(g + 1) * 8]), reads=[grp], writes=[m8])
            P.op("dve", lambda e: e.tensor_tensor(gs.t[:], m8.t[:, :, 0], m8.t[:, :, 1], ALU.add), reads=[m8], writes=[gs])
            P.op("dve", lambda e: e.reduce_max(gmax.t[:], gs.t[:], AX.X), reads=[gs], writes=[gmax])
            P.op("dve", lambda e: e.tensor_scalar(oh.t[:], gs.t[:], gmax.t[:, 0:1], None, ALU.is_equal), reads=[gs, gmax], writes=[oh])
            P.op("dve", lambda e: e.tensor_tensor(tmp4.t[:], oh.t[:], m8.t[:, :, 1], ALU.mult), reads=[oh, m8], writes=[tmp4])
            P.op("dve", lambda e: e.reduce_sum(thr.t[:], tmp4.t[:], AX.X), reads=[tmp4], writes=[thr])
            P.op("dve", lambda e: e.tensor_scalar(ge.t[:], grp.t[:], thr.t[:, 0:1], None, ALU.is_ge), reads=[grp, thr], writes=[ge])
            P.op("dve", lambda e: e.tensor_tensor(sel.t[:].rearrange("p (g j) -> p g j", j=8), ge.t[:].rearrange("p (g j) -> p g j", j=8),
                                                  oh.t[:].unsqueeze(2).to_broadcast([128, 4, 8]), ALU.mult), reads=[ge, oh], writes=[sel])
            P.op("dve", lambda e: e.tensor_tensor(ws.t[:], sc.t[:], sel.t[:], ALU.mult), reads=[sc, sel], writes=[ws])
            P.op("dve", lambda e: e.reduce_sum(wsum.t[:], ws.t[:], AX.X), reads=[ws], writes=[wsum])
            P.op("dve", lambda e: e.reciprocal(wsum.t[:], wsum.t[:]), reads=[wsum], writes=[wsum])
            P.op("dve", lambda e: e.tensor_scalar(wt.t[:], ws.t[:], wsum.t[:, 0:1], None, ALU.mult), reads=[ws, wsum], writes=[wt])
            P.op("dve", lambda e: e.tensor_copy(selb.t[:], sel.t[:]), reads=[sel], writes=[selb])
            P.op("pe", lambda e: e.matmul(psS.t[:, 64:64 + NEXP], tri.t[:], selb.t[:], start=True, stop=True), reads=[tri, selb], writes=[psLog])
            P.op("pe", lambda e: e.matmul(psS.t[:, 128:128 + NEXP], ones.t[:], selb.t[:], start=True, stop=True), reads=[ones, selb], writes=[psLog])
            P.op("dve", lambda e: e.tensor_tensor(dall.t[:], psS.t[:, 64:64 + NEXP], base.t[:], ALU.add), reads=[psLog, base], writes=[dall])
            P.op("dve", lambda e: e.tensor_tensor(dall.t[:], dall.t[:], offsB.t[:], ALU.add), reads=[dall, offsB], writes=[dall])
            P.op("dve", lambda e: e.tensor_tensor(dall.t[:], dall.t[:], sel.t[:], ALU.mult), reads=[dall, sel], writes=[dall])
            P.op("dve", lambda e: e.tensor_tensor(base.t[:], base.t[:], psS.t[:, 128:128 + NEXP], ALU.add), reads=[psLog, base], writes=[base])
            P.op("dve", lambda e: e.reduce_max(dhi.t[:], dall.t[:], AX.X), reads=[dall], writes=[dhi])
            P.op("dve", lambda e: e.reduce_sum(dsum.t[:], dall.t[:], AX.X), reads=[dall], writes=[dsum])
            P.op("dve", lambda e: e.tensor_scalar(dpair.t[:, 0:1], dhi.t[:], -1.0, None, ALU.add), reads=[dhi], writes=[dpair])
            P.op("dve", lambda e: e.scalar_tensor_tensor(dpair.t[:, 1:2], dsum.t[:], -1.0, dhi.t[:], ALU.add, ALU.subtract),
                 reads=[dsum, dhi], writes=[dpair])
            P.op("dve", lambda e, t=t: e.tensor_copy(desti.t[:, t, :], dpair.t[:]), reads=[dpair], writes=[desti])
            P.op("dve", lambda e: e.tensor_scalar(eq.t[:], dall.t[:], dhi.t[:, 0:1], None, ALU.is_equal), reads=[dall, dhi], writes=[eq])
            P.op("dve", lambda e: e.tensor_tensor(eq.t[:], eq.t[:], wt.t[:], ALU.mult), reads=[eq, wt], writes=[eq])
            P.op("dve", lambda e, t=t: e.reduce_sum(wts.t[:, t, 0:1], eq.t[:], AX.X), reads=[eq], writes=[wts])
            P.op("dve", lambda e, t=t: e.tensor_scalar(wts.t[:, t, 1:2], wts.t[:, t, 0:1], -1.0, 1.0, ALU.mult, ALU.add), reads=[wts], writes=[wts])
            for j in range(2):
                P.dma("pool", lambda e, b=b, t=t, j=j: e.indirect_dma_start(
                    out=XS[:, :], out_offset=bass.IndirectOffsetOnAxis(ap=desti.t[:, t, j:j + 1], axis=0),
                    in_=hb[b].t[:, :], in_offset=None), reads=[hb[b], desti], writes=[XSb])

        if dbg == 1:
            Db = Buf("dbg")
            P.dma("sp", lambda e: e.dma_start(out=dbg_i, in_=desti.t[:].rearrange("p t j -> p (t j)")), reads=[desti], writes=[Db])
            P.dma("sp", lambda e: e.dma_start(out=dbg_w, in_=wts.t[:].rearrange("p t j -> p (t j)")), reads=[wts], writes=[Db])
            for i_, tl in enumerate([g1B, sh2B, sc2B, g2B]):
                P.dma("sp", lambda e, i_=i_, tl=tl: e.dma_start(out=dbg_m[:, i_ * D:(i_ + 1) * D], in_=tl.t[:]), reads=[tl], writes=[Db])
            P.wait_all("sp", [Db, X1b])
            P.pop()
            P.emit()
            return nc
        P.pop()
        P.push()
        wgs = [sb("wgs%d" % i, [128, 8, 512], BF16) for i in range(2)]
        wus = [sb("wus%d" % i, [128, 8, 512], BF16) for i in range(2)]
        wds = [sb("wds%d" % i, [128, 4, D], BF16) for i in range(2)]
        xs_tok = [sb("xs_tok%d" % i, [128, D], BF16) for i in range(2)]
        xsT = [sb("xsT%d" % i, [128, 8, CAP], BF16) for i in range(2)]
        sg = [sb("sg%d" % i, [128, 512], F32) for i in range(2)]
        hT = [sb("hT%d" % i, [128, 4, CAP], BF16) for i in range(2)]
        ysb = [sb("ysb%d" % i, [128, D], F32) for i in range(2)]
        psG = [Buf("psG0", psT.t), Buf("psG1", psU.t)]
        YSb = Buf("YS")
        RB = CAP // 128
        nx = 0
        ny = 0
        for ex in range(NE_):
            wb = ex % 2
            P.dma("pool", lambda e, ex=ex, wb=wb: e.dma_start(out=wgs[wb].t[:], in_=wg_d[ex].rearrange("(k p) n -> p k n", p=128)), writes=[wgs[wb]])
            P.dma("pool", lambda e, ex=ex, wb=wb: e.dma_start(out=wus[wb].t[:], in_=wu_d[ex].rearrange("(k p) n -> p k n", p=128)), writes=[wus[wb]])
            P.dma("pool", lambda e, ex=ex, wb=wb: e.dma_start(out=wds[wb].t[:], in_=wd_d[ex].rearrange("(k p) n -> p k n", p=128)), writes=[wds[wb]])
            xT = xsT[wb]
            for rb in range(RB):
                xb = xs_tok[nx % 2]
                nx += 1
                r0 = ex * CAP + rb * 128
                P.dma("sp", lambda e, xb=xb, r0=r0: e.dma_start(out=xb.t[:], in_=XS[r0:r0 + 128, :]), reads=[XSb], writes=[xb])
                for k in range(8):
                    P.op("pe", lambda e, xb=xb, k=k: e.transpose(psA.t[:, k * 128:(k + 1) * 128], xb.t[:, k * 128:(k + 1) * 128], idnb.t[:]),
                         reads=[xb, idnb], writes=[psA])
                P.op("act", lambda e, xT=xT, rb=rb: e.copy(xT.t[:, :, rb * 128:(rb + 1) * 128], psA.t[:].rearrange("p (k m) -> p k m", m=128)),
                     reads=[psA], writes=[xT])
            hh = hT[wb]
            for fc in range(4):
              for hf in range(CAP // 512):
                pg = psG[(fc * (CAP // 512) + hf) % 2]
                s0 = hf * 512
                for k in range(8):
                    P.op("pe", lambda e, k=k, fc=fc, pg=pg, wb=wb, xT=xT, s0=s0: e.matmul(
                        pg.t[:, 0:512], wgs[wb].t[:, k, fc * 128:(fc + 1) * 128], xT.t[:, k, s0:s0 + 512], start=(k == 0), stop=(k == 7)),
                        reads=[wgs[wb], xT], writes=[pg])
                for k in range(8):
                    P.op("pe", lambda e, k=k, fc=fc, pg=pg, wb=wb, xT=xT, s0=s0: e.matmul(
                        pg.t[:, 512:1024], wus[wb].t[:, k, fc * 128:(fc + 1) * 128], xT.t[:, k, s0:s0 + 512], start=(k == 0), stop=(k == 7)),
                        reads=[wus[wb], xT], writes=[pg])
                s_ = sg[(fc * (CAP // 512) + hf) % 2]
                P.op("act", lambda e, pg=pg, s_=s_: e.activation(s_.t[:], pg.t[:, 0:512], AF.Silu), reads=[pg], writes=[s_])
                P.op("dve", lambda e, pg=pg, s_=s_, hh=hh, fc=fc, s0=s0: e.tensor_tensor(hh.t[:, fc, s0:s0 + 512], s_.t[:], pg.t[:, 512:1024], ALU.mult),
                     reads=[pg, s_], writes=[hh])
            for rb in range(RB):
                yb = ysb[ny % 2]
                ny += 1
                for nh in range(2):
                    for fc in range(4):
                        P.op("pe", lambda e, rb=rb, nh=nh, fc=fc, hh=hh, wb=wb: e.matmul(
                            psY.t[:, nh * 512:(nh + 1) * 512], hh.t[:, fc, rb * 128:(rb + 1) * 128],
                            wds[wb].t[:, fc, nh * 512:(nh + 1) * 512], start=(fc == 0), stop=(fc == 3)),
                            reads=[hh, wds[wb]], writes=[psY])
                P.op("act", lambda e, yb=yb: e.copy(yb.t[:], psY.t[:]), reads=[psY], writes=[yb])
                r0 = ex * CAP + rb * 128
                P.dma("sp", lambda e, yb=yb, r0=r0: e.dma_start(out=YS[r0:r0 + 128, :], in_=yb.t[:]), reads=[yb], writes=[YSb])

        if dbg == 2:
            Db = Buf("dbg")
            P.dma("sp", lambda e: e.dma_start(out=dbg_xs, in_=XS[0:2 * CAP, :]), reads=[XSb], writes=[Db])
            P.dma("sp", lambda e: e.dma_start(out=dbg_ys, in_=YS[0:2 * CAP, :]), reads=[YSb], writes=[Db])
            P.dma("sp", lambda e: e.dma_start(out=dbg_i, in_=desti.t[:].rearrange("p t j -> p (t j)")), reads=[desti], writes=[Db])
            P.wait_all("sp", [Db])
            P.pop()
            P.emit()
            return nc
        P.pop()
        P.push()
        cxt = [sb("cxt%d" % i, [128, D], F32) for i in range(2)]
        ct1 = sb("ct1", [128, D], F32)
        cz = sb("cz", [128, D], F32)
        cxn = sb("cxn", [128, D], F32)
        cstats = sb("cstats", [128, 2, 6], F32)
        cmv = sb("cmv", [128, 2], F32)
        crstd = sb("crstd", [128, 1], F32)
        yh = [sb("yh%d" % i, [128, D], F32) for i in range(2)]
        yl = [sb("yl%d" % i, [128, D], F32) for i in range(2)]
        outt = [sb("outt%d" % i, [128, D], F32) for i in range(2)]
        Yb = Buf("y")
        import os
        COMB = os.environ.get("COMB", "full")
        for t in range(NT):
            b = t % 2
            r0 = t * 128
            if COMB == "a":
                P.dma("sp", lambda e, b=b, r0=r0: e.dma_start(out=cxt[b].t[:], in_=X1[r0:r0 + 128, :]), reads=[X1b], writes=[cxt[b]])
                P.dma("sp", lambda e, b=b, r0=r0: e.dma_start(out=y_d[r0:r0 + 128, :], in_=cxt[b].t[:]), reads=[cxt[b]], writes=[Yb])
                continue
            if COMB == "none":
                continue
            P.dma("pool", lambda e, b=b, t=t: e.indirect_dma_start(
                out=yh[b].t[:, :], out_offset=None, in_=YS[:, :],
                in_offset=bass.IndirectOffsetOnAxis(ap=desti.t[:, t, 0:1], axis=0)), reads=[YSb, desti], writes=[yh[b]])
            P.dma("pool", lambda e, b=b, t=t: e.indirect_dma_start(
                out=yl[b].t[:, :], out_offset=None, in_=YS[:, :],
                in_offset=bass.IndirectOffsetOnAxis(ap=desti.t[:, t, 1:2], axis=0)), reads=[YSb, desti], writes=[yl[b]])
            P.dma("sp", lambda e, b=b, r0=r0: e.dma_start(out=cxt[b].t[:], in_=X1[r0:r0 + 128, :]), reads=[X1b], writes=[cxt[b]])
            if dbg == 4 and t == 0:
                Dbb = Buf("dbb")
                P.dma("sp", lambda e: e.dma_start(out=dbg_a[:, 0:D], in_=yh[0].t[:]), reads=[yh[0]], writes=[Dbb])
                P.dma("sp", lambda e: e.dma_start(out=dbg_a[:, D:2 * D], in_=yl[0].t[:]), reads=[yl[0]], writes=[Dbb])
                P.dma("sp", lambda e: e.dma_start(out=dbg_a[:, 2 * D:3 * D], in_=cxt[0].t[:]), reads=[cxt[0]], writes=[Dbb])
            P.op("dve", lambda e, b=b, t=t: e.tensor_scalar(yh[b].t[:], yh[b].t[:], wts.t[:, t, 0:1], None, ALU.mult), reads=[yh[b], wts], writes=[yh[b]])
            P.op("dve", lambda e, b=b, t=t: e.scalar_tensor_tensor(yh[b].t[:], yl[b].t[:], wts.t[:, t, 1:2], yh[b].t[:], ALU.mult, ALU.add),
                 reads=[yl[b], yh[b], wts], writes=[yh[b]])
            P.op("pool", lambda e, b=b: e.tensor_tensor(ct1.t[:], yh[b].t[:], g2B.t[:], ALU.mult), reads=[yh[b], g2B], writes=[ct1])
            P.op("dve", lambda e, b=b: e.scalar_tensor_tensor(cz.t[:], cxt[b].t[:], ALPHA, ct1.t[:], ALU.mult, ALU.add), reads=[cxt[b], ct1], writes=[cz])
            if dbg == 4 and t == 0:
                P.dma("sp", lambda e: e.dma_start(out=dbg_a[:, 3 * D:4 * D], in_=cz.t[:]), reads=[cz], writes=[Dbb])
            emit_layernorm(P, cz, cstats, cmv, crstd, cxn)
            P.op("pool", lambda e, b=b: e.tensor_tensor(outt[b].t[:], cxn.t[:], lgB[1].t[:], ALU.mult), reads=[cxn, lgB[1]], writes=[outt[b]])
            P.op("pool", lambda e, b=b: e.tensor_tensor(outt[b].t[:], outt[b].t[:], lbB[1].t[:], ALU.add), reads=[outt[b], lbB[1]], writes=[outt[b]])
            P.dma("sp", lambda e, b=b, r0=r0: e.dma_start(out=y_d[r0:r0 + 128, :], in_=outt[b].t[:]), reads=[outt[b]], writes=[Yb])
        if dbg in (3, 4):
            P.dma("sp", lambda e: e.dma_start(out=dbg_i, in_=desti.t[:].rearrange("p t j -> p (t j)")), reads=[desti], writes=[Yb])
        P.wait_all("sp", [Yb])
        P.pop()
        P.emit()
    return nc


_CACHE = {}


def _consts():
    idn = np.eye(128, dtype=np.float32)
    tri = np.triu(np.ones((128, 128), np.float32), 1)
    offs = (np.arange(NEXP, dtype=np.float32) * CAP + 1.0)[None, :]
    return idn, tri, offs


def run_ffn(x, o, c, ada_w_l, ada_b_l, ln_g_l, ln_b_l, w_o, router_w, router_b, wg, wu, wd):
    if "ffn" not in _CACHE:
        _CACHE["ffn"] = build_ffn()
    nc = _CACHE["ffn"]
    idn, tri, offs = _consts()
    B, S, _ = x.shape
    xf = x.reshape(B * S, D)
    of = o.reshape(B * S, D)
    in_maps = []
    for core in range(8):
        r0 = core * NTOK
        b = r0 // S
        in_maps.append({
            "x": np.ascontiguousarray(xf[r0:r0 + NTOK]), "o": np.ascontiguousarray(of[r0:r0 + NTOK]),
            "cT": np.ascontiguousarray(c[b].reshape(8, 128).T),
            "adaw": ada_w_l, "adab": ada_b_l, "lng": ln_g_l, "lnb": ln_b_l, "wo": w_o,
            "rw": router_w, "rb": router_b.reshape(1, NEXP), "wg": wg, "wu": wu, "wd": wd,
            "idn": idn, "tri": tri, "offs": offs,
        })
    res = run_bass_kernel_spmd(nc, in_maps, core_ids=list(range(8)))
    return np.concatenate([r["y"] for r in res.results], axis=0).reshape(B, S, D)


S_LEN = 16384
SPAN = 2048
TWO_PI = 6.283185307179586
PI = 3.141592653589793


def rope_inv_table():
    inv = 500000.0 ** (-np.arange(8, dtype=np.float64) * (2.0 / 16))
    t = np.zeros((128, 1), np.float32)
    for p in range(128):
        f = p % 64
        if f < 16:
            t[p, 0] = np.float32(inv[f % 8])
    return t


def emit_mod_cols(P, nc, adaw_d, adabT_d, cT_d, ncols, shp, psum_buf):
    sb = P.sbuf
    cT = sb("cT", [128, 8], F32)
    cond = sb("cond", [128, 8], F32)
    modp = sb("modp", [128, ncols // 128], F32)
    abT = sb("abT", [128, ncols // 128], F32)
    P.dma("sp", lambda e: e.dma_start(out=cT.t[:], in_=cT_d), writes=[cT])
    P.dma("sp", lambda e: e.dma_start(out=abT.t[:], in_=adabT_d), writes=[abT])
    P.op("act", lambda e: e.activation(cond.t[:], cT.t[:], AF.Silu), reads=[cT], writes=[cond])
    P.push()
    awb = [sb("awb%d" % i, [128, 8, 512], F32) for i in range(2)]
    for blk in range(ncols // 512):
        wb = awb[blk % 2]
        P.dma(["sp", "act"][blk % 2], lambda e, blk=blk, wb=wb: e.dma_start(
            out=wb.t[:], in_=adaw_d[:, blk * 512:(blk + 1) * 512].rearrange("(k p) n -> p k n", p=128)), writes=[wb])
        for j in range(4):
            for kk in range(8):
                P.op("pe", lambda e, j=j, kk=kk, wb=wb, blk=blk: e.matmul(
                    psum_buf.t[:, blk * 4 + j:blk * 4 + j + 1], wb.t[:, kk, j * 128:(j + 1) * 128], cond.t[:, kk:kk + 1],
                    start=(kk == 0), stop=(kk == 7)), reads=[wb, cond], writes=[psum_buf])
    P.op("dve", lambda e: e.tensor_tensor(modp.t[:], psum_buf.t[:, 0:ncols // 128], abT.t[:], ALU.add),
         reads=[psum_buf, abT], writes=[modp])
    P.pop()
    return modp


def emit_rope_tables(P, posB, posI, invp, negpi, Ct, St, tmp, pos_ap, n):
    C1 = 6.28125
    C2 = TWO_PI - C1
    P.dma("sp", lambda e: e.dma_start(out=posI.t[:, 0:n], in_=pos_ap.to_broadcast([128, n])), writes=[posI])
    P.op("dve", lambda e: e.tensor_copy(posB.t[:, 0:n], posI.t[:, 0:n]), reads=[posI], writes=[posB])
    for off, dst in ((0.0, St), (0.5 * PI, Ct)):
        P.op("dve", lambda e, off=off: e.tensor_scalar(tmp.t[:, 0:n], posB.t[:, 0:n], invp.t[:, 0:1], off, ALU.mult, ALU.add),
             reads=[posB, invp], writes=[tmp])
        P.op("dve", lambda e: e.tensor_scalar(dst.t[:, 0:n], tmp.t[:, 0:n], 1.0 / TWO_PI, None, ALU.mult), reads=[tmp], writes=[dst])
        P.op("dve", lambda e: e.tensor_copy(posI.t[:, 0:n], dst.t[:, 0:n]), reads=[dst], writes=[posI])
        P.op("dve", lambda e, dst=dst: e.tensor_copy(dst.t[:, 0:n], posI.t[:, 0:n]), reads=[posI], writes=[dst])
        P.op("dve", lambda e, dst=dst: e.scalar_tensor_tensor(tmp.t[:, 0:n], dst.t[:, 0:n], -C1, tmp.t[:, 0:n], ALU.mult, ALU.add),
             reads=[dst, tmp], writes=[tmp])
        P.op("dve", lambda e, dst=dst: e.scalar_tensor_tensor(tmp.t[:, 0:n], dst.t[:, 0:n], -C2, tmp.t[:, 0:n], ALU.mult, ALU.add),
             reads=[dst, tmp], writes=[tmp])
        P.op("dve", lambda e, dst=dst: e.tensor_scalar(dst.t[:, 0:n], tmp.t[:, 0:n], PI, -TWO_PI, ALU.is_gt, ALU.mult), reads=[tmp], writes=[dst])
        P.op("dve", lambda e, dst=dst: e.tensor_tensor(tmp.t[:, 0:n], tmp.t[:, 0:n], dst.t[:, 0:n], ALU.add), reads=[tmp, dst], writes=[tmp])
        P.op("dve", lambda e, dst=dst: e.tensor_scalar(dst.t[:, 0:n], tmp.t[:, 0:n], -PI, TWO_PI, ALU.is_lt, ALU.mult), reads=[tmp], writes=[dst])
        P.op("dve", lambda e, dst=dst: e.tensor_tensor(tmp.t[:, 0:n], tmp.t[:, 0:n], dst.t[:, 0:n], ALU.add), reads=[tmp, dst], writes=[tmp])
        P.op("dve", lambda e: e.tensor_scalar(tmp.t[:, 0:n], tmp.t[:, 0:n], PI, -PI, ALU.min, ALU.max), reads=[tmp], writes=[tmp])
        P.op("act", lambda e, dst=dst: e.activation(dst.t[:, 0:n], tmp.t[:, 0:n], AF.Sin), reads=[tmp], writes=[dst])


def emit_rot_weights(P, w, wr, nheads):
    P.op("pool", lambda e: e.memset(wr.t[:], 0.0), writes=[wr])
    for k in range(8):
        wv = w.t[:, k, :].rearrange("p (h e) -> p h e", e=64)
        rv = wr.t[:, k, :].rearrange("p (h e) -> p h e", e=64)
        P.op("dve", lambda e, wv=wv, rv=rv: e.tensor_scalar(rv[:, :, 0:8], wv[:, :, 8:16], -1.0, None, ALU.mult), reads=[w], writes=[wr])
        P.op("dve", lambda e, wv=wv, rv=rv: e.tensor_copy(rv[:, :, 8:16], wv[:, :, 0:8]), reads=[w], writes=[wr])


def emit_hmodT_tile(P, x_d, tok0, xt, psX, idn, modp, hT, col0, sc_off, q, width=1024):
    P.dma(q, lambda e: e.dma_start(out=xt.t[:], in_=x_d[tok0:tok0 + 128, :]), writes=[xt])
    nb_ = width // 128
    for k0 in range(0, 8, nb_):
        for kk in range(nb_):
            k = k0 + kk
            P.op("pe", lambda e, k=k, kk=kk: e.transpose(psX.t[:, kk * 128:(kk + 1) * 128], xt.t[:, k * 128:(k + 1) * 128], idn.t[:]),
                 reads=[xt, idn], writes=[psX])
        for kk in range(nb_):
            k = k0 + kk
            P.op("act", lambda e, k=k, kk=kk: e.activation(hT.t[:, k, col0:col0 + 128], psX.t[:, kk * 128:(kk + 1) * 128], AF.Identity,
                                                         bias=modp.t[:, k:k + 1], scale=modp.t[:, sc_off + k:sc_off + k + 1]),
                 reads=[psX, modp], writes=[hT])


def emit_proj_rope(P, hT, t0, n, w, wr, wc0, ps_a, ps_b, Ct, St, tcol0, tmpa, tmpb, out_ap_fn, out_buf):
    for k in range(8):
        P.op("pe", lambda e, k=k: e.matmul(ps_a.t[:, 0:n], w.t[:, k, wc0:wc0 + 128], hT.t[:, k, t0:t0 + n], start=(k == 0), stop=(k == 7)),
             reads=[w, hT], writes=[ps_a])
    for k in range(8):
        P.op("pe", lambda e, k=k: e.matmul(ps_b.t[:, 0:n], wr.t[:, k, wc0:wc0 + 128], hT.t[:, k, t0:t0 + n], start=(k == 0), stop=(k == 7)),
             reads=[wr, hT], writes=[ps_b])
    P.op("dve", lambda e: e.tensor_tensor(tmpa.t[:, 0:n], ps_a.t[:, 0:n], Ct.t[:, tcol0:tcol0 + n], ALU.mult), reads=[ps_a, Ct], writes=[tmpa])
    P.op("dve", lambda e: e.tensor_tensor(tmpb.t[:, 0:n], ps_b.t[:, 0:n], St.t[:, tcol0:tcol0 + n], ALU.mult), reads=[ps_b, St], writes=[tmpb])
    P.op("pool", lambda e: e.tensor_tensor(out_ap_fn(), tmpa.t[:, 0:n], tmpb.t[:, 0:n], ALU.add), reads=[tmpa, tmpb], writes=[out_buf])


def tri_masks():
    p = np.arange(128)[:, None]
    f = np.arange(128)[None, :]
    m0 = np.where(f <= p, 0.0, NEG).astype(np.float32)
    m1 = np.where(f >= p, 0.0, NEG).astype(np.float32)
    return np.stack([np.tile(m0, (1, 4)), np.tile(m1, (1, 4))], axis=1)


DIL = (1, 4, 16)


def build_dil():
    nc = bass.Bass("TRN2", target_bir_lowering=False)
    dt_in = lambda name, shape, dt=F32: nc.dram_tensor(name, list(shape), dt, kind="ExternalInput").ap()
    x_d = dt_in("x", [S_LEN, D])
    cT_d = dt_in("cT", [128, 8])
    adaw_d = dt_in("adaw", [D, 2 * D])
    adabT_d = dt_in("adabT", [128, 16])
    wq_d = dt_in("wq", [D, 256])
    wk_d = dt_in("wk", [3, D, 64])
    wv_d = dt_in("wv", [3, D, 64])
    pos_d = dt_in("pos", [1, S_LEN], I32)
    inv_d = dt_in("inv", [128, 1])
    idn_d = dt_in("idn", [128, 128])
    msk_d = dt_in("msk", [128, 2, 512])
    o_d = nc.dram_tensor("o", [S_LEN, 256], F32, kind="ExternalOutput").ap()
    OP = [nc.dram_tensor("OP%d" % p, [S_LEN, 260], F32).ap() for p in range(3)]

    with ExitStack() as st:
        P = Prog(nc, st)
        sb = P.sbuf
        idn = sb("idn", [128, 128], F32)
        idnb = sb("idnb", [128, 128], BF16)
        msk = sb("msk", [128, 2, 512], BF16)
        invp = sb("invp", [128, 1], F32)
        negpi = sb("negpi", [128, 1], F32)
        wq = sb("wq", [128, 8, 256], BF16)
        wqr = sb("wqr", [128, 8, 256], BF16)
        wk = [sb("wk%d" % p, [128, 8, 128], BF16) for p in range(3)]
        wkr = [sb("wkr%d" % p, [128, 8, 128], BF16) for p in range(3)]
        wv = [sb("wv%d" % p, [128, 8, 64], BF16) for p in range(3)]
        ps = [P.psum("pb%d" % i, [128, 512], F32) for i in range(6)]
        psX = P.psum("psX", [128, 1024], F32)

        P.dma("sp", lambda e: e.dma_start(out=idn.t[:], in_=idn_d), writes=[idn])
        P.dma("pool", lambda e: e.dma_start(out=idnb.t[:], in_=idn_d), writes=[idnb])
        P.dma("pool", lambda e: e.dma_start(out=msk.t[:], in_=msk_d), writes=[msk])
        P.dma("sp", lambda e: e.dma_start(out=invp.t[:], in_=inv_d), writes=[invp])
        P.op("pool", lambda e: e.memset(negpi.t[:], -PI), writes=[negpi])
        P.dma("pool", lambda e: e.dma_start(out=wq.t[:], in_=wq_d.rearrange("(k p) n -> p k n", p=128)), writes=[wq])
        for p in range(3):
            for h in range(2):
                P.dma("pool", lambda e, p=p, h=h: e.dma_start(out=wk[p].t[:, :, h * 64:(h + 1) * 64],
                                                              in_=wk_d[p].rearrange("(k p) n -> p k n", p=128)), writes=[wk[p]])
            P.dma("pool", lambda e, p=p: e.dma_start(out=wv[p].t[:], in_=wv_d[p].rearrange("(k p) n -> p k n", p=128)), writes=[wv[p]])
        emit_rot_weights(P, wq, wqr, 4)
        for p in range(3):
            emit_rot_weights(P, wk[p], wkr[p], 2)
        modp = emit_mod_cols(P, nc, adaw_d, adabT_d, cT_d, 2 * D, None, ps[0])
        P.op("dve", lambda e: e.tensor_scalar(modp.t[:, 8:16], modp.t[:, 8:16], 1.0, None, ALU.add), reads=[modp], writes=[modp])

        NSP = S_LEN // SPAN
        hT = sb("hT", [128, 8, SPAN], BF16)
        xts = [sb("xts%d" % i, [128, D], F32) for i in range(2)]
        qT = [sb("qT%d" % i, [128, 2, SPAN], BF16) for i in range(2)]
        kT = [[sb("kT%d_%d" % (p, s_), [128, SPAN], BF16) for s_ in range(2)] for p in range(3)]
        V = [[sb("V%d_%d" % (p, s_), [128, 16, 80], BF16) for s_ in range(2)] for p in range(3)]
        posI = sb("posI", [128, SPAN], I32)
        posB = sb("posB", [128, SPAN], F32)
        Ct = sb("Ct", [128, SPAN], F32)
        St = sb("St", [128, SPAN], F32)
        tmpT = sb("tmpT", [128, SPAN], F32)
        tmpa = sb("tmpa", [128, 512], F32)
        tmpb = sb("tmpb", [128, 512], F32)
        PT = [sb("PT%d" % i, [128, 512], BF16) for i in range(4)]
        accs = [sb("accs%d" % i, [128, 260], F32) for i in range(3)]
        for p in range(3):
            for s_ in range(2):
                P.op("pool", lambda e, p=p, s_=s_: e.memset(V[p][s_].t[:, :, 64:65], 1.0), writes=[V[p][s_]])
        OPb = [Buf("OP%d" % p) for p in range(3)]
        import os
        STG = int(os.environ.get("DILSTAGE", "9"))
        ATT = int(os.environ.get("DILATT", "9"))
        NSP = int(os.environ.get("DILNSP", str(NSP)))
        nS = 0
        nPT = 0
        nacc = 0
        for s in range(NSP):
            sl = s % 2
            tok0 = s * SPAN
            for ti in range(16):
                emit_hmodT_tile(P, x_d, tok0 + ti * 128, xts[ti % 2], psX, idn, modp, hT, ti * 128, 8, ["sp", "act"][ti % 2])
            if STG < 2:
                continue
            emit_rope_tables(P, posB, posI, invp, negpi, Ct, St, tmpT, pos_d[0:1, tok0:tok0 + SPAN], SPAN)
            if STG < 3:
                continue
            for qc in range(2):
                for tg in range(4):
                    emit_proj_rope(P, hT, tg * 512, 512, wq, wqr, qc * 128, ps[0], ps[1], Ct, St, tg * 512, tmpa, tmpb,
                                   lambda qc=qc, tg=tg, sl=sl: qT[sl].t[:, qc, tg * 512:(tg + 1) * 512], qT[sl])
            for p in range(3):
                for tg in range(4):
                    emit_proj_rope(P, hT, tg * 512, 512, wk[p], wkr[p], 0, ps[0], ps[1], Ct, St, tg * 512, tmpa, tmpb,
                                   lambda p=p, tg=tg, sl=sl: kT[p][sl].t[:, tg * 512:(tg + 1) * 512], kT[p][sl])
            if STG < 4:
                continue
            for p, d in enumerate(DIL):
                ncb = 16 // d
                for r in range(d):
                    for c in range(ncb):
                        idx = r * ncb + c
                        a0 = r + d * 128 * c
                        pv = ps[2 + idx % 2]
                        for k in range(8):
                            P.op("pe", lambda e, k=k, a0=a0, d=d, p=p, pv=pv: e.matmul(
                                pv.t[:, 0:64], hT.t[:, k, ss(a0, 128, d)], wv[p].t[:, k, :],
                                start=(k == 0), stop=(k == 7)), reads=[hT, wv[p]], writes=[pv])
                        P.op("act", lambda e, p=p, sl=sl, idx=idx, pv=pv: e.copy(V[p][sl].t[:, idx, 0:64], pv.t[:, 0:64]),
                             reads=[pv], writes=[V[p][sl]])
            if STG < 5:
                continue
            for p, d in enumerate(DIL):
                ncb = 16 // d
                for r in range(d):
                    for j in range(ncb):
                        acc = ps[4 + nacc % 2]
                        chunks = [(j - 1, 0), (j, 1)]
                        chunks = [(kb, mc) for kb, mc in chunks if not (s == 0 and kb < 0)]
                        pts = []
                        for ci, (kb, mc) in enumerate(chunks):
                            ksl = sl if kb >= 0 else 1 - sl
                            kbb = kb if kb >= 0 else ncb - 1
                            ka0 = r + d * 128 * kbb
                            qa0 = r + d * 128 * j
                            Sp = ps[nS % 2]
                            nS += 1
                            for h in range(4):
                                qc, hf = h // 2, h % 2
                                rows = slice(hf * 64, (hf + 1) * 64)
                                P.op("pe", lambda e, h=h, qc=qc, rows=rows, p=p, ksl=ksl, ka0=ka0, qa0=qa0, d=d, sl=sl, Sp=Sp: e.matmul(
                                    Sp.t[:, h * 128:(h + 1) * 128],
                                    kT[p][ksl].t[rows, ss(ka0, 128, d)],
                                    qT[sl].t[rows, qc, ss(qa0, 128, d)],
                                    start=True, stop=False), reads=[kT[p][ksl], qT[sl]], writes=[Sp])
                                P.op("pe", lambda e, h=h, mc=mc, Sp=Sp: e.matmul(Sp.t[:, h * 128:(h + 1) * 128], idnb.t[:], msk.t[:, mc, 0:128],
                                                                              start=False, stop=True), reads=[idnb, msk], writes=[Sp])
                            pt = PT[nPT % 4]
                            nPT += 1
                            P.op("act", lambda e, pt=pt, Sp=Sp: e.activation(pt.t[:], Sp.t[:, 0:512], AF.Exp, scale=0.125), reads=[Sp], writes=[pt])
                            pts.append((pt, ksl, r * ncb + kbb))
                        for h in range(4):
                            for ci, (pt, ksl, vidx) in enumerate(pts):
                                P.op("pe", lambda e, h=h, pt=pt, p=p, ksl=ksl, vidx=vidx, acc=acc, ci=ci, nch=len(pts): e.matmul(
                                    acc.t[:, h * 80:h * 80 + 65], pt.t[:, h * 128:(h + 1) * 128], V[p][ksl].t[:, vidx, 0:65],
                                    start=(ci == 0), stop=(ci == nch - 1)), reads=[pt, V[p][ksl]], writes=[acc])
                        ab = accs[nacc % 3]
                        nacc += 1
                        if ATT < 5:
                            continue
                        P.op("dve", lambda e, ab=ab, acc=acc: e.tensor_copy(ab.t[:].rearrange("p (h e) -> p h e", e=65), acc.t[:, 0:320].rearrange("p (h e) -> p h e", e=80)[:, :, 0:65]), reads=[acc], writes=[ab])
                        g0 = tok0 + r + d * 128 * j
                        if ATT < 6:
                            continue
                        P.dma("sp", lambda e, ab=ab, p=p, g0=g0, d=d: e.dma_start(
                            out=OP[p][ss(g0, 128, d), :], in_=ab.t[:]), reads=[ab], writes=[OPb[p]])
        P.barrier()
        if STG < 6:
            P.emit()
            return nc
        ld = [[sb("ld%d_%d" % (p, i), [128, 260], F32) for i in range(2)] for p in range(3)]
        rl = sb("rl", [128, 4], F32)
        ot = [sb("otl%d" % i, [128, 256], F32) for i in range(2)]
        Ob = Buf("o")
        for T in range(S_LEN // 128):
            b = T % 2
            for p in range(3):
                P.dma(["sp", "act", "sp"][p], lambda e, p=p, b=b, T=T: e.dma_start(out=ld[p][b].t[:], in_=OP[p][T * 128:(T + 1) * 128, :]),
                      reads=[OPb[p]], writes=[ld[p][b]])
            P.op("dve", lambda e, b=b: e.tensor_tensor(ld[0][b].t[:], ld[0][b].t[:], ld[1][b].t[:], ALU.add), reads=[ld[0][b], ld[1][b]], writes=[ld[0][b]])
            P.op("dve", lambda e, b=b: e.tensor_tensor(ld[0][b].t[:], ld[0][b].t[:], ld[2][b].t[:], ALU.add), reads=[ld[0][b], ld[2][b]], writes=[ld[0][b]])
            a3 = ld[0][b].t[:].rearrange("p (h e) -> p h e", e=65)
            P.op("dve", lambda e, a3=a3: e.reciprocal(rl.t[:], a3[:, :, 64]), reads=[ld[0][b]], writes=[rl])
            P.op("dve", lambda e, a3=a3, b=b: e.tensor_tensor(ot[b].t[:].rearrange("p (h e) -> p h e", e=64), a3[:, :, 0:64],
                                                             rl.t[:].unsqueeze(2).to_broadcast([128, 4, 64]), ALU.mult),
                 reads=[ld[0][b], rl], writes=[ot[b]])
            P.dma("sp", lambda e, b=b, T=T: e.dma_start(out=o_d[T * 128:(T + 1) * 128, :], in_=ot[b].t[:]), reads=[ot[b]], writes=[Ob])
        P.wait_all("sp", [Ob])
        P.emit()
    return nc


def run_dil(x, c, positions, ada_w_s, ada_b_s, w_in):
    if "dil" not in _CACHE:
        _CACHE["dil"] = build_dil()
    nc = _CACHE["dil"]
    idn = np.eye(128, dtype=np.float32)
    inv = rope_inv_table()
    msk = tri_masks()
    in_maps = []
    for core in range(8):
        b, g = core // 4, core % 4
        wk = np.stack([w_in[:, D + p * 512 + g * 64: D + p * 512 + g * 64 + 64] for p in range(3)])
        wv = np.stack([w_in[:, D + p * 512 + 256 + g * 64: D + p * 512 + 256 + g * 64 + 64] for p in range(3)])
        in_maps.append({
            "x": x[b], "cT": np.ascontiguousarray(c[b].reshape(8, 128).T),
            "adaw": np.ascontiguousarray(ada_w_s[:, 0:2 * D]), "adabT": np.ascontiguousarray(ada_b_s[0:2 * D].reshape(16, 128).T),
            "wq": np.ascontiguousarray(w_in[:, g * 256:(g + 1) * 256]), "wk": np.ascontiguousarray(wk), "wv": np.ascontiguousarray(wv),
            "pos": np.ascontiguousarray(positions[b:b + 1]), "inv": inv, "idn": idn, "msk": msk,
        })
    res = run_bass_kernel_spmd(nc, in_maps, core_ids=list(range(8)))
    B = x.shape[0]
    o = np.zeros((B, S_LEN, D), np.float32)
    for core in range(8):
        b, g = core // 4, core % 4
        o[b, :, g * 256:(g + 1) * 256] = res.results[core]["o"]
    return o


QG = 512
NG = S_LEN // QG
NCMP = 1023


def nsa_masks():
    p = np.arange(128)[:, None]
    f = np.arange(512)[None, :]
    ms = []
    for jj in range(4):
        ms.append(np.where(f - p - 128 * jj >= 0, 0.0, NEG))
    for c in range(4):
        ms.append(np.where(f - p < 128 * c, 0.0, NEG))
    for m in range(5):
        ms.append(np.where(f - 16 * p + 512 * m - 31 >= 0, 0.0, NEG))
    return np.stack(ms, axis=1).astype(np.float32)


def nsa_indc():
    t = np.zeros((128, 64, 128), np.float32)
    for jm in range(64):
        t[2 * jm, jm, 0:64] = 1.0
        t[2 * jm + 1, jm, 64:128] = 1.0
    return t


def nsa_vc_const():
    t = np.zeros((1024, 257), np.float32)
    t[:, 0] = 1.0
    for s_ in range(256):
        for n in range(max(0, 4 * s_ - 1), min(NCMP, 4 * s_ + 4)):
            t[n, 1 + s_] = 1.0
    return t


def nsa_forced():
    t = np.zeros((128, 3), np.float32)
    for p in range(128):
        c = p // 64
        for x in (-1, 0, 1):
            if x == c or x == c - 1:
                t[p, x + 1] = 1.0e4
    return t


def build_nsa():
    nc = bass.Bass("TRN2", target_bir_lowering=False)
    dt_in = lambda name, shape, dt=F32: nc.dram_tensor(name, list(shape), dt, kind="ExternalInput").ap()
    x_d = dt_in("x", [S_LEN, D])
    cT_d = dt_in("cT", [128, 8])
    adaw_d = dt_in("adaw", [D, 2 * D])
    adabT_d = dt_in("adabT", [128, 16])
    wq_d = dt_in("wq", [D, 256])
    wkv_d = dt_in("wkv", [6, D, 64])
    wgt_d = dt_in("wgt", [D, 12])
    pos_d = dt_in("pos", [1, S_LEN], I32)
    posc_d = dt_in("posc", [1, 1024], I32)
    inv_d = dt_in("inv", [128, 1])
    idn_d = dt_in("idn", [128, 128])
    msk_d = dt_in("msk", [128, 13, 512])
    indc_d = dt_in("indc", [128, 64, 128])
    vcc_d = dt_in("vcc", [1024, 257])
    frc_d = dt_in("frc", [128, 3])
    w1_d = dt_in("w1", [2, 2048, 256])
    w2_d = dt_in("w2", [2, 256, 64])
    cpos_d = dt_in("cposT", [128, 32])
    o_d = nc.dram_tensor("o", [S_LEN, 256], F32, kind="ExternalOutput").ap()

    with ExitStack() as st:
        P = Prog(nc, st)
        sb = P.sbuf
        idn = sb("idn", [128, 128], F32)
        idnb = sb("idnb", [128, 128], BF16)
        msk = sb("msk", [128, 13, 512], BF16)
        indc = sb("indc", [128, 64, 128], BF16)
        frc = sb("frc", [128, 3], F32)
        invp = sb("invp", [128, 1], F32)
        wq = sb("wq", [128, 8, 256], BF16)
        wqr = sb("wqr", [128, 8, 256], BF16)
        wks = sb("wks", [128, 8, 128], BF16)
        wksr = sb("wksr", [128, 8, 128], BF16)
        wkw = sb("wkw", [128, 8, 128], BF16)
        wkwr = sb("wkwr", [128, 8, 128], BF16)
        wkvc = sb("wkvc", [128, 8, 128], BF16)
        wvs = sb("wvs", [128, 8, 64], BF16)
        wvw = sb("wvw", [128, 8, 64], BF16)
        wgt = sb("wgt", [128, 8, 12], BF16)
        kselT = sb("kselT", [128, S_LEN], BF16)
        Vsel = sb("Vsel", [128, 128, 80], BF16)
        kcT = sb("kcT", [128, 1024], BF16)
        VC = sb("VC", [128, 8, 336], BF16)
        ps = [P.psum("pb%d" % i, [128, 512], F32) for i in range(6)]
        psXb = P.psum("psX", [128, 512], F32)
        psS3 = P.psum("psS3", [128, 512], F32)
        spr = [ps[0], ps[1], psS3]

        def ld(q, dst, src, ap=None):
            P.dma(q, lambda e: e.dma_start(out=dst.t[:] if ap is None else ap, in_=src), writes=[dst])
        ld("sp", idn, idn_d)
        ld("pool", idnb, idn_d)
        ld("pool", msk, msk_d)
        ld("pool", indc, indc_d)
        ld("sp", frc, frc_d)
        ld("sp", invp, inv_d)
        kp = lambda a: a.rearrange("(k p) n -> p k n", p=128)
        ld("pool", wq, kp(wq_d))
        for h in range(2):
            ld("pool", wks, kp(wkv_d[2]), wks.t[:, :, h * 64:(h + 1) * 64])
            ld("pool", wkw, kp(wkv_d[4]), wkw.t[:, :, h * 64:(h + 1) * 64])
        ld("pool", wkvc, kp(wkv_d[0]), wkvc.t[:, :, 0:64])
        ld("pool", wkvc, kp(wkv_d[1]), wkvc.t[:, :, 64:128])
        ld("pool", wvs, kp(wkv_d[3]))
        ld("pool", wvw, kp(wkv_d[5]))
        ld("pool", wgt, kp(wgt_d))
        emit_rot_weights(P, wq, wqr, 4)
        emit_rot_weights(P, wks, wksr, 2)
        emit_rot_weights(P, wkw, wkwr, 2)
        P.op("pool", lambda e: e.memset(Vsel.t[:, :, 64:65], 1.0), writes=[Vsel])
        for c in range(8):
            P.dma("pool", lambda e, c=c: e.dma_start(out=VC.t[:, c, 64:321], in_=vcc_d[c * 128:(c + 1) * 128, :]), writes=[VC])
        modp = emit_mod_cols(P, nc, adaw_d, adabT_d, cT_d, 2 * D, None, ps[0])
        P.op("dve", lambda e: e.tensor_scalar(modp.t[:, 8:16], modp.t[:, 8:16], 1.0, None, ALU.add), reads=[modp], writes=[modp])

        hT = sb("hT", [128, 8, QG], BF16)
        xts = [sb("xts%d" % i, [128, D], F32) for i in range(2)]
        posI = sb("posI", [128, 512], I32)
        posB = sb("posB", [128, 512], F32)
        Ct = sb("Ct", [128, 512], F32)
        St = sb("St", [128, 512], F32)
        tmpT = sb("tmpT", [128, 512], F32)
        tmpa = sb("tmpa", [128, 512], F32)
        tmpb = sb("tmpb", [128, 512], F32)

        import os
        NST = int(os.environ.get("NSA_STAGE", "9"))
        if NST < 2:
            P.barrier(); P.emit(); return nc
        P.push()
        kvcT = sb("kvcT", [128, S_LEN], BF16)
        for G in range(NG):
            t0 = G * QG
            for ti in range(4):
                emit_hmodT_tile(P, x_d, t0 + ti * 128, xts[ti % 2], psXb, idn, modp, hT, ti * 128, 8, ["sp", "act"][ti % 2], width=512)
            emit_rope_tables(P, posB, posI, invp, None, Ct, St, tmpT, pos_d[0:1, t0:t0 + QG], QG)
            emit_proj_rope(P, hT, 0, QG, wks, wksr, 0, ps[0], ps[1], Ct, St, 0, tmpa, tmpb,
                           lambda t0=t0: kselT.t[:, t0:t0 + QG], kselT)
            for k in range(8):
                P.op("pe", lambda e, k=k: e.matmul(ps[2].t[:, 0:QG], wkvc.t[:, k, :], hT.t[:, k, :], start=(k == 0), stop=(k == 7)),
                     reads=[wkvc, hT], writes=[ps[2]])
            P.op("act", lambda e, t0=t0: e.copy(kvcT.t[:, t0:t0 + QG], ps[2].t[:, 0:QG]), reads=[ps[2]], writes=[kvcT])
            for ti in range(4):
                pv = ps[3 + ti % 2]
                for k in range(8):
                    P.op("pe", lambda e, k=k, ti=ti, pv=pv: e.matmul(pv.t[:, 0:64], hT.t[:, k, ti * 128:(ti + 1) * 128], wvs.t[:, k, :],
                                                                     start=(k == 0), stop=(k == 7)), reads=[hT, wvs], writes=[pv])
                P.op("act", lambda e, ti=ti, G=G, pv=pv: e.copy(Vsel.t[:, G * 4 + ti, 0:64], pv.t[:, 0:64]), reads=[pv], writes=[Vsel])

        if NST < 3:
            P.barrier(); P.emit(); return nc
        w1 = sb("w1", [128, 32, 256], BF16)
        w2k = sb("w2k", [128, 2, 128], BF16)
        w2kr = sb("w2kr", [128, 2, 128], BF16)
        w2v = sb("w2v", [128, 2, 64], BF16)
        cposT = sb("cposT", [128, 32], BF16)
        cbias = sb("cbias", [128, 4], F32)
        hid = [[sb("hid%d_%d" % (kv, hc), [128, 1024], BF16) for hc in range(2)] for kv in range(2)]
        xg = sb("xg", [128, 512], F32)
        ug = sb("ug", [128, 512], F32)
        for kv in range(2):
            P.dma("pool", lambda e, kv=kv: e.dma_start(out=w1.t[kv * 64:(kv + 1) * 64, :, :], in_=w1_d[kv].rearrange("(l e) h -> e l h", e=64)), writes=[w1])
        for h in range(2):
            P.dma("pool", lambda e, h=h: e.dma_start(out=w2k.t[:, :, h * 64:(h + 1) * 64], in_=w2_d[0].rearrange("(k p) n -> p k n", p=128)), writes=[w2k])
        P.dma("pool", lambda e: e.dma_start(out=w2v.t[:], in_=w2_d[1].rearrange("(k p) n -> p k n", p=128)), writes=[w2v])
        P.dma("pool", lambda e: e.dma_start(out=cposT.t[:], in_=cpos_d), writes=[cposT])
        P.op("pool", lambda e: e.memset(w2kr.t[:], 0.0), writes=[w2kr])
        for k in range(2):
            wv_ = w2k.t[:, k, :].rearrange("p (h e) -> p h e", e=64)
            rv_ = w2kr.t[:, k, :].rearrange("p (h e) -> p h e", e=64)
            P.op("dve", lambda e, wv_=wv_, rv_=rv_: e.tensor_scalar(rv_[:, :, 0:8], wv_[:, :, 8:16], -1.0, None, ALU.mult), reads=[w2k], writes=[w2kr])
            P.op("dve", lambda e, wv_=wv_, rv_=rv_: e.tensor_copy(rv_[:, :, 8:16], wv_[:, :, 0:8]), reads=[w2k], writes=[w2kr])
        for kv in range(2):
            for hc in range(2):
                P.op("pool", lambda e, kv=kv, hc=hc: e.memset(hid[kv][hc].t[:], 0.0), writes=[hid[kv][hc]])
        for kv in range(2):
            rows = slice(kv * 64, (kv + 1) * 64)
            for hc in range(2):
                col = kv * 2 + hc
                for l in range(32):
                    P.op("pe", lambda e, rows=rows, hc=hc, l=l, col=col: e.matmul(
                        ps[0].t[:, col:col + 1], w1.t[rows, l, hc * 128:(hc + 1) * 128], cposT.t[rows, l:l + 1],
                        start=(l == 0), stop=(l == 31)), reads=[w1, cposT], writes=[ps[0]])
        P.op("dve", lambda e: e.tensor_copy(cbias.t[:], ps[0].t[:, 0:4]), reads=[ps[0]], writes=[cbias])
        for kv in range(2):
            rows = slice(kv * 64, (kv + 1) * 64)
            for hc in range(2):
                col = kv * 2 + hc
                for gi in range(2):
                    n0 = gi * 512
                    nn = 512 if gi == 0 else 511
                    pz = ps[1 + (hc * 2 + gi) % 2]
                    for l in range(32):
                        P.op("pe", lambda e, rows=rows, hc=hc, l=l, n0=n0, nn=nn, pz=pz: e.matmul(
                            pz.t[:, 0:nn], w1.t[rows, l, hc * 128:(hc + 1) * 128], kvcT.t[rows, ss(16 * n0 + l, nn, 16)],
                            start=(l == 0), stop=(l == 31)), reads=[w1, kvcT], writes=[pz])
                    P.op("act", lambda e, pz=pz, nn=nn, col=col: e.activation(xg.t[:, 0:nn], pz.t[:, 0:nn], AF.Identity, bias=cbias.t[:, col:col + 1]),
                         reads=[pz, cbias], writes=[xg])
                    P.op("dve", lambda e, nn=nn: e.tensor_tensor(ug.t[:, 0:nn], xg.t[:, 0:nn], xg.t[:, 0:nn], ALU.mult), reads=[xg], writes=[ug])
                    P.op("dve", lambda e, nn=nn: e.tensor_scalar(ug.t[:, 0:nn], ug.t[:, 0:nn], 0.044715, 1.0, ALU.mult, ALU.add), reads=[ug], writes=[ug])
                    P.op("dve", lambda e, nn=nn: e.tensor_tensor(ug.t[:, 0:nn], ug.t[:, 0:nn], xg.t[:, 0:nn], ALU.mult), reads=[ug, xg], writes=[ug])
                    P.op("act", lambda e, nn=nn: e.activation(ug.t[:, 0:nn], ug.t[:, 0:nn], AF.Sigmoid, scale=1.5957691216057308), reads=[ug], writes=[ug])
                    P.op("dve", lambda e, nn=nn, kv=kv, hc=hc, n0=n0: e.tensor_tensor(hid[kv][hc].t[:, n0:n0 + nn], ug.t[:, 0:nn], xg.t[:, 0:nn], ALU.mult),
                         reads=[ug, xg], writes=[hid[kv][hc]])
        for gi in range(2):
            n0 = gi * 512
            emit_rope_tables(P, posB, posI, invp, None, Ct, St, tmpT, posc_d[0:1, n0:n0 + 512], 512)
            for hc in range(2):
                P.op("pe", lambda e, hc=hc, n0=n0: e.matmul(ps[0].t[:, 0:512], w2k.t[:, hc, :], hid[0][hc].t[:, n0:n0 + 512], start=(hc == 0), stop=(hc == 1)),
                     reads=[w2k, hid[0][hc]], writes=[ps[0]])
            for hc in range(2):
                P.op("pe", lambda e, hc=hc, n0=n0: e.matmul(ps[1].t[:, 0:512], w2kr.t[:, hc, :], hid[0][hc].t[:, n0:n0 + 512], start=(hc == 0), stop=(hc == 1)),
                     reads=[w2kr, hid[0][hc]], writes=[ps[1]])
            P.op("dve", lambda e, n0=n0: e.tensor_tensor(tmpa.t[:], ps[0].t[:, 0:512], Ct.t[:, 0:512], ALU.mult), reads=[ps[0], Ct], writes=[tmpa])
            P.op("dve", lambda e, n0=n0: e.tensor_tensor(tmpb.t[:], ps[1].t[:, 0:512], St.t[:, 0:512], ALU.mult), reads=[ps[1], St], writes=[tmpb])
            P.op("pool", lambda e, n0=n0: e.tensor_tensor(kcT.t[:, n0:n0 + 512], tmpa.t[:], tmpb.t[:], ALU.add), reads=[tmpa, tmpb], writes=[kcT])
        for c in range(8):
            pv = ps[2 + c % 2]
            for hc in range(2):
                P.op("pe", lambda e, hc=hc, c=c, pv=pv: e.matmul(pv.t[:, 0:64], hid[1][hc].t[:, c * 128:(c + 1) * 128], w2v.t[:, hc, :],
                                                                 start=(hc == 0), stop=(hc == 1)), reads=[hid[1][hc], w2v], writes=[pv])
            P.op("act", lambda e, c=c, pv=pv: e.copy(VC.t[:, c, 0:64], pv.t[:, 0:64]), reads=[pv], writes=[VC])
        P.pop()
        if NST < 4:
            P.barrier(); P.emit(); return nc

        qT = sb("qT", [128, 2, QG], BF16)
        kwT = [sb("kwT%d" % i, [128, QG], BF16) for i in range(2)]
        Vw = sb("Vw", [128, 8, 80], BF16)
        gts = sb("gts", [128, 4, 12], F32)
        PT = [sb("PT%d" % i, [128, 512], BF16) for i in range(4)]
        nbT = sb("nbT", [128, 2, QG], BF16)
        imp = sb("imp", [128, 256], F32)
        impw = sb("impw", [128, 256], F32)
        m8a = sb("m8a", [128, 8], F32)
        m8b = sb("m8b", [128, 8], F32)
        nb = sb("nb", [128, 256], F32)
        nbb = sb("nbb", [128, 256], BF16)
        rl = sb("rl", [128, 1], F32)
        ocs = sb("ocs", [128, 64], F32)
        osb = [sb("osb%d" % i, [128, 256], F32) for i in range(4)]
        impU = [sb("impU%d" % i, [128, 256], F32) for i in range(2)]
        P.op("pool", lambda e: e.memset(Vw.t[:, :, 64:65], 1.0), writes=[Vw])
        Ob = Buf("o")
        acc = [ps[2], ps[3], ps[4], ps[5]]
        cnt = {"S": 0, "PT": 0, "first": [True] * 4}

        def attend(h, chunks, ncols, G):
            qc, hf = h // 2, h % 2
            rows = slice(hf * 64, (hf + 1) * 64)
            first = {s_: True for s_ in range(4)}
            last_idx = {}
            for ci, ch in enumerate(chunks):
                for s_ in range(ch[3], ch[4] + 1):
                    last_idx[s_] = ci
            def qk(ci):
                kfn, masks, vfn, slo, shi = chunks[ci]
                Sp = spr[cnt["S"] % 3]
                cnt["S"] += 1
                P.op("pe", lambda e, kfn=kfn, Sp=Sp, nm0=len(masks): e.matmul(Sp.t[:, 0:QG], kfn(rows), qT.t[rows, qc, :], start=True, stop=(nm0 == 0)),
                     reads=[kselT, kcT, kwT[0], kwT[1], qT], writes=[Sp])
                for mi, (lf, rf) in enumerate(masks):
                    P.op("pe", lambda e, lf=lf, rf=rf, Sp=Sp, mi=mi, nm=len(masks): e.matmul(Sp.t[:, 0:QG], lf(), rf(), start=False, stop=(mi == nm - 1)),
                         reads=[idnb, msk, indc, nbT], writes=[Sp])
                return Sp

            def ex(ci, Sp):
                pt = PT[cnt["PT"] % 4]
                cnt["PT"] += 1
                P.op("act", lambda e, pt=pt, Sp=Sp: e.activation(pt.t[:], Sp.t[:, 0:QG], AF.Exp, scale=0.125), reads=[Sp], writes=[pt])
                return pt

            def pv(ci, pt):
                kfn, masks, vfn, slo, shi = chunks[ci]
                for s_ in range(slo, shi + 1):
                    P.op("pe", lambda e, s_=s_, pt=pt, vfn=vfn, st_=first[s_], sp_=(last_idx[s_] == ci): e.matmul(
                        acc[s_].t[:, 0:ncols], pt.t[:, s_ * 128:(s_ + 1) * 128], vfn(), start=st_, stop=sp_),
                        reads=[pt, Vsel, VC, Vw], writes=[acc[s_]])
                    first[s_] = False

            sps = {0: qk(0)}
            if len(chunks) > 1:
                sps[1] = qk(1)
            for ci in range(len(chunks)):
                pt = ex(ci, sps.pop(ci))
                if ci + 2 < len(chunks):
                    sps[ci + 2] = qk(ci + 2)
                pv(ci, pt)

        stg = [sb("stg%d" % br, [128, 4, 4, 65], F32) for br in range(3)]
        stgB = [[Buf("stgB%d_%d" % (br, s_), stg[br].t) for s_ in range(4)] for br in range(3)]
        impS = sb("impS", [128, 4, 4, 256], F32)
        impSB = [Buf("impSB%d" % s_, impS.t) for s_ in range(4)]
        osb4 = sb("osb4", [128, 4, 4, 64], F32)
        otmp = sb("otmp", [128, 4, 4, 64], F32)
        rlb = sb("rlb", [128, 4, 4], F32)
        wgb = sb("wgb", [128, 4, 4], F32)
        impacc4 = sb("impacc4", [128, 4, 256], F32)

        def evac(h, s_, br):
            P.op("dve", lambda e: e.tensor_copy(stg[br].t[:, s_, h, :], acc[s_].t[:, 0:65]), reads=[acc[s_]], writes=[stgB[br][s_]])
            if br == 0:
                P.op("dve", lambda e: e.tensor_copy(impS.t[:, s_, h, :], acc[s_].t[:, 65:321]), reads=[acc[s_]], writes=[impSB[s_]])

        def finish_branch(br, first_branch):
            P.op("dve", lambda e: e.tensor_scalar(rlb.t[:], stg[br].t[:, :, :, 64], 1e-30, None, ALU.max), reads=stgB[br], writes=[rlb])
            P.op("dve", lambda e: e.reciprocal(rlb.t[:], rlb.t[:]), reads=[rlb], writes=[rlb])
            P.op("dve", lambda e: e.tensor_tensor(wgb.t[:], rlb.t[:], gts.t[:, :, ss(br, 4, 3)], ALU.mult), reads=[rlb, gts], writes=[wgb])
            dst = osb4 if first_branch else otmp
            P.op("dve", lambda e: e.tensor_tensor(dst.t[:], stg[br].t[:, :, :, 0:64], wgb.t[:].unsqueeze(3).to_broadcast([128, 4, 4, 64]), ALU.mult),
                 reads=stgB[br] + [wgb], writes=[dst])
            if not first_branch:
                P.op("pool", lambda e: e.tensor_tensor(osb4.t[:], osb4.t[:], otmp.t[:], ALU.add), reads=[otmp, osb4], writes=[osb4])
            if br == 0:
                P.op("dve", lambda e: e.tensor_tensor(impS.t[:], impS.t[:], rlb.t[:].unsqueeze(3).to_broadcast([128, 4, 4, 256]), ALU.mult),
                     reads=impSB + [rlb], writes=impSB)
                P.op("pool", lambda e: e.tensor_tensor(impacc4.t[:], impS.t[:, :, 0, :], impS.t[:, :, 1, :], ALU.add), reads=impSB, writes=[impacc4])
                P.op("pool", lambda e: e.tensor_tensor(impacc4.t[:], impacc4.t[:], impS.t[:, :, 2, :], ALU.add), reads=impSB + [impacc4], writes=[impacc4])
                P.op("pool", lambda e: e.tensor_tensor(impacc4.t[:], impacc4.t[:], impS.t[:, :, 3, :], ALU.add), reads=impSB + [impacc4], writes=[impacc4])

        import os
        NG_RUN = int(os.environ.get("NSA_NG", str(NG)))
        for G in range(NG_RUN):
            t0 = G * QG
            sl = G % 2
            for ti in range(4):
                emit_hmodT_tile(P, x_d, t0 + ti * 128, xts[ti % 2], psXb, idn, modp, hT, ti * 128, 8, ["sp", "act"][ti % 2], width=512)
            emit_rope_tables(P, posB, posI, invp, None, Ct, St, tmpT, pos_d[0:1, t0:t0 + QG], QG)
            for qc in range(2):
                emit_proj_rope(P, hT, 0, QG, wq, wqr, qc * 128, ps[0], ps[1], Ct, St, 0, tmpa, tmpb,
                               lambda qc=qc: qT.t[:, qc, :], qT)
            emit_proj_rope(P, hT, 0, QG, wkw, wkwr, 0, ps[0], ps[1], Ct, St, 0, tmpa, tmpb, lambda sl=sl: kwT[sl].t[:, :], kwT[sl])
            for ti in range(4):
                pv = ps[2 + ti % 2]
                for k in range(8):
                    P.op("pe", lambda e, k=k, ti=ti, pv=pv: e.matmul(pv.t[:, 0:64], hT.t[:, k, ti * 128:(ti + 1) * 128], wvw.t[:, k, :],
                                                                     start=(k == 0), stop=(k == 7)), reads=[hT, wvw], writes=[pv])
                P.op("act", lambda e, ti=ti, sl=sl, pv=pv: e.copy(Vw.t[:, sl * 4 + ti, 0:64], pv.t[:, 0:64]), reads=[pv], writes=[Vw])
                pg = ps[4 + ti % 2]
                for k in range(8):
                    P.op("pe", lambda e, k=k, ti=ti, pg=pg: e.matmul(pg.t[:, 0:12], hT.t[:, k, ti * 128:(ti + 1) * 128], wgt.t[:, k, :],
                                                                     start=(k == 0), stop=(k == 7)), reads=[hT, wgt], writes=[pg])
                P.op("act", lambda e, ti=ti, pg=pg: e.activation(gts.t[:, ti, :], pg.t[:, 0:12], AF.Sigmoid), reads=[pg], writes=[gts])

            if NST < 5:
                continue
            cmax = min(7, G // 4)
            for h in range(4):
                chunks = []
                for c in range(cmax + 1):
                    m = G - 4 * c
                    masks = []
                    if m <= 4:
                        masks = [(lambda: idnb.t[:], lambda m=m: msk.t[:, 8 + m, :])]
                    chunks.append((lambda rows, c=c: kcT.t[rows, c * 128:(c + 1) * 128], masks, lambda c=c: VC.t[:, c, 0:321], 0, 3))
                attend(h, chunks, 321, G)
                for s_ in range(4):
                    evac(h, s_, 0)
            finish_branch(0, True)
            if NST < 6:
                continue
            for s_ in range(4):
                T = G * 4 + s_
                ia = Buf("iav", impacc4.t[:, s_, :])
                lo = max(0, 2 * T - 1)
                hi = min(256, 2 * T + 2)
                P.op("dve", lambda e, ia=ia, lo=lo, hi=hi, T=T: e.tensor_tensor(ia.t[:, lo:hi], ia.t[:, lo:hi], frc.t[:, lo - (2 * T - 1):hi - (2 * T - 1)], ALU.add),
                     reads=[impacc4, frc], writes=[impacc4])
                P.op("dve", lambda e, ia=ia: e.tensor_scalar(ia.t[:, 0:1], ia.t[:, 0:1], 1.0e4, None, ALU.add), reads=[impacc4], writes=[impacc4])
                P.op("dve", lambda e, ia=ia: e.max(out=m8a.t[:], in_=ia.t[:]), reads=[impacc4], writes=[m8a])
                P.op("dve", lambda e, ia=ia: e.match_replace(out=impw.t[:], in_to_replace=m8a.t[:], in_values=ia.t[:], imm_value=-1.0e30),
                     reads=[impacc4, m8a], writes=[impw])
                P.op("dve", lambda e: e.max(out=m8b.t[:], in_=impw.t[:]), reads=[impw], writes=[m8b])
                P.op("dve", lambda e, ia=ia: e.tensor_scalar(nb.t[:], ia.t[:], m8b.t[:, 7:8], NEG, ALU.is_lt, ALU.mult), reads=[impacc4, m8b], writes=[nb])
                for tb in range(2):
                    P.op("pe", lambda e, tb=tb: e.transpose(psXb.t[:, tb * 128:(tb + 1) * 128], nb.t[:, tb * 128:(tb + 1) * 128], idn.t[:]),
                         reads=[nb, idn], writes=[psXb])
                P.op("act", lambda e, s_=s_: e.copy(nbT.t[:, :, s_ * 128:(s_ + 1) * 128], psXb.t[:, 0:256].rearrange("p (t q) -> p t q", q=128)),
                     reads=[psXb], writes=[nbT])
            if NST < 7:
                continue
            for h in range(4):
                chunks = []
                for j in range(4 * G + 4):
                    tb = j // 64
                    jm = j % 64
                    masks = [(lambda jm=jm: indc.t[:, jm, :], lambda tb=tb: nbT.t[:, tb, :])]
                    jj = j - 4 * G
                    if jj >= 0:
                        masks.append((lambda: idnb.t[:], lambda jj=jj: msk.t[:, jj, :]))
                    chunks.append((lambda rows, j=j: kselT.t[rows, j * 128:(j + 1) * 128], masks, lambda j=j: Vsel.t[:, j, 0:65],
                                   max(0, jj), 3))
                attend(h, chunks, 65, G)
                for s_ in range(4):
                    evac(h, s_, 1)
            finish_branch(1, False)
            if NST < 8:
                continue
            for h in range(4):
                chunks = []
                for c in range(8):
                    if t0 - 512 + 128 * c < 0:
                        continue
                    if c < 4:
                        kfn = lambda rows, c=c, sl=sl: kwT[1 - sl].t[rows, c * 128:(c + 1) * 128]
                        vfn = lambda c=c, sl=sl: Vw.t[:, (1 - sl) * 4 + c, 0:65]
                        mk = 4 + c
                    else:
                        kfn = lambda rows, c=c, sl=sl: kwT[sl].t[rows, (c - 4) * 128:(c - 3) * 128]
                        vfn = lambda c=c, sl=sl: Vw.t[:, sl * 4 + (c - 4), 0:65]
                        mk = c - 4
                    masks = [(lambda: idnb.t[:], lambda mk=mk: msk.t[:, mk, :])]
                    chunks.append((kfn, masks, vfn, max(0, c - 4), min(3, c)))
                attend(h, chunks, 65, G)
                for s_ in range(4):
                    evac(h, s_, 2)
            finish_branch(2, False)
            for s_ in range(4):
                r0 = t0 + s_ * 128
                P.dma("sp", lambda e, s_=s_, r0=r0: e.dma_start(out=o_d[r0:r0 + 128, :], in_=osb4.t[:, s_, :, :].rearrange("p h e -> p (h e)")), reads=[osb4], writes=[Ob])
        P.barrier()
        P.wait_all("sp", [Ob])
        P.emit()
    return nc


def nsa_in_maps(x, c, positions, ada_w_s, ada_b_s, w_in, pos_k, w1_k, w2_k, pos_v, w1_v, w2_v, cores=range(8)):
    idn = np.eye(128, dtype=np.float32)
    inv = rope_inv_table()
    msk = nsa_masks()
    indc = nsa_indc()
    vcc = nsa_vc_const()
    frc = nsa_forced()
    cposT = np.ascontiguousarray(np.concatenate([pos_k.T, pos_v.T], axis=0))
    w1 = np.ascontiguousarray(np.stack([w1_k, w1_v]))
    w2 = np.ascontiguousarray(np.stack([w2_k, w2_v]))
    in_maps = []
    for core in cores:
        b, g = core // 4, core % 4
        wkv = np.stack([w_in[:, D + br * 512 + kv * 256 + g * 64: D + br * 512 + kv * 256 + g * 64 + 64] for br in range(3) for kv in range(2)])
        posc = np.zeros((1, 1024), np.int32)
        posc[0, :NCMP] = positions[b, 31::16][:NCMP]
        in_maps.append({
            "x": x[b], "cT": np.ascontiguousarray(c[b].reshape(8, 128).T),
            "adaw": np.ascontiguousarray(ada_w_s[:, 0:2 * D]), "adabT": np.ascontiguousarray(ada_b_s[0:2 * D].reshape(16, 128).T),
            "wq": np.ascontiguousarray(w_in[:, g * 256:(g + 1) * 256]), "wkv": np.ascontiguousarray(wkv),
            "wgt": np.ascontiguousarray(w_in[:, D + 1536 + 12 * g: D + 1536 + 12 * g + 12]),
            "pos": np.ascontiguousarray(positions[b:b + 1]), "posc": posc, "inv": inv, "idn": idn, "msk": msk,
            "indc": indc, "vcc": vcc, "frc": frc, "w1": w1, "w2": w2, "cposT": cposT,
        })
    return in_maps


def run_nsa(x, c, positions, ada_w_s, ada_b_s, w_in, pos_k, w1_k, w2_k, pos_v, w1_v, w2_v):
    if "nsa" not in _CACHE:
        _CACHE["nsa"] = build_nsa()
    nc = _CACHE["nsa"]
    in_maps = nsa_in_maps(x, c, positions, ada_w_s, ada_b_s, w_in, pos_k, w1_k, w2_k, pos_v, w1_v, w2_v)
    res = run_bass_kernel_spmd(nc, in_maps, core_ids=list(range(8)))
    B = x.shape[0]
    o = np.zeros((B, S_LEN, D), np.float32)
    for core in range(8):
        b, g = core // 4, core % 4
        o[b, :, g * 256:(g + 1) * 256] = res.results[core]["o"]
    return o


def kernel(x, c, positions, ada_w, ada_b, ln_g, ln_b,
           nsa_w_in, nsa_cmp_pos_k, nsa_cmp_w1_k, nsa_cmp_w2_k,
           nsa_cmp_pos_v, nsa_cmp_w1_v, nsa_cmp_w2_v, nsa_w_o,
           dil_w_in, dil_w_o, router_w, router_b, moe_w_gate, moe_w_up, moe_w_down):
    f = lambda a: np.ascontiguousarray(np.asarray(a))
    x, c, positions = f(x), f(c), f(positions)
    ada_w, ada_b, ln_g, ln_b = f(ada_w), f(ada_b), f(ln_g), f(ln_b)
    nsa_w_in, nsa_w_o, dil_w_in, dil_w_o = f(nsa_w_in), f(nsa_w_o), f(dil_w_in), f(dil_w_o)
    router_w, router_b = f(router_w), f(router_b)
    moe_w_gate, moe_w_up, moe_w_down = f(moe_w_gate), f(moe_w_up), f(moe_w_down)
    o0 = run_nsa(x, c, positions, ada_w[0, 0], ada_b[0, 0], nsa_w_in[0],
                 f(nsa_cmp_pos_k)[0], f(nsa_cmp_w1_k)[0], f(nsa_cmp_w2_k)[0],
                 f(nsa_cmp_pos_v)[0], f(nsa_cmp_w1_v)[0], f(nsa_cmp_w2_v)[0])
    x1 = run_ffn(x, o0, c, ada_w[0], ada_b[0], ln_g[0], ln_b[0], nsa_w_o[0], router_w, router_b,
                 moe_w_gate[0], moe_w_up[0], moe_w_down[0])
    o1 = run_dil(x1, c, positions, ada_w[1, 0], ada_b[1, 0], dil_w_in[0])
    out = run_ffn(x1, o1, c, ada_w[1], ada_b[1], ln_g[1], ln_b[1], dil_w_o[0], router_w, router_b,
                  moe_w_gate[1], moe_w_up[1], moe_w_down[1])
    return out.astype(np.float32)
```

```python
import numpy as np
from contextlib import ExitStack
import concourse.bass as bass
import concourse.mybir as mybir
from concourse.bass_utils import run_bass_kernel_spmd

F32 = mybir.dt.float32
BF16 = mybir.dt.bfloat16
I32 = mybir.dt.int32
AF = mybir.ActivationFunctionType
ALU = mybir.AluOpType
AX = mybir.AxisListType

ENGS = ("pe", "act", "dve", "pool", "sp")

D = 1024
ALPHA = (2.0 * 2) ** 0.25
LN_EPS = 1e-5
NEG = -30000.0
import os as _os
SAME_ENGINE_SYNC = _os.environ.get("SES", "1") == "1"


_UID = [0]


class Buf:
    __slots__ = ("name", "t", "writer", "readers", "dma_sem", "dma_cnt", "uid")

    def __init__(self, name, t=None):
        _UID[0] += 1
        self.uid = _UID[0]
        self.name = name
        self.t = t
        self.writer = None
        self.readers = []
        self.dma_sem = None
        self.dma_cnt = 0


class Prog:
    def __init__(self, nc, stack):
        self.nc = nc
        self.stack = stack
        self.ops = {e: [] for e in ENGS}
        self.cnt = {e: 0 for e in ENGS}
        self.waited = {}
        self.sem = {}
        for e in ENGS:
            self.sem[e] = stack.enter_context(nc.semaphore("s_" + e))
        self.n_dma_sems = 0
        self.dma_bufs = []
        self.scopes = []

    def sbuf(self, name, shape, dt):
        st = self.scopes[-1] if self.scopes else self.stack
        t = st.enter_context(self.nc.sbuf_tensor("sb_" + name, list(shape), dt))
        return Buf(name, t)

    def push(self):
        self.scopes.append(ExitStack())

    def pop(self):
        self.barrier()
        self.scopes.pop().close()

    def barrier(self):
        for eng in ENGS:
            waits = []
            for e2 in ENGS:
                if self.cnt[e2] > 0:
                    self._need(eng, ("eng", e2, self.cnt[e2]), waits)
            if eng == "pe" and self.cnt["pe"] > 0:
                pass
            for b in self.dma_bufs:
                self._need(eng, ("dma", b, b.dma_cnt), waits)
            self.ops[eng].append((waits, None, None))

    def psum(self, name, shape, dt=F32):
        t = self.stack.enter_context(self.nc.psum_tensor("ps_" + name, list(shape), dt))
        return Buf(name, t)

    def _need(self, eng, dep, waits):
        if dep[0] == "eng":
            _, e, idx = dep
            if e == eng and (e == "pe" or not SAME_ENGINE_SYNC):
                return
            key = (eng, "E", e)
            val = idx
            sem = self.sem[e]
        else:
            _, b, c = dep
            key = (eng, "D", b.uid)
            val = 16 * c
            sem = b.dma_sem
        if self.waited.get(key, 0) >= val:
            return
        self.waited[key] = val
        waits.append((sem, val))

    def _deps(self, eng, reads, writes):
        waits = []
        for b in reads:
            if b.writer is not None:
                self._need(eng, b.writer, waits)
        for b in writes:
            if b.writer is not None:
                self._need(eng, b.writer, waits)
            for r in b.readers:
                self._need(eng, r, waits)
        return waits

    def op(self, eng, fn, reads=(), writes=()):
        waits = self._deps(eng, reads, writes)
        self.cnt[eng] += 1
        me = ("eng", eng, self.cnt[eng])
        for b in reads:
            b.readers.append(me)
        for b in writes:
            b.writer = me
            b.readers = []
        self.ops[eng].append((waits, fn, (self.sem[eng], 1)))

    def dma(self, eng, fn, reads=(), writes=()):
        waits = self._deps(eng, reads, writes)
        dst = writes[0]
        if dst.dma_sem is None:
            dst.dma_sem = self.stack.enter_context(self.nc.semaphore("d%d" % self.n_dma_sems))
            self.n_dma_sems += 1
            self.dma_bufs.append(dst)
        dst.dma_cnt += 1
        me = ("dma", dst, dst.dma_cnt)
        for b in reads:
            b.readers.append(me)
        for b in writes:
            b.writer = me
            b.readers = []
        self.ops[eng].append((waits, fn, (dst.dma_sem, 16)))

    def wait_all(self, eng, bufs):
        waits = []
        for b in bufs:
            if b.writer is not None:
                self._need(eng, b.writer, waits)
        self.ops[eng].append((waits, None, None))

    def emit(self):
        nc = self.nc
        ops = self.ops
        with nc.Block() as block:
            def run(e, lst):
                for waits, fn, inc in lst:
                    for sem, val in waits:
                        e.wait_ge(sem, val)
                    if fn is not None:
                        fn(e).then_inc(inc[0], inc[1])

            @block.tensor
            def _(e):
                run(e, ops["pe"])

            @block.scalar
            def _(e):
                run(e, ops["act"])

            @block.vector
            def _(e):
                run(e, ops["dve"])

            @block.gpsimd
            def _(e):
                run(e, ops["pool"])

            @block.sync
            def _(e):
                run(e, ops["sp"])


def ss(a0, n, d):
    return slice(a0, a0 + (n - 1) * d + 1, d) if d > 1 else slice(a0, a0 + n)


def bcast_row(ap_row, n):
    return ap_row.to_broadcast([128, n])


def emit_layernorm(P, z, stats, mv, rstd, xn):
    for h in range(2):
        P.op("dve", lambda e, h=h: e.bn_stats(stats.t[:, h, :], z.t[:, h * 512:(h + 1) * 512]), reads=[z], writes=[stats])
    P.op("dve", lambda e: e.bn_aggr(mv.t[:], stats.t[:]), reads=[stats], writes=[mv])
    P.op("dve", lambda e: e.tensor_scalar(rstd.t[:], mv.t[:, 1:2], LN_EPS, None, ALU.add), reads=[mv], writes=[rstd])
    P.op("act", lambda e: e.sqrt(rstd.t[:], rstd.t[:]), reads=[rstd], writes=[rstd])
    P.op("dve", lambda e: e.reciprocal(rstd.t[:], rstd.t[:]), reads=[rstd], writes=[rstd])
    P.op("dve", lambda e: e.tensor_scalar(xn.t[:], z.t[:], mv.t[:, 0:1], rstd.t[:, 0:1], ALU.subtract, ALU.mult),
         reads=[z, mv, rstd], writes=[xn])


NTOK = 4096
CAP = 1024
NEXP = 32


def build_ffn(dbg=False):
    nc = bass.Bass("TRN2", target_bir_lowering=False)
    NT = NTOK // 128
    dt_in = lambda name, shape, dt=F32: nc.dram_tensor(name, list(shape), dt, kind="ExternalInput").ap()
    x_d = dt_in("x", [NTOK, D])
    o_d = dt_in("o", [NTOK, D])
    cT_d = dt_in("cT", [128, 8])
    adaw_d = dt_in("adaw", [2, D, 3 * D])
    adab_d = dt_in("adab", [2, 3 * D])
    lng_d = dt_in("lng", [2, D])
    lnb_d = dt_in("lnb", [2, D])
    wo_d = dt_in("wo", [D, D])
    rw_d = dt_in("rw", [D, NEXP])
    rb_d = dt_in("rb", [1, NEXP])
    NE_ = (1 if dbg == 1 else 2) if dbg in (1, 2, 3) else NEXP
    if dbg == 4:
        dbg_a = nc.dram_tensor("dbg_a", [128, 4 * D], F32, kind="ExternalOutput").ap()
    if dbg in (3, 4):
        dbg_i = nc.dram_tensor("dbg_i", [128, NTOK // 128 * 2], I32, kind="ExternalOutput").ap()
    wg_d = dt_in("wg", [NE_, D, 512])
    wu_d = dt_in("wu", [NE_, D, 512])
    wd_d = dt_in("wd", [NE_, 512, D])
    idn_d = dt_in("idn", [128, 128])
    tri_d = dt_in("tri", [128, 128])
    offs_d = dt_in("offs", [1, NEXP])
    y_d = nc.dram_tensor("y", [NTOK, D], F32, kind="ExternalOutput").ap()
    XS = nc.dram_tensor("XS", [NEXP * CAP, D], BF16).ap()
    YS = nc.dram_tensor("YS", [NEXP * CAP, D], F32).ap()
    X1 = nc.dram_tensor("X1", [NTOK, D], F32, kind="ExternalOutput" if dbg else "Internal").ap()
    if dbg == 2:
        dbg_xs = nc.dram_tensor("dbg_xs", [2 * CAP, D], BF16, kind="ExternalOutput").ap()
        dbg_ys = nc.dram_tensor("dbg_ys", [2 * CAP, D], F32, kind="ExternalOutput").ap()
    if dbg in (1, 2):
        dbg_i = nc.dram_tensor("dbg_i", [128, NTOK // 128 * 2], I32, kind="ExternalOutput").ap()
        dbg_w = nc.dram_tensor("dbg_w", [128, NTOK // 128 * 2], F32, kind="ExternalOutput").ap()
        dbg_m = nc.dram_tensor("dbg_m", [128, 4 * D], F32, kind="ExternalOutput").ap()

    with ExitStack() as st:
        P = Prog(nc, st)
        sb = P.sbuf
        idn = sb("idn", [128, 128], F32)
        idnb = sb("idnb", [128, 128], BF16)
        tri = sb("tri", [128, 128], BF16)
        ones = sb("ones", [128, 128], BF16)
        offsB = sb("offsB", [128, NEXP], F32)
        rbB = sb("rbB", [128, NEXP], F32)
        rw = sb("rw", [128, 8, NEXP], F32)
        wo = sb("wo", [128, 8, D], BF16)
        g1B = sb("g1B", [128, D], F32)
        sh2B = sb("sh2B", [128, D], F32)
        sc2B = sb("sc2B", [128, D], F32)
        g2B = sb("g2B", [128, D], F32)
        lgB = [sb("lgB%d" % i, [128, D], F32) for i in range(2)]
        lbB = [sb("lbB%d" % i, [128, D], F32) for i in range(2)]
        cT = sb("cT", [128, 8], F32)
        cond = sb("cond", [128, 8], F32)
        condB = sb("condB", [128, 8, 128], F32)
        base = sb("base", [128, NEXP], F32)
        desti = sb("desti", [128, NT, 2], I32)
        wts = sb("wts", [128, NT, 2], F32)
        psA = P.psum("psA", [128, 1024], BF16)
        psY = P.psum("psY", [128, 1024], F32)
        psT = P.psum("psT", [128, 1024], F32)
        psU = P.psum("psU", [128, 1024], F32)
        psS = P.psum("psS", [128, 512], F32)

        dmaq = ["sp", "act"]
        P.dma("sp", lambda e: e.dma_start(out=idn.t[:], in_=idn_d), writes=[idn])
        P.dma("pool", lambda e: e.dma_start(out=idnb.t[:], in_=idn_d), writes=[idnb])
        P.dma("pool", lambda e: e.dma_start(out=tri.t[:], in_=tri_d), writes=[tri])
        P.dma("sp", lambda e: e.dma_start(out=offsB.t[:], in_=bcast_row(offs_d, NEXP)), writes=[offsB])
        P.dma("sp", lambda e: e.dma_start(out=rbB.t[:], in_=bcast_row(rb_d, NEXP)), writes=[rbB])
        P.dma("sp", lambda e: e.dma_start(out=rw.t[:], in_=rw_d.rearrange("(k p) n -> p k n", p=128)), writes=[rw])
        P.dma("pool", lambda e: e.dma_start(out=wo.t[:], in_=wo_d.rearrange("(k p) n -> p k n", p=128)), writes=[wo])
        for i in range(2):
            P.dma("sp", lambda e, i=i: e.dma_start(out=lgB[i].t[:], in_=bcast_row(lng_d[i:i + 1, :], D)), writes=[lgB[i]])
            P.dma("sp", lambda e, i=i: e.dma_start(out=lbB[i].t[:], in_=bcast_row(lnb_d[i:i + 1, :], D)), writes=[lbB[i]])
        P.dma("sp", lambda e: e.dma_start(out=cT.t[:], in_=cT_d), writes=[cT])
        P.op("pool", lambda e: e.memset(ones.t[:], 1.0), writes=[ones])
        P.op("pool", lambda e: e.memset(base.t[:], 0.0), writes=[base])
        P.op("act", lambda e: e.activation(cond.t[:], cT.t[:], AF.Silu), reads=[cT], writes=[cond])
        for k in range(8):
            P.op("dve", lambda e, k=k: e.tensor_copy(condB.t[:, k, :], cond.t[:, k:k + 1].to_broadcast([128, 128])),
                 reads=[cond], writes=[condB])

        P.push()
        awb = [sb("awb%d" % i, [128, 8, 512], F32) for i in range(2)]
        abb = [sb("abb%d" % i, [128, 512], F32) for i in range(2)]
        jobs = []
        for h in range(2):
            jobs.append((0, 2 * D + h * 512, g1B, h * 512, False))
        for h in range(2):
            jobs.append((1, 0 * D + h * 512, sh2B, h * 512, False))
        for h in range(2):
            jobs.append((1, 1 * D + h * 512, sc2B, h * 512, True))
        for h in range(2):
            jobs.append((1, 2 * D + h * 512, g2B, h * 512, False))
        psM = [Buf("psM0", psS.t), Buf("psM1", psU.t)]
        for j, (s, c0, dst, d0, plus1) in enumerate(jobs):
            wb = awb[j % 2]
            bb = abb[j % 2]
            pm = psM[j % 2]
            P.dma(dmaq[j % 2], lambda e, s=s, c0=c0, wb=wb: e.dma_start(
                out=wb.t[:], in_=adaw_d[s, :, c0:c0 + 512].rearrange("(k p) n -> p k n", p=128)), writes=[wb])
            P.dma("sp", lambda e, s=s, c0=c0, bb=bb: e.dma_start(
                out=bb.t[:], in_=bcast_row(adab_d[s:s + 1, c0:c0 + 512], 512)), writes=[bb])
            for k in range(8):
                P.op("pe", lambda e, k=k, wb=wb, pm=pm: e.matmul(pm.t[:, 0:512], condB.t[:, k, :], wb.t[:, k, :],
                                                                 start=(k == 0), stop=(k == 7)),
                     reads=[condB, wb], writes=[pm])
            P.op("dve", lambda e, pm=pm, bb=bb, dst=dst, d0=d0: e.tensor_tensor(
                dst.t[:, d0:d0 + 512], pm.t[:, 0:512], bb.t[:], ALU.add), reads=[pm, bb], writes=[dst])
            if plus1:
                P.op("dve", lambda e, dst=dst, d0=d0: e.tensor_scalar(
                    dst.t[:, d0:d0 + 512], dst.t[:, d0:d0 + 512], 1.0, None, ALU.add), reads=[dst], writes=[dst])

        P.pop()
        P.push()
        NB = 2
        xt = [sb("xt%d" % i, [128, D], F32) for i in range(NB)]
        ot = [sb("ot%d" % i, [128, D], F32) for i in range(NB)]
        ob = [sb("ob%d" % i, [128, D], BF16) for i in range(NB)]
        oT = [sb("oT%d" % i, [128, 8, 128], BF16) for i in range(NB)]
        t1 = sb("t1", [128, D], F32)
        z = sb("z", [128, D], F32)
        xn = sb("xn", [128, D], F32)
        x1 = [sb("x1_%d" % i, [128, D], F32) for i in range(NB)]
        h2 = [sb("h2_%d" % i, [128, D], F32) for i in range(NB)]
        hb = [sb("hb%d" % i, [128, D], BF16) for i in range(NB)]
        h2T = [sb("h2T%d" % i, [128, 8, 128], F32) for i in range(NB)]
        stats = sb("stats", [128, 2, 6], F32)
        mv = sb("mv", [128, 2], F32)
        rstd = sb("rstd", [128, 1], F32)
        sc = sb("sc", [128, NEXP], F32)
        grp = sb("grp", [128, NEXP], F32)
        m8 = sb("m8", [128, 4, 8], F32)
        gs = sb("gs", [128, 4], F32)
        gmax = sb("gmax", [128, 1], F32)
        oh = sb("oh", [128, 4], F32)
        tmp4 = sb("tmp4", [128, 4], F32)
        thr = sb("thr", [128, 1], F32)
        ge = sb("ge", [128, NEXP], F32)
        sel = sb("sel", [128, NEXP], F32)
        selb = sb("selb", [128, NEXP], BF16)
        ws = sb("ws", [128, NEXP], F32)
        wsum = sb("wsum", [128, 1], F32)
        wt = sb("wt", [128, NEXP], F32)
        dall = sb("dall", [128, NEXP], F32)
        dhi = sb("dhi", [128, 1], F32)
        dsum = sb("dsum", [128, 1], F32)
        dpair = sb("dpair", [128, 2], F32)
        eq = sb("eq", [128, NEXP], F32)
        XSb = Buf("XS")
        X1b = Buf("X1")
        psLog = Buf("psLog", psS.t)

        for t in range(NT):
            b = t % NB
            r0 = t * 128
            P.dma("sp", lambda e, b=b, r0=r0: e.dma_start(out=xt[b].t[:], in_=x_d[r0:r0 + 128, :]), writes=[xt[b]])
            P.dma("act", lambda e, b=b, r0=r0: e.dma_start(out=ot[b].t[:], in_=o_d[r0:r0 + 128, :]), writes=[ot[b]])
            P.op("pool", lambda e, b=b: e.tensor_copy(ob[b].t[:], ot[b].t[:]), reads=[ot[b]], writes=[ob[b]])
            for k in range(8):
                P.op("pe", lambda e, b=b, k=k: e.transpose(psA.t[:, k * 128:(k + 1) * 128], ob[b].t[:, k * 128:(k + 1) * 128], idnb.t[:]),
                     reads=[ob[b], idnb], writes=[psA])
            P.op("act", lambda e, b=b: e.copy(oT[b].t[:].rearrange("p k m -> p (k m)"), psA.t[:]), reads=[psA], writes=[oT[b]])
            for nh in range(2):
                for k in range(8):
                    P.op("pe", lambda e, b=b, k=k, nh=nh: e.matmul(psY.t[:, nh * 512:(nh + 1) * 512], oT[b].t[:, k, :],
                                                                   wo.t[:, k, nh * 512:(nh + 1) * 512], start=(k == 0), stop=(k == 7)),
                         reads=[oT[b], wo], writes=[psY])
            for nh in range(2):
                sl = slice(nh * 512, (nh + 1) * 512)
                P.op("dve", lambda e, sl=sl: e.tensor_tensor(t1.t[:, sl], psY.t[:, sl], g1B.t[:, sl], ALU.mult),
                     reads=[psY, g1B], writes=[t1])
            P.op("dve", lambda e, b=b: e.scalar_tensor_tensor(z.t[:], xt[b].t[:], ALPHA, t1.t[:], ALU.mult, ALU.add),
                 reads=[xt[b], t1], writes=[z])
            emit_layernorm(P, z, stats, mv, rstd, xn)
            P.op("pool", lambda e, b=b: e.tensor_tensor(x1[b].t[:], xn.t[:], lgB[0].t[:], ALU.mult), reads=[xn, lgB[0]], writes=[x1[b]])
            P.op("pool", lambda e, b=b: e.tensor_tensor(x1[b].t[:], x1[b].t[:], lbB[0].t[:], ALU.add), reads=[x1[b], lbB[0]], writes=[x1[b]])
            P.dma("sp", lambda e, b=b, r0=r0: e.dma_start(out=X1[r0:r0 + 128, :], in_=x1[b].t[:]), reads=[x1[b]], writes=[X1b])
            P.op("pool", lambda e, b=b: e.tensor_tensor(h2[b].t[:], x1[b].t[:], sc2B.t[:], ALU.mult), reads=[x1[b], sc2B], writes=[h2[b]])
            P.op("pool", lambda e, b=b: e.tensor_tensor(h2[b].t[:], h2[b].t[:], sh2B.t[:], ALU.add), reads=[h2[b], sh2B], writes=[h2[b]])
            P.op("pool", lambda e, b=b: e.tensor_copy(hb[b].t[:], h2[b].t[:]), reads=[h2[b]], writes=[hb[b]])
            for k in range(8):
                P.op("pe", lambda e, b=b, k=k: e.transpose(psT.t[:, k * 128:(k + 1) * 128], h2[b].t[:, k * 128:(k + 1) * 128], idn.t[:]),
                     reads=[h2[b], idn], writes=[psT])
            P.op("act", lambda e, b=b: e.copy(h2T[b].t[:].rearrange("p k m -> p (k m)"), psT.t[:]), reads=[psT], writes=[h2T[b]])
            for k in range(8):
                P.op("pe", lambda e, b=b, k=k: e.matmul(psS.t[:, 0:NEXP], h2T[b].t[:, k, :], rw.t[:, k, :], start=(k == 0), stop=(k == 7)),
                     reads=[h2T[b], rw], writes=[psLog])
            P.op("act", lambda e: e.activation(sc.t[:], psS.t[:, 0:NEXP], AF.Sigmoid), reads=[psLog], writes=[sc])
            P.op("dve", lambda e: e.tensor_tensor(grp.t[:], sc.t[:], rbB.t[:], ALU.add), reads=[sc, rbB], writes=[grp])
            for g in range(4):
                P.op("dve", lambda e, g=g: e.max(out=m8.t[:, g, :], in_=grp.t[:, g * 8:(g + 1) * 8]), reads=[grp], writes=[m8])
            P.op("dve", lambda e: e.tensor_tensor(gs.t[:], m8.t[:, :, 0], m8.t[:, :, 1], ALU.add), reads=[m8], writes=[gs])
            P.op("dve", lambda e: e.reduce_max(gmax.t[:], gs.t[:], AX.X), reads=[gs], writes=[gmax])
            P.op("dve", lambda e: e.tensor_scalar(oh.t[:], gs.t[:], gmax.t[:, 0:1], None, ALU.is_equal), reads=[gs, gmax], writes=[oh])
            P.op("dve", lambda e: e.tensor_tensor(tmp4.t[:], oh.t[:], m8.t[:, :, 1], ALU.mult), reads=[oh, m8], writes=[tmp4])
            P.op("dve", lambda e: e.reduce_sum(thr.t[:], tmp4.t[:], AX.X), reads=[tmp4], writes=[thr])
            P.op("dve", lambda e: e.tensor_scalar(ge.t[:], grp.t[:], thr.t[:, 0:1], None, ALU.is_ge), reads=[grp, thr], writes=[ge])
            P.op("dve", lambda e: e.tensor_tensor(sel.t[:].rearrange("p (g j) -> p g j", j=8), ge.t[:].rearrange("p (g j) -> p g j", j=8),
                                                  oh.t[:].unsqueeze(2).to_broadcast([128, 4, 8]), ALU.mult), reads=[ge, oh], writes=[sel])
            P.op("dve", lambda e: e.tensor_tensor(ws.t[:], sc.t[:], sel.t[:], ALU.mult), reads=[sc, sel], writes=[ws])
            P.op("dve", lambda e: e.reduce_sum(wsum.t[:], ws.t[:], AX.X), reads=[ws], writes=[wsum])
            P.op("dve", lambda e: e.reciprocal(wsum.t[:], wsum.t[:]), reads=[wsum], writes=[wsum])
            P.op("dve", lambda e: e.tensor_scalar(wt.t[:], ws.t[:], wsum.t[:, 0:1], None, ALU.mult), reads=[ws, wsum], writes=[wt])
            P.op("dve", lambda e: e.tensor_copy(selb.t[:], sel.t[:]), reads=[sel], writes=[selb])
            P.op("pe", lambda e: e.matmul(psS.t[:, 64:64 + NEXP], tri.t[:], selb.t[:], start=True, stop=True), reads=[tri, selb], writes=[psLog])
            P.op("pe", lambda e: e.matmul(psS.t[:, 128:128 + NEXP], ones.t[:], selb.t[:], start=True, stop=True), reads=[ones, selb], writes=[psLog])
            P.op("dve", lambda e: e.tensor_tensor(dall.t[:], psS.t[:, 64:64 + NEXP], base.t[:], ALU.add), reads=[psLog, base], writes=[dall])
            P.op("dve", lambda e: e.tensor_tensor(dall.t[:], dall.t[:], offsB.t[:], ALU.add), reads=[dall, offsB], writes=[dall])
            P.op("dve", lambda e: e.tensor_tensor(dall.t[:], dall.t[:], sel.t[:], ALU.mult), reads=[dall, sel], writes=[dall])
            P.op("dve", lambda e: e.tensor_tensor(base.t[:], base.t[:], psS.t[:, 128:128 + NEXP], ALU.add), reads=[psLog, base], writes=[base])
            P.op("dve", lambda e: e.reduce_max(dhi.t[:], dall.t[:], AX.X), reads=[dall], writes=[dhi])
            P.op("dve", lambda e: e.reduce_sum(dsum.t[:], dall.t[:], AX.X), reads=[dall], writes=[dsum])
            P.op("dve", lambda e: e.tensor_scalar(dpair.t[:, 0:1], dhi.t[:], -1.0, None, ALU.add), reads=[dhi], writes=[dpair])
            P.op("dve", lambda e: e.scalar_tensor_tensor(dpair.t[:, 1:2], dsum.t[:], -1.0, dhi.t[:], ALU.add, ALU.subtract),
                 reads=[dsum, dhi], writes=[dpair])
            P.op("dve", lambda e, t=t: e.tensor_copy(desti.t[:, t, :], dpair.t[:]), reads=[dpair], writes=[desti])
            P.op("dve", lambda e: e.tensor_scalar(eq.t[:], dall.t[:], dhi.t[:, 0:1], None, ALU.is_equal), reads=[dall, dhi], writes=[eq])
            P.op("dve", lambda e: e.tensor_tensor(eq.t[:], eq.t[:], wt.t[:], ALU.mult), reads=[eq, wt], writes=[eq])
            P.op("dve", lambda e, t=t: e.reduce_sum(wts.t[:, t, 0:1], eq.t[:], AX.X), reads=[eq], writes=[wts])
            P.op("dve", lambda e, t=t: e.tensor_scalar(wts.t[:, t, 1:2], wts.t[:, t, 0:1], -1.0, 1.0, ALU.mult, ALU.add), reads=[wts], writes=[wts])
            for j in range(2):
                P.dma("pool", lambda e, b=b, t=t, j=j: e.indirect_dma_start(
                    out=XS[:, :], out_offset=bass.IndirectOffsetOnAxis(ap=desti.t[:, t, j:j + 1], axis=0),
                    in_=hb[b].t[:, :], in_offset=None), reads=[hb[b], desti], writes=[XSb])

        if dbg == 1:
            Db = Buf("dbg")
            P.dma("sp", lambda e: e.dma_start(out=dbg_i, in_=desti.t[:].rearrange("p t j -> p (t j)")), reads=[desti], writes=[Db])
            P.dma("sp", lambda e: e.dma_start(out=dbg_w, in_=wts.t[:].rearrange("p t j -> p (t j)")), reads=[wts], writes=[Db])
            for i_, tl in enumerate([g1B, sh2B, sc2B, g2B]):
                P.dma("sp", lambda e, i_=i_, tl=tl: e.dma_start(out=dbg_m[:, i_ * D:(i_ + 1) * D], in_=tl.t[:]), reads=[tl], writes=[Db])
            P.wait_all("sp", [Db, X1b])
            P.pop()
            P.emit()
            return nc
        P.pop()
        P.push()
        wgs = [sb("wgs%d" % i, [128, 8, 512], BF16) for i in range(2)]
        wus = [sb("wus%d" % i, [128, 8, 512], BF16) for i in range(2)]
        wds = [sb("wds%d" % i, [128, 4, D], BF16) for i in range(2)]
        xs_tok = [sb("xs_tok%d" % i, [128, D], BF16) for i in range(2)]
        xsT = [sb("xsT%d" % i, [128, 8, CAP], BF16) for i in range(2)]
        sg = [sb("sg%d" % i, [128, 512], F32) for i in range(2)]
        hT = [sb("hT%d" % i, [128, 4, CAP], BF16) for i in range(2)]
        ysb = [sb("ysb%d" % i, [128, D], F32) for i in range(2)]
        psG = [Buf("psG0", psT.t), Buf("psG1", psU.t)]
        YSb = Buf("YS")
        RB = CAP // 128
        nx = 0
        ny = 0
        for ex in range(NE_):
            wb = ex % 2
            P.dma("pool", lambda e, ex=ex, wb=wb: e.dma_start(out=wgs[wb].t[:], in_=wg_d[ex].rearrange("(k p) n -> p k n", p=128)), writes=[wgs[wb]])
            P.dma("pool", lambda e, ex=ex, wb=wb: e.dma_start(out=wus[wb].t[:], in_=wu_d[ex].rearrange("(k p) n -> p k n", p=128)), writes=[wus[wb]])
            P.dma("pool", lambda e, ex=ex, wb=wb: e.dma_start(out=wds[wb].t[:], in_=wd_d[ex].rearrange("(k p) n -> p k n", p=128)), writes=[wds[wb]])
            xT = xsT[wb]
            for rb in range(RB):
                xb = xs_tok[nx % 2]
                nx += 1
                r0 = ex * CAP + rb * 128
                P.dma("sp", lambda e, xb=xb, r0=r0: e.dma_start(out=xb.t[:], in_=XS[r0:r0 + 128, :]), reads=[XSb], writes=[xb])
                for k in range(8):
                    P.op("pe", lambda e, xb=xb, k=k: e.transpose(psA.t[:, k * 128:(k + 1) * 128], xb.t[:, k * 128:(k + 1) * 128], idnb.t[:]),
                         reads=[xb, idnb], writes=[psA])
                P.op("act", lambda e, xT=xT, rb=rb: e.copy(xT.t[:, :, rb * 128:(rb + 1) * 128], psA.t[:].rearrange("p (k m) -> p k m", m=128)),
                     reads=[psA], writes=[xT])
            hh = hT[wb]
            for fc in range(4):
              for hf in range(CAP // 512):
                pg = psG[(fc * (CAP // 512) + hf) % 2]
                s0 = hf * 512
                for k in range(8):
                    P.op("pe", lambda e, k=k, fc=fc, pg=pg, wb=wb, xT=xT, s0=s0: e.matmul(
                        pg.t[:, 0:512], wgs[wb].t[:, k, fc * 128:(fc + 1) * 128], xT.t[:, k, s0:s0 + 512], start=(k == 0), stop=(k == 7)),
                        reads=[wgs[wb], xT], writes=[pg])
                for k in range(8):
                    P.op("pe", lambda e, k=k, fc=fc, pg=pg, wb=wb, xT=xT, s0=s0: e.matmul(
                        pg.t[:, 512:1024], wus[wb].t[:, k, fc * 128:(fc + 1) * 128], xT.t[:, k, s0:s0 + 512], start=(k == 0), stop=(k == 7)),
                        reads=[wus[wb], xT], writes=[pg])
                s_ = sg[(fc * (CAP // 512) + hf) % 2]
                P.op("act", lambda e, pg=pg, s_=s_: e.activation(s_.t[:], pg.t[:, 0:512], AF.Silu), reads=[pg], writes=[s_])
                P.op("dve", lambda e, pg=pg, s_=s_, hh=hh, fc=fc, s0=s0: e.tensor_tensor(hh.t[:, fc, s0:s0 + 512], s_.t[:], pg.t[:, 512:1024], ALU.mult),
                     reads=[pg, s_], writes=[hh])
            for rb in range(RB):
                yb = ysb[ny % 2]
                ny += 1
                for nh in range(2):
                    for fc in range(4):
                        P.op("pe", lambda e, rb=rb, nh=nh, fc=fc, hh=hh, wb=wb: e.matmul(
                            psY.t[:, nh * 512:(nh + 1) * 512], hh.t[:, fc, rb * 128:(rb + 1) * 128],
                            wds[wb].t[:, fc, nh * 512:(nh + 1) * 512], start=(fc == 0), stop=(fc == 3)),
                            reads=[hh, wds[wb]], writes=[psY])
                P.op("act", lambda e, yb=yb: e.copy(yb.t[:], psY.t[:]), reads=[psY], writes=[yb])
                r0 = ex * CAP + rb * 128
                P.dma("sp", lambda e, yb=yb, r0=r0: e.dma_start(out=YS[r0:r0 + 128, :], in_=yb.t[:]), reads=[yb], writes=[YSb])

        if dbg == 2:
            Db = Buf("dbg")
            P.dma("sp", lambda e: e.dma_start(out=dbg_xs, in_=XS[0:2 * CAP, :]), reads=[XSb], writes=[Db])
            P.dma("sp", lambda e: e.dma_start(out=dbg_ys, in_=YS[0:2 * CAP, :]), reads=[YSb], writes=[Db])
            P.dma("sp", lambda e: e.dma_start(out=dbg_i, in_=desti.t[:].rearrange("p t j -> p (t j)")), reads=[desti], writes=[Db])
            P.wait_all("sp", [Db])
            P.pop()
            P.emit()
            return nc
        P.pop()
        P.push()
        cxt = [sb("cxt%d" % i, [128, D], F32) for i in range(2)]
        ct1 = sb("ct1", [128, D], F32)
        cz = sb("cz", [128, D], F32)
        cxn = sb("cxn", [128, D], F32)
        cstats = sb("cstats", [128, 2, 6], F32)
        cmv = sb("cmv", [128, 2], F32)
        crstd = sb("crstd", [128, 1], F32)
        yh = [sb("yh%d" % i, [128, D], F32) for i in range(2)]
        yl = [sb("yl%d" % i, [128, D], F32) for i in range(2)]
        outt = [sb("outt%d" % i, [128, D], F32) for i in range(2)]
        Yb = Buf("y")
        import os
        COMB = os.environ.get("COMB", "full")
        for t in range(NT):
            b = t % 2
            r0 = t * 128
            if COMB == "a":
                P.dma("sp", lambda e, b=b, r0=r0: e.dma_start(out=cxt[b].t[:], in_=X1[r0:r0 + 128, :]), reads=[X1b], writes=[cxt[b]])
                P.dma("sp", lambda e, b=b, r0=r0: e.dma_start(out=y_d[r0:r0 + 128, :], in_=cxt[b].t[:]), reads=[cxt[b]], writes=[Yb])
                continue
            if COMB == "none":
                continue
            P.dma("pool", lambda e, b=b, t=t: e.indirect_dma_start(
                out=yh[b].t[:, :], out_offset=None, in_=YS[:, :],
                in_offset=bass.IndirectOffsetOnAxis(ap=desti.t[:, t, 0:1], axis=0)), reads=[YSb, desti], writes=[yh[b]])
            P.dma("pool", lambda e, b=b, t=t: e.indirect_dma_start(
                out=yl[b].t[:, :], out_offset=None, in_=YS[:, :],
                in_offset=bass.IndirectOffsetOnAxis(ap=desti.t[:, t, 1:2], axis=0)), reads=[YSb, desti], writes=[yl[b]])
            P.dma("sp", lambda e, b=b, r0=r0: e.dma_start(out=cxt[b].t[:], in_=X1[r0:r0 + 128, :]), reads=[X1b], writes=[cxt[b]])
            if dbg == 4 and t == 0:
                Dbb = Buf("dbb")
                P.dma("sp", lambda e: e.dma_start(out=dbg_a[:, 0:D], in_=yh[0].t[:]), reads=[yh[0]], writes=[Dbb])
                P.dma("sp", lambda e: e.dma_start(out=dbg_a[:, D:2 * D], in_=yl[0].t[:]), reads=[yl[0]], writes=[Dbb])
                P.dma("sp", lambda e: e.dma_start(out=dbg_a[:, 2 * D:3 * D], in_=cxt[0].t[:]), reads=[cxt[0]], writes=[Dbb])
            P.op("dve", lambda e, b=b, t=t: e.tensor_scalar(yh[b].t[:], yh[b].t[:], wts.t[:, t, 0:1], None, ALU.mult), reads=[yh[b], wts], writes=[yh[b]])
            P.op("dve", lambda e, b=b, t=t: e.scalar_tensor_tensor(yh[b].t[:], yl[b].t[:], wts.t[:, t, 1:2], yh[b].t[:], ALU.mult, ALU.add),
                 reads=[yl[b], yh[b], wts], writes=[yh[b]])
            P.op("pool", lambda e, b=b: e.tensor_tensor(ct1.t[:], yh[b].t[:], g2B.t[:], ALU.mult), reads=[yh[b], g2B], writes=[ct1])
            P.op("dve", lambda e, b=b: e.scalar_tensor_tensor(cz.t[:], cxt[b].t[:], ALPHA, ct1.t[:], ALU.mult, ALU.add), reads=[cxt[b], ct1], writes=[cz])
            if dbg == 4 and t == 0:
                P.dma("sp", lambda e: e.dma_start(out=dbg_a[:, 3 * D:4 * D], in_=cz.t[:]), reads=[cz], writes=[Dbb])
            emit_layernorm(P, cz, cstats, cmv, crstd, cxn)
            P.op("pool", lambda e, b=b: e.tensor_tensor(outt[b].t[:], cxn.t[:], lgB[1].t[:], ALU.mult), reads=[cxn, lgB[1]], writes=[outt[b]])
            P.op("pool", lambda e, b=b: e.tensor_tensor(outt[b].t[:], outt[b].t[:], lbB[1].t[:], ALU.add), reads=[outt[b], lbB[1]], writes=[outt[b]])
            P.dma("sp", lambda e, b=b, r0=r0: e.dma_start(out=y_d[r0:r0 + 128, :], in_=outt[b].t[:]), reads=[outt[b]], writes=[Yb])
        if dbg in (3, 4):
            P.dma("sp", lambda e: e.dma_start(out=dbg_i, in_=desti.t[:].rearrange("p t j -> p (t j)")), reads=[desti], writes=[Yb])
        P.wait_all("sp", [Yb])
        P.pop()
        P.emit()
    return nc


_CACHE = {}


def _consts():
    idn = np.eye(128, dtype=np.float32)
    tri = np.triu(np.ones((128, 128), np.float32), 1)
    offs = (np.arange(NEXP, dtype=np.float32) * CAP + 1.0)[None, :]
    return idn, tri, offs


def run_ffn(x, o, c, ada_w_l, ada_b_l, ln_g_l, ln_b_l, w_o, router_w, router_b, wg, wu, wd):
    if "ffn" not in _CACHE:
        _CACHE["ffn"] = build_ffn()
    nc = _CACHE["ffn"]
    idn, tri, offs = _consts()
    B, S, _ = x.shape
    xf = x.reshape(B * S, D)
    of = o.reshape(B * S, D)
    in_maps = []
    for core in range(8):
        r0 = core * NTOK
        b = r0 // S
        in_maps.append({
            "x": np.ascontiguousarray(xf[r0:r0 + NTOK]), "o": np.ascontiguousarray(of[r0:r0 + NTOK]),
            "cT": np.ascontiguousarray(c[b].reshape(8, 128).T),
            "adaw": ada_w_l, "adab": ada_b_l, "lng": ln_g_l, "lnb": ln_b_l, "wo": w_o,
            "rw": router_w, "rb": router_b.reshape(1, NEXP), "wg": wg, "wu": wu, "wd": wd,
            "idn": idn, "tri": tri, "offs": offs,
        })
    res = run_bass_kernel_spmd(nc, in_maps, core_ids=list(range(8)))
    return np.concatenate([r["y"] for r in res.results], axis=0).reshape(B, S, D)


S_LEN = 16384
SPAN = 2048
TWO_PI = 6.283185307179586
PI = 3.141592653589793


def rope_inv_table():
    inv = 500000.0 ** (-np.arange(8, dtype=np.float64) * (2.0 / 16))
    t = np.zeros((128, 1), np.float32)
    for p in range(128):
        f = p % 64
        if f < 16:
            t[p, 0] = np.float32(inv[f % 8])
    return t


def emit_mod_cols(P, nc, adaw_d, adabT_d, cT_d, ncols, shp, psum_buf):
    sb = P.sbuf
    cT = sb("cT", [128, 8], F32)
    cond = sb("cond", [128, 8], F32)
    modp = sb("modp", [128, ncols // 128], F32)
    abT = sb("abT", [128, ncols // 128], F32)
    P.dma("sp", lambda e: e.dma_start(out=cT.t[:], in_=cT_d), writes=[cT])
    P.dma("sp", lambda e: e.dma_start(out=abT.t[:], in_=adabT_d), writes=[abT])
    P.op("act", lambda e: e.activation(cond.t[:], cT.t[:], AF.Silu), reads=[cT], writes=[cond])
    P.push()
    awb = [sb("awb%d" % i, [128, 8, 512], F32) for i in range(2)]
    for blk in range(ncols // 512):
        wb = awb[blk % 2]
        P.dma(["sp", "act"][blk % 2], lambda e, blk=blk, wb=wb: e.dma_start(
            out=wb.t[:], in_=adaw_d[:, blk * 512:(blk + 1) * 512].rearrange("(k p) n -> p k n", p=128)), writes=[wb])
        for j in range(4):
            for kk in range(8):
                P.op("pe", lambda e, j=j, kk=kk, wb=wb, blk=blk: e.matmul(
                    psum_buf.t[:, blk * 4 + j:blk * 4 + j + 1], wb.t[:, kk, j * 128:(j + 1) * 128], cond.t[:, kk:kk + 1],
                    start=(kk == 0), stop=(kk == 7)), reads=[wb, cond], writes=[psum_buf])
    P.op("dve", lambda e: e.tensor_tensor(modp.t[:], psum_buf.t[:, 0:ncols // 128], abT.t[:], ALU.add),
         reads=[psum_buf, abT], writes=[modp])
    P.pop()
    return modp


def emit_rope_tables(P, posB, posI, invp, negpi, Ct, St, tmp, pos_ap, n):
    C1 = 6.28125
    C2 = TWO_PI - C1
    P.dma("sp", lambda e: e.dma_start(out=posI.t[:, 0:n], in_=pos_ap.to_broadcast([128, n])), writes=[posI])
    P.op("dve", lambda e: e.tensor_copy(posB.t[:, 0:n], posI.t[:, 0:n]), reads=[posI], writes=[posB])
    for off, dst in ((0.0, St), (0.5 * PI, Ct)):
        P.op("dve", lambda e, off=off: e.tensor_scalar(tmp.t[:, 0:n], posB.t[:, 0:n], invp.t[:, 0:1], off, ALU.mult, ALU.add),
             reads=[posB, invp], writes=[tmp])
        P.op("dve", lambda e: e.tensor_scalar(dst.t[:, 0:n], tmp.t[:, 0:n], 1.0 / TWO_PI, None, ALU.mult), reads=[tmp], writes=[dst])
        P.op("dve", lambda e: e.tensor_copy(posI.t[:, 0:n], dst.t[:, 0:n]), reads=[dst], writes=[posI])
        P.op("dve", lambda e, dst=dst: e.tensor_copy(dst.t[:, 0:n], posI.t[:, 0:n]), reads=[posI], writes=[dst])
        P.op("dve", lambda e, dst=dst: e.scalar_tensor_tensor(tmp.t[:, 0:n], dst.t[:, 0:n], -C1, tmp.t[:, 0:n], ALU.mult, ALU.add),
             reads=[dst, tmp], writes=[tmp])
        P.op("dve", lambda e, dst=dst: e.scalar_tensor_tensor(tmp.t[:, 0:n], dst.t[:, 0:n], -C2, tmp.t[:, 0:n], ALU.mult, ALU.add),
             reads=[dst, tmp], writes=[tmp])
        P.op("dve", lambda e, dst=dst: e.tensor_scalar(dst.t[:, 0:n], tmp.t[:, 0:n], PI, -TWO_PI, ALU.is_gt, ALU.mult), reads=[tmp], writes=[dst])
        P.op("dve", lambda e, dst=dst: e.tensor_tensor(tmp.t[:, 0:n], tmp.t[:, 0:n], dst.t[:, 0:n], ALU.add), reads=[tmp, dst], writes=[tmp])
        P.op("dve", lambda e, dst=dst: e.tensor_scalar(dst.t[:, 0:n], tmp.t[:, 0:n], -PI, TWO_PI, ALU.is_lt, ALU.mult), reads=[tmp], writes=[dst])
        P.op("dve", lambda e, dst=dst: e.tensor_tensor(tmp.t[:, 0:n], tmp.t[:, 0:n], dst.t[:, 0:n], ALU.add), reads=[tmp, dst], writes=[tmp])
        P.op("dve", lambda e: e.tensor_scalar(tmp.t[:, 0:n], tmp.t[:, 0:n], PI, -PI, ALU.min, ALU.max), reads=[tmp], writes=[tmp])
        P.op("act", lambda e, dst=dst: e.activation(dst.t[:, 0:n], tmp.t[:, 0:n], AF.Sin), reads=[tmp], writes=[dst])


def emit_rot_weights(P, w, wr, nheads):
    P.op("pool", lambda e: e.memset(wr.t[:], 0.0), writes=[wr])
    for k in range(8):
        wv = w.t[:, k, :].rearrange("p (h e) -> p h e", e=64)
        rv = wr.t[:, k, :].rearrange("p (h e) -> p h e", e=64)
        P.op("dve", lambda e, wv=wv, rv=rv: e.tensor_scalar(rv[:, :, 0:8], wv[:, :, 8:16], -1.0, None, ALU.mult), reads=[w], writes=[wr])
        P.op("dve", lambda e, wv=wv, rv=rv: e.tensor_copy(rv[:, :, 8:16], wv[:, :, 0:8]), reads=[w], writes=[wr])


def emit_hmodT_tile(P, x_d, tok0, xt, psX, idn, modp, hT, col0, sc_off, q, width=1024):
    P.dma(q, lambda e: e.dma_start(out=xt.t[:], in_=x_d[tok0:tok0 + 128, :]), writes=[xt])
    nb_ = width // 128
    for k0 in range(0, 8, nb_):
        for kk in range(nb_):
            k = k0 + kk
            P.op("pe", lambda e, k=k, kk=kk: e.transpose(psX.t[:, kk * 128:(kk + 1) * 128], xt.t[:, k * 128:(k + 1) * 128], idn.t[:]),
                 reads=[xt, idn], writes=[psX])
        for kk in range(nb_):
            k = k0 + kk
            P.op("act", lambda e, k=k, kk=kk: e.activation(hT.t[:, k, col0:col0 + 128], psX.t[:, kk * 128:(kk + 1) * 128], AF.Identity,
                                                         bias=modp.t[:, k:k + 1], scale=modp.t[:, sc_off + k:sc_off + k + 1]),
                 reads=[psX, modp], writes=[hT])


def emit_proj_rope(P, hT, t0, n, w, wr, wc0, ps_a, ps_b, Ct, St, tcol0, tmpa, tmpb, out_ap_fn, out_buf):
    for k in range(8):
        P.op("pe", lambda e, k=k: e.matmul(ps_a.t[:, 0:n], w.t[:, k, wc0:wc0 + 128], hT.t[:, k, t0:t0 + n], start=(k == 0), stop=(k == 7)),
             reads=[w, hT], writes=[ps_a])
    for k in range(8):
        P.op("pe", lambda e, k=k: e.matmul(ps_b.t[:, 0:n], wr.t[:, k, wc0:wc0 + 128], hT.t[:, k, t0:t0 + n], start=(k == 0), stop=(k == 7)),
             reads=[wr, hT], writes=[ps_b])
    P.op("dve", lambda e: e.tensor_tensor(tmpa.t[:, 0:n], ps_a.t[:, 0:n], Ct.t[:, tcol0:tcol0 + n], ALU.mult), reads=[ps_a, Ct], writes=[tmpa])
    P.op("dve", lambda e: e.tensor_tensor(tmpb.t[:, 0:n], ps_b.t[:, 0:n], St.t[:, tcol0:tcol0 + n], ALU.mult), reads=[ps_b, St], writes=[tmpb])
    P.op("pool", lambda e: e.tensor_tensor(out_ap_fn(), tmpa.t[:, 0:n], tmpb.t[:, 0:n], ALU.add), reads=[tmpa, tmpb], writes=[out_buf])


def tri_masks():
    p = np.arange(128)[:, None]
    f = np.arange(128)[None, :]
    m0 = np.where(f <= p, 0.0, NEG).astype(np.float32)
    m1 = np.where(f >= p, 0.0, NEG).astype(np.float32)
    return np.stack([np.tile(m0, (1, 4)), np.tile(m1, (1, 4))], axis=1)


DIL = (1, 4, 16)


def build_dil():
    nc = bass.Bass("TRN2", target_bir_lowering=False)
    dt_in = lambda name, shape, dt=F32: nc.dram_tensor(name, list(shape), dt, kind="ExternalInput").ap()
    x_d = dt_in("x", [S_LEN, D])
    cT_d = dt_in("cT", [128, 8])
    adaw_d = dt_in("adaw", [D, 2 * D])
    adabT_d = dt_in("adabT", [128, 16])
    wq_d = dt_in("wq", [D, 256])
    wk_d = dt_in("wk", [3, D, 64])
    wv_d = dt_in("wv", [3, D, 64])
    pos_d = dt_in("pos", [1, S_LEN], I32)
    inv_d = dt_in("inv", [128, 1])
    idn_d = dt_in("idn", [128, 128])
    msk_d = dt_in("msk", [128, 2, 512])
    o_d = nc.dram_tensor("o", [S_LEN, 256], F32, kind="ExternalOutput").ap()
    OP = [nc.dram_tensor("OP%d" % p, [S_LEN, 260], F32).ap() for p in range(3)]

    with ExitStack() as st:
        P = Prog(nc, st)
        sb = P.sbuf
        idn = sb("idn", [128, 128], F32)
        idnb = sb("idnb", [128, 128], BF16)
        msk = sb("msk", [128, 2, 512], BF16)
        invp = sb("invp", [128, 1], F32)
        negpi = sb("negpi", [128, 1], F32)
        wq = sb("wq", [128, 8, 256], BF16)
        wqr = sb("wqr", [128, 8, 256], BF16)
        wk = [sb("wk%d" % p, [128, 8, 128], BF16) for p in range(3)]
        wkr = [sb("wkr%d" % p, [128, 8, 128], BF16) for p in range(3)]
        wv = [sb("wv%d" % p, [128, 8, 64], BF16) for p in range(3)]
        ps = [P.psum("pb%d" % i, [128, 512], F32) for i in range(6)]
        psX = P.psum("psX", [128, 1024], F32)

        P.dma("sp", lambda e: e.dma_start(out=idn.t[:], in_=idn_d), writes=[idn])
        P.dma("pool", lambda e: e.dma_start(out=idnb.t[:], in_=idn_d), writes=[idnb])
        P.dma("pool", lambda e: e.dma_start(out=msk.t[:], in_=msk_d), writes=[msk])
        P.dma("sp", lambda e: e.dma_start(out=invp.t[:], in_=inv_d), writes=[invp])
        P.op("pool", lambda e: e.memset(negpi.t[:], -PI), writes=[negpi])
        P.dma("pool", lambda e: e.dma_start(out=wq.t[:], in_=wq_d.rearrange("(k p) n -> p k n", p=128)), writes=[wq])
        for p in range(3):
            for h in range(2):
                P.dma("pool", lambda e, p=p, h=h: e.dma_start(out=wk[p].t[:, :, h * 64:(h + 1) * 64],
                                                              in_=wk_d[p].rearrange("(k p) n -> p k n", p=128)), writes=[wk[p]])
            P.dma("pool", lambda e, p=p: e.dma_start(out=wv[p].t[:], in_=wv_d[p].rearrange("(k p) n -> p k n", p=128)), writes=[wv[p]])
        emit_rot_weights(P, wq, wqr, 4)
        for p in range(3):
            emit_rot_weights(P, wk[p], wkr[p], 2)
        modp = emit_mod_cols(P, nc, adaw_d, adabT_d, cT_d, 2 * D, None, ps[0])
        P.op("dve", lambda e: e.tensor_scalar(modp.t[:, 8:16], modp.t[:, 8:16], 1.0, None, ALU.add), reads=[modp], writes=[modp])

        NSP = S_LEN // SPAN
        hT = sb("hT", [128, 8, SPAN], BF16)
        xts = [sb("xts%d" % i, [128, D], F32) for i in range(2)]
        qT = [sb("qT%d" % i, [128, 2, SPAN], BF16) for i in range(2)]
        kT = [[sb("kT%d_%d" % (p, s_), [128, SPAN], BF16) for s_ in range(2)] for p in range(3)]
        V = [[sb("V%d_%d" % (p, s_), [128, 16, 80], BF16) for s_ in range(2)] for p in range(3)]
        posI = sb("posI", [128, SPAN], I32)
        posB = sb("posB", [128, SPAN], F32)
        Ct = sb("Ct", [128, SPAN], F32)
        St = sb("St", [128, SPAN], F32)
        tmpT = sb("tmpT", [128, SPAN], F32)
        tmpa = sb("tmpa", [128, 512], F32)
        tmpb = sb("tmpb", [128, 512], F32)
        PT = [sb("PT%d" % i, [128, 512], BF16) for i in range(4)]
        accs = [sb("accs%d" % i, [128, 260], F32) for i in range(3)]
        for p in range(3):
            for s_ in range(2):
                P.op("pool", lambda e, p=p, s_=s_: e.memset(V[p][s_].t[:, :, 64:65], 1.0), writes=[V[p][s_]])
        OPb = [Buf("OP%d" % p) for p in range(3)]
        import os
        STG = int(os.environ.get("DILSTAGE", "9"))
        ATT = int(os.environ.get("DILATT", "9"))
        NSP = int(os.environ.get("DILNSP", str(NSP)))
        nS = 0
        nPT = 0
        nacc = 0
        for s in range(NSP):
            sl = s % 2
            tok0 = s * SPAN
            for ti in range(16):
                emit_hmodT_tile(P, x_d, tok0 + ti * 128, xts[ti % 2], psX, idn, modp, hT, ti * 128, 8, ["sp", "act"][ti % 2])
            if STG < 2:
                continue
            emit_rope_tables(P, posB, posI, invp, negpi, Ct, St, tmpT, pos_d[0:1, tok0:tok0 + SPAN], SPAN)
            if STG < 3:
                continue
            for qc in range(2):
                for tg in range(4):
                    emit_proj_rope(P, hT, tg * 512, 512, wq, wqr, qc * 128, ps[0], ps[1], Ct, St, tg * 512, tmpa, tmpb,
                                   lambda qc=qc, tg=tg, sl=sl: qT[sl].t[:, qc, tg * 512:(tg + 1) * 512], qT[sl])
            for p in range(3):
                for tg in range(4):
                    emit_proj_rope(P, hT, tg * 512, 512, wk[p], wkr[p], 0, ps[0], ps[1], Ct, St, tg * 512, tmpa, tmpb,
                                   lambda p=p, tg=tg, sl=sl: kT[p][sl].t[:, tg * 512:(tg + 1) * 512], kT[p][sl])
            if STG < 4:
                continue
            for p, d in enumerate(DIL):
                ncb = 16 // d
                for r in range(d):
                    for c in range(ncb):
                        idx = r * ncb + c
                        a0 = r + d * 128 * c
                        pv = ps[2 + idx % 2]
                        for k in range(8):
                            P.op("pe", lambda e, k=k, a0=a0, d=d, p=p, pv=pv: e.matmul(
                                pv.t[:, 0:64], hT.t[:, k, ss(a0, 128, d)], wv[p].t[:, k, :],
                                start=(k == 0), stop=(k == 7)), reads=[hT, wv[p]], writes=[pv])
                        P.op("act", lambda e, p=p, sl=sl, idx=idx, pv=pv: e.copy(V[p][sl].t[:, idx, 0:64], pv.t[:, 0:64]),
                             reads=[pv], writes=[V[p][sl]])
            if STG < 5:
                continue
            for p, d in enumerate(DIL):
                ncb = 16 // d
                for r in range(d):
                    for j in range(ncb):
                        acc = ps[4 + nacc % 2]
                        chunks = [(j - 1, 0), (j, 1)]
                        chunks = [(kb, mc) for kb, mc in chunks if not (s == 0 and kb < 0)]
                        pts = []
                        for ci, (kb, mc) in enumerate(chunks):
                            ksl = sl if kb >= 0 else 1 - sl
                            kbb = kb if kb >= 0 else ncb - 1
                            ka0 = r + d * 128 * kbb
                            qa0 = r + d * 128 * j
                            Sp = ps[nS % 2]
                            nS += 1
                            for h in range(4):
                                qc, hf = h // 2, h % 2
                                rows = slice(hf * 64, (hf + 1) * 64)
                                P.op("pe", lambda e, h=h, qc=qc, rows=rows, p=p, ksl=ksl, ka0=ka0, qa0=qa0, d=d, sl=sl, Sp=Sp: e.matmul(
                                    Sp.t[:, h * 128:(h + 1) * 128],
                                    kT[p][ksl].t[rows, ss(ka0, 128, d)],
                                    qT[sl].t[rows, qc, ss(qa0, 128, d)],
                                    start=True, stop=False), reads=[kT[p][ksl], qT[sl]], writes=[Sp])
                                P.op("pe", lambda e, h=h, mc=mc, Sp=Sp: e.matmul(Sp.t[:, h * 128:(h + 1) * 128], idnb.t[:], msk.t[:, mc, 0:128],
                                                                              start=False, stop=True), reads=[idnb, msk], writes=[Sp])
                            pt = PT[nPT % 4]
                            nPT += 1
                            P.op("act", lambda e, pt=pt, Sp=Sp: e.activation(pt.t[:], Sp.t[:, 0:512], AF.Exp, scale=0.125), reads=[Sp], writes=[pt])
                            pts.append((pt, ksl, r * ncb + kbb))
                        for h in range(4):
                            for ci, (pt, ksl, vidx) in enumerate(pts):
                                P.op("pe", lambda e, h=h, pt=pt, p=p, ksl=ksl, vidx=vidx, acc=acc, ci=ci, nch=len(pts): e.matmul(
                                    acc.t[:, h * 80:h * 80 + 65], pt.t[:, h * 128:(h + 1) * 128], V[p][ksl].t[:, vidx, 0:65],
                                    start=(ci == 0), stop=(ci == nch - 1)), reads=[pt, V[p][ksl]], writes=[acc])
                        ab = accs[nacc % 3]
                        nacc += 1
                        if ATT < 5:
                            continue
                        P.op("dve", lambda e, ab=ab, acc=acc: e.tensor_copy(ab.t[:].rearrange("p (h e) -> p h e", e=65), acc.t[:, 0:320].rearrange("p (h e) -> p h e", e=80)[:, :, 0:65]), reads=[acc], writes=[ab])
                        g0 = tok0 + r + d * 128 * j
                        if ATT < 6:
                            continue
                        P.dma("sp", lambda e, ab=ab, p=p, g0=g0, d=d: e.dma_start(
                            out=OP[p][ss(g0, 128, d), :], in_=ab.t[:]), reads=[ab], writes=[OPb[p]])
        P.barrier()
        if STG < 6:
            P.emit()
            return nc
        ld = [[sb("ld%d_%d" % (p, i), [128, 260], F32) for i in range(2)] for p in range(3)]
        rl = sb("rl", [128, 4], F32)
        ot = [sb("otl%d" % i, [128, 256], F32) for i in range(2)]
        Ob = Buf("o")
        for T in range(S_LEN // 128):
            b = T % 2
            for p in range(3):
                P.dma(["sp", "act", "sp"][p], lambda e, p=p, b=b, T=T: e.dma_start(out=ld[p][b].t[:], in_=OP[p][T * 128:(T + 1) * 128, :]),
                      reads=[OPb[p]], writes=[ld[p][b]])
            P.op("dve", lambda e, b=b: e.tensor_tensor(ld[0][b].t[:], ld[0][b].t[:], ld[1][b].t[:], ALU.add), reads=[ld[0][b], ld[1][b]], writes=[ld[0][b]])
            P.op("dve", lambda e, b=b: e.tensor_tensor(ld[0][b].t[:], ld[0][b].t[:], ld[2][b].t[:], ALU.add), reads=[ld[0][b], ld[2][b]], writes=[ld[0][b]])
            a3 = ld[0][b].t[:].rearrange("p (h e) -> p h e", e=65)
            P.op("dve", lambda e, a3=a3: e.reciprocal(rl.t[:], a3[:, :, 64]), reads=[ld[0][b]], writes=[rl])
            P.op("dve", lambda e, a3=a3, b=b: e.tensor_tensor(ot[b].t[:].rearrange("p (h e) -> p h e", e=64), a3[:, :, 0:64],
                                                             rl.t[:].unsqueeze(2).to_broadcast([128, 4, 64]), ALU.mult),
                 reads=[ld[0][b], rl], writes=[ot[b]])
            P.dma("sp", lambda e, b=b, T=T: e.dma_start(out=o_d[T * 128:(T + 1) * 128, :], in_=ot[b].t[:]), reads=[ot[b]], writes=[Ob])
        P.wait_all("sp", [Ob])
        P.emit()
    return nc


def run_dil(x, c, positions, ada_w_s, ada_b_s, w_in):
    if "dil" not in _CACHE:
        _CACHE["dil"] = build_dil()
    nc = _CACHE["dil"]
    idn = np.eye(128, dtype=np.float32)
    inv = rope_inv_table()
    msk = tri_masks()
    in_maps = []
    for core in range(8):
        b, g = core // 4, core % 4
        wk = np.stack([w_in[:, D + p * 512 + g * 64: D + p * 512 + g * 64 + 64] for p in range(3)])
        wv = np.stack([w_in[:, D + p * 512 + 256 + g * 64: D + p * 512 + 256 + g * 64 + 64] for p in range(3)])
        in_maps.append({
            "x": x[b], "cT": np.ascontiguousarray(c[b].reshape(8, 128).T),
            "adaw": np.ascontiguousarray(ada_w_s[:, 0:2 * D]), "adabT": np.ascontiguousarray(ada_b_s[0:2 * D].reshape(16, 128).T),
            "wq": np.ascontiguousarray(w_in[:, g * 256:(g + 1) * 256]), "wk": np.ascontiguousarray(wk), "wv": np.ascontiguousarray(wv),
            "pos": np.ascontiguousarray(positions[b:b + 1]), "inv": inv, "idn": idn, "msk": msk,
        })
    res = run_bass_kernel_spmd(nc, in_maps, core_ids=list(range(8)))
    B = x.shape[0]
    o = np.zeros((B, S_LEN, D), np.float32)
    for core in range(8):
        b, g = core // 4, core % 4
        o[b, :, g * 256:(g + 1) * 256] = res.results[core]["o"]
    return o


QG = 512
NG = S_LEN // QG
NCMP = 1023


def nsa_masks():
    p = np.arange(128)[:, None]
    f = np.arange(512)[None, :]
    ms = []
    for jj in range(4):
        ms.append(np.where(f - p - 128 * jj >= 0, 0.0, NEG))
    for c in range(4):
        ms.append(np.where(f - p < 128 * c, 0.0, NEG))
    for m in range(5):
        ms.append(np.where(f - 16 * p + 512 * m - 31 >= 0, 0.0, NEG))
    return np.stack(ms, axis=1).astype(np.float32)


def nsa_indc():
    t = np.zeros((128, 64, 128), np.float32)
    for jm in range(64):
        t[2 * jm, jm, 0:64] = 1.0
        t[2 * jm + 1, jm, 64:128] = 1.0
    return t


def nsa_vc_const():
    t = np.zeros((1024, 257), np.float32)
    t[:, 0] = 1.0
    for s_ in range(256):
        for n in range(max(0, 4 * s_ - 1), min(NCMP, 4 * s_ + 4)):
            t[n, 1 + s_] = 1.0
    return t


def nsa_forced():
    t = np.zeros((128, 3), np.float32)
    for p in range(128):
        c = p // 64
        for x in (-1, 0, 1):
            if x == c or x == c - 1:
                t[p, x + 1] = 1.0e4
    return t


def build_nsa():
    nc = bass.Bass("TRN2", target_bir_lowering=False)
    dt_in = lambda name, shape, dt=F32: nc.dram_tensor(name, list(shape), dt, kind="ExternalInput").ap()
    x_d = dt_in("x", [S_LEN, D])
    cT_d = dt_in("cT", [128, 8])
    adaw_d = dt_in("adaw", [D, 2 * D])
    adabT_d = dt_in("adabT", [128, 16])
    wq_d = dt_in("wq", [D, 256])
    wkv_d = dt_in("wkv", [6, D, 64])
    wgt_d = dt_in("wgt", [D, 12])
    pos_d = dt_in("pos", [1, S_LEN], I32)
    posc_d = dt_in("posc", [1, 1024], I32)
    inv_d = dt_in("inv", [128, 1])
    idn_d = dt_in("idn", [128, 128])
    msk_d = dt_in("msk", [128, 13, 512])
    gp_d = dt_in("gp", [64, S_LEN])
    vcc_d = dt_in("vcc", [1024, 257])
    frc_d = dt_in("frc", [128, 3])
    w1_d = dt_in("w1", [2, 2048, 256])
    w2_d = dt_in("w2", [2, 256, 64])
    cpos_d = dt_in("cposT", [128, 32])
    o_d = nc.dram_tensor("o", [S_LEN, 256], F32, kind="ExternalOutput").ap()

    with ExitStack() as st:
        P = Prog(nc, st)
        sb = P.sbuf
        idn = sb("idn", [128, 128], F32)
        idnb = sb("idnb", [128, 128], BF16)
        msk = sb("msk", [128, 13, 512], BF16)
        frc = sb("frc", [128, 3], F32)
        invp = sb("invp", [128, 1], F32)
        wqh = [sb("wqh%d" % h_, [128, 8, 128], BF16) for h_ in range(4)]
        wqhr = [sb("wqhr%d" % h_, [128, 8, 128], BF16) for h_ in range(4)]
        wks = sb("wks", [128, 8, 128], BF16)
        wksr = sb("wksr", [128, 8, 128], BF16)
        wkw = sb("wkw", [128, 8, 128], BF16)
        wkwr = sb("wkwr", [128, 8, 128], BF16)
        wkvc = sb("wkvc", [128, 8, 128], BF16)
        wvs = sb("wvs", [128, 8, 64], BF16)
        wvw = sb("wvw", [128, 8, 64], BF16)
        wgt = sb("wgt", [128, 8, 12], BF16)
        kselT = sb("kselT", [128, S_LEN], BF16)
        Vsel = sb("Vsel", [128, 128, 80], BF16)
        kcT = sb("kcT", [128, 1024], BF16)
        VC = sb("VC", [128, 8, 336], BF16)
        ps = [P.psum("pb%d" % i, [128, 512], F32) for i in range(6)]
        psXb = P.psum("psX", [128, 512], F32)
        psS3 = P.psum("psS3", [128, 512], F32)
        spr = [ps[0], ps[1], psS3]

        def ld(q, dst, src, ap=None):
            P.dma(q, lambda e: e.dma_start(out=dst.t[:] if ap is None else ap, in_=src), writes=[dst])
        ld("sp", idn, idn_d)
        ld("pool", idnb, idn_d)
        ld("pool", msk, msk_d)
        ld("sp", frc, frc_d)
        ld("sp", invp, inv_d)
        kp = lambda a: a.rearrange("(k p) n -> p k n", p=128)
        for h_ in range(4):
            P.op("pool", lambda e, h_=h_: e.memset(wqh[h_].t[:], 0.0), writes=[wqh[h_]])
            ld("pool", wqh[h_], kp(wq_d[:, h_ * 64:(h_ + 1) * 64]), wqh[h_].t[:, :, 0:64])
        for h in range(2):
            ld("pool", wks, kp(wkv_d[2]), wks.t[:, :, h * 64:(h + 1) * 64])
            ld("pool", wkw, kp(wkv_d[4]), wkw.t[:, :, h * 64:(h + 1) * 64])
        ld("pool", wkvc, kp(wkv_d[0]), wkvc.t[:, :, 0:64])
        ld("pool", wkvc, kp(wkv_d[1]), wkvc.t[:, :, 64:128])
        ld("pool", wvs, kp(wkv_d[3]))
        ld("pool", wvw, kp(wkv_d[5]))
        ld("pool", wgt, kp(wgt_d))
        for h_ in range(4):
            emit_rot_weights(P, wqh[h_], wqhr[h_], 2)
        emit_rot_weights(P, wks, wksr, 2)
        emit_rot_weights(P, wkw, wkwr, 2)
        P.op("pool", lambda e: e.memset(Vsel.t[:, :, 64:65], 1.0), writes=[Vsel])
        for c in range(8):
            P.dma("pool", lambda e, c=c: e.dma_start(out=VC.t[:, c, 64:321], in_=vcc_d[c * 128:(c + 1) * 128, :]), writes=[VC])
        modp = emit_mod_cols(P, nc, adaw_d, adabT_d, cT_d, 2 * D, None, ps[0])
        P.op("dve", lambda e: e.tensor_scalar(modp.t[:, 8:16], modp.t[:, 8:16], 1.0, None, ALU.add), reads=[modp], writes=[modp])

        hT = sb("hT", [128, 8, QG], BF16)
        xts = [sb("xts%d" % i, [128, D], F32) for i in range(2)]
        posI = sb("posI", [128, 512], I32)
        posB = sb("posB", [128, 512], F32)
        Ct = sb("Ct", [128, 512], F32)
        St = sb("St", [128, 512], F32)
        tmpT = sb("tmpT", [128, 512], F32)
        tmpa = sb("tmpa", [128, 512], F32)
        tmpb = sb("tmpb", [128, 512], F32)

        import os
        NST = int(os.environ.get("NSA_STAGE", "9"))
        if NST < 2:
            P.barrier(); P.emit(); return nc
        P.push()
        kvcT = sb("kvcT", [128, S_LEN], BF16)
        for G in range(NG):
            t0 = G * QG
            for ti in range(4):
                emit_hmodT_tile(P, x_d, t0 + ti * 128, xts[ti % 2], psXb, idn, modp, hT, ti * 128, 8, ["sp", "act"][ti % 2], width=512)
            emit_rope_tables(P, posB, posI, invp, None, Ct, St, tmpT, pos_d[0:1, t0:t0 + QG], QG)
            emit_proj_rope(P, hT, 0, QG, wks, wksr, 0, ps[0], ps[1], Ct, St, 0, tmpa, tmpb,
                           lambda t0=t0: kselT.t[:, t0:t0 + QG], kselT)
            for k in range(8):
                P.op("pe", lambda e, k=k: e.matmul(ps[2].t[:, 0:QG], wkvc.t[:, k, :], hT.t[:, k, :], start=(k == 0), stop=(k == 7)),
                     reads=[wkvc, hT], writes=[ps[2]])
            P.op("act", lambda e, t0=t0: e.copy(kvcT.t[:, t0:t0 + QG], ps[2].t[:, 0:QG]), reads=[ps[2]], writes=[kvcT])
            for ti in range(4):
                pv = ps[3 + ti % 2]
                for k in range(8):
                    P.op("pe", lambda e, k=k, ti=ti, pv=pv: e.matmul(pv.t[:, 0:64], hT.t[:, k, ti * 128:(ti + 1) * 128], wvs.t[:, k, :],
                                                                     start=(k == 0), stop=(k == 7)), reads=[hT, wvs], writes=[pv])
                P.op("act", lambda e, ti=ti, G=G, pv=pv: e.copy(Vsel.t[:, G * 4 + ti, 0:64], pv.t[:, 0:64]), reads=[pv], writes=[Vsel])

        P.dma("pool", lambda e: e.dma_start(out=kselT.t[64:128, :], in_=gp_d), writes=[kselT])
        if NST < 3:
            P.barrier(); P.emit(); return nc
        w1 = sb("w1", [128, 32, 256], BF16)
        w2k = sb("w2k", [128, 2, 128], BF16)
        w2kr = sb("w2kr", [128, 2, 128], BF16)
        w2v = sb("w2v", [128, 2, 64], BF16)
        cposT = sb("cposT", [128, 32], BF16)
        cbias = sb("cbias", [128, 4], F32)
        hid = [[sb("hid%d_%d" % (kv, hc), [128, 1024], BF16) for hc in range(2)] for kv in range(2)]
        xg = sb("xg", [128, 512], F32)
        ug = sb("ug", [128, 512], F32)
        for kv in range(2):
            P.dma("pool", lambda e, kv=kv: e.dma_start(out=w1.t[kv * 64:(kv + 1) * 64, :, :], in_=w1_d[kv].rearrange("(l e) h -> e l h", e=64)), writes=[w1])
        for h in range(2):
            P.dma("pool", lambda e, h=h: e.dma_start(out=w2k.t[:, :, h * 64:(h + 1) * 64], in_=w2_d[0].rearrange("(k p) n -> p k n", p=128)), writes=[w2k])
        P.dma("pool", lambda e: e.dma_start(out=w2v.t[:], in_=w2_d[1].rearrange("(k p) n -> p k n", p=128)), writes=[w2v])
        P.dma("pool", lambda e: e.dma_start(out=cposT.t[:], in_=cpos_d), writes=[cposT])
        P.op("pool", lambda e: e.memset(w2kr.t[:], 0.0), writes=[w2kr])
        for k in range(2):
            wv_ = w2k.t[:, k, :].rearrange("p (h e) -> p h e", e=64)
            rv_ = w2kr.t[:, k, :].rearrange("p (h e) -> p h e", e=64)
            P.op("dve", lambda e, wv_=wv_, rv_=rv_: e.tensor_scalar(rv_[:, :, 0:8], wv_[:, :, 8:16], -1.0, None, ALU.mult), reads=[w2k], writes=[w2kr])
            P.op("dve", lambda e, wv_=wv_, rv_=rv_: e.tensor_copy(rv_[:, :, 8:16], wv_[:, :, 0:8]), reads=[w2k], writes=[w2kr])
        for kv in range(2):
            for hc in range(2):
                P.op("pool", lambda e, kv=kv, hc=hc: e.memset(hid[kv][hc].t[:], 0.0), writes=[hid[kv][hc]])
        for kv in range(2):
            rows = slice(kv * 64, (kv + 1) * 64)
            for hc in range(2):
                col = kv * 2 + hc
                for l in range(32):
                    P.op("pe", lambda e, rows=rows, hc=hc, l=l, col=col: e.matmul(
                        ps[0].t[:, col:col + 1], w1.t[rows, l, hc * 128:(hc + 1) * 128], cposT.t[rows, l:l + 1],
                        start=(l == 0), stop=(l == 31)), reads=[w1, cposT], writes=[ps[0]])
        P.op("dve", lambda e: e.tensor_copy(cbias.t[:], ps[0].t[:, 0:4]), reads=[ps[0]], writes=[cbias])
        for kv in range(2):
            rows = slice(kv * 64, (kv + 1) * 64)
            for hc in range(2):
                col = kv * 2 + hc
                for gi in range(2):
                    n0 = gi * 512
                    nn = 512 if gi == 0 else 511
                    pz = ps[1 + (hc * 2 + gi) % 2]
                    for l in range(32):
                        P.op("pe", lambda e, rows=rows, hc=hc, l=l, n0=n0, nn=nn, pz=pz: e.matmul(
                            pz.t[:, 0:nn], w1.t[rows, l, hc * 128:(hc + 1) * 128], kvcT.t[rows, ss(16 * n0 + l, nn, 16)],
                            start=(l == 0), stop=(l == 31)), reads=[w1, kvcT], writes=[pz])
                    P.op("act", lambda e, pz=pz, nn=nn, col=col: e.activation(xg.t[:, 0:nn], pz.t[:, 0:nn], AF.Identity, bias=cbias.t[:, col:col + 1]),
                         reads=[pz, cbias], writes=[xg])
                    P.op("dve", lambda e, nn=nn: e.tensor_tensor(ug.t[:, 0:nn], xg.t[:, 0:nn], xg.t[:, 0:nn], ALU.mult), reads=[xg], writes=[ug])
                    P.op("dve", lambda e, nn=nn: e.tensor_scalar(ug.t[:, 0:nn], ug.t[:, 0:nn], 0.044715, 1.0, ALU.mult, ALU.add), reads=[ug], writes=[ug])
                    P.op("dve", lambda e, nn=nn: e.tensor_tensor(ug.t[:, 0:nn], ug.t[:, 0:nn], xg.t[:, 0:nn], ALU.mult), reads=[ug, xg], writes=[ug])
                    P.op("act", lambda e, nn=nn: e.activation(ug.t[:, 0:nn], ug.t[:, 0:nn], AF.Sigmoid, scale=1.5957691216057308), reads=[ug], writes=[ug])
                    P.op("dve", lambda e, nn=nn, kv=kv, hc=hc, n0=n0: e.tensor_tensor(hid[kv][hc].t[:, n0:n0 + nn], ug.t[:, 0:nn], xg.t[:, 0:nn], ALU.mult),
                         reads=[ug, xg], writes=[hid[kv][hc]])
        for gi in range(2):
            n0 = gi * 512
            emit_rope_tables(P, posB, posI, invp, None, Ct, St, tmpT, posc_d[0:1, n0:n0 + 512], 512)
            for hc in range(2):
                P.op("pe", lambda e, hc=hc, n0=n0: e.matmul(ps[0].t[:, 0:512], w2k.t[:, hc, :], hid[0][hc].t[:, n0:n0 + 512], start=(hc == 0), stop=(hc == 1)),
                     reads=[w2k, hid[0][hc]], writes=[ps[0]])
            for hc in range(2):
                P.op("pe", lambda e, hc=hc, n0=n0: e.matmul(ps[1].t[:, 0:512], w2kr.t[:, hc, :], hid[0][hc].t[:, n0:n0 + 512], start=(hc == 0), stop=(hc == 1)),
                     reads=[w2kr, hid[0][hc]], writes=[ps[1]])
            P.op("dve", lambda e, n0=n0: e.tensor_tensor(tmpa.t[:], ps[0].t[:, 0:512], Ct.t[:, 0:512], ALU.mult), reads=[ps[0], Ct], writes=[tmpa])
            P.op("dve", lambda e, n0=n0: e.tensor_tensor(tmpb.t[:], ps[1].t[:, 0:512], St.t[:, 0:512], ALU.mult), reads=[ps[1], St], writes=[tmpb])
            P.op("pool", lambda e, n0=n0: e.tensor_tensor(kcT.t[:, n0:n0 + 512], tmpa.t[:], tmpb.t[:], ALU.add), reads=[tmpa, tmpb], writes=[kcT])
        for c in range(8):
            pv = ps[2 + c % 2]
            for hc in range(2):
                P.op("pe", lambda e, hc=hc, c=c, pv=pv: e.matmul(pv.t[:, 0:64], hid[1][hc].t[:, c * 128:(c + 1) * 128], w2v.t[:, hc, :],
                                                                 start=(hc == 0), stop=(hc == 1)), reads=[hid[1][hc], w2v], writes=[pv])
            P.op("act", lambda e, c=c, pv=pv: e.copy(VC.t[:, c, 0:64], pv.t[:, 0:64]), reads=[pv], writes=[VC])
        P.pop()
        if NST < 4:
            P.barrier(); P.emit(); return nc

        R = [[sb("R%d_%d" % (h_, w_), [128, QG], BF16) for w_ in range(4)] for h_ in range(4)]
        nbTw = [sb("nbTw%d" % w_, [128, QG], BF16) for w_ in range(4)]
        nbp = sb("nbp", [128, 320], F32)
        P.op("pool", lambda e: e.memset(nbp.t[:], 0.0), writes=[nbp])
        kwT = [sb("kwT%d" % i, [128, QG], BF16) for i in range(2)]
        Vw = sb("Vw", [128, 8, 80], BF16)
        gts = sb("gts", [128, 4, 12], F32)
        PT = [sb("PT%d" % i, [128, 512], BF16) for i in range(4)]
        impw = sb("impw", [128, 256], F32)
        m8a = sb("m8a", [128, 8], F32)
        m8b = sb("m8b", [128, 8], F32)
        P.op("pool", lambda e: e.memset(Vw.t[:, :, 64:65], 1.0), writes=[Vw])
        Ob = Buf("o")
        acc = [ps[2], ps[3], ps[4], ps[5]]
        psW = acc
        cnt = {"S": 0, "PT": 0, "first": [True] * 4}

        def attend(h, chunks, ncols, G):
            qc, hf = h // 2, h % 2
            rows = slice(hf * 64, (hf + 1) * 64)
            first = {s_: True for s_ in range(4)}
            last_idx = {}
            for ci, ch in enumerate(chunks):
                for s_ in range(ch[3], ch[4] + 1):
                    last_idx[s_] = ci
            def qk(ci):
                kfn, masks, vfn, slo, shi = chunks[ci]
                Sp = spr[cnt["S"] % 3]
                cnt["S"] += 1
                P.op("pe", lambda e, kfn=kfn, Sp=Sp, nm0=len(masks): e.matmul(Sp.t[:, 0:QG], kfn(rows)[0], kfn(rows)[1], start=True, stop=(nm0 == 0)),
                     reads=[kselT, kcT, kwT[0], kwT[1]] + R[h], writes=[Sp])
                for mi, (lf, rf) in enumerate(masks):
                    P.op("pe", lambda e, lf=lf, rf=rf, Sp=Sp, mi=mi, nm=len(masks): e.matmul(Sp.t[:, 0:QG], lf(), rf(), start=False, stop=(mi == nm - 1)),
                         reads=[idnb, msk], writes=[Sp])
                return Sp

            def ex(ci, Sp):
                pt = PT[cnt["PT"] % 4]
                cnt["PT"] += 1
                P.op("act", lambda e, pt=pt, Sp=Sp: e.activation(pt.t[:], Sp.t[:, 0:QG], AF.Exp, scale=0.125), reads=[Sp], writes=[pt])
                return pt

            def pv(ci, pt):
                kfn, masks, vfn, slo, shi = chunks[ci]
                for s_ in range(slo, shi + 1):
                    P.op("pe", lambda e, s_=s_, pt=pt, vfn=vfn, st_=first[s_], sp_=(last_idx[s_] == ci): e.matmul(
                        acc[s_].t[:, 0:ncols], pt.t[:, s_ * 128:(s_ + 1) * 128], vfn(), start=st_, stop=sp_),
                        reads=[pt, Vsel, VC, Vw], writes=[acc[s_]])
                    first[s_] = False

            sps = {0: qk(0)}
            if len(chunks) > 1:
                sps[1] = qk(1)
            for ci in range(len(chunks)):
                pt = ex(ci, sps.pop(ci))
                if ci + 2 < len(chunks):
                    sps[ci + 2] = qk(ci + 2)
                pv(ci, pt)

        stg = [sb("stg%d" % br, [128, 4, 4, 65], F32) for br in range(3)]
        stgB = [[Buf("stgB%d_%d" % (br, s_), stg[br].t) for s_ in range(4)] for br in range(3)]
        impS = sb("impS", [128, 4, 4, 256], F32)
        impSB = [Buf("impSB%d" % s_, impS.t) for s_ in range(4)]
        osb4 = sb("osb4", [128, 4, 4, 64], F32)
        otmp = sb("otmp", [128, 4, 4, 64], F32)
        rlb = sb("rlb", [128, 4, 4], F32)
        wgb = sb("wgb", [128, 4, 4], F32)
        impacc4 = sb("impacc4", [128, 4, 256], F32)

        def evac(h, s_, br):
            P.op("dve", lambda e: e.tensor_copy(stg[br].t[:, s_, h, :], acc[s_].t[:, 0:65]), reads=[acc[s_]], writes=[stgB[br][s_]])
            if br == 0:
                P.op("dve", lambda e: e.tensor_copy(impS.t[:, s_, h, :], acc[s_].t[:, 65:321]), reads=[acc[s_]], writes=[impSB[s_]])

        def finish_branch(br, first_branch):
            P.op("dve", lambda e: e.tensor_scalar(rlb.t[:], stg[br].t[:, :, :, 64], 1e-30, None, ALU.max), reads=stgB[br], writes=[rlb])
            P.op("dve", lambda e: e.reciprocal(rlb.t[:], rlb.t[:]), reads=[rlb], writes=[rlb])
            P.op("dve", lambda e: e.tensor_tensor(wgb.t[:], rlb.t[:], gts.t[:, :, ss(br, 4, 3)], ALU.mult), reads=[rlb, gts], writes=[wgb])
            dst = osb4 if first_branch else otmp
            P.op("dve", lambda e: e.tensor_tensor(dst.t[:], stg[br].t[:, :, :, 0:64], wgb.t[:].unsqueeze(3).to_broadcast([128, 4, 4, 64]), ALU.mult),
                 reads=stgB[br] + [wgb], writes=[dst])
            if not first_branch:
                P.op("pool", lambda e: e.tensor_tensor(osb4.t[:], osb4.t[:], otmp.t[:], ALU.add), reads=[otmp, osb4], writes=[osb4])
            if br == 0:
                P.op("dve", lambda e: e.tensor_tensor(impS.t[:], impS.t[:], rlb.t[:].unsqueeze(3).to_broadcast([128, 4, 4, 256]), ALU.mult),
                     reads=impSB + [rlb], writes=impSB)
                P.op("pool", lambda e: e.tensor_tensor(impacc4.t[:], impS.t[:, :, 0, :], impS.t[:, :, 1, :], ALU.add), reads=impSB, writes=[impacc4])
                P.op("pool", lambda e: e.tensor_tensor(impacc4.t[:], impacc4.t[:], impS.t[:, :, 2, :], ALU.add), reads=impSB + [impacc4], writes=[impacc4])
                P.op("pool", lambda e: e.tensor_tensor(impacc4.t[:], impacc4.t[:], impS.t[:, :, 3, :], ALU.add), reads=impSB + [impacc4], writes=[impacc4])

        import os
        NG_RUN = int(os.environ.get("NSA_NG", str(NG)))
        for G in range(NG_RUN):
            t0 = G * QG
            sl = G % 2
            for ti in range(4):
                emit_hmodT_tile(P, x_d, t0 + ti * 128, xts[ti % 2], psXb, idn, modp, hT, ti * 128, 8, ["sp", "act"][ti % 2], width=512)
            emit_rope_tables(P, posB, posI, invp, None, Ct, St, tmpT, pos_d[0:1, t0:t0 + QG], QG)
            for h_ in range(4):
                emit_proj_rope(P, hT, 0, QG, wqh[h_], wqhr[h_], 0, ps[0], ps[1], Ct, St, 0, tmpa, tmpb,
                               lambda h_=h_: R[h_][0].t[:, :], R[h_][0])
            emit_proj_rope(P, hT, 0, QG, wkw, wkwr, 0, ps[0], ps[1], Ct, St, 0, tmpa, tmpb, lambda sl=sl: kwT[sl].t[:, :], kwT[sl])
            for ti in range(4):
                pv = ps[2 + ti % 2]
                for k in range(8):
                    P.op("pe", lambda e, k=k, ti=ti, pv=pv: e.matmul(pv.t[:, 0:64], hT.t[:, k, ti * 128:(ti + 1) * 128], wvw.t[:, k, :],
                                                                     start=(k == 0), stop=(k == 7)), reads=[hT, wvw], writes=[pv])
                P.op("act", lambda e, ti=ti, sl=sl, pv=pv: e.copy(Vw.t[:, sl * 4 + ti, 0:64], pv.t[:, 0:64]), reads=[pv], writes=[Vw])
                pg = ps[4 + ti % 2]
                for k in range(8):
                    P.op("pe", lambda e, k=k, ti=ti, pg=pg: e.matmul(pg.t[:, 0:12], hT.t[:, k, ti * 128:(ti + 1) * 128], wgt.t[:, k, :],
                                                                     start=(k == 0), stop=(k == 7)), reads=[hT, wgt], writes=[pg])
                P.op("act", lambda e, ti=ti, pg=pg: e.activation(gts.t[:, ti, :], pg.t[:, 0:12], AF.Sigmoid), reads=[pg], writes=[gts])

            if NST < 5:
                continue
            cmax = min(7, G // 4)
            for h in range(4):
                chunks = []
                for c in range(cmax + 1):
                    m = G - 4 * c
                    masks = []
                    if m <= 4:
                        masks = [(lambda: idnb.t[:], lambda m=m: msk.t[:, 8 + m, :])]
                    chunks.append((lambda rows, c=c, h=h: (kcT.t[0:64, c * 128:(c + 1) * 128], R[h][0].t[0:64, :]), masks, lambda c=c: VC.t[:, c, 0:321], 0, 3))
                attend(h, chunks, 321, G)
                for s_ in range(4):
                    evac(h, s_, 0)
            finish_branch(0, True)
            if NST < 6:
                continue
            for s_ in range(4):
                T = G * 4 + s_
                ia = Buf("iav", impacc4.t[:, s_, :])
                lo = max(0, 2 * T - 1)
                hi = min(256, 2 * T + 2)
                P.op("dve", lambda e, ia=ia, lo=lo, hi=hi, T=T: e.tensor_tensor(ia.t[:, lo:hi], ia.t[:, lo:hi], frc.t[:, lo - (2 * T - 1):hi - (2 * T - 1)], ALU.add),
                     reads=[impacc4, frc], writes=[impacc4])
                P.op("dve", lambda e, ia=ia: e.tensor_scalar(ia.t[:, 0:1], ia.t[:, 0:1], 1.0e4, None, ALU.add), reads=[impacc4], writes=[impacc4])
                P.op("dve", lambda e, ia=ia: e.max(out=m8a.t[:], in_=ia.t[:]), reads=[impacc4], writes=[m8a])
                P.op("dve", lambda e, ia=ia: e.match_replace(out=impw.t[:], in_to_replace=m8a.t[:], in_values=ia.t[:], imm_value=-1.0e30),
                     reads=[impacc4, m8a], writes=[impw])
                P.op("dve", lambda e: e.max(out=m8b.t[:], in_=impw.t[:]), reads=[impw], writes=[m8b])
                P.op("dve", lambda e, ia=ia: e.tensor_scalar(nbp.t[:, 64:320], ia.t[:], m8b.t[:, 7:8], NEG, ALU.is_lt, ALU.mult), reads=[impacc4, m8b], writes=[nbp])
                nW = (4 * G + 3) // 32 + 1
                for w_ in range(nW):
                    P.op("pe", lambda e, w_=w_, s_=s_: e.transpose(psW[w_].t[:, s_ * 128:(s_ + 1) * 128], nbp.t[:, 64 * w_:64 * w_ + 128], idn.t[:]),
                         reads=[nbp, idn], writes=[psW[w_]])
            nW = (4 * G + 3) // 32 + 1
            for w_ in range(nW):
                P.op("act", lambda e, w_=w_: e.copy(nbTw[w_].t[:], psW[w_].t[:, 0:QG]), reads=[psW[w_]], writes=[nbTw[w_]])
                for h_ in range(4):
                    if w_ > 0:
                        P.op("pool", lambda e, w_=w_, h_=h_: e.tensor_copy(R[h_][w_].t[0:64, :], R[h_][0].t[0:64, :]), reads=[R[h_][0]], writes=[R[h_][w_]])
                    P.op("pool", lambda e, w_=w_, h_=h_: e.tensor_copy(R[h_][w_].t[64:128, :], nbTw[w_].t[64:128, :]), reads=[nbTw[w_]], writes=[R[h_][w_]])
            if NST < 7:
                continue
            for h in range(4):
                chunks = []
                for j in range(4 * G + 4):
                    w_ = j // 32
                    masks = []
                    jj = j - 4 * G
                    if jj >= 0:
                        masks.append((lambda: idnb.t[:], lambda jj=jj: msk.t[:, jj, :]))
                    chunks.append((lambda rows, j=j, w_=w_, h=h: (kselT.t[:, j * 128:(j + 1) * 128], R[h][w_].t[:, :]), masks, lambda j=j: Vsel.t[:, j, 0:65],
                                   max(0, jj), 3))
                attend(h, chunks, 65, G)
                for s_ in range(4):
                    evac(h, s_, 1)
            finish_branch(1, False)
            if NST < 8:
                continue
            for h in range(4):
                chunks = []
                for c in range(8):
                    if t0 - 512 + 128 * c < 0:
                        continue
                    if c < 4:
                        kfn = lambda rows, c=c, sl=sl, h=h: (kwT[1 - sl].t[0:64, c * 128:(c + 1) * 128], R[h][0].t[0:64, :])
                        vfn = lambda c=c, sl=sl: Vw.t[:, (1 - sl) * 4 + c, 0:65]
                        mk = 4 + c
                    else:
                        kfn = lambda rows, c=c, sl=sl, h=h: (kwT[sl].t[0:64, (c - 4) * 128:(c - 3) * 128], R[h][0].t[0:64, :])
                        vfn = lambda c=c, sl=sl: Vw.t[:, sl * 4 + (c - 4), 0:65]
                        mk = c - 4
                    masks = [(lambda: idnb.t[:], lambda mk=mk: msk.t[:, mk, :])]
                    chunks.append((kfn, masks, vfn, max(0, c - 4), min(3, c)))
                attend(h, chunks, 65, G)
                for s_ in range(4):
                    evac(h, s_, 2)
            finish_branch(2, False)
            for s_ in range(4):
                r0 = t0 + s_ * 128
                P.dma("sp", lambda e, s_=s_, r0=r0: e.dma_start(out=o_d[r0:r0 + 128, :], in_=osb4.t[:, s_, :, :].rearrange("p h e -> p (h e)")), reads=[osb4], writes=[Ob])
        P.barrier()
        P.wait_all("sp", [Ob])
        P.emit()
    return nc


def nsa_in_maps(x, c, positions, ada_w_s, ada_b_s, w_in, pos_k, w1_k, w2_k, pos_v, w1_v, w2_v, cores=range(8)):
    idn = np.eye(128, dtype=np.float32)
    inv = rope_inv_table()
    msk = nsa_masks()
    gp = np.zeros((64, S_LEN), np.float32)
    kk = np.arange(S_LEN)
    gp[(kk // 64) % 64, kk] = 1.0
    vcc = nsa_vc_const()
    frc = nsa_forced()
    cposT = np.ascontiguousarray(np.concatenate([pos_k.T, pos_v.T], axis=0))
    w1 = np.ascontiguousarray(np.stack([w1_k, w1_v]))
    w2 = np.ascontiguousarray(np.stack([w2_k, w2_v]))
    in_maps = []
    for core in cores:
        b, g = core // 4, core % 4
        wkv = np.stack([w_in[:, D + br * 512 + kv * 256 + g * 64: D + br * 512 + kv * 256 + g * 64 + 64] for br in range(3) for kv in range(2)])
        posc = np.zeros((1, 1024), np.int32)
        posc[0, :NCMP] = positions[b, 31::16][:NCMP]
        in_maps.append({
            "x": x[b], "cT": np.ascontiguousarray(c[b].reshape(8, 128).T),
            "adaw": np.ascontiguousarray(ada_w_s[:, 0:2 * D]), "adabT": np.ascontiguousarray(ada_b_s[0:2 * D].reshape(16, 128).T),
            "wq": np.ascontiguousarray(w_in[:, g * 256:(g + 1) * 256]), "wkv": np.ascontiguousarray(wkv),
            "wgt": np.ascontiguousarray(w_in[:, D + 1536 + 12 * g: D + 1536 + 12 * g + 12]),
            "pos": np.ascontiguousarray(positions[b:b + 1]), "posc": posc, "inv": inv, "idn": idn, "msk": msk,
            "gp": gp, "vcc": vcc, "frc": frc, "w1": w1, "w2": w2, "cposT": cposT,
        })
    return in_maps


def run_nsa(x, c, positions, ada_w_s, ada_b_s, w_in, pos_k, w1_k, w2_k, pos_v, w1_v, w2_v):
    if "nsa" not in _CACHE:
        _CACHE["nsa"] = build_nsa()
    nc = _CACHE["nsa"]
    in_maps = nsa_in_maps(x, c, positions, ada_w_s, ada_b_s, w_in, pos_k, w1_k, w2_k, pos_v, w1_v, w2_v)
    res = run_bass_kernel_spmd(nc, in_maps, core_ids=list(range(8)))
    B = x.shape[0]
    o = np.zeros((B, S_LEN, D), np.float32)
    for core in range(8):
        b, g = core // 4, core % 4
        o[b, :, g * 256:(g + 1) * 256] = res.results[core]["o"]
    return o


def kernel(x, c, positions, ada_w, ada_b, ln_g, ln_b,
           nsa_w_in, nsa_cmp_pos_k, nsa_cmp_w1_k, nsa_cmp_w2_k,
           nsa_cmp_pos_v, nsa_cmp_w1_v, nsa_cmp_w2_v, nsa_w_o,
           dil_w_in, dil_w_o, router_w, router_b, moe_w_gate, moe_w_up, moe_w_down):
    f = lambda a: np.ascontiguousarray(np.asarray(a))
    x, c, positions = f(x), f(c), f(positions)
    ada_w, ada_b, ln_g, ln_b = f(ada_w), f(ada_b), f(ln_g), f(ln_b)
    nsa_w_in, nsa_w_o, dil_w_in, dil_w_o = f(nsa_w_in), f(nsa_w_o), f(dil_w_in), f(dil_w_o)
    router_w, router_b = f(router_w), f(router_b)
    moe_w_gate, moe_w_up, moe_w_down = f(moe_w_gate), f(moe_w_up), f(moe_w_down)
    o0 = run_nsa(x, c, positions, ada_w[0, 0], ada_b[0, 0], nsa_w_in[0],
                 f(nsa_cmp_pos_k)[0], f(nsa_cmp_w1_k)[0], f(nsa_cmp_w2_k)[0],
                 f(nsa_cmp_pos_v)[0], f(nsa_cmp_w1_v)[0], f(nsa_cmp_w2_v)[0])
    x1 = run_ffn(x, o0, c, ada_w[0], ada_b[0], ln_g[0], ln_b[0], nsa_w_o[0], router_w, router_b,
                 moe_w_gate[0], moe_w_up[0], moe_w_down[0])
    o1 = run_dil(x1, c, positions, ada_w[1, 0], ada_b[1, 0], dil_w_in[0])
    out = run_ffn(x1, o1, c, ada_w[1], ada_b[1], ln_g[1], ln_b[1], dil_w_o[0], router_w, router_b,
                  moe_w_gate[1], moe_w_up[1], moe_w_down[1])
    return out.astype(np.float32)
```

```python
import numpy as np
from contextlib import ExitStack
import concourse.bass as bass
import concourse.mybir as mybir
from concourse.bass_utils import run_bass_kernel_spmd

F32 = mybir.dt.float32
BF16 = mybir.dt.bfloat16
I32 = mybir.dt.int32
AF = mybir.ActivationFunctionType
ALU = mybir.AluOpType
AX = mybir.AxisListType

ENGS = ("pe", "act", "dve", "pool", "sp")

D = 1024
ALPHA = (2.0 * 2) ** 0.25
LN_EPS = 1e-5
NEG = -30000.0
import os as _os
SAME_ENGINE_SYNC = _os.environ.get("SES", "1") == "1"


_UID = [0]


class Buf:
    __slots__ = ("name", "t", "writer", "readers", "dma_sem", "dma_cnt", "uid")

    def __init__(self, name, t=None):
        _UID[0] += 1
        self.uid = _UID[0]
        self.name = name
        self.t = t
        self.writer = None
        self.readers = []
        self.dma_sem = None
        self.dma_cnt = 0


class Prog:
    def __init__(self, nc, stack):
        self.nc = nc
        self.stack = stack
        self.ops = {e: [] for e in ENGS}
        self.cnt = {e: 0 for e in ENGS}
        self.waited = {}
        self.sem = {}
        for e in ENGS:
            self.sem[e] = stack.enter_context(nc.semaphore("s_" + e))
        self.n_dma_sems = 0
        self.dma_bufs = []
        self.scopes = []

    def sbuf(self, name, shape, dt):
        st = self.scopes[-1] if self.scopes else self.stack
        t = st.enter_context(self.nc.sbuf_tensor("sb_" + name, list(shape), dt))
        return Buf(name, t)

    def push(self):
        self.scopes.append(ExitStack())

    def pop(self):
        self.barrier()
        self.scopes.pop().close()

    def barrier(self):
        for eng in ENGS:
            waits = []
            for e2 in ENGS:
                if self.cnt[e2] > 0:
                    self._need(eng, ("eng", e2, self.cnt[e2]), waits)
            if eng == "pe" and self.cnt["pe"] > 0:
                pass
            for b in self.dma_bufs:
                self._need(eng, ("dma", b, b.dma_cnt), waits)
            self.ops[eng].append((waits, None, None))

    def psum(self, name, shape, dt=F32):
        t = self.stack.enter_context(self.nc.psum_tensor("ps_" + name, list(shape), dt))
        return Buf(name, t)

    def _need(self, eng, dep, waits):
        if dep[0] == "eng":
            _, e, idx = dep
            if e == eng and (e == "pe" or not SAME_ENGINE_SYNC):
                return
            key = (eng, "E", e)
            val = idx
            sem = self.sem[e]
        else:
            _, b, c = dep
            key = (eng, "D", b.uid)
            val = 16 * c
            sem = b.dma_sem
        if self.waited.get(key, 0) >= val:
            return
        self.waited[key] = val
        waits.append((sem, val))

    def _deps(self, eng, reads, writes):
        waits = []
        for b in reads:
            if b.writer is not None:
                self._need(eng, b.writer, waits)
        for b in writes:
            if b.writer is not None:
                self._need(eng, b.writer, waits)
            for r in b.readers:
                self._need(eng, r, waits)
        return waits

    def op(self, eng, fn, reads=(), writes=()):
        waits = self._deps(eng, reads, writes)
        self.cnt[eng] += 1
        me = ("eng", eng, self.cnt[eng])
        for b in reads:
            b.readers.append(me)
        for b in writes:
            b.writer = me
            b.readers = []
        self.ops[eng].append((waits, fn, (self.sem[eng], 1)))

    def dma(self, eng, fn, reads=(), writes=()):
        waits = self._deps(eng, reads, writes)
        dst = writes[0]
        if dst.dma_sem is None:
            dst.dma_sem = self.stack.enter_context(self.nc.semaphore("d%d" % self.n_dma_sems))
            self.n_dma_sems += 1
            self.dma_bufs.append(dst)
        dst.dma_cnt += 1
        me = ("dma", dst, dst.dma_cnt)
        for b in reads:
            b.readers.append(me)
        for b in writes:
            b.writer = me
            b.readers = []
        self.ops[eng].append((waits, fn, (dst.dma_sem, 16)))

    def wait_all(self, eng, bufs):
        waits = []
        for b in bufs:
            if b.writer is not None:
                self._need(eng, b.writer, waits)
        self.ops[eng].append((waits, None, None))

    def emit(self):
        nc = self.nc
        ops = self.ops
        with nc.Block() as block:
            def run(e, lst):
                for waits, fn, inc in lst:
                    for sem, val in waits:
                        e.wait_ge(sem, val)
                    if fn is not None:
                        fn(e).then_inc(inc[0], inc[1])

            @block.tensor
            def _(e):
                run(e, ops["pe"])

            @block.scalar
            def _(e):
                run(e, ops["act"])

            @block.vector
            def _(e):
                run(e, ops["dve"])

            @block.gpsimd
            def _(e):
                run(e, ops["pool"])

            @block.sync
            def _(e):
                run(e, ops["sp"])


def ss(a0, n, d):
    return slice(a0, a0 + (n - 1) * d + 1, d) if d > 1 else slice(a0, a0 + n)


def bcast_row(ap_row, n):
    return ap_row.to_broadcast([128, n])


def emit_layernorm(P, z, stats, mv, rstd, xn):
    for h in range(2):
        P.op("dve", lambda e, h=h: e.bn_stats(stats.t[:, h, :], z.t[:, h * 512:(h + 1) * 512]), reads=[z], writes=[stats])
    P.op("dve", lambda e: e.bn_aggr(mv.t[:], stats.t[:]), reads=[stats], writes=[mv])
    P.op("dve", lambda e: e.tensor_scalar(rstd.t[:], mv.t[:, 1:2], LN_EPS, None, ALU.add), reads=[mv], writes=[rstd])
    P.op("act", lambda e: e.sqrt(rstd.t[:], rstd.t[:]), reads=[rstd], writes=[rstd])
    P.op("dve", lambda e: e.reciprocal(rstd.t[:], rstd.t[:]), reads=[rstd], writes=[rstd])
    P.op("dve", lambda e: e.tensor_scalar(xn.t[:], z.t[:], mv.t[:, 0:1], rstd.t[:, 0:1], ALU.subtract, ALU.mult),
         reads=[z, mv, rstd], writes=[xn])


NTOK = 4096
CAP = 1024
NEXP = 32


def build_ffn(dbg=False):
    nc = bass.Bass("TRN2", target_bir_lowering=False)
    NT = NTOK // 128
    dt_in = lambda name, shape, dt=F32: nc.dram_tensor(name, list(shape), dt, kind="ExternalInput").ap()
    x_d = dt_in("x", [NTOK, D])
    o_d = dt_in("o", [NTOK, D])
    cT_d = dt_in("cT", [128, 8])
    adaw_d = dt_in("adaw", [2, D, 3 * D])
    adab_d = dt_in("adab", [2, 3 * D])
    lng_d = dt_in("lng", [2, D])
    lnb_d = dt_in("lnb", [2, D])
    wo_d = dt_in("wo", [D, D])
    rw_d = dt_in("rw", [D, NEXP])
    rb_d = dt_in("rb", [1, NEXP])
    NE_ = (1 if dbg == 1 else 2) if dbg in (1, 2, 3) else NEXP
    if dbg == 4:
        dbg_a = nc.dram_tensor("dbg_a", [128, 4 * D], F32, kind="ExternalOutput").ap()
    if dbg in (3, 4):
        dbg_i = nc.dram_tensor("dbg_i", [128, NTOK // 128 * 2], I32, kind="ExternalOutput").ap()
    wg_d = dt_in("wg", [NE_, D, 512])
    wu_d = dt_in("wu", [NE_, D, 512])
    wd_d = dt_in("wd", [NE_, 512, D])
    idn_d = dt_in("idn", [128, 128])
    tri_d = dt_in("tri", [128, 128])
    offs_d = dt_in("offs", [1, NEXP])
    y_d = nc.dram_tensor("y", [NTOK, D], F32, kind="ExternalOutput").ap()
    XS = nc.dram_tensor("XS", [NEXP * CAP, D], BF16).ap()
    YS = nc.dram_tensor("YS", [NEXP * CAP, D], F32).ap()
    X1 = nc.dram_tensor("X1", [NTOK, D], F32, kind="ExternalOutput" if dbg else "Internal").ap()
    if dbg == 2:
        dbg_xs = nc.dram_tensor("dbg_xs", [2 * CAP, D], BF16, kind="ExternalOutput").ap()
        dbg_ys = nc.dram_tensor("dbg_ys", [2 * CAP, D], F32, kind="ExternalOutput").ap()
    if dbg in (1, 2):
        dbg_i = nc.dram_tensor("dbg_i", [128, NTOK // 128 * 2], I32, kind="ExternalOutput").ap()
        dbg_w = nc.dram_tensor("dbg_w", [128, NTOK // 128 * 2], F32, kind="ExternalOutput").ap()
        dbg_m = nc.dram_tensor("dbg_m", [128, 4 * D], F32, kind="ExternalOutput").ap()

    with ExitStack() as st:
        P = Prog(nc, st)
        sb = P.sbuf
        idn = sb("idn", [128, 128], F32)
        idnb = sb("idnb", [128, 128], BF16)
        tri = sb("tri", [128, 128], BF16)
        ones = sb("ones", [128, 128], BF16)
        offsB = sb("offsB", [128, NEXP], F32)
        rbB = sb("rbB", [128, NEXP], F32)
        rw = sb("rw", [128, 8, NEXP], F32)
        wo = sb("wo", [128, 8, D], BF16)
        g1B = sb("g1B", [128, D], F32)
        sh2B = sb("sh2B", [128, D], F32)
        sc2B = sb("sc2B", [128, D], F32)
        g2B = sb("g2B", [128, D], F32)
        lgB = [sb("lgB%d" % i, [128, D], F32) for i in range(2)]
        lbB = [sb("lbB%d" % i, [128, D], F32) for i in range(2)]
        cT = sb("cT", [128, 8], F32)
        cond = sb("cond", [128, 8], F32)
        condB = sb("condB", [128, 8, 128], F32)
        base = sb("base", [128, NEXP], F32)
        desti = sb("desti", [128, NT, 2], I32)
        wts = sb("wts", [128, NT, 2], F32)
        psA = P.psum("psA", [128, 1024], BF16)
        psY = P.psum("psY", [128, 1024], F32)
        psT = P.psum("psT", [128, 1024], F32)
        psU = P.psum("psU", [128, 1024], F32)
        psS = P.psum("psS", [128, 512], F32)

        dmaq = ["sp", "act"]
        P.dma("sp", lambda e: e.dma_start(out=idn.t[:], in_=idn_d), writes=[idn])
        P.dma("pool", lambda e: e.dma_start(out=idnb.t[:], in_=idn_d), writes=[idnb])
        P.dma("pool", lambda e: e.dma_start(out=tri.t[:], in_=tri_d), writes=[tri])
        P.dma("sp", lambda e: e.dma_start(out=offsB.t[:], in_=bcast_row(offs_d, NEXP)), writes=[offsB])
        P.dma("sp", lambda e: e.dma_start(out=rbB.t[:], in_=bcast_row(rb_d, NEXP)), writes=[rbB])
        P.dma("sp", lambda e: e.dma_start(out=rw.t[:], in_=rw_d.rearrange("(k p) n -> p k n", p=128)), writes=[rw])
        P.dma("pool", lambda e: e.dma_start(out=wo.t[:], in_=wo_d.rearrange("(k p) n -> p k n", p=128)), writes=[wo])
        for i in range(2):
            P.dma("sp", lambda e, i=i: e.dma_start(out=lgB[i].t[:], in_=bcast_row(lng_d[i:i + 1, :], D)), writes=[lgB[i]])
            P.dma("sp", lambda e, i=i: e.dma_start(out=lbB[i].t[:], in_=bcast_row(lnb_d[i:i + 1, :], D)), writes=[lbB[i]])
        P.dma("sp", lambda e: e.dma_start(out=cT.t[:], in_=cT_d), writes=[cT])
        P.op("pool", lambda e: e.memset(ones.t[:], 1.0), writes=[ones])
        P.op("pool", lambda e: e.memset(base.t[:], 0.0), writes=[base])
        P.op("act", lambda e: e.activation(cond.t[:], cT.t[:], AF.Silu), reads=[cT], writes=[cond])
        for k in range(8):
            P.op("dve", lambda e, k=k: e.tensor_copy(condB.t[:, k, :], cond.t[:, k:k + 1].to_broadcast([128, 128])),
                 reads=[cond], writes=[condB])

        P.push()
        awb = [sb("awb%d" % i, [128, 8, 512], F32) for i in range(2)]
        abb = [sb("abb%d" % i, [128, 512], F32) for i in range(2)]
        jobs = []
        for h in range(2):
            jobs.append((0, 2 * D + h * 512, g1B, h * 512, False))
        for h in range(2):
            jobs.append((1, 0 * D + h * 512, sh2B, h * 512, False))
        for h in range(2):
            jobs.append((1, 1 * D + h * 512, sc2B, h * 512, True))
        for h in range(2):
            jobs.append((1, 2 * D + h * 512, g2B, h * 512, False))
        psM = [Buf("psM0", psS.t), Buf("psM1", psU.t)]
        for j, (s, c0, dst, d0, plus1) in enumerate(jobs):
            wb = awb[j % 2]
            bb = abb[j % 2]
            pm = psM[j % 2]
            P.dma(dmaq[j % 2], lambda e, s=s, c0=c0, wb=wb: e.dma_start(
                out=wb.t[:], in_=adaw_d[s, :, c0:c0 + 512].rearrange("(k p) n -> p k n", p=128)), writes=[wb])
            P.dma("sp", lambda e, s=s, c0=c0, bb=bb: e.dma_start(
                out=bb.t[:], in_=bcast_row(adab_d[s:s + 1, c0:c0 + 512], 512)), writes=[bb])
            for k in range(8):
                P.op("pe", lambda e, k=k, wb=wb, pm=pm: e.matmul(pm.t[:, 0:512], condB.t[:, k, :], wb.t[:, k, :],
                                                                 start=(k == 0), stop=(k == 7)),
                     reads=[condB, wb], writes=[pm])
            P.op("dve", lambda e, pm=pm, bb=bb, dst=dst, d0=d0: e.tensor_tensor(
                dst.t[:, d0:d0 + 512], pm.t[:, 0:512], bb.t[:], ALU.add), reads=[pm, bb], writes=[dst])
            if plus1:
                P.op("dve", lambda e, dst=dst, d0=d0: e.tensor_scalar(
                    dst.t[:, d0:d0 + 512], dst.t[:, d0:d0 + 512], 1.0, None, ALU.add), reads=[dst], writes=[dst])

        P.pop()
        P.push()
        NB = 2
        xt = [sb("xt%d" % i, [128, D], F32) for i in range(NB)]
        ot = [sb("ot%d" % i, [128, D], F32) for i in range(NB)]
        ob = [sb("ob%d" % i, [128, D], BF16) for i in range(NB)]
        oT = [sb("oT%d" % i, [128, 8, 128], BF16) for i in range(NB)]
        t1 = [sb("t1%d" % i_, [128, D], F32) for i_ in range(2)]
        z = [sb("z%d" % i_, [128, D], F32) for i_ in range(2)]
        xn = [sb("xn%d" % i_, [128, D], F32) for i_ in range(2)]
        x1 = [sb("x1_%d" % i, [128, D], F32) for i in range(NB)]
        h2 = [sb("h2_%d" % i, [128, D], F32) for i in range(NB)]
        hb = [sb("hb%d" % i, [128, D], BF16) for i in range(NB)]
        h2T = [sb("h2T%d" % i, [128, 8, 128], F32) for i in range(NB)]
        stats = [sb("stats%d" % i_, [128, 2, 6], F32) for i_ in range(2)]
        mv = [sb("mv%d" % i_, [128, 2], F32) for i_ in range(2)]
        rstd = [sb("rstd%d" % i_, [128, 1], F32) for i_ in range(2)]
        sc = [sb("sc%d" % i_, [128, NEXP], F32) for i_ in range(2)]
        grp = [sb("grp%d" % i_, [128, NEXP], F32) for i_ in range(2)]
        m8 = [sb("m8%d" % i_, [128, 4, 8], F32) for i_ in range(2)]
        gs = [sb("gs%d" % i_, [128, 4], F32) for i_ in range(2)]
        gmax = [sb("gmax%d" % i_, [128, 1], F32) for i_ in range(2)]
        oh = [sb("oh%d" % i_, [128, 4], F32) for i_ in range(2)]
        tmp4 = [sb("tmp4%d" % i_, [128, 4], F32) for i_ in range(2)]
        thr = [sb("thr%d" % i_, [128, 1], F32) for i_ in range(2)]
        ge = [sb("ge%d" % i_, [128, NEXP], F32) for i_ in range(2)]
        sel = [sb("sel%d" % i_, [128, NEXP], F32) for i_ in range(2)]
        selb = [sb("selb%d" % i_, [128, NEXP], BF16) for i_ in range(2)]
        ws = [sb("ws%d" % i_, [128, NEXP], F32) for i_ in range(2)]
        wsum = [sb("wsum%d" % i_, [128, 1], F32) for i_ in range(2)]
        wt = [sb("wt%d" % i_, [128, NEXP], F32) for i_ in range(2)]
        dall = [sb("dall%d" % i_, [128, NEXP], F32) for i_ in range(2)]
        dhi = [sb("dhi%d" % i_, [128, 1], F32) for i_ in range(2)]
        dsum = [sb("dsum%d" % i_, [128, 1], F32) for i_ in range(2)]
        dpair = [sb("dpair%d" % i_, [128, 2], F32) for i_ in range(2)]
        eq = [sb("eq%d" % i_, [128, NEXP], F32) for i_ in range(2)]
        XSb = Buf("XS")
        X1b = Buf("X1")
        psLog = Buf("psLog", psS.t)

        def chain(t):
            b = t % NB
            yield
            r0 = t * 128
            yield
            P.dma("sp", lambda e, b=b, r0=r0: e.dma_start(out=xt[b].t[:], in_=x_d[r0:r0 + 128, :]), writes=[xt[b]])
            yield
            P.dma("act", lambda e, b=b, r0=r0: e.dma_start(out=ot[b].t[:], in_=o_d[r0:r0 + 128, :]), writes=[ot[b]])
            yield
            P.op("pool", lambda e, b=b: e.tensor_copy(ob[b].t[:], ot[b].t[:]), reads=[ot[b]], writes=[ob[b]])
            yield
            for k in range(8):
                P.op("pe", lambda e, b=b, k=k: e.transpose(psA.t[:, k * 128:(k + 1) * 128], ob[b].t[:, k * 128:(k + 1) * 128], idnb.t[:]),
                     reads=[ob[b], idnb], writes=[psA])
            P.op("act", lambda e, b=b: e.copy(oT[b].t[:].rearrange("p k m -> p (k m)"), psA.t[:]), reads=[psA], writes=[oT[b]])
            yield
            for nh in range(2):
                for k in range(8):
                    P.op("pe", lambda e, b=b, k=k, nh=nh: e.matmul(psY.t[:, nh * 512:(nh + 1) * 512], oT[b].t[:, k, :],
                                                                   wo.t[:, k, nh * 512:(nh + 1) * 512], start=(k == 0), stop=(k == 7)),
                         reads=[oT[b], wo], writes=[psY])
            for nh in range(2):
                sl = slice(nh * 512, (nh + 1) * 512)
                P.op("dve", lambda e, sl=sl: e.tensor_tensor(t1[b].t[:, sl], psY.t[:, sl], g1B.t[:, sl], ALU.mult),
                     reads=[psY, g1B], writes=[t1[b]])
            P.op("dve", lambda e, b=b: e.scalar_tensor_tensor(z[b].t[:], xt[b].t[:], ALPHA, t1[b].t[:], ALU.mult, ALU.add),
                 reads=[xt[b], t1[b]], writes=[z[b]])
            emit_layernorm(P, z[b], stats[b], mv[b], rstd[b], xn[b])
            yield
            P.op("pool", lambda e, b=b: e.tensor_tensor(x1[b].t[:], xn[b].t[:], lgB[0].t[:], ALU.mult), reads=[xn[b], lgB[0]], writes=[x1[b]])
            yield
            P.op("pool", lambda e, b=b: e.tensor_tensor(x1[b].t[:], x1[b].t[:], lbB[0].t[:], ALU.add), reads=[x1[b], lbB[0]], writes=[x1[b]])
            yield
            P.dma("sp", lambda e, b=b, r0=r0: e.dma_start(out=X1[r0:r0 + 128, :], in_=x1[b].t[:]), reads=[x1[b]], writes=[X1b])
            yield
            P.op("pool", lambda e, b=b: e.tensor_tensor(h2[b].t[:], x1[b].t[:], sc2B.t[:], ALU.mult), reads=[x1[b], sc2B], writes=[h2[b]])
            yield
            P.op("pool", lambda e, b=b: e.tensor_tensor(h2[b].t[:], h2[b].t[:], sh2B.t[:], ALU.add), reads=[h2[b], sh2B], writes=[h2[b]])
            yield
            P.op("pool", lambda e, b=b: e.tensor_copy(hb[b].t[:], h2[b].t[:]), reads=[h2[b]], writes=[hb[b]])
            yield
            for k in range(8):
                P.op("pe", lambda e, b=b, k=k: e.transpose(psT.t[:, k * 128:(k + 1) * 128], h2[b].t[:, k * 128:(k + 1) * 128], idn.t[:]),
                     reads=[h2[b], idn], writes=[psT])
            P.op("act", lambda e, b=b: e.copy(h2T[b].t[:].rearrange("p k m -> p (k m)"), psT.t[:]), reads=[psT], writes=[h2T[b]])
            yield
            for k in range(8):
                P.op("pe", lambda e, b=b, k=k: e.matmul(psS.t[:, 0:NEXP], h2T[b].t[:, k, :], rw.t[:, k, :], start=(k == 0), stop=(k == 7)),
                     reads=[h2T[b], rw], writes=[psLog])
            P.op("act", lambda e: e.activation(sc[b].t[:], psS.t[:, 0:NEXP], AF.Sigmoid), reads=[psLog], writes=[sc[b]])
            yield
            P.op("dve", lambda e: e.tensor_tensor(grp[b].t[:], sc[b].t[:], rbB.t[:], ALU.add), reads=[sc[b], rbB], writes=[grp[b]])
            yield
            for g in range(4):
                P.op("dve", lambda e, g=g: e.max(out=m8[b].t[:, g, :], in_=grp[b].t[:, g * 8:(g + 1) * 8]), reads=[grp[b]], writes=[m8[b]])
            P.op("dve", lambda e: e.tensor_tensor(gs[b].t[:], m8[b].t[:, :, 0], m8[b].t[:, :, 1], ALU.add), reads=[m8[b]], writes=[gs[b]])
            yield
            P.op("dve", lambda e: e.reduce_max(gmax[b].t[:], gs[b].t[:], AX.X), reads=[gs[b]], writes=[gmax[b]])
            yield
            P.op("dve", lambda e: e.tensor_scalar(oh[b].t[:], gs[b].t[:], gmax[b].t[:, 0:1], None, ALU.is_equal), reads=[gs[b], gmax[b]], writes=[oh[b]])
            yield
            P.op("dve", lambda e: e.tensor_tensor(tmp4[b].t[:], oh[b].t[:], m8[b].t[:, :, 1], ALU.mult), reads=[oh[b], m8[b]], writes=[tmp4[b]])
            yield
            P.op("dve", lambda e: e.reduce_sum(thr[b].t[:], tmp4[b].t[:], AX.X), reads=[tmp4[b]], writes=[thr[b]])
            yield
            P.op("dve", lambda e: e.tensor_scalar(ge[b].t[:], grp[b].t[:], thr[b].t[:, 0:1], None, ALU.is_ge), reads=[grp[b], thr[b]], writes=[ge[b]])
            yield
            P.op("dve", lambda e: e.tensor_tensor(sel[b].t[:].rearrange("p (g j) -> p g j", j=8), ge[b].t[:].rearrange("p (g j) -> p g j", j=8),
                                                  oh[b].t[:].unsqueeze(2).to_broadcast([128, 4, 8]), ALU.mult), reads=[ge[b], oh[b]], writes=[sel[b]])
            P.op("dve", lambda e: e.tensor_tensor(ws[b].t[:], sc[b].t[:], sel[b].t[:], ALU.mult), reads=[sc[b], sel[b]], writes=[ws[b]])
            yield
            P.op("dve", lambda e: e.reduce_sum(wsum[b].t[:], ws[b].t[:], AX.X), reads=[ws[b]], writes=[wsum[b]])
            yield
            P.op("dve", lambda e: e.reciprocal(wsum[b].t[:], wsum[b].t[:]), reads=[wsum[b]], writes=[wsum[b]])
            yield
            P.op("dve", lambda e: e.tensor_scalar(wt[b].t[:], ws[b].t[:], wsum[b].t[:, 0:1], None, ALU.mult), reads=[ws[b], wsum[b]], writes=[wt[b]])
            yield
            P.op("dve", lambda e: e.tensor_copy(selb[b].t[:], sel[b].t[:]), reads=[sel[b]], writes=[selb[b]])
            yield
            P.op("pe", lambda e: e.matmul(psS.t[:, 64:64 + NEXP], tri.t[:], selb[b].t[:], start=True, stop=True), reads=[tri, selb[b]], writes=[psLog])
            yield
            P.op("pe", lambda e: e.matmul(psS.t[:, 128:128 + NEXP], ones.t[:], selb[b].t[:], start=True, stop=True), reads=[ones, selb[b]], writes=[psLog])
            yield
            P.op("dve", lambda e: e.tensor_tensor(dall[b].t[:], psS.t[:, 64:64 + NEXP], base.t[:], ALU.add), reads=[psLog, base], writes=[dall[b]])
            yield
            P.op("dve", lambda e: e.tensor_tensor(dall[b].t[:], dall[b].t[:], offsB.t[:], ALU.add), reads=[dall[b], offsB], writes=[dall[b]])
            yield
            P.op("dve", lambda e: e.tensor_tensor(dall[b].t[:], dall[b].t[:], sel[b].t[:], ALU.mult), reads=[dall[b], sel[b]], writes=[dall[b]])
            yield
            P.op("dve", lambda e: e.tensor_tensor(base.t[:], base.t[:], psS.t[:, 128:128 + NEXP], ALU.add), reads=[psLog, base], writes=[base])
            yield
            P.op("dve", lambda e: e.reduce_max(dhi[b].t[:], dall[b].t[:], AX.X), reads=[dall[b]], writes=[dhi[b]])
            yield
            P.op("dve", lambda e: e.reduce_sum(dsum[b].t[:], dall[b].t[:], AX.X), reads=[dall[b]], writes=[dsum[b]])
            yield
            P.op("dve", lambda e: e.tensor_scalar(dpair[b].t[:, 0:1], dhi[b].t[:], -1.0, None, ALU.add), reads=[dhi[b]], writes=[dpair[b]])
            yield
            P.op("dve", lambda e: e.scalar_tensor_tensor(dpair[b].t[:, 1:2], dsum[b].t[:], -1.0, dhi[b].t[:], ALU.add, ALU.subtract),
                 reads=[dsum[b], dhi[b]], writes=[dpair[b]])
            P.op("dve", lambda e, t=t: e.tensor_copy(desti.t[:, t, :], dpair[b].t[:]), reads=[dpair[b]], writes=[desti])
            yield
            P.op("dve", lambda e: e.tensor_scalar(eq[b].t[:], dall[b].t[:], dhi[b].t[:, 0:1], None, ALU.is_equal), reads=[dall[b], dhi[b]], writes=[eq[b]])
            yield
            P.op("dve", lambda e: e.tensor_tensor(eq[b].t[:], eq[b].t[:], wt[b].t[:], ALU.mult), reads=[eq[b], wt[b]], writes=[eq[b]])
            yield
            P.op("dve", lambda e, t=t: e.reduce_sum(wts.t[:, t, 0:1], eq[b].t[:], AX.X), reads=[eq[b]], writes=[wts])
            yield
            P.op("dve", lambda e, t=t: e.tensor_scalar(wts.t[:, t, 1:2], wts.t[:, t, 0:1], -1.0, 1.0, ALU.mult, ALU.add), reads=[wts], writes=[wts])
            yield
            for j in range(2):
                P.dma("pool", lambda e, b=b, t=t, j=j: e.indirect_dma_start(
                    out=XS[:, :], out_offset=bass.IndirectOffsetOnAxis(ap=desti.t[:, t, j:j + 1], axis=0),
                    in_=hb[b].t[:, :], in_offset=None), reads=[hb[b], desti], writes=[XSb])


        LAG = 10
        for t0_ in range(0, NT, 2):
            ga, gb = chain(t0_), chain(t0_ + 1)
            la = lb = True
            na = 0
            while la or lb:
                if la:
                    try:
                        next(ga)
                        na += 1
                    except StopIteration:
                        la = False
                if lb and (na >= LAG or not la):
                    try:
                        next(gb)
                    except StopIteration:
                        lb = False
        if dbg == 1:
            Db = Buf("dbg")
            P.dma("sp", lambda e: e.dma_start(out=dbg_i, in_=desti.t[:].rearrange("p t j -> p (t j)")), reads=[desti], writes=[Db])
            P.dma("sp", lambda e: e.dma_start(out=dbg_w, in_=wts.t[:].rearrange("p t j -> p (t j)")), reads=[wts], writes=[Db])
            for i_, tl in enumerate([g1B, sh2B, sc2B, g2B]):
                P.dma("sp", lambda e, i_=i_, tl=tl: e.dma_start(out=dbg_m[:, i_ * D:(i_ + 1) * D], in_=tl.t[:]), reads=[tl], writes=[Db])
            P.wait_all("sp", [Db, X1b])
            P.pop()
            P.emit()
            return nc
        P.pop()
        P.push()
        wgs = [sb("wgs%d" % i, [128, 8, 512], BF16) for i in range(2)]
        wus = [sb("wus%d" % i, [128, 8, 512], BF16) for i in range(2)]
        wds = [sb("wds%d" % i, [128, 4, D], BF16) for i in range(2)]
        xs_tok = [sb("xs_tok%d" % i, [128, D], BF16) for i in range(2)]
        xsT = [sb("xsT%d" % i, [128, 8, CAP], BF16) for i in range(2)]
        sg = [sb("sg%d" % i, [128, 512], F32) for i in range(2)]
        hT = [sb("hT%d" % i, [128, 4, CAP], BF16) for i in range(2)]
        ysb = [sb("ysb%d" % i, [128, D], F32) for i in range(2)]
        psG = [Buf("psG0", psT.t), Buf("psG1", psU.t)]
        YSb = Buf("YS")
        RB = CAP // 128
        nx = 0
        ny = 0
        for ex in range(NE_):
            wb = ex % 2
            P.dma("pool", lambda e, ex=ex, wb=wb: e.dma_start(out=wgs[wb].t[:], in_=wg_d[ex].rearrange("(k p) n -> p k n", p=128)), writes=[wgs[wb]])
            P.dma("pool", lambda e, ex=ex, wb=wb: e.dma_start(out=wus[wb].t[:], in_=wu_d[ex].rearrange("(k p) n -> p k n", p=128)), writes=[wus[wb]])
            P.dma("pool", lambda e, ex=ex, wb=wb: e.dma_start(out=wds[wb].t[:], in_=wd_d[ex].rearrange("(k p) n -> p k n", p=128)), writes=[wds[wb]])
            xT = xsT[wb]
            for rb in range(RB):
                xb = xs_tok[nx % 2]
                nx += 1
                r0 = ex * CAP + rb * 128
                P.dma("sp", lambda e, xb=xb, r0=r0: e.dma_start(out=xb.t[:], in_=XS[r0:r0 + 128, :]), reads=[XSb], writes=[xb])
                for k in range(8):
                    P.op("pe", lambda e, xb=xb, k=k: e.transpose(psA.t[:, k * 128:(k + 1) * 128], xb.t[:, k * 128:(k + 1) * 128], idnb.t[:]),
                         reads=[xb, idnb], writes=[psA])
                P.op("act", lambda e, xT=xT, rb=rb: e.copy(xT.t[:, :, rb * 128:(rb + 1) * 128], psA.t[:].rearrange("p (k m) -> p k m", m=128)),
                     reads=[psA], writes=[xT])
            hh = hT[wb]
            for fc in range(4):
              for hf in range(CAP // 512):
                pg = psG[(fc * (CAP // 512) + hf) % 2]
                s0 = hf * 512
                for k in range(8):
                    P.op("pe", lambda e, k=k, fc=fc, pg=pg, wb=wb, xT=xT, s0=s0: e.matmul(
                        pg.t[:, 0:512], wgs[wb].t[:, k, fc * 128:(fc + 1) * 128], xT.t[:, k, s0:s0 + 512], start=(k == 0), stop=(k == 7)),
                        reads=[wgs[wb], xT], writes=[pg])
                for k in range(8):
                    P.op("pe", lambda e, k=k, fc=fc, pg=pg, wb=wb, xT=xT, s0=s0: e.matmul(
                        pg.t[:, 512:1024], wus[wb].t[:, k, fc * 128:(fc + 1) * 128], xT.t[:, k, s0:s0 + 512], start=(k == 0), stop=(k == 7)),
                        reads=[wus[wb], xT], writes=[pg])
                s_ = sg[(fc * (CAP // 512) + hf) % 2]
                P.op("act", lambda e, pg=pg, s_=s_: e.activation(s_.t[:], pg.t[:, 0:512], AF.Silu), reads=[pg], writes=[s_])
                P.op("dve", lambda e, pg=pg, s_=s_, hh=hh, fc=fc, s0=s0: e.tensor_tensor(hh.t[:, fc, s0:s0 + 512], s_.t[:], pg.t[:, 512:1024], ALU.mult),
                     reads=[pg, s_], writes=[hh])
            for rb in range(RB):
                yb = ysb[ny % 2]
                ny += 1
                for nh in range(2):
                    for fc in range(4):
                        P.op("pe", lambda e, rb=rb, nh=nh, fc=fc, hh=hh, wb=wb: e.matmul(
                            psY.t[:, nh * 512:(nh + 1) * 512], hh.t[:, fc, rb * 128:(rb + 1) * 128],
                            wds[wb].t[:, fc, nh * 512:(nh + 1) * 512], start=(fc == 0), stop=(fc == 3)),
                            reads=[hh, wds[wb]], writes=[psY])
                P.op("act", lambda e, yb=yb: e.copy(yb.t[:], psY.t[:]), reads=[psY], writes=[yb])
                r0 = ex * CAP + rb * 128
                P.dma("sp", lambda e, yb=yb, r0=r0: e.dma_start(out=YS[r0:r0 + 128, :], in_=yb.t[:]), reads=[yb], writes=[YSb])

        if dbg == 2:
            Db = Buf("dbg")
            P.dma("sp", lambda e: e.dma_start(out=dbg_xs, in_=XS[0:2 * CAP, :]), reads=[XSb], writes=[Db])
            P.dma("sp", lambda e: e.dma_start(out=dbg_ys, in_=YS[0:2 * CAP, :]), reads=[YSb], writes=[Db])
            P.dma("sp", lambda e: e.dma_start(out=dbg_i, in_=desti.t[:].rearrange("p t j -> p (t j)")), reads=[desti], writes=[Db])
            P.wait_all("sp", [Db])
            P.pop()
            P.emit()
            return nc
        P.pop()
        P.push()
        cxt = [sb("cxt%d" % i, [128, D], F32) for i in range(2)]
        ct1 = sb("ct1", [128, D], F32)
        cz = sb("cz", [128, D], F32)
        cxn = sb("cxn", [128, D], F32)
        cstats = sb("cstats", [128, 2, 6], F32)
        cmv = sb("cmv", [128, 2], F32)
        crstd = sb("crstd", [128, 1], F32)
        yh = [sb("yh%d" % i, [128, D], F32) for i in range(2)]
        yl = [sb("yl%d" % i, [128, D], F32) for i in range(2)]
        outt = [sb("outt%d" % i, [128, D], F32) for i in range(2)]
        Yb = Buf("y")
        import os
        COMB = os.environ.get("COMB", "full")
        for t in range(NT):
            b = t % 2
            r0 = t * 128
            if COMB == "a":
                P.dma("sp", lambda e, b=b, r0=r0: e.dma_start(out=cxt[b].t[:], in_=X1[r0:r0 + 128, :]), reads=[X1b], writes=[cxt[b]])
                P.dma("sp", lambda e, b=b, r0=r0: e.dma_start(out=y_d[r0:r0 + 128, :], in_=cxt[b].t[:]), reads=[cxt[b]], writes=[Yb])
                continue
            if COMB == "none":
                continue
            P.dma("pool", lambda e, b=b, t=t: e.indirect_dma_start(
                out=yh[b].t[:, :], out_offset=None, in_=YS[:, :],
                in_offset=bass.IndirectOffsetOnAxis(ap=desti.t[:, t, 0:1], axis=0)), reads=[YSb, desti], writes=[yh[b]])
            P.dma("pool", lambda e, b=b, t=t: e.indirect_dma_start(
                out=yl[b].t[:, :], out_offset=None, in_=YS[:, :],
                in_offset=bass.IndirectOffsetOnAxis(ap=desti.t[:, t, 1:2], axis=0)), reads=[YSb, desti], writes=[yl[b]])
            P.dma("sp", lambda e, b=b, r0=r0: e.dma_start(out=cxt[b].t[:], in_=X1[r0:r0 + 128, :]), reads=[X1b], writes=[cxt[b]])
            if dbg == 4 and t == 0:
                Dbb = Buf("dbb")
                P.dma("sp", lambda e: e.dma_start(out=dbg_a[:, 0:D], in_=yh[0].t[:]), reads=[yh[0]], writes=[Dbb])
                P.dma("sp", lambda e: e.dma_start(out=dbg_a[:, D:2 * D], in_=yl[0].t[:]), reads=[yl[0]], writes=[Dbb])
                P.dma("sp", lambda e: e.dma_start(out=dbg_a[:, 2 * D:3 * D], in_=cxt[0].t[:]), reads=[cxt[0]], writes=[Dbb])
            P.op("dve", lambda e, b=b, t=t: e.tensor_scalar(yh[b].t[:], yh[b].t[:], wts.t[:, t, 0:1], None, ALU.mult), reads=[yh[b], wts], writes=[yh[b]])
            P.op("dve", lambda e, b=b, t=t: e.scalar_tensor_tensor(yh[b].t[:], yl[b].t[:], wts.t[:, t, 1:2], yh[b].t[:], ALU.mult, ALU.add),
                 reads=[yl[b], yh[b], wts], writes=[yh[b]])
            P.op("pool", lambda e, b=b: e.tensor_tensor(ct1.t[:], yh[b].t[:], g2B.t[:], ALU.mult), reads=[yh[b], g2B], writes=[ct1])
            P.op("dve", lambda e, b=b: e.scalar_tensor_tensor(cz.t[:], cxt[b].t[:], ALPHA, ct1.t[:], ALU.mult, ALU.add), reads=[cxt[b], ct1], writes=[cz])
            if dbg == 4 and t == 0:
                P.dma("sp", lambda e: e.dma_start(out=dbg_a[:, 3 * D:4 * D], in_=cz.t[:]), reads=[cz], writes=[Dbb])
            emit_layernorm(P, cz, cstats, cmv, crstd, cxn)
            P.op("pool", lambda e, b=b: e.tensor_tensor(outt[b].t[:], cxn.t[:], lgB[1].t[:], ALU.mult), reads=[cxn, lgB[1]], writes=[outt[b]])
            P.op("pool", lambda e, b=b: e.tensor_tensor(outt[b].t[:], outt[b].t[:], lbB[1].t[:], ALU.add), reads=[outt[b], lbB[1]], writes=[outt[b]])
            P.dma("sp", lambda e, b=b, r0=r0: e.dma_start(out=y_d[r0:r0 + 128, :], in_=outt[b].t[:]), reads=[outt[b]], writes=[Yb])
        if dbg in (3, 4):
            P.dma("sp", lambda e: e.dma_start(out=dbg_i, in_=desti.t[:].rearrange("p t j -> p (t j)")), reads=[desti], writes=[Yb])
        P.wait_all("sp", [Yb])
        P.pop()
        P.emit()
    return nc


_CACHE = {}


def _consts():
    idn = np.eye(128, dtype=np.float32)
    tri = np.triu(np.ones((128, 128), np.float32), 1)
    offs = (np.arange(NEXP, dtype=np.float32) * CAP + 1.0)[None, :]
    return idn, tri, offs


def run_ffn(x, o, c, ada_w_l, ada_b_l, ln_g_l, ln_b_l, w_o, router_w, router_b, wg, wu, wd):
    if "ffn" not in _CACHE:
        _CACHE["ffn"] = build_ffn()
    nc = _CACHE["ffn"]
    idn, tri, offs = _consts()
    B, S, _ = x.shape
    xf = x.reshape(B * S, D)
    of = o.reshape(B * S, D)
    in_maps = []
    for core in range(8):
        r0 = core * NTOK
        b = r0 // S
        in_maps.append({
            "x": np.ascontiguousarray(xf[r0:r0 + NTOK]), "o": np.ascontiguousarray(of[r0:r0 + NTOK]),
            "cT": np.ascontiguousarray(c[b].reshape(8, 128).T),
            "adaw": ada_w_l, "adab": ada_b_l, "lng": ln_g_l, "lnb": ln_b_l, "wo": w_o,
            "rw": router_w, "rb": router_b.reshape(1, NEXP), "wg": wg, "wu": wu, "wd": wd,
            "idn": idn, "tri": tri, "offs": offs,
        })
    res = run_bass_kernel_spmd(nc, in_maps, core_ids=list(range(8)))
    return np.concatenate([r["y"] for r in res.results], axis=0).reshape(B, S, D)


S_LEN = 16384
SPAN = 2048
TWO_PI = 6.283185307179586
PI = 3.141592653589793


def rope_inv_table():
    inv = 500000.0 ** (-np.arange(8, dtype=np.float64) * (2.0 / 16))
    t = np.zeros((128, 1), np.float32)
    for p in range(128):
        f = p % 64
        if f < 16:
            t[p, 0] = np.float32(inv[f % 8])
    return t


def emit_mod_cols(P, nc, adaw_d, adabT_d, cT_d, ncols, shp, psum_buf):
    sb = P.sbuf
    cT = sb("cT", [128, 8], F32)
    cond = sb("cond", [128, 8], F32)
    modp = sb("modp", [128, ncols // 128], F32)
    abT = sb("abT", [128, ncols // 128], F32)
    P.dma("sp", lambda e: e.dma_start(out=cT.t[:], in_=cT_d), writes=[cT])
    P.dma("sp", lambda e: e.dma_start(out=abT.t[:], in_=adabT_d), writes=[abT])
    P.op("act", lambda e: e.activation(cond.t[:], cT.t[:], AF.Silu), reads=[cT], writes=[cond])
    P.push()
    awb = [sb("awb%d" % i, [128, 8, 512], F32) for i in range(2)]
    for blk in range(ncols // 512):
        wb = awb[blk % 2]
        P.dma(["sp", "act"][blk % 2], lambda e, blk=blk, wb=wb: e.dma_start(
            out=wb.t[:], in_=adaw_d[:, blk * 512:(blk + 1) * 512].rearrange("(k p) n -> p k n", p=128)), writes=[wb])
        for j in range(4):
            for kk in range(8):
                P.op("pe", lambda e, j=j, kk=kk, wb=wb, blk=blk: e.matmul(
                    psum_buf.t[:, blk * 4 + j:blk * 4 + j + 1], wb.t[:, kk, j * 128:(j + 1) * 128], cond.t[:, kk:kk + 1],
                    start=(kk == 0), stop=(kk == 7)), reads=[wb, cond], writes=[psum_buf])
    P.op("dve", lambda e: e.tensor_tensor(modp.t[:], psum_buf.t[:, 0:ncols // 128], abT.t[:], ALU.add),
         reads=[psum_buf, abT], writes=[modp])
    P.pop()
    return modp


def emit_rope_tables(P, posB, posI, invp, negpi, Ct, St, tmp, pos_ap, n):
    C1 = 6.28125
    C2 = TWO_PI - C1
    P.dma("sp", lambda e: e.dma_start(out=posI.t[:, 0:n], in_=pos_ap.to_broadcast([128, n])), writes=[posI])
    P.op("dve", lambda e: e.tensor_copy(posB.t[:, 0:n], posI.t[:, 0:n]), reads=[posI], writes=[posB])
    for off, dst in ((0.0, St), (0.5 * PI, Ct)):
        P.op("dve", lambda e, off=off: e.tensor_scalar(tmp.t[:, 0:n], posB.t[:, 0:n], invp.t[:, 0:1], off, ALU.mult, ALU.add),
             reads=[posB, invp], writes=[tmp])
        P.op("dve", lambda e: e.tensor_scalar(dst.t[:, 0:n], tmp.t[:, 0:n], 1.0 / TWO_PI, None, ALU.mult), reads=[tmp], writes=[dst])
        P.op("dve", lambda e: e.tensor_copy(posI.t[:, 0:n], dst.t[:, 0:n]), reads=[dst], writes=[posI])
        P.op("dve", lambda e, dst=dst: e.tensor_copy(dst.t[:, 0:n], posI.t[:, 0:n]), reads=[posI], writes=[dst])
        P.op("dve", lambda e, dst=dst: e.scalar_tensor_tensor(tmp.t[:, 0:n], dst.t[:, 0:n], -C1, tmp.t[:, 0:n], ALU.mult, ALU.add),
             reads=[dst, tmp], writes=[tmp])
        P.op("dve", lambda e, dst=dst: e.scalar_tensor_tensor(tmp.t[:, 0:n], dst.t[:, 0:n], -C2, tmp.t[:, 0:n], ALU.mult, ALU.add),
             reads=[dst, tmp], writes=[tmp])
        P.op("dve", lambda e, dst=dst: e.tensor_scalar(dst.t[:, 0:n], tmp.t[:, 0:n], PI, -TWO_PI, ALU.is_gt, ALU.mult), reads=[tmp], writes=[dst])
        P.op("dve", lambda e, dst=dst: e.tensor_tensor(tmp.t[:, 0:n], tmp.t[:, 0:n], dst.t[:, 0:n], ALU.add), reads=[tmp, dst], writes=[tmp])
        P.op("dve", lambda e, dst=dst: e.tensor_scalar(dst.t[:, 0:n], tmp.t[:, 0:n], -PI, TWO_PI, ALU.is_lt, ALU.mult), reads=[tmp], writes=[dst])
        P.op("dve", lambda e, dst=dst: e.tensor_tensor(tmp.t[:, 0:n], tmp.t[:, 0:n], dst.t[:, 0:n], ALU.add), reads=[tmp, dst], writes=[tmp])
        P.op("dve", lambda e: e.tensor_scalar(tmp.t[:, 0:n], tmp.t[:, 0:n], PI, -PI, ALU.min, ALU.max), reads=[tmp], writes=[tmp])
        P.op("act", lambda e, dst=dst: e.activation(dst.t[:, 0:n], tmp.t[:, 0:n], AF.Sin), reads=[tmp], writes=[dst])


def emit_rot_weights(P, w, wr, nheads):
    P.op("pool", lambda e: e.memset(wr.t[:], 0.0), writes=[wr])
    for k in range(8):
        wv = w.t[:, k, :].rearrange("p (h e) -> p h e", e=64)
        rv = wr.t[:, k, :].rearrange("p (h e) -> p h e", e=64)
        P.op("dve", lambda e, wv=wv, rv=rv: e.tensor_scalar(rv[:, :, 0:8], wv[:, :, 8:16], -1.0, None, ALU.mult), reads=[w], writes=[wr])
        P.op("dve", lambda e, wv=wv, rv=rv: e.tensor_copy(rv[:, :, 8:16], wv[:, :, 0:8]), reads=[w], writes=[wr])


def emit_hmodT_tile(P, x_d, tok0, xt, psX, idn, modp, hT, col0, sc_off, q, width=1024):
    P.dma(q, lambda e: e.dma_start(out=xt.t[:], in_=x_d[tok0:tok0 + 128, :]), writes=[xt])
    nb_ = width // 128
    for k0 in range(0, 8, nb_):
        for kk in range(nb_):
            k = k0 + kk
            P.op("pe", lambda e, k=k, kk=kk: e.transpose(psX.t[:, kk * 128:(kk + 1) * 128], xt.t[:, k * 128:(k + 1) * 128], idn.t[:]),
                 reads=[xt, idn], writes=[psX])
        for kk in range(nb_):
            k = k0 + kk
            P.op("act", lambda e, k=k, kk=kk: e.activation(hT.t[:, k, col0:col0 + 128], psX.t[:, kk * 128:(kk + 1) * 128], AF.Identity,
                                                         bias=modp.t[:, k:k + 1], scale=modp.t[:, sc_off + k:sc_off + k + 1]),
                 reads=[psX, modp], writes=[hT])


def emit_proj_rope(P, hT, t0, n, w, wr, wc0, ps_a, ps_b, Ct, St, tcol0, tmpa, tmpb, out_ap_fn, out_buf):
    for k in range(8):
        P.op("pe", lambda e, k=k: e.matmul(ps_a.t[:, 0:n], w.t[:, k, wc0:wc0 + 128], hT.t[:, k, t0:t0 + n], start=(k == 0), stop=(k == 7)),
             reads=[w, hT], writes=[ps_a])
    for k in range(8):
        P.op("pe", lambda e, k=k: e.matmul(ps_b.t[:, 0:n], wr.t[:, k, wc0:wc0 + 128], hT.t[:, k, t0:t0 + n], start=(k == 0), stop=(k == 7)),
             reads=[wr, hT], writes=[ps_b])
    P.op("dve", lambda e: e.tensor_tensor(tmpa.t[:, 0:n], ps_a.t[:, 0:n], Ct.t[:, tcol0:tcol0 + n], ALU.mult), reads=[ps_a, Ct], writes=[tmpa])
    P.op("dve", lambda e: e.tensor_tensor(tmpb.t[:, 0:n], ps_b.t[:, 0:n], St.t[:, tcol0:tcol0 + n], ALU.mult), reads=[ps_b, St], writes=[tmpb])
    P.op("pool", lambda e: e.tensor_tensor(out_ap_fn(), tmpa.t[:, 0:n], tmpb.t[:, 0:n], ALU.add), reads=[tmpa, tmpb], writes=[out_buf])


def tri_masks():
    p = np.arange(128)[:, None]
    f = np.arange(128)[None, :]
    m0 = np.where(f <= p, 0.0, NEG).astype(np.float32)
    m1 = np.where(f >= p, 0.0, NEG).astype(np.float32)
    return np.stack([np.tile(m0, (1, 4)), np.tile(m1, (1, 4))], axis=1)


DIL = (1, 4, 16)


def build_dil():
    nc = bass.Bass("TRN2", target_bir_lowering=False)
    dt_in = lambda name, shape, dt=F32: nc.dram_tensor(name, list(shape), dt, kind="ExternalInput").ap()
    x_d = dt_in("x", [S_LEN, D])
    cT_d = dt_in("cT", [128, 8])
    adaw_d = dt_in("adaw", [D, 2 * D])
    adabT_d = dt_in("adabT", [128, 16])
    wq_d = dt_in("wq", [D, 256])
    wk_d = dt_in("wk", [3, D, 64])
    wv_d = dt_in("wv", [3, D, 64])
    pos_d = dt_in("pos", [1, S_LEN], I32)
    inv_d = dt_in("inv", [128, 1])
    idn_d = dt_in("idn", [128, 128])
    msk_d = dt_in("msk", [128, 2, 512])
    o_d = nc.dram_tensor("o", [S_LEN, 256], F32, kind="ExternalOutput").ap()
    OP = [nc.dram_tensor("OP%d" % p, [S_LEN, 260], F32).ap() for p in range(3)]

    with ExitStack() as st:
        P = Prog(nc, st)
        sb = P.sbuf
        idn = sb("idn", [128, 128], F32)
        idnb = sb("idnb", [128, 128], BF16)
        msk = sb("msk", [128, 2, 512], BF16)
        invp = sb("invp", [128, 1], F32)
        negpi = sb("negpi", [128, 1], F32)
        wq = sb("wq", [128, 8, 256], BF16)
        wqr = sb("wqr", [128, 8, 256], BF16)
        wk = [sb("wk%d" % p, [128, 8, 128], BF16) for p in range(3)]
        wkr = [sb("wkr%d" % p, [128, 8, 128], BF16) for p in range(3)]
        wv = [sb("wv%d" % p, [128, 8, 64], BF16) for p in range(3)]
        ps = [P.psum("pb%d" % i, [128, 512], F32) for i in range(6)]
        psX = P.psum("psX", [128, 1024], F32)

        P.dma("sp", lambda e: e.dma_start(out=idn.t[:], in_=idn_d), writes=[idn])
        P.dma("pool", lambda e: e.dma_start(out=idnb.t[:], in_=idn_d), writes=[idnb])
        P.dma("pool", lambda e: e.dma_start(out=msk.t[:], in_=msk_d), writes=[msk])
        P.dma("sp", lambda e: e.dma_start(out=invp.t[:], in_=inv_d), writes=[invp])
        P.op("pool", lambda e: e.memset(negpi.t[:], -PI), writes=[negpi])
        P.dma("pool", lambda e: e.dma_start(out=wq.t[:], in_=wq_d.rearrange("(k p) n -> p k n", p=128)), writes=[wq])
        for p in range(3):
            for h in range(2):
                P.dma("pool", lambda e, p=p, h=h: e.dma_start(out=wk[p].t[:, :, h * 64:(h + 1) * 64],
                                                              in_=wk_d[p].rearrange("(k p) n -> p k n", p=128)), writes=[wk[p]])
            P.dma("pool", lambda e, p=p: e.dma_start(out=wv[p].t[:], in_=wv_d[p].rearrange("(k p) n -> p k n", p=128)), writes=[wv[p]])
        emit_rot_weights(P, wq, wqr, 4)
        for p in range(3):
            emit_rot_weights(P, wk[p], wkr[p], 2)
        modp = emit_mod_cols(P, nc, adaw_d, adabT_d, cT_d, 2 * D, None, ps[0])
        P.op("dve", lambda e: e.tensor_scalar(modp.t[:, 8:16], modp.t[:, 8:16], 1.0, None, ALU.add), reads=[modp], writes=[modp])

        NSP = S_LEN // SPAN
        hT = sb("hT", [128, 8, SPAN], BF16)
        xts = [sb("xts%d" % i, [128, D], F32) for i in range(2)]
        qT = [sb("qT%d" % i, [128, 2, SPAN], BF16) for i in range(2)]
        kT = [[sb("kT%d_%d" % (p, s_), [128, SPAN], BF16) for s_ in range(2)] for p in range(3)]
        V = [[sb("V%d_%d" % (p, s_), [128, 16, 80], BF16) for s_ in range(2)] for p in range(3)]
        posI = sb("posI", [128, SPAN], I32)
        posB = sb("posB", [128, SPAN], F32)
        Ct = sb("Ct", [128, SPAN], F32)
        St = sb("St", [128, SPAN], F32)
        tmpT = sb("tmpT", [128, SPAN], F32)
        tmpa = sb("tmpa", [128, 512], F32)
        tmpb = sb("tmpb", [128, 512], F32)
        PT = [sb("PT%d" % i, [128, 512], BF16) for i in range(6)]
        accs = [sb("accs%d" % i, [128, 260], F32) for i in range(3)]
        for p in range(3):
            for s_ in range(2):
                P.op("pool", lambda e, p=p, s_=s_: e.memset(V[p][s_].t[:, :, 64:65], 1.0), writes=[V[p][s_]])
        OPb = [Buf("OP%d" % p) for p in range(3)]
        import os
        STG = int(os.environ.get("DILSTAGE", "9"))
        ATT = int(os.environ.get("DILATT", "9"))
        NSP = int(os.environ.get("DILNSP", str(NSP)))
        cntd = {"PT": 0, "acc": 0}
        for s in range(NSP):
            sl = s % 2
            tok0 = s * SPAN
            for ti in range(16):
                emit_hmodT_tile(P, x_d, tok0 + ti * 128, xts[ti % 2], psX, idn, modp, hT, ti * 128, 8, ["sp", "act"][ti % 2])
            if STG < 2:
                continue
            emit_rope_tables(P, posB, posI, invp, negpi, Ct, St, tmpT, pos_d[0:1, tok0:tok0 + SPAN], SPAN)
            if STG < 3:
                continue
            for qc in range(2):
                for tg in range(4):
                    emit_proj_rope(P, hT, tg * 512, 512, wq, wqr, qc * 128, ps[0], ps[1], Ct, St, tg * 512, tmpa, tmpb,
                                   lambda qc=qc, tg=tg, sl=sl: qT[sl].t[:, qc, tg * 512:(tg + 1) * 512], qT[sl])
            for p in range(3):
                for tg in range(4):
                    emit_proj_rope(P, hT, tg * 512, 512, wk[p], wkr[p], 0, ps[0], ps[1], Ct, St, tg * 512, tmpa, tmpb,
                                   lambda p=p, tg=tg, sl=sl: kT[p][sl].t[:, tg * 512:(tg + 1) * 512], kT[p][sl])
            if STG < 4:
                continue
            for p, d in enumerate(DIL):
                ncb = 16 // d
                for r in range(d):
                    for c in range(ncb):
                        idx = r * ncb + c
                        a0 = r + d * 128 * c
                        pv = ps[2 + idx % 2]
                        for k in range(8):
                            P.op("pe", lambda e, k=k, a0=a0, d=d, p=p, pv=pv: e.matmul(
                                pv.t[:, 0:64], hT.t[:, k, ss(a0, 128, d)], wv[p].t[:, k, :],
                                start=(k == 0), stop=(k == 7)), reads=[hT, wv[p]], writes=[pv])
                        P.op("act", lambda e, p=p, sl=sl, idx=idx, pv=pv: e.copy(V[p][sl].t[:, idx, 0:64], pv.t[:, 0:64]),
                             reads=[pv], writes=[V[p][sl]])
            if STG < 5:
                continue
            tiles = []
            for p, d in enumerate(DIL):
                ncb = 16 // d
                for r in range(d):
                    for j in range(ncb):
                        chunks = [(j - 1, 0), (j, 1)]
                        chunks = [(kb, mc) for kb, mc in chunks if not (s == 0 and kb < 0)]
                        tiles.append((p, d, ncb, r, j, chunks))

            def stage_a(ti):
                p, d, ncb, r, j, chunks = tiles[ti]
                pts = []
                for ci, (kb, mc) in enumerate(chunks):
                    ksl = sl if kb >= 0 else 1 - sl
                    kbb = kb if kb >= 0 else ncb - 1
                    ka0 = r + d * 128 * kbb
                    qa0 = r + d * 128 * j
                    Sp = ps[(ti % 2) * 2 + ci]
                    for h in range(4):
                        qc, hf = h // 2, h % 2
                        rows = slice(hf * 64, (hf + 1) * 64)
                        P.op("pe", lambda e, h=h, qc=qc, rows=rows, p=p, ksl=ksl, ka0=ka0, qa0=qa0, d=d, Sp=Sp, sl=sl: e.matmul(
                            Sp.t[:, h * 128:(h + 1) * 128],
                            kT[p][ksl].t[rows, ss(ka0, 128, d)],
                            qT[sl].t[rows, qc, ss(qa0, 128, d)],
                            start=True, stop=False), reads=[kT[p][ksl], qT[sl]], writes=[Sp])
                        P.op("pe", lambda e, h=h, mc=mc, Sp=Sp: e.matmul(Sp.t[:, h * 128:(h + 1) * 128], idnb.t[:], msk.t[:, mc, 0:128],
                                                                      start=False, stop=True), reads=[idnb, msk], writes=[Sp])
                    pt = PT[cntd["PT"] % 6]
                    cntd["PT"] += 1
                    P.op("act", lambda e, pt=pt, Sp=Sp: e.activation(pt.t[:], Sp.t[:, 0:512], AF.Exp, scale=0.125), reads=[Sp], writes=[pt])
                    pts.append((pt, ksl, r * ncb + kbb))
                return pts

            def stage_b(ti, pts):
                p, d, ncb, r, j, chunks = tiles[ti]
                acc = ps[4 + ti % 2]
                for h in range(4):
                    for ci, (pt, ksl, vidx) in enumerate(pts):
                        P.op("pe", lambda e, h=h, pt=pt, p=p, ksl=ksl, vidx=vidx, acc=acc, ci=ci, nch=len(pts): e.matmul(
                            acc.t[:, h * 80:h * 80 + 65], pt.t[:, h * 128:(h + 1) * 128], V[p][ksl].t[:, vidx, 0:65],
                            start=(ci == 0), stop=(ci == nch - 1)), reads=[pt, V[p][ksl]], writes=[acc])
                ab = accs[cntd["acc"] % 3]
                cntd["acc"] += 1
                P.op("dve", lambda e, ab=ab, acc=acc: e.tensor_copy(ab.t[:].rearrange("p (h e) -> p h e", e=65), acc.t[:, 0:320].rearrange("p (h e) -> p h e", e=80)[:, :, 0:65]), reads=[acc], writes=[ab])
                g0 = tok0 + r + d * 128 * j
                P.dma("sp", lambda e, ab=ab, p=p, g0=g0, d=d: e.dma_start(out=OP[p][ss(g0, 128, d), :], in_=ab.t[:]), reads=[ab], writes=[OPb[p]])

            nxt = stage_a(0)
            for ti in range(len(tiles)):
                cur = nxt
                if ti + 1 < len(tiles):
                    nxt = stage_a(ti + 1)
                stage_b(ti, cur)
        P.barrier()
        if STG < 6:
            P.emit()
            return nc
        ld = [[sb("ld%d_%d" % (p, i), [128, 260], F32) for i in range(2)] for p in range(3)]
        rl = sb("rl", [128, 4], F32)
        ot = [sb("otl%d" % i, [128, 256], F32) for i in range(2)]
        Ob = Buf("o")
        for T in range(S_LEN // 128):
            b = T % 2
            for p in range(3):
                P.dma(["sp", "act", "sp"][p], lambda e, p=p, b=b, T=T: e.dma_start(out=ld[p][b].t[:], in_=OP[p][T * 128:(T + 1) * 128, :]),
                      reads=[OPb[p]], writes=[ld[p][b]])
            P.op("dve", lambda e, b=b: e.tensor_tensor(ld[0][b].t[:], ld[0][b].t[:], ld[1][b].t[:], ALU.add), reads=[ld[0][b], ld[1][b]], writes=[ld[0][b]])
            P.op("dve", lambda e, b=b: e.tensor_tensor(ld[0][b].t[:], ld[0][b].t[:], ld[2][b].t[:], ALU.add), reads=[ld[0][b], ld[2][b]], writes=[ld[0][b]])
            a3 = ld[0][b].t[:].rearrange("p (h e) -> p h e", e=65)
            P.op("dve", lambda e, a3=a3: e.reciprocal(rl.t[:], a3[:, :, 64]), reads=[ld[0][b]], writes=[rl])
            P.op("dve", lambda e, a3=a3, b=b: e.tensor_tensor(ot[b].t[:].rearrange("p (h e) -> p h e", e=64), a3[:, :, 0:64],
                                                             rl.t[:].unsqueeze(2).to_broadcast([128, 4, 64]), ALU.mult),
                 reads=[ld[0][b], rl], writes=[ot[b]])
            P.dma("sp", lambda e, b=b, T=T: e.dma_start(out=o_d[T * 128:(T + 1) * 128, :], in_=ot[b].t[:]), reads=[ot[b]], writes=[Ob])
        P.wait_all("sp", [Ob])
        P.emit()
    return nc


def run_dil(x, c, positions, ada_w_s, ada_b_s, w_in):
    if "dil" not in _CACHE:
        _CACHE["dil"] = build_dil()
    nc = _CACHE["dil"]
    idn = np.eye(128, dtype=np.float32)
    inv = rope_inv_table()
    msk = tri_masks()
    in_maps = []
    for core in range(8):
        b, g = core // 4, core % 4
        wk = np.stack([w_in[:, D + p * 512 + g * 64: D + p * 512 + g * 64 + 64] for p in range(3)])
        wv = np.stack([w_in[:, D + p * 512 + 256 + g * 64: D + p * 512 + 256 + g * 64 + 64] for p in range(3)])
        in_maps.append({
            "x": x[b], "cT": np.ascontiguousarray(c[b].reshape(8, 128).T),
            "adaw": np.ascontiguousarray(ada_w_s[:, 0:2 * D]), "adabT": np.ascontiguousarray(ada_b_s[0:2 * D].reshape(16, 128).T),
            "wq": np.ascontiguousarray(w_in[:, g * 256:(g + 1) * 256]), "wk": np.ascontiguousarray(wk), "wv": np.ascontiguousarray(wv),
            "pos": np.ascontiguousarray(positions[b:b + 1]), "inv": inv, "idn": idn, "msk": msk,
        })
    res = run_bass_kernel_spmd(nc, in_maps, core_ids=list(range(8)))
    B = x.shape[0]
    o = np.zeros((B, S_LEN, D), np.float32)
    for core in range(8):
        b, g = core // 4, core % 4
        o[b, :, g * 256:(g + 1) * 256] = res.results[core]["o"]
    return o


QG = 512
NG = S_LEN // QG
NCMP = 1023


def nsa_masks():
    p = np.arange(128)[:, None]
    f = np.arange(512)[None, :]
    ms = []
    for jj in range(4):
        ms.append(np.where(f - p - 128 * jj >= 0, 0.0, NEG))
    for c in range(4):
        ms.append(np.where(f - p < 128 * c, 0.0, NEG))
    for m in range(5):
        ms.append(np.where(f - 16 * p + 512 * m - 31 >= 0, 0.0, NEG))
    return np.stack(ms, axis=1).astype(np.float32)


def nsa_indc():
    t = np.zeros((128, 64, 128), np.float32)
    for jm in range(64):
        t[2 * jm, jm, 0:64] = 1.0
        t[2 * jm + 1, jm, 64:128] = 1.0
    return t


def nsa_vc_const():
    t = np.zeros((1024, 257), np.float32)
    t[:, 0] = 1.0
    for s_ in range(256):
        for n in range(max(0, 4 * s_ - 1), min(NCMP, 4 * s_ + 4)):
            t[n, 1 + s_] = 1.0
    return t


def nsa_forced():
    t = np.zeros((128, 3), np.float32)
    for p in range(128):
        c = p // 64
        for x in (-1, 0, 1):
            if x == c or x == c - 1:
                t[p, x + 1] = 1.0e4
    return t


def build_nsa():
    nc = bass.Bass("TRN2", target_bir_lowering=False)
    dt_in = lambda name, shape, dt=F32: nc.dram_tensor(name, list(shape), dt, kind="ExternalInput").ap()
    x_d = dt_in("x", [S_LEN, D])
    cT_d = dt_in("cT", [128, 8])
    adaw_d = dt_in("adaw", [D, 2 * D])
    adabT_d = dt_in("adabT", [128, 16])
    wq_d = dt_in("wq", [D, 256])
    wkv_d = dt_in("wkv", [6, D, 64])
    wgt_d = dt_in("wgt", [D, 12])
    pos_d = dt_in("pos", [1, S_LEN], I32)
    posc_d = dt_in("posc", [1, 1024], I32)
    inv_d = dt_in("inv", [128, 1])
    idn_d = dt_in("idn", [128, 128])
    msk_d = dt_in("msk", [128, 13, 512])
    gp_d = dt_in("gp", [64, S_LEN])
    vcc_d = dt_in("vcc", [1024, 257])
    frc_d = dt_in("frc", [128, 3])
    w1_d = dt_in("w1", [2, 2048, 256])
    w2_d = dt_in("w2", [2, 256, 64])
    cpos_d = dt_in("cposT", [128, 32])
    o_d = nc.dram_tensor("o", [S_LEN, 256], F32, kind="ExternalOutput").ap()

    with ExitStack() as st:
        P = Prog(nc, st)
        sb = P.sbuf
        idn = sb("idn", [128, 128], F32)
        idnb = sb("idnb", [128, 128], BF16)
        msk = sb("msk", [128, 13, 512], BF16)
        frc = sb("frc", [128, 3], F32)
        invp = sb("invp", [128, 1], F32)
        wqh = [sb("wqh%d" % h_, [128, 8, 128], BF16) for h_ in range(4)]
        wqhr = [sb("wqhr%d" % h_, [128, 8, 128], BF16) for h_ in range(4)]
        wks = sb("wks", [128, 8, 128], BF16)
        wksr = sb("wksr", [128, 8, 128], BF16)
        wkw = sb("wkw", [128, 8, 128], BF16)
        wkwr = sb("wkwr", [128, 8, 128], BF16)
        wkvc = sb("wkvc", [128, 8, 128], BF16)
        wvs = sb("wvs", [128, 8, 64], BF16)
        wvw = sb("wvw", [128, 8, 64], BF16)
        wgt = sb("wgt", [128, 8, 12], BF16)
        kselT = sb("kselT", [128, S_LEN], BF16)
        Vsel = sb("Vsel", [128, 128, 80], BF16)
        kcT = sb("kcT", [128, 1024], BF16)
        VC = sb("VC", [128, 8, 336], BF16)
        ps = [P.psum("pb%d" % i, [128, 512], F32) for i in range(6)]
        psXb = P.psum("psX", [128, 512], F32)
        psS3 = P.psum("psS3", [128, 512], F32)
        spr = [ps[0], ps[1], psS3]

        def ld(q, dst, src, ap=None):
            P.dma(q, lambda e: e.dma_start(out=dst.t[:] if ap is None else ap, in_=src), writes=[dst])
        ld("sp", idn, idn_d)
        ld("pool", idnb, idn_d)
        ld("pool", msk, msk_d)
        ld("sp", frc, frc_d)
        ld("sp", invp, inv_d)
        kp = lambda a: a.rearrange("(k p) n -> p k n", p=128)
        for h_ in range(4):
            P.op("pool", lambda e, h_=h_: e.memset(wqh[h_].t[:], 0.0), writes=[wqh[h_]])
            ld("pool", wqh[h_], kp(wq_d[:, h_ * 64:(h_ + 1) * 64]), wqh[h_].t[:, :, 0:64])
        for h in range(2):
            ld("pool", wks, kp(wkv_d[2]), wks.t[:, :, h * 64:(h + 1) * 64])
            ld("pool", wkw, kp(wkv_d[4]), wkw.t[:, :, h * 64:(h + 1) * 64])
        ld("pool", wkvc, kp(wkv_d[0]), wkvc.t[:, :, 0:64])
        ld("pool", wkvc, kp(wkv_d[1]), wkvc.t[:, :, 64:128])
        ld("pool", wvs, kp(wkv_d[3]))
        ld("pool", wvw, kp(wkv_d[5]))
        ld("pool", wgt, kp(wgt_d))
        for h_ in range(4):
            emit_rot_weights(P, wqh[h_], wqhr[h_], 2)
        emit_rot_weights(P, wks, wksr, 2)
        emit_rot_weights(P, wkw, wkwr, 2)
        P.op("pool", lambda e: e.memset(Vsel.t[:, :, 64:65], 1.0), writes=[Vsel])
        for c in range(8):
            P.dma("pool", lambda e, c=c: e.dma_start(out=VC.t[:, c, 64:321], in_=vcc_d[c * 128:(c + 1) * 128, :]), writes=[VC])
        modp = emit_mod_cols(P, nc, adaw_d, adabT_d, cT_d, 2 * D, None, ps[0])
        P.op("dve", lambda e: e.tensor_scalar(modp.t[:, 8:16], modp.t[:, 8:16], 1.0, None, ALU.add), reads=[modp], writes=[modp])

        hT = sb("hT", [128, 8, QG], BF16)
        xts = [sb("xts%d" % i, [128, D], F32) for i in range(2)]
        posI = sb("posI", [128, 512], I32)
        posB = sb("posB", [128, 512], F32)
        Ct = sb("Ct", [128, 512], F32)
        St = sb("St", [128, 512], F32)
        tmpT = sb("tmpT", [128, 512], F32)
        tmpa = sb("tmpa", [128, 512], F32)
        tmpb = sb("tmpb", [128, 512], F32)

        import os
        NST = int(os.environ.get("NSA_STAGE", "9"))
        if NST < 2:
            P.barrier(); P.emit(); return nc
        P.push()
        kvcT = sb("kvcT", [128, S_LEN], BF16)
        for G in range(NG):
            t0 = G * QG
            for ti in range(4):
                emit_hmodT_tile(P, x_d, t0 + ti * 128, xts[ti % 2], psXb, idn, modp, hT, ti * 128, 8, ["sp", "act"][ti % 2], width=512)
            emit_rope_tables(P, posB, posI, invp, None, Ct, St, tmpT, pos_d[0:1, t0:t0 + QG], QG)
            emit_proj_rope(P, hT, 0, QG, wks, wksr, 0, ps[0], ps[1], Ct, St, 0, tmpa, tmpb,
                           lambda t0=t0: kselT.t[:, t0:t0 + QG], kselT)
            for k in range(8):
                P.op("pe", lambda e, k=k: e.matmul(ps[2].t[:, 0:QG], wkvc.t[:, k, :], hT.t[:, k, :], start=(k == 0), stop=(k == 7)),
                     reads=[wkvc, hT], writes=[ps[2]])
            P.op("act", lambda e, t0=t0: e.copy(kvcT.t[:, t0:t0 + QG], ps[2].t[:, 0:QG]), reads=[ps[2]], writes=[kvcT])
            for ti in range(4):
                pv = ps[3 + ti % 2]
                for k in range(8):
                    P.op("pe", lambda e, k=k, ti=ti, pv=pv: e.matmul(pv.t[:, 0:64], hT.t[:, k, ti * 128:(ti + 1) * 128], wvs.t[:, k, :],
                                                                     start=(k == 0), stop=(k == 7)), reads=[hT, wvs], writes=[pv])
                P.op("act", lambda e, ti=ti, G=G, pv=pv: e.copy(Vsel.t[:, G * 4 + ti, 0:64], pv.t[:, 0:64]), reads=[pv], writes=[Vsel])

        P.dma("pool", lambda e: e.dma_start(out=kselT.t[64:128, :], in_=gp_d), writes=[kselT])
        if NST < 3:
            P.barrier(); P.emit(); return nc
        w1 = sb("w1", [128, 32, 256], BF16)
        w2k = sb("w2k", [128, 2, 128], BF16)
        w2kr = sb("w2kr", [128, 2, 128], BF16)
        w2v = sb("w2v", [128, 2, 64], BF16)
        cposT = sb("cposT", [128, 32], BF16)
        cbias = sb("cbias", [128, 4], F32)
        hid = [[sb("hid%d_%d" % (kv, hc), [128, 1024], BF16) for hc in range(2)] for kv in range(2)]
        xg = sb("xg", [128, 512], F32)
        ug = sb("ug", [128, 512], F32)
        for kv in range(2):
            P.dma("pool", lambda e, kv=kv: e.dma_start(out=w1.t[kv * 64:(kv + 1) * 64, :, :], in_=w1_d[kv].rearrange("(l e) h -> e l h", e=64)), writes=[w1])
        for h in range(2):
            P.dma("pool", lambda e, h=h: e.dma_start(out=w2k.t[:, :, h * 64:(h + 1) * 64], in_=w2_d[0].rearrange("(k p) n -> p k n", p=128)), writes=[w2k])
        P.dma("pool", lambda e: e.dma_start(out=w2v.t[:], in_=w2_d[1].rearrange("(k p) n -> p k n", p=128)), writes=[w2v])
        P.dma("pool", lambda e: e.dma_start(out=cposT.t[:], in_=cpos_d), writes=[cposT])
        P.op("pool", lambda e: e.memset(w2kr.t[:], 0.0), writes=[w2kr])
        for k in range(2):
            wv_ = w2k.t[:, k, :].rearrange("p (h e) -> p h e", e=64)
            rv_ = w2kr.t[:, k, :].rearrange("p (h e) -> p h e", e=64)
            P.op("dve", lambda e, wv_=wv_, rv_=rv_: e.tensor_scalar(rv_[:, :, 0:8], wv_[:, :, 8:16], -1.0, None, ALU.mult), reads=[w2k], writes=[w2kr])
            P.op("dve", lambda e, wv_=wv_, rv_=rv_: e.tensor_copy(rv_[:, :, 8:16], wv_[:, :, 0:8]), reads=[w2k], writes=[w2kr])
        for kv in range(2):
            for hc in range(2):
                P.op("pool", lambda e, kv=kv, hc=hc: e.memset(hid[kv][hc].t[:], 0.0), writes=[hid[kv][hc]])
        for kv in range(2):
            rows = slice(kv * 64, (kv + 1) * 64)
            for hc in range(2):
                col = kv * 2 + hc
                for l in range(32):
                    P.op("pe", lambda e, rows=rows, hc=hc, l=l, col=col: e.matmul(
                        ps[0].t[:, col:col + 1], w1.t[rows, l, hc * 128:(hc + 1) * 128], cposT.t[rows, l:l + 1],
                        start=(l == 0), stop=(l == 31)), reads=[w1, cposT], writes=[ps[0]])
        P.op("dve", lambda e: e.tensor_copy(cbias.t[:], ps[0].t[:, 0:4]), reads=[ps[0]], writes=[cbias])
        for kv in range(2):
            rows = slice(kv * 64, (kv + 1) * 64)
            for hc in range(2):
                col = kv * 2 + hc
                for gi in range(2):
                    n0 = gi * 512
                    nn = 512 if gi == 0 else 511
                    pz = ps[1 + (hc * 2 + gi) % 2]
                    for l in range(32):
                        P.op("pe", lambda e, rows=rows, hc=hc, l=l, n0=n0, nn=nn, pz=pz: e.matmul(
                            pz.t[:, 0:nn], w1.t[rows, l, hc * 128:(hc + 1) * 128], kvcT.t[rows, ss(16 * n0 + l, nn, 16)],
                            start=(l == 0), stop=(l == 31)), reads=[w1, kvcT], writes=[pz])
                    P.op("act", lambda e, pz=pz, nn=nn, col=col: e.activation(xg.t[:, 0:nn], pz.t[:, 0:nn], AF.Identity, bias=cbias.t[:, col:col + 1]),
                         reads=[pz, cbias], writes=[xg])
                    P.op("dve", lambda e, nn=nn: e.tensor_tensor(ug.t[:, 0:nn], xg.t[:, 0:nn], xg.t[:, 0:nn], ALU.mult), reads=[xg], writes=[ug])
                    P.op("dve", lambda e, nn=nn: e.tensor_scalar(ug.t[:, 0:nn], ug.t[:, 0:nn], 0.044715, 1.0, ALU.mult, ALU.add), reads=[ug], writes=[ug])
                    P.op("dve", lambda e, nn=nn: e.tensor_tensor(ug.t[:, 0:nn], ug.t[:, 0:nn], xg.t[:, 0:nn], ALU.mult), reads=[ug, xg], writes=[ug])
                    P.op("act", lambda e, nn=nn: e.activation(ug.t[:, 0:nn], ug.t[:, 0:nn], AF.Sigmoid, scale=1.5957691216057308), reads=[ug], writes=[ug])
                    P.op("dve", lambda e, nn=nn, kv=kv, hc=hc, n0=n0: e.tensor_tensor(hid[kv][hc].t[:, n0:n0 + nn], ug.t[:, 0:nn], xg.t[:, 0:nn], ALU.mult),
                         reads=[ug, xg], writes=[hid[kv][hc]])
        for gi in range(2):
            n0 = gi * 512
            emit_rope_tables(P, posB, posI, invp, None, Ct, St, tmpT, posc_d[0:1, n0:n0 + 512], 512)
            for hc in range(2):
                P.op("pe", lambda e, hc=hc, n0=n0: e.matmul(ps[0].t[:, 0:512], w2k.t[:, hc, :], hid[0][hc].t[:, n0:n0 + 512], start=(hc == 0), stop=(hc == 1)),
                     reads=[w2k, hid[0][hc]], writes=[ps[0]])
            for hc in range(2):
                P.op("pe", lambda e, hc=hc, n0=n0: e.matmul(ps[1].t[:, 0:512], w2kr.t[:, hc, :], hid[0][hc].t[:, n0:n0 + 512], start=(hc == 0), stop=(hc == 1)),
                     reads=[w2kr, hid[0][hc]], writes=[ps[1]])
            P.op("dve", lambda e, n0=n0: e.tensor_tensor(tmpa.t[:], ps[0].t[:, 0:512], Ct.t[:, 0:512], ALU.mult), reads=[ps[0], Ct], writes=[tmpa])
            P.op("dve", lambda e, n0=n0: e.tensor_tensor(tmpb.t[:], ps[1].t[:, 0:512], St.t[:, 0:512], ALU.mult), reads=[ps[1], St], writes=[tmpb])
            P.op("pool", lambda e, n0=n0: e.tensor_tensor(kcT.t[:, n0:n0 + 512], tmpa.t[:], tmpb.t[:], ALU.add), reads=[tmpa, tmpb], writes=[kcT])
        for c in range(8):
            pv = ps[2 + c % 2]
            for hc in range(2):
                P.op("pe", lambda e, hc=hc, c=c, pv=pv: e.matmul(pv.t[:, 0:64], hid[1][hc].t[:, c * 128:(c + 1) * 128], w2v.t[:, hc, :],
                                                                 start=(hc == 0), stop=(hc == 1)), reads=[hid[1][hc], w2v], writes=[pv])
            P.op("act", lambda e, c=c, pv=pv: e.copy(VC.t[:, c, 0:64], pv.t[:, 0:64]), reads=[pv], writes=[VC])
        P.pop()
        if NST < 4:
            P.barrier(); P.emit(); return nc

        R = [[sb("R%d_%d" % (h_, w_), [128, QG], BF16) for w_ in range(4)] for h_ in range(4)]
        nbTw = [sb("nbTw%d" % w_, [128, QG], BF16) for w_ in range(4)]
        nbp = sb("nbp", [128, 320], F32)
        P.op("pool", lambda e: e.memset(nbp.t[:], 0.0), writes=[nbp])
        kwT = [sb("kwT%d" % i, [128, QG], BF16) for i in range(2)]
        Vw = sb("Vw", [128, 8, 80], BF16)
        gts = sb("gts", [128, 4, 12], F32)
        PT = [sb("PT%d" % i, [128, 512], BF16) for i in range(4)]
        impw = sb("impw", [128, 256], F32)
        m8a = sb("m8a", [128, 8], F32)
        m8b = sb("m8b", [128, 8], F32)
        P.op("pool", lambda e: e.memset(Vw.t[:, :, 64:65], 1.0), writes=[Vw])
        Ob = Buf("o")
        acc = [ps[2], ps[3], ps[4], ps[5]]
        psW = acc
        cnt = {"S": 0, "PT": 0, "first": [True] * 4}

        def attend(h, chunks, ncols, G):
            qc, hf = h // 2, h % 2
            rows = slice(hf * 64, (hf + 1) * 64)
            first = {s_: True for s_ in range(4)}
            last_idx = {}
            for ci, ch in enumerate(chunks):
                for s_ in range(ch[3], ch[4] + 1):
                    last_idx[s_] = ci
            def qk(ci):
                kfn, masks, vfn, slo, shi = chunks[ci]
                Sp = spr[cnt["S"] % 3]
                cnt["S"] += 1
                P.op("pe", lambda e, kfn=kfn, Sp=Sp, nm0=len(masks): e.matmul(Sp.t[:, 0:QG], kfn(rows)[0], kfn(rows)[1], start=True, stop=(nm0 == 0)),
                     reads=[kselT, kcT, kwT[0], kwT[1]] + R[h], writes=[Sp])
                for mi, (lf, rf) in enumerate(masks):
                    P.op("pe", lambda e, lf=lf, rf=rf, Sp=Sp, mi=mi, nm=len(masks): e.matmul(Sp.t[:, 0:QG], lf(), rf(), start=False, stop=(mi == nm - 1)),
                         reads=[idnb, msk], writes=[Sp])
                return Sp

            def ex(ci, Sp):
                pt = PT[cnt["PT"] % 4]
                cnt["PT"] += 1
                P.op("act", lambda e, pt=pt, Sp=Sp: e.activation(pt.t[:], Sp.t[:, 0:QG], AF.Exp, scale=0.125), reads=[Sp], writes=[pt])
                return pt

            def pv(ci, pt):
                kfn, masks, vfn, slo, shi = chunks[ci]
                for s_ in range(slo, shi + 1):
                    P.op("pe", lambda e, s_=s_, pt=pt, vfn=vfn, st_=first[s_], sp_=(last_idx[s_] == ci): e.matmul(
                        acc[s_].t[:, 0:ncols], pt.t[:, s_ * 128:(s_ + 1) * 128], vfn(), start=st_, stop=sp_),
                        reads=[pt, Vsel, VC, Vw], writes=[acc[s_]])
                    first[s_] = False

            sps = {0: qk(0)}
            if len(chunks) > 1:
                sps[1] = qk(1)
            for ci in range(len(chunks)):
                pt = ex(ci, sps.pop(ci))
                if ci + 2 < len(chunks):
                    sps[ci + 2] = qk(ci + 2)
                pv(ci, pt)

        stg = [sb("stg%d" % br, [128, 4, 4, 65], F32) for br in range(3)]
        stgB = [[Buf("stgB%d_%d" % (br, s_), stg[br].t) for s_ in range(4)] for br in range(3)]
        impS = sb("impS", [128, 4, 4, 256], F32)
        impSB = [Buf("impSB%d" % s_, impS.t) for s_ in range(4)]
        osb4 = sb("osb4", [128, 4, 4, 64], F32)
        otmp = sb("otmp", [128, 4, 4, 64], F32)
        rlb = sb("rlb", [128, 4, 4], F32)
        wgb = sb("wgb", [128, 4, 4], F32)
        impacc4 = sb("impacc4", [128, 4, 256], F32)

        def evac(h, s_, br):
            P.op("dve", lambda e: e.tensor_copy(stg[br].t[:, s_, h, :], acc[s_].t[:, 0:65]), reads=[acc[s_]], writes=[stgB[br][s_]])
            if br == 0:
                P.op("dve", lambda e: e.tensor_copy(impS.t[:, s_, h, :], acc[s_].t[:, 65:321]), reads=[acc[s_]], writes=[impSB[s_]])

        def finish_branch(br, first_branch):
            P.op("dve", lambda e: e.tensor_scalar(rlb.t[:], stg[br].t[:, :, :, 64], 1e-30, None, ALU.max), reads=stgB[br], writes=[rlb])
            P.op("dve", lambda e: e.reciprocal(rlb.t[:], rlb.t[:]), reads=[rlb], writes=[rlb])
            P.op("dve", lambda e: e.tensor_tensor(wgb.t[:], rlb.t[:], gts.t[:, :, ss(br, 4, 3)], ALU.mult), reads=[rlb, gts], writes=[wgb])
            dst = osb4 if first_branch else otmp
            P.op("dve", lambda e: e.tensor_tensor(dst.t[:], stg[br].t[:, :, :, 0:64], wgb.t[:].unsqueeze(3).to_broadcast([128, 4, 4, 64]), ALU.mult),
                 reads=stgB[br] + [wgb], writes=[dst])
            if not first_branch:
                P.op("pool", lambda e: e.tensor_tensor(osb4.t[:], osb4.t[:], otmp.t[:], ALU.add), reads=[otmp, osb4], writes=[osb4])
            if br == 0:
                P.op("dve", lambda e: e.tensor_tensor(impS.t[:], impS.t[:], rlb.t[:].unsqueeze(3).to_broadcast([128, 4, 4, 256]), ALU.mult),
                     reads=impSB + [rlb], writes=impSB)
                P.op("pool", lambda e: e.tensor_tensor(impacc4.t[:], impS.t[:, :, 0, :], impS.t[:, :, 1, :], ALU.add), reads=impSB, writes=[impacc4])
                P.op("pool", lambda e: e.tensor_tensor(impacc4.t[:], impacc4.t[:], impS.t[:, :, 2, :], ALU.add), reads=impSB + [impacc4], writes=[impacc4])
                P.op("pool", lambda e: e.tensor_tensor(impacc4.t[:], impacc4.t[:], impS.t[:, :, 3, :], ALU.add), reads=impSB + [impacc4], writes=[impacc4])

        import os
        NG_RUN = int(os.environ.get("NSA_NG", str(NG)))
        for G in range(NG_RUN):
            t0 = G * QG
            sl = G % 2
            for ti in range(4):
                emit_hmodT_tile(P, x_d, t0 + ti * 128, xts[ti % 2], psXb, idn, modp, hT, ti * 128, 8, ["sp", "act"][ti % 2], width=512)
            emit_rope_tables(P, posB, posI, invp, None, Ct, St, tmpT, pos_d[0:1, t0:t0 + QG], QG)
            for h_ in range(4):
                emit_proj_rope(P, hT, 0, QG, wqh[h_], wqhr[h_], 0, ps[0], ps[1], Ct, St, 0, tmpa, tmpb,
                               lambda h_=h_: R[h_][0].t[:, :], R[h_][0])
            emit_proj_rope(P, hT, 0, QG, wkw, wkwr, 0, ps[0], ps[1], Ct, St, 0, tmpa, tmpb, lambda sl=sl: kwT[sl].t[:, :], kwT[sl])
            for ti in range(4):
                pv = ps[2 + ti % 2]
                for k in range(8):
                    P.op("pe", lambda e, k=k, ti=ti, pv=pv: e.matmul(pv.t[:, 0:64], hT.t[:, k, ti * 128:(ti + 1) * 128], wvw.t[:, k, :],
                                                                     start=(k == 0), stop=(k == 7)), reads=[hT, wvw], writes=[pv])
                P.op("act", lambda e, ti=ti, sl=sl, pv=pv: e.copy(Vw.t[:, sl * 4 + ti, 0:64], pv.t[:, 0:64]), reads=[pv], writes=[Vw])
                pg = ps[4 + ti % 2]
                for k in range(8):
                    P.op("pe", lambda e, k=k, ti=ti, pg=pg: e.matmul(pg.t[:, 0:12], hT.t[:, k, ti * 128:(ti + 1) * 128], wgt.t[:, k, :],
                                                                     start=(k == 0), stop=(k == 7)), reads=[hT, wgt], writes=[pg])
                P.op("act", lambda e, ti=ti, pg=pg: e.activation(gts.t[:, ti, :], pg.t[:, 0:12], AF.Sigmoid), reads=[pg], writes=[gts])

            if NST < 5:
                continue
            cmax = min(7, G // 4)
            for h in range(4):
                chunks = []
                for c in range(cmax + 1):
                    m = G - 4 * c
                    masks = []
                    if m <= 4:
                        masks = [(lambda: idnb.t[:], lambda m=m: msk.t[:, 8 + m, :])]
                    chunks.append((lambda rows, c=c, h=h: (kcT.t[0:64, c * 128:(c + 1) * 128], R[h][0].t[0:64, :]), masks, lambda c=c: VC.t[:, c, 0:321], 0, 3))
                attend(h, chunks, 321, G)
                for s_ in range(4):
                    evac(h, s_, 0)
            finish_branch(0, True)
            if NST < 6:
                continue
            for s_ in range(4):
                T = G * 4 + s_
                ia = Buf("iav", impacc4.t[:, s_, :])
                lo = max(0, 2 * T - 1)
                hi = min(256, 2 * T + 2)
                P.op("dve", lambda e, ia=ia, lo=lo, hi=hi, T=T: e.tensor_tensor(ia.t[:, lo:hi], ia.t[:, lo:hi], frc.t[:, lo - (2 * T - 1):hi - (2 * T - 1)], ALU.add),
                     reads=[impacc4, frc], writes=[impacc4])
                P.op("dve", lambda e, ia=ia: e.tensor_scalar(ia.t[:, 0:1], ia.t[:, 0:1], 1.0e4, None, ALU.add), reads=[impacc4], writes=[impacc4])
                P.op("dve", lambda e, ia=ia: e.max(out=m8a.t[:], in_=ia.t[:]), reads=[impacc4], writes=[m8a])
                P.op("dve", lambda e, ia=ia: e.match_replace(out=impw.t[:], in_to_replace=m8a.t[:], in_values=ia.t[:], imm_value=-1.0e30),
                     reads=[impacc4, m8a], writes=[impw])
                P.op("dve", lambda e: e.max(out=m8b.t[:], in_=impw.t[:]), reads=[impw], writes=[m8b])
                P.op("dve", lambda e, ia=ia: e.tensor_scalar(nbp.t[:, 64:320], ia.t[:], m8b.t[:, 7:8], NEG, ALU.is_lt, ALU.mult), reads=[impacc4, m8b], writes=[nbp])
                nW = (4 * G + 3) // 32 + 1
                for w_ in range(nW):
                    P.op("pe", lambda e, w_=w_, s_=s_: e.transpose(psW[w_].t[:, s_ * 128:(s_ + 1) * 128], nbp.t[:, 64 * w_:64 * w_ + 128], idn.t[:]),
                         reads=[nbp, idn], writes=[psW[w_]])
            nW = (4 * G + 3) // 32 + 1
            for w_ in range(nW):
                P.op("act", lambda e, w_=w_: e.copy(nbTw[w_].t[:], psW[w_].t[:, 0:QG]), reads=[psW[w_]], writes=[nbTw[w_]])
                for h_ in range(4):
                    if w_ > 0:
                        P.op("pool", lambda e, w_=w_, h_=h_: e.tensor_copy(R[h_][w_].t[0:64, :], R[h_][0].t[0:64, :]), reads=[R[h_][0]], writes=[R[h_][w_]])
                    P.op("pool", lambda e, w_=w_, h_=h_: e.tensor_copy(R[h_][w_].t[64:128, :], nbTw[w_].t[64:128, :]), reads=[nbTw[w_]], writes=[R[h_][w_]])
            if NST < 7:
                continue
            for h in range(4):
                chunks = []
                for j in range(4 * G + 4):
                    w_ = j // 32
                    masks = []
                    jj = j - 4 * G
                    if jj >= 0:
                        masks.append((lambda: idnb.t[:], lambda jj=jj: msk.t[:, jj, :]))
                    chunks.append((lambda rows, j=j, w_=w_, h=h: (kselT.t[:, j * 128:(j + 1) * 128], R[h][w_].t[:, :]), masks, lambda j=j: Vsel.t[:, j, 0:65],
                                   max(0, jj), 3))
                attend(h, chunks, 65, G)
                for s_ in range(4):
                    evac(h, s_, 1)
            finish_branch(1, False)
            if NST < 8:
                continue
            for h in range(4):
                chunks = []
                for c in range(8):
                    if t0 - 512 + 128 * c < 0:
                        continue
                    if c < 4:
                        kfn = lambda rows, c=c, sl=sl, h=h: (kwT[1 - sl].t[0:64, c * 128:(c + 1) * 128], R[h][0].t[0:64, :])
                        vfn = lambda c=c, sl=sl: Vw.t[:, (1 - sl) * 4 + c, 0:65]
                        mk = 4 + c
                    else:
                        kfn = lambda rows, c=c, sl=sl, h=h: (kwT[sl].t[0:64, (c - 4) * 128:(c - 3) * 128], R[h][0].t[0:64, :])
                        vfn = lambda c=c, sl=sl: Vw.t[:, sl * 4 + (c - 4), 0:65]
                        mk = c - 4
                    masks = [(lambda: idnb.t[:], lambda mk=mk: msk.t[:, mk, :])]
                    chunks.append((kfn, masks, vfn, max(0, c - 4), min(3, c)))
                attend(h, chunks, 65, G)
                for s_ in range(4):
                    evac(h, s_, 2)
            finish_branch(2, False)
            for s_ in range(4):
                r0 = t0 + s_ * 128
                P.dma("sp", lambda e, s_=s_, r0=r0: e.dma_start(out=o_d[r0:r0 + 128, :], in_=osb4.t[:, s_, :, :].rearrange("p h e -> p (h e)")), reads=[osb4], writes=[Ob])
        P.barrier()
        P.wait_all("sp", [Ob])
        P.emit()
    return nc


def nsa_in_maps(x, c, positions, ada_w_s, ada_b_s, w_in, pos_k, w1_k, w2_k, pos_v, w1_v, w2_v, cores=range(8)):
    idn = np.eye(128, dtype=np.float32)
    inv = rope_inv_table()
    msk = nsa_masks()
    gp = np.zeros((64, S_LEN), np.float32)
    kk = np.arange(S_LEN)
    gp[(kk // 64) % 64, kk] = 1.0
    vcc = nsa_vc_const()
    frc = nsa_forced()
    cposT = np.ascontiguousarray(np.concatenate([pos_k.T, pos_v.T], axis=0))
    w1 = np.ascontiguousarray(np.stack([w1_k, w1_v]))
    w2 = np.ascontiguousarray(np.stack([w2_k, w2_v]))
    in_maps = []
    for core in cores:
        b, g = core // 4, core % 4
        wkv = np.stack([w_in[:, D + br * 512 + kv * 256 + g * 64: D + br * 512 + kv * 256 + g * 64 + 64] for br in range(3) for kv in range(2)])
        posc = np.zeros((1, 1024), np.int32)
        posc[0, :NCMP] = positions[b, 31::16][:NCMP]
        in_maps.append({
            "x": x[b], "cT": np.ascontiguousarray(c[b].reshape(8, 128).T),
            "adaw": np.ascontiguousarray(ada_w_s[:, 0:2 * D]), "adabT": np.ascontiguousarray(ada_b_s[0:2 * D].reshape(16, 128).T),
            "wq": np.ascontiguousarray(w_in[:, g * 256:(g + 1) * 256]), "wkv": np.ascontiguousarray(wkv),
            "wgt": np.ascontiguousarray(w_in[:, D + 1536 + 12 * g: D + 1536 + 12 * g + 12]),
            "pos": np.ascontiguousarray(positions[b:b + 1]), "posc": posc, "inv": inv, "idn": idn, "msk": msk,
            "gp": gp, "vcc": vcc, "frc": frc, "w1": w1, "w2": w2, "cposT": cposT,
        })
    return in_maps


def run_nsa(x, c, positions, ada_w_s, ada_b_s, w_in, pos_k, w1_k, w2_k, pos_v, w1_v, w2_v):
    if "nsa" not in _CACHE:
        _CACHE["nsa"] = build_nsa()
    nc = _CACHE["nsa"]
    in_maps = nsa_in_maps(x, c, positions, ada_w_s, ada_b_s, w_in, pos_k, w1_k, w2_k, pos_v, w1_v, w2_v)
    res = run_bass_kernel_spmd(nc, in_maps, core_ids=list(range(8)))
    B = x.shape[0]
    o = np.zeros((B, S_LEN, D), np.float32)
    for core in range(8):
        b, g = core // 4, core % 4
        o[b, :, g * 256:(g + 1) * 256] = res.results[core]["o"]
    return o


def kernel(x, c, positions, ada_w, ada_b, ln_g, ln_b,
           nsa_w_in, nsa_cmp_pos_k, nsa_cmp_w1_k, nsa_cmp_w2_k,
           nsa_cmp_pos_v, nsa_cmp_w1_v, nsa_cmp_w2_v, nsa_w_o,
           dil_w_in, dil_w_o, router_w, router_b, moe_w_gate, moe_w_up, moe_w_down):
    f = lambda a: np.ascontiguousarray(np.asarray(a))
    x, c, positions = f(x), f(c), f(positions)
    ada_w, ada_b, ln_g, ln_b = f(ada_w), f(ada_b), f(ln_g), f(ln_b)
    nsa_w_in, nsa_w_o, dil_w_in, dil_w_o = f(nsa_w_in), f(nsa_w_o), f(dil_w_in), f(dil_w_o)
    router_w, router_b = f(router_w), f(router_b)
    moe_w_gate, moe_w_up, moe_w_down = f(moe_w_gate), f(moe_w_up), f(moe_w_down)
    o0 = run_nsa(x, c, positions, ada_w[0, 0], ada_b[0, 0], nsa_w_in[0],
                 f(nsa_cmp_pos_k)[0], f(nsa_cmp_w1_k)[0], f(nsa_cmp_w2_k)[0],
                 f(nsa_cmp_pos_v)[0], f(nsa_cmp_w1_v)[0], f(nsa_cmp_w2_v)[0])
    x1 = run_ffn(x, o0, c, ada_w[0], ada_b[0], ln_g[0], ln_b[0], nsa_w_o[0], router_w, router_b,
                 moe_w_gate[0], moe_w_up[0], moe_w_down[0])
    o1 = run_dil(x1, c, positions, ada_w[1, 0], ada_b[1, 0], dil_w_in[0])
    out = run_ffn(x1, o1, c, ada_w[1], ada_b[1], ln_g[1], ln_b[1], dil_w_o[0], router_w, router_b,
                  moe_w_gate[1], moe_w_up[1], moe_w_down[1])
    return out.astype(np.float32)
```

```python
import numpy as np
from contextlib import ExitStack
import concourse.bass as bass
import concourse.mybir as mybir
from concourse.bass_utils import run_bass_kernel_spmd

F32 = mybir.dt.float32
BF16 = mybir.dt.bfloat16
I32 = mybir.dt.int32
AF = mybir.ActivationFunctionType
ALU = mybir.AluOpType
AX = mybir.AxisListType

ENGS = ("pe", "act", "dve", "pool", "sp")

D = 1024
ALPHA = (2.0 * 2) ** 0.25
LN_EPS = 1e-5
NEG = -30000.0
import os as _os
SAME_ENGINE_SYNC = _os.environ.get("SES", "1") == "1"


_UID = [0]


class Buf:
    __slots__ = ("name", "t", "writer", "readers", "dma_sem", "dma_cnt", "uid")

    def __init__(self, name, t=None):
        _UID[0] += 1
        self.uid = _UID[0]
        self.name = name
        self.t = t
        self.writer = None
        self.readers = []
        self.dma_sem = None
        self.dma_cnt = 0


class Prog:
    def __init__(self, nc, stack):
        self.nc = nc
        self.stack = stack
        self.ops = {e: [] for e in ENGS}
        self.cnt = {e: 0 for e in ENGS}
        self.waited = {}
        self.sem = {}
        for e in ENGS:
            self.sem[e] = stack.enter_context(nc.semaphore("s_" + e))
        self.n_dma_sems = 0
        self.dma_bufs = []
        self.scopes = []

    def sbuf(self, name, shape, dt):
        st = self.scopes[-1] if self.scopes else self.stack
        t = st.enter_context(self.nc.sbuf_tensor("sb_" + name, list(shape), dt))
        return Buf(name, t)

    def push(self):
        self.scopes.append(ExitStack())

    def pop(self):
        self.barrier()
        self.scopes.pop().close()

    def barrier(self):
        for eng in ENGS:
            waits = []
            for e2 in ENGS:
                if self.cnt[e2] > 0:
                    self._need(eng, ("eng", e2, self.cnt[e2]), waits)
            if eng == "pe" and self.cnt["pe"] > 0:
                pass
            for b in self.dma_bufs:
                self._need(eng, ("dma", b, b.dma_cnt), waits)
            self.ops[eng].append((waits, None, None))

    def psum(self, name, shape, dt=F32):
        t = self.stack.enter_context(self.nc.psum_tensor("ps_" + name, list(shape), dt))
        return Buf(name, t)

    def _need(self, eng, dep, waits):
        if dep[0] == "eng":
            _, e, idx = dep
            if e == eng and (e == "pe" or not SAME_ENGINE_SYNC):
                return
            key = (eng, "E", e)
            val = idx
            sem = self.sem[e]
        else:
            _, b, c = dep
            key = (eng, "D", b.uid)
            val = 16 * c
            sem = b.dma_sem
        if self.waited.get(key, 0) >= val:
            return
        self.waited[key] = val
        waits.append((sem, val))

    def _deps(self, eng, reads, writes):
        waits = []
        for b in reads:
            if b.writer is not None:
                self._need(eng, b.writer, waits)
        for b in writes:
            if b.writer is not None:
                self._need(eng, b.writer, waits)
            for r in b.readers:
                self._need(eng, r, waits)
        return waits

    def op(self, eng, fn, reads=(), writes=()):
        waits = self._deps(eng, reads, writes)
        self.cnt[eng] += 1
        me = ("eng", eng, self.cnt[eng])
        for b in reads:
            b.readers.append(me)
        for b in writes:
            b.writer = me
            b.readers = []
        self.ops[eng].append((waits, fn, (self.sem[eng], 1)))

    def dma(self, eng, fn, reads=(), writes=()):
        waits = self._deps(eng, reads, writes)
        dst = writes[0]
        if dst.dma_sem is None:
            dst.dma_sem = self.stack.enter_context(self.nc.semaphore("d%d" % self.n_dma_sems))
            self.n_dma_sems += 1
            self.dma_bufs.append(dst)
        dst.dma_cnt += 1
        me = ("dma", dst, dst.dma_cnt)
        for b in reads:
            b.readers.append(me)
        for b in writes:
            b.writer = me
            b.readers = []
        self.ops[eng].append((waits, fn, (dst.dma_sem, 16)))

    def wait_all(self, eng, bufs):
        waits = []
        for b in bufs:
            if b.writer is not None:
                self._need(eng, b.writer, waits)
        self.ops[eng].append((waits, None, None))

    def emit(self):
        nc = self.nc
        ops = self.ops
        with nc.Block() as block:
            def run(e, lst):
                for waits, fn, inc in lst:
                    for sem, val in waits:
                        e.wait_ge(sem, val)
                    if fn is not None:
                        fn(e).then_inc(inc[0], inc[1])

            @block.tensor
            def _(e):
                run(e, ops["pe"])

            @block.scalar
            def _(e):
                run(e, ops["act"])

            @block.vector
            def _(e):
                run(e, ops["dve"])

            @block.gpsimd
            def _(e):
                run(e, ops["pool"])

            @block.sync
            def _(e):
                run(e, ops["sp"])


def ss(a0, n, d):
    return slice(a0, a0 + (n - 1) * d + 1, d) if d > 1 else slice(a0, a0 + n)


def bcast_row(ap_row, n):
    return ap_row.to_broadcast([128, n])


def emit_layernorm(P, z, stats, mv, rstd, xn):
    for h in range(2):
        P.op("dve", lambda e, h=h: e.bn_stats(stats.t[:, h, :], z.t[:, h * 512:(h + 1) * 512]), reads=[z], writes=[stats])
    P.op("dve", lambda e: e.bn_aggr(mv.t[:], stats.t[:]), reads=[stats], writes=[mv])
    P.op("dve", lambda e: e.tensor_scalar(rstd.t[:], mv.t[:, 1:2], LN_EPS, None, ALU.add), reads=[mv], writes=[rstd])
    P.op("act", lambda e: e.sqrt(rstd.t[:], rstd.t[:]), reads=[rstd], writes=[rstd])
    P.op("dve", lambda e: e.reciprocal(rstd.t[:], rstd.t[:]), reads=[rstd], writes=[rstd])
    P.op("dve", lambda e: e.tensor_scalar(xn.t[:], z.t[:], mv.t[:, 0:1], rstd.t[:, 0:1], ALU.subtract, ALU.mult),
         reads=[z, mv, rstd], writes=[xn])


NTOK = 4096
CAP = 1024
NEXP = 32


def build_ffn(dbg=False):
    nc = bass.Bass("TRN2", target_bir_lowering=False)
    NT = NTOK // 128
    dt_in = lambda name, shape, dt=F32: nc.dram_tensor(name, list(shape), dt, kind="ExternalInput").ap()
    x_d = dt_in("x", [NTOK, D])
    o_d = dt_in("o", [NTOK, D])
    cT_d = dt_in("cT", [128, 8])
    adaw_d = dt_in("adaw", [2, D, 3 * D])
    adab_d = dt_in("adab", [2, 3 * D])
    lng_d = dt_in("lng", [2, D])
    lnb_d = dt_in("lnb", [2, D])
    wo_d = dt_in("wo", [D, D])
    rw_d = dt_in("rw", [D, NEXP])
    rb_d = dt_in("rb", [1, NEXP])
    NE_ = (1 if dbg == 1 else 2) if dbg in (1, 2, 3) else NEXP
    if dbg == 4:
        dbg_a = nc.dram_tensor("dbg_a", [128, 4 * D], F32, kind="ExternalOutput").ap()
    if dbg in (3, 4):
        dbg_i = nc.dram_tensor("dbg_i", [128, NTOK // 128 * 2], I32, kind="ExternalOutput").ap()
    wg_d = dt_in("wg", [NE_, D, 512])
    wu_d = dt_in("wu", [NE_, D, 512])
    wd_d = dt_in("wd", [NE_, 512, D])
    idn_d = dt_in("idn", [128, 128])
    tri_d = dt_in("tri", [128, 128])
    offs_d = dt_in("offs", [1, NEXP])
    y_d = nc.dram_tensor("y", [NTOK, D], F32, kind="ExternalOutput").ap()
    XS = nc.dram_tensor("XS", [NEXP * CAP, D], BF16).ap()
    YS = nc.dram_tensor("YS", [NEXP * CAP, D], F32).ap()
    X1 = nc.dram_tensor("X1", [NTOK, D], F32, kind="ExternalOutput" if dbg else "Internal").ap()
    if dbg == 2:
        dbg_xs = nc.dram_tensor("dbg_xs", [2 * CAP, D], BF16, kind="ExternalOutput").ap()
        dbg_ys = nc.dram_tensor("dbg_ys", [2 * CAP, D], F32, kind="ExternalOutput").ap()
    if dbg in (1, 2):
        dbg_i = nc.dram_tensor("dbg_i", [128, NTOK // 128 * 2], I32, kind="ExternalOutput").ap()
        dbg_w = nc.dram_tensor("dbg_w", [128, NTOK // 128 * 2], F32, kind="ExternalOutput").ap()
        dbg_m = nc.dram_tensor("dbg_m", [128, 4 * D], F32, kind="ExternalOutput").ap()

    with ExitStack() as st:
        P = Prog(nc, st)
        sb = P.sbuf
        idn = sb("idn", [128, 128], F32)
        idnb = sb("idnb", [128, 128], BF16)
        tri = sb("tri", [128, 128], BF16)
        ones = sb("ones", [128, 128], BF16)
        offsB = sb("offsB", [128, NEXP], F32)
        rbB = sb("rbB", [128, NEXP], F32)
        rw = sb("rw", [128, 8, NEXP], F32)
        wo = sb("wo", [128, 8, D], BF16)
        g1B = sb("g1B", [128, D], F32)
        sh2B = sb("sh2B", [128, D], F32)
        sc2B = sb("sc2B", [128, D], F32)
        g2B = sb("g2B", [128, D], F32)
        lgB = [sb("lgB%d" % i, [128, D], F32) for i in range(2)]
        lbB = [sb("lbB%d" % i, [128, D], F32) for i in range(2)]
        cT = sb("cT", [128, 8], F32)
        cond = sb("cond", [128, 8], F32)
        condB = sb("condB", [128, 8, 128], F32)
        base = sb("base", [128, NEXP], F32)
        desti = sb("desti", [128, NT, 2], I32)
        wts = sb("wts", [128, NT, 2], F32)
        psA = P.psum("psA", [128, 1024], BF16)
        psY = P.psum("psY", [128, 1024], F32)
        psT = P.psum("psT", [128, 1024], F32)
        psU = P.psum("psU", [128, 1024], F32)
        psS = P.psum("psS", [128, 512], F32)

        dmaq = ["sp", "act"]
        P.dma("sp", lambda e: e.dma_start(out=idn.t[:], in_=idn_d), writes=[idn])
        P.dma("pool", lambda e: e.dma_start(out=idnb.t[:], in_=idn_d), writes=[idnb])
        P.dma("pool", lambda e: e.dma_start(out=tri.t[:], in_=tri_d), writes=[tri])
        P.dma("sp", lambda e: e.dma_start(out=offsB.t[:], in_=bcast_row(offs_d, NEXP)), writes=[offsB])
        P.dma("sp", lambda e: e.dma_start(out=rbB.t[:], in_=bcast_row(rb_d, NEXP)), writes=[rbB])
        P.dma("sp", lambda e: e.dma_start(out=rw.t[:], in_=rw_d.rearrange("(k p) n -> p k n", p=128)), writes=[rw])
        P.dma("pool", lambda e: e.dma_start(out=wo.t[:], in_=wo_d.rearrange("(k p) n -> p k n", p=128)), writes=[wo])
        for i in range(2):
            P.dma("sp", lambda e, i=i: e.dma_start(out=lgB[i].t[:], in_=bcast_row(lng_d[i:i + 1, :], D)), writes=[lgB[i]])
            P.dma("sp", lambda e, i=i: e.dma_start(out=lbB[i].t[:], in_=bcast_row(lnb_d[i:i + 1, :], D)), writes=[lbB[i]])
        P.dma("sp", lambda e: e.dma_start(out=cT.t[:], in_=cT_d), writes=[cT])
        P.op("pool", lambda e: e.memset(ones.t[:], 1.0), writes=[ones])
        P.op("pool", lambda e: e.memset(base.t[:], 0.0), writes=[base])
        P.op("act", lambda e: e.activation(cond.t[:], cT.t[:], AF.Silu), reads=[cT], writes=[cond])
        for k in range(8):
            P.op("dve", lambda e, k=k: e.tensor_copy(condB.t[:, k, :], cond.t[:, k:k + 1].to_broadcast([128, 128])),
                 reads=[cond], writes=[condB])

        P.push()
        awb = [sb("awb%d" % i, [128, 8, 512], F32) for i in range(2)]
        abb = [sb("abb%d" % i, [128, 512], F32) for i in range(2)]
        jobs = []
        for h in range(2):
            jobs.append((0, 2 * D + h * 512, g1B, h * 512, False))
        for h in range(2):
            jobs.append((1, 0 * D + h * 512, sh2B, h * 512, False))
        for h in range(2):
            jobs.append((1, 1 * D + h * 512, sc2B, h * 512, True))
        for h in range(2):
            jobs.append((1, 2 * D + h * 512, g2B, h * 512, False))
        psM = [Buf("psM0", psS.t), Buf("psM1", psU.t)]
        for j, (s, c0, dst, d0, plus1) in enumerate(jobs):
            wb = awb[j % 2]
            bb = abb[j % 2]
            pm = psM[j % 2]
            P.dma(dmaq[j % 2], lambda e, s=s, c0=c0, wb=wb: e.dma_start(
                out=wb.t[:], in_=adaw_d[s, :, c0:c0 + 512].rearrange("(k p) n -> p k n", p=128)), writes=[wb])
            P.dma("sp", lambda e, s=s, c0=c0, bb=bb: e.dma_start(
                out=bb.t[:], in_=bcast_row(adab_d[s:s + 1, c0:c0 + 512], 512)), writes=[bb])
            for k in range(8):
                P.op("pe", lambda e, k=k, wb=wb, pm=pm: e.matmul(pm.t[:, 0:512], condB.t[:, k, :], wb.t[:, k, :],
                                                                 start=(k == 0), stop=(k == 7)),
                     reads=[condB, wb], writes=[pm])
            P.op("dve", lambda e, pm=pm, bb=bb, dst=dst, d0=d0: e.tensor_tensor(
                dst.t[:, d0:d0 + 512], pm.t[:, 0:512], bb.t[:], ALU.add), reads=[pm, bb], writes=[dst])
            if plus1:
                P.op("dve", lambda e, dst=dst, d0=d0: e.tensor_scalar(
                    dst.t[:, d0:d0 + 512], dst.t[:, d0:d0 + 512], 1.0, None, ALU.add), reads=[dst], writes=[dst])

        P.pop()
        P.push()
        NB = 2
        xt = [sb("xt%d" % i, [128, D], F32) for i in range(NB)]
        ot = [sb("ot%d" % i, [128, D], F32) for i in range(NB)]
        ob = [sb("ob%d" % i, [128, D], BF16) for i in range(NB)]
        oT = [sb("oT%d" % i, [128, 8, 128], BF16) for i in range(NB)]
        t1 = [sb("t1%d" % i_, [128, D], F32) for i_ in range(2)]
        z = [sb("z%d" % i_, [128, D], F32) for i_ in range(2)]
        xn = [sb("xn%d" % i_, [128, D], F32) for i_ in range(2)]
        x1 = [sb("x1_%d" % i, [128, D], F32) for i in range(NB)]
        h2 = [sb("h2_%d" % i, [128, D], F32) for i in range(NB)]
        hb = [sb("hb%d" % i, [128, D], BF16) for i in range(NB)]
        h2T = [sb("h2T%d" % i, [128, 8, 128], F32) for i in range(NB)]
        stats = [sb("stats%d" % i_, [128, 2, 6], F32) for i_ in range(2)]
        mv = [sb("mv%d" % i_, [128, 2], F32) for i_ in range(2)]
        rstd = [sb("rstd%d" % i_, [128, 1], F32) for i_ in range(2)]
        sc = [sb("sc%d" % i_, [128, NEXP], F32) for i_ in range(2)]
        grp = [sb("grp%d" % i_, [128, NEXP], F32) for i_ in range(2)]
        m8 = [sb("m8%d" % i_, [128, 4, 8], F32) for i_ in range(2)]
        gs = [sb("gs%d" % i_, [128, 4], F32) for i_ in range(2)]
        gmax = [sb("gmax%d" % i_, [128, 1], F32) for i_ in range(2)]
        oh = [sb("oh%d" % i_, [128, 4], F32) for i_ in range(2)]
        tmp4 = [sb("tmp4%d" % i_, [128, 4], F32) for i_ in range(2)]
        thr = [sb("thr%d" % i_, [128, 1], F32) for i_ in range(2)]
        ge = [sb("ge%d" % i_, [128, NEXP], F32) for i_ in range(2)]
        sel = [sb("sel%d" % i_, [128, NEXP], F32) for i_ in range(2)]
        selb = [sb("selb%d" % i_, [128, NEXP], BF16) for i_ in range(2)]
        ws = [sb("ws%d" % i_, [128, NEXP], F32) for i_ in range(2)]
        wsum = [sb("wsum%d" % i_, [128, 1], F32) for i_ in range(2)]
        wt = [sb("wt%d" % i_, [128, NEXP], F32) for i_ in range(2)]
        dall = [sb("dall%d" % i_, [128, NEXP], F32) for i_ in range(2)]
        dhi = [sb("dhi%d" % i_, [128, 1], F32) for i_ in range(2)]
        dsum = [sb("dsum%d" % i_, [128, 1], F32) for i_ in range(2)]
        dpair = [sb("dpair%d" % i_, [128, 2], F32) for i_ in range(2)]
        eq = [sb("eq%d" % i_, [128, NEXP], F32) for i_ in range(2)]
        XSb = Buf("XS")
        X1b = Buf("X1")
        psLog = Buf("psLog", psS.t)

        def chain(t):
            b = t % NB
            yield
            r0 = t * 128
            yield
            P.dma("sp", lambda e, b=b, r0=r0: e.dma_start(out=xt[b].t[:], in_=x_d[r0:r0 + 128, :]), writes=[xt[b]])
            yield
            P.dma("act", lambda e, b=b, r0=r0: e.dma_start(out=ot[b].t[:], in_=o_d[r0:r0 + 128, :]), writes=[ot[b]])
            yield
            P.op("dve", lambda e, b=b: e.tensor_copy(ob[b].t[:], ot[b].t[:]), reads=[ot[b]], writes=[ob[b]])
            yield
            for k in range(8):
                P.op("pe", lambda e, b=b, k=k: e.transpose(psA.t[:, k * 128:(k + 1) * 128], ob[b].t[:, k * 128:(k + 1) * 128], idnb.t[:]),
                     reads=[ob[b], idnb], writes=[psA])
            P.op("act", lambda e, b=b: e.copy(oT[b].t[:].rearrange("p k m -> p (k m)"), psA.t[:]), reads=[psA], writes=[oT[b]])
            yield
            for nh in range(2):
                for k in range(8):
                    P.op("pe", lambda e, b=b, k=k, nh=nh: e.matmul(psY.t[:, nh * 512:(nh + 1) * 512], oT[b].t[:, k, :],
                                                                   wo.t[:, k, nh * 512:(nh + 1) * 512], start=(k == 0), stop=(k == 7)),
                         reads=[oT[b], wo], writes=[psY])
            for nh in range(2):
                sl = slice(nh * 512, (nh + 1) * 512)
                P.op("dve", lambda e, sl=sl: e.tensor_tensor(t1[b].t[:, sl], psY.t[:, sl], g1B.t[:, sl], ALU.mult),
                     reads=[psY, g1B], writes=[t1[b]])
            P.op("dve", lambda e, b=b: e.scalar_tensor_tensor(z[b].t[:], xt[b].t[:], ALPHA, t1[b].t[:], ALU.mult, ALU.add),
                 reads=[xt[b], t1[b]], writes=[z[b]])
            emit_layernorm(P, z[b], stats[b], mv[b], rstd[b], xn[b])
            yield
            P.op("dve", lambda e, b=b: e.tensor_tensor(x1[b].t[:], xn[b].t[:], lgB[0].t[:], ALU.mult), reads=[xn[b], lgB[0]], writes=[x1[b]])
            yield
            P.op("dve", lambda e, b=b: e.tensor_tensor(x1[b].t[:], x1[b].t[:], lbB[0].t[:], ALU.add), reads=[x1[b], lbB[0]], writes=[x1[b]])
            yield
            P.dma("sp", lambda e, b=b, r0=r0: e.dma_start(out=X1[r0:r0 + 128, :], in_=x1[b].t[:]), reads=[x1[b]], writes=[X1b])
            yield
            P.op("dve", lambda e, b=b: e.tensor_tensor(h2[b].t[:], x1[b].t[:], sc2B.t[:], ALU.mult), reads=[x1[b], sc2B], writes=[h2[b]])
            yield
            P.op("dve", lambda e, b=b: e.tensor_tensor(h2[b].t[:], h2[b].t[:], sh2B.t[:], ALU.add), reads=[h2[b], sh2B], writes=[h2[b]])
            yield
            P.op("act", lambda e, b=b: e.copy(hb[b].t[:], h2[b].t[:]), reads=[h2[b]], writes=[hb[b]])
            yield
            for k in range(8):
                P.op("pe", lambda e, b=b, k=k: e.transpose(psT.t[:, k * 128:(k + 1) * 128], h2[b].t[:, k * 128:(k + 1) * 128], idn.t[:]),
                     reads=[h2[b], idn], writes=[psT])
            P.op("act", lambda e, b=b: e.copy(h2T[b].t[:].rearrange("p k m -> p (k m)"), psT.t[:]), reads=[psT], writes=[h2T[b]])
            yield
            for k in range(8):
                P.op("pe", lambda e, b=b, k=k: e.matmul(psS.t[:, 0:NEXP], h2T[b].t[:, k, :], rw.t[:, k, :], start=(k == 0), stop=(k == 7)),
                     reads=[h2T[b], rw], writes=[psLog])
            P.op("act", lambda e: e.activation(sc[b].t[:], psS.t[:, 0:NEXP], AF.Sigmoid), reads=[psLog], writes=[sc[b]])
            yield
            P.op("dve", lambda e: e.tensor_tensor(grp[b].t[:], sc[b].t[:], rbB.t[:], ALU.add), reads=[sc[b], rbB], writes=[grp[b]])
            yield
            for g in range(4):
                P.op("dve", lambda e, g=g: e.max(out=m8[b].t[:, g, :], in_=grp[b].t[:, g * 8:(g + 1) * 8]), reads=[grp[b]], writes=[m8[b]])
            P.op("dve", lambda e: e.tensor_tensor(gs[b].t[:], m8[b].t[:, :, 0], m8[b].t[:, :, 1], ALU.add), reads=[m8[b]], writes=[gs[b]])
            yield
            P.op("dve", lambda e: e.reduce_max(gmax[b].t[:], gs[b].t[:], AX.X), reads=[gs[b]], writes=[gmax[b]])
            yield
            P.op("dve", lambda e: e.tensor_scalar(oh[b].t[:], gs[b].t[:], gmax[b].t[:, 0:1], None, ALU.is_equal), reads=[gs[b], gmax[b]], writes=[oh[b]])
            yield
            P.op("dve", lambda e: e.tensor_tensor(tmp4[b].t[:], oh[b].t[:], m8[b].t[:, :, 1], ALU.mult), reads=[oh[b], m8[b]], writes=[tmp4[b]])
            yield
            P.op("dve", lambda e: e.reduce_sum(thr[b].t[:], tmp4[b].t[:], AX.X), reads=[tmp4[b]], writes=[thr[b]])
            yield
            P.op("dve", lambda e: e.tensor_scalar(ge[b].t[:], grp[b].t[:], thr[b].t[:, 0:1], None, ALU.is_ge), reads=[grp[b], thr[b]], writes=[ge[b]])
            yield
            P.op("dve", lambda e: e.tensor_tensor(sel[b].t[:].rearrange("p (g j) -> p g j", j=8), ge[b].t[:].rearrange("p (g j) -> p g j", j=8),
                                                  oh[b].t[:].unsqueeze(2).to_broadcast([128, 4, 8]), ALU.mult), reads=[ge[b], oh[b]], writes=[sel[b]])
            P.op("dve", lambda e: e.tensor_tensor(ws[b].t[:], sc[b].t[:], sel[b].t[:], ALU.mult), reads=[sc[b], sel[b]], writes=[ws[b]])
            yield
            P.op("dve", lambda e: e.reduce_sum(wsum[b].t[:], ws[b].t[:], AX.X), reads=[ws[b]], writes=[wsum[b]])
            yield
            P.op("dve", lambda e: e.reciprocal(wsum[b].t[:], wsum[b].t[:]), reads=[wsum[b]], writes=[wsum[b]])
            yield
            P.op("dve", lambda e: e.tensor_scalar(wt[b].t[:], ws[b].t[:], wsum[b].t[:, 0:1], None, ALU.mult), reads=[ws[b], wsum[b]], writes=[wt[b]])
            yield
            P.op("dve", lambda e: e.tensor_copy(selb[b].t[:], sel[b].t[:]), reads=[sel[b]], writes=[selb[b]])
            yield
            P.op("pe", lambda e: e.matmul(psS.t[:, 64:64 + NEXP], tri.t[:], selb[b].t[:], start=True, stop=True), reads=[tri, selb[b]], writes=[psLog])
            yield
            P.op("pe", lambda e: e.matmul(psS.t[:, 128:128 + NEXP], ones.t[:], selb[b].t[:], start=True, stop=True), reads=[ones, selb[b]], writes=[psLog])
            yield
            P.op("dve", lambda e: e.tensor_tensor(dall[b].t[:], psS.t[:, 64:64 + NEXP], base.t[:], ALU.add), reads=[psLog, base], writes=[dall[b]])
            yield
            P.op("dve", lambda e: e.tensor_tensor(dall[b].t[:], dall[b].t[:], offsB.t[:], ALU.add), reads=[dall[b], offsB], writes=[dall[b]])
            yield
            P.op("dve", lambda e: e.tensor_tensor(dall[b].t[:], dall[b].t[:], sel[b].t[:], ALU.mult), reads=[dall[b], sel[b]], writes=[dall[b]])
            yield
            P.op("dve", lambda e: e.tensor_tensor(base.t[:], base.t[:], psS.t[:, 128:128 + NEXP], ALU.add), reads=[psLog, base], writes=[base])
            yield
            P.op("dve", lambda e: e.reduce_max(dhi[b].t[:], dall[b].t[:], AX.X), reads=[dall[b]], writes=[dhi[b]])
            yield
            P.op("dve", lambda e: e.reduce_sum(dsum[b].t[:], dall[b].t[:], AX.X), reads=[dall[b]], writes=[dsum[b]])
            yield
            P.op("dve", lambda e: e.tensor_scalar(dpair[b].t[:, 0:1], dhi[b].t[:], -1.0, None, ALU.add), reads=[dhi[b]], writes=[dpair[b]])
            yield
            P.op("dve", lambda e: e.scalar_tensor_tensor(dpair[b].t[:, 1:2], dsum[b].t[:], -1.0, dhi[b].t[:], ALU.add, ALU.subtract),
                 reads=[dsum[b], dhi[b]], writes=[dpair[b]])
            P.op("dve", lambda e, t=t: e.tensor_copy(desti.t[:, t, :], dpair[b].t[:]), reads=[dpair[b]], writes=[desti])
            yield
            P.op("dve", lambda e: e.tensor_scalar(eq[b].t[:], dall[b].t[:], dhi[b].t[:, 0:1], None, ALU.is_equal), reads=[dall[b], dhi[b]], writes=[eq[b]])
            yield
            P.op("dve", lambda e: e.tensor_tensor(eq[b].t[:], eq[b].t[:], wt[b].t[:], ALU.mult), reads=[eq[b], wt[b]], writes=[eq[b]])
            yield
            P.op("dve", lambda e, t=t: e.reduce_sum(wts.t[:, t, 0:1], eq[b].t[:], AX.X), reads=[eq[b]], writes=[wts])
            yield
            P.op("dve", lambda e, t=t: e.tensor_scalar(wts.t[:, t, 1:2], wts.t[:, t, 0:1], -1.0, 1.0, ALU.mult, ALU.add), reads=[wts], writes=[wts])
            yield
            for j in range(2):
                P.dma("pool", lambda e, b=b, t=t, j=j: e.indirect_dma_start(
                    out=XS[:, :], out_offset=bass.IndirectOffsetOnAxis(ap=desti.t[:, t, j:j + 1], axis=0),
                    in_=hb[b].t[:, :], in_offset=None), reads=[hb[b], desti], writes=[XSb])


        LAG = 10
        for t0_ in range(0, NT, 2):
            ga, gb = chain(t0_), chain(t0_ + 1)
            la = lb = True
            na = 0
            while la or lb:
                if la:
                    try:
                        next(ga)
                        na += 1
                    except StopIteration:
                        la = False
                if lb and (na >= LAG or not la):
                    try:
                        next(gb)
                    except StopIteration:
                        lb = False
        if dbg == 1:
            Db = Buf("dbg")
            P.dma("sp", lambda e: e.dma_start(out=dbg_i, in_=desti.t[:].rearrange("p t j -> p (t j)")), reads=[desti], writes=[Db])
            P.dma("sp", lambda e: e.dma_start(out=dbg_w, in_=wts.t[:].rearrange("p t j -> p (t j)")), reads=[wts], writes=[Db])
            for i_, tl in enumerate([g1B, sh2B, sc2B, g2B]):
                P.dma("sp", lambda e, i_=i_, tl=tl: e.dma_start(out=dbg_m[:, i_ * D:(i_ + 1) * D], in_=tl.t[:]), reads=[tl], writes=[Db])
            P.wait_all("sp", [Db, X1b])
            P.pop()
            P.emit()
            return nc
        P.pop()
        P.push()
        wgs = [sb("wgs%d" % i, [128, 8, 512], BF16) for i in range(2)]
        wus = [sb("wus%d" % i, [128, 8, 512], BF16) for i in range(2)]
        wds = [sb("wds%d" % i, [128, 4, D], BF16) for i in range(2)]
        xs_tok = [sb("xs_tok%d" % i, [128, D], BF16) for i in range(2)]
        xsT = [sb("xsT%d" % i, [128, 8, CAP], BF16) for i in range(2)]
        sg = [sb("sg%d" % i, [128, 512], F32) for i in range(2)]
        hT = [sb("hT%d" % i, [128, 4, CAP], BF16) for i in range(2)]
        ysb = [sb("ysb%d" % i, [128, D], F32) for i in range(2)]
        psG = [Buf("psG0", psT.t), Buf("psG1", psU.t)]
        YSb = Buf("YS")
        RB = CAP // 128
        nx = 0
        ny = 0
        for ex in range(NE_):
            wb = ex % 2
            P.dma("pool", lambda e, ex=ex, wb=wb: e.dma_start(out=wgs[wb].t[:], in_=wg_d[ex].rearrange("(k p) n -> p k n", p=128)), writes=[wgs[wb]])
            P.dma("pool", lambda e, ex=ex, wb=wb: e.dma_start(out=wus[wb].t[:], in_=wu_d[ex].rearrange("(k p) n -> p k n", p=128)), writes=[wus[wb]])
            P.dma("pool", lambda e, ex=ex, wb=wb: e.dma_start(out=wds[wb].t[:], in_=wd_d[ex].rearrange("(k p) n -> p k n", p=128)), writes=[wds[wb]])
            xT = xsT[wb]
            for rb in range(RB):
                xb = xs_tok[nx % 2]
                nx += 1
                r0 = ex * CAP + rb * 128
                P.dma("sp", lambda e, xb=xb, r0=r0: e.dma_start(out=xb.t[:], in_=XS[r0:r0 + 128, :]), reads=[XSb], writes=[xb])
                for k in range(8):
                    P.op("pe", lambda e, xb=xb, k=k: e.transpose(psA.t[:, k * 128:(k + 1) * 128], xb.t[:, k * 128:(k + 1) * 128], idnb.t[:]),
                         reads=[xb, idnb], writes=[psA])
                P.op("act", lambda e, xT=xT, rb=rb: e.copy(xT.t[:, :, rb * 128:(rb + 1) * 128], psA.t[:].rearrange("p (k m) -> p k m", m=128)),
                     reads=[psA], writes=[xT])
            hh = hT[wb]
            for fc in range(4):
              for hf in range(CAP // 512):
                pg = psG[(fc * (CAP // 512) + hf) % 2]
                s0 = hf * 512
                for k in range(8):
                    P.op("pe", lambda e, k=k, fc=fc, pg=pg, wb=wb, xT=xT, s0=s0: e.matmul(
                        pg.t[:, 0:512], wgs[wb].t[:, k, fc * 128:(fc + 1) * 128], xT.t[:, k, s0:s0 + 512], start=(k == 0), stop=(k == 7)),
                        reads=[wgs[wb], xT], writes=[pg])
                for k in range(8):
                    P.op("pe", lambda e, k=k, fc=fc, pg=pg, wb=wb, xT=xT, s0=s0: e.matmul(
                        pg.t[:, 512:1024], wus[wb].t[:, k, fc * 128:(fc + 1) * 128], xT.t[:, k, s0:s0 + 512], start=(k == 0), stop=(k == 7)),
                        reads=[wus[wb], xT], writes=[pg])
                s_ = sg[(fc * (CAP // 512) + hf) % 2]
                P.op("act", lambda e, pg=pg, s_=s_: e.activation(s_.t[:], pg.t[:, 0:512], AF.Silu), reads=[pg], writes=[s_])
                P.op("dve", lambda e, pg=pg, s_=s_, hh=hh, fc=fc, s0=s0: e.tensor_tensor(hh.t[:, fc, s0:s0 + 512], s_.t[:], pg.t[:, 512:1024], ALU.mult),
                     reads=[pg, s_], writes=[hh])
            for rb in range(RB):
                yb = ysb[ny % 2]
                ny += 1
                for nh in range(2):
                    for fc in range(4):
                        P.op("pe", lambda e, rb=rb, nh=nh, fc=fc, hh=hh, wb=wb: e.matmul(
                            psY.t[:, nh * 512:(nh + 1) * 512], hh.t[:, fc, rb * 128:(rb + 1) * 128],
                            wds[wb].t[:, fc, nh * 512:(nh + 1) * 512], start=(fc == 0), stop=(fc == 3)),
                            reads=[hh, wds[wb]], writes=[psY])
                P.op("act", lambda e, yb=yb: e.copy(yb.t[:], psY.t[:]), reads=[psY], writes=[yb])
                r0 = ex * CAP + rb * 128
                P.dma("sp", lambda e, yb=yb, r0=r0: e.dma_start(out=YS[r0:r0 + 128, :], in_=yb.t[:]), reads=[yb], writes=[YSb])

        if dbg == 2:
            Db = Buf("dbg")
            P.dma("sp", lambda e: e.dma_start(out=dbg_xs, in_=XS[0:2 * CAP, :]), reads=[XSb], writes=[Db])
            P.dma("sp", lambda e: e.dma_start(out=dbg_ys, in_=YS[0:2 * CAP, :]), reads=[YSb], writes=[Db])
            P.dma("sp", lambda e: e.dma_start(out=dbg_i, in_=desti.t[:].rearrange("p t j -> p (t j)")), reads=[desti], writes=[Db])
            P.wait_all("sp", [Db])
            P.pop()
            P.emit()
            return nc
        P.pop()
        P.push()
        cxt = [sb("cxt%d" % i, [128, D], F32) for i in range(2)]
        ct1 = sb("ct1", [128, D], F32)
        cz = sb("cz", [128, D], F32)
        cxn = sb("cxn", [128, D], F32)
        cstats = sb("cstats", [128, 2, 6], F32)
        cmv = sb("cmv", [128, 2], F32)
        crstd = sb("crstd", [128, 1], F32)
        yh = [sb("yh%d" % i, [128, D], F32) for i in range(2)]
        yl = [sb("yl%d" % i, [128, D], F32) for i in range(2)]
        outt = [sb("outt%d" % i, [128, D], F32) for i in range(2)]
        Yb = Buf("y")
        import os
        COMB = os.environ.get("COMB", "full")
        for t in range(NT):
            b = t % 2
            r0 = t * 128
            if COMB == "a":
                P.dma("sp", lambda e, b=b, r0=r0: e.dma_start(out=cxt[b].t[:], in_=X1[r0:r0 + 128, :]), reads=[X1b], writes=[cxt[b]])
                P.dma("sp", lambda e, b=b, r0=r0: e.dma_start(out=y_d[r0:r0 + 128, :], in_=cxt[b].t[:]), reads=[cxt[b]], writes=[Yb])
                continue
            if COMB == "none":
                continue
            P.dma("pool", lambda e, b=b, t=t: e.indirect_dma_start(
                out=yh[b].t[:, :], out_offset=None, in_=YS[:, :],
                in_offset=bass.IndirectOffsetOnAxis(ap=desti.t[:, t, 0:1], axis=0)), reads=[YSb, desti], writes=[yh[b]])
            P.dma("pool", lambda e, b=b, t=t: e.indirect_dma_start(
                out=yl[b].t[:, :], out_offset=None, in_=YS[:, :],
                in_offset=bass.IndirectOffsetOnAxis(ap=desti.t[:, t, 1:2], axis=0)), reads=[YSb, desti], writes=[yl[b]])
            P.dma("sp", lambda e, b=b, r0=r0: e.dma_start(out=cxt[b].t[:], in_=X1[r0:r0 + 128, :]), reads=[X1b], writes=[cxt[b]])
            if dbg == 4 and t == 0:
                Dbb = Buf("dbb")
                P.dma("sp", lambda e: e.dma_start(out=dbg_a[:, 0:D], in_=yh[0].t[:]), reads=[yh[0]], writes=[Dbb])
                P.dma("sp", lambda e: e.dma_start(out=dbg_a[:, D:2 * D], in_=yl[0].t[:]), reads=[yl[0]], writes=[Dbb])
                P.dma("sp", lambda e: e.dma_start(out=dbg_a[:, 2 * D:3 * D], in_=cxt[0].t[:]), reads=[cxt[0]], writes=[Dbb])
            P.op("dve", lambda e, b=b, t=t: e.tensor_scalar(yh[b].t[:], yh[b].t[:], wts.t[:, t, 0:1], None, ALU.mult), reads=[yh[b], wts], writes=[yh[b]])
            P.op("dve", lambda e, b=b, t=t: e.scalar_tensor_tensor(yh[b].t[:], yl[b].t[:], wts.t[:, t, 1:2], yh[b].t[:], ALU.mult, ALU.add),
                 reads=[yl[b], yh[b], wts], writes=[yh[b]])
            P.op("dve", lambda e, b=b: e.tensor_tensor(ct1.t[:], yh[b].t[:], g2B.t[:], ALU.mult), reads=[yh[b], g2B], writes=[ct1])
            P.op("dve", lambda e, b=b: e.scalar_tensor_tensor(cz.t[:], cxt[b].t[:], ALPHA, ct1.t[:], ALU.mult, ALU.add), reads=[cxt[b], ct1], writes=[cz])
            if dbg == 4 and t == 0:
                P.dma("sp", lambda e: e.dma_start(out=dbg_a[:, 3 * D:4 * D], in_=cz.t[:]), reads=[cz], writes=[Dbb])
            emit_layernorm(P, cz, cstats, cmv, crstd, cxn)
            P.op("dve", lambda e, b=b: e.tensor_tensor(outt[b].t[:], cxn.t[:], lgB[1].t[:], ALU.mult), reads=[cxn, lgB[1]], writes=[outt[b]])
            P.op("dve", lambda e, b=b: e.tensor_tensor(outt[b].t[:], outt[b].t[:], lbB[1].t[:], ALU.add), reads=[outt[b], lbB[1]], writes=[outt[b]])
            P.dma("sp", lambda e, b=b, r0=r0: e.dma_start(out=y_d[r0:r0 + 128, :], in_=outt[b].t[:]), reads=[outt[b]], writes=[Yb])
        if dbg in (3, 4):
            P.dma("sp", lambda e: e.dma_start(out=dbg_i, in_=desti.t[:].rearrange("p t j -> p (t j)")), reads=[desti], writes=[Yb])
        P.wait_all("sp", [Yb])
        P.pop()
        P.emit()
    return nc


_CACHE = {}


def _consts():
    idn = np.eye(128, dtype=np.float32)
    tri = np.triu(np.ones((128, 128), np.float32), 1)
    offs = (np.arange(NEXP, dtype=np.float32) * CAP + 1.0)[None, :]
    return idn, tri, offs


def run_ffn(x, o, c, ada_w_l, ada_b_l, ln_g_l, ln_b_l, w_o, router_w, router_b, wg, wu, wd):
    if "ffn" not in _CACHE:
        _CACHE["ffn"] = build_ffn()
    nc = _CACHE["ffn"]
    idn, tri, offs = _consts()
    B, S, _ = x.shape
    xf = x.reshape(B * S, D)
    of = o.reshape(B * S, D)
    in_maps = []
    for core in range(8):
        r0 = core * NTOK
        b = r0 // S
        in_maps.append({
            "x": np.ascontiguousarray(xf[r0:r0 + NTOK]), "o": np.ascontiguousarray(of[r0:r0 + NTOK]),
            "cT": np.ascontiguousarray(c[b].reshape(8, 128).T),
            "adaw": ada_w_l, "adab": ada_b_l, "lng": ln_g_l, "lnb": ln_b_l, "wo": w_o,
            "rw": router_w, "rb": router_b.reshape(1, NEXP), "wg": wg, "wu": wu, "wd": wd,
            "idn": idn, "tri": tri, "offs": offs,
        })
    res = run_bass_kernel_spmd(nc, in_maps, core_ids=list(range(8)))
    return np.concatenate([r["y"] for r in res.results], axis=0).reshape(B, S, D)


S_LEN = 16384
SPAN = 2048
TWO_PI = 6.283185307179586
PI = 3.141592653589793


def rope_inv_table():
    inv = 500000.0 ** (-np.arange(8, dtype=np.float64) * (2.0 / 16))
    t = np.zeros((128, 1), np.float32)
    for p in range(128):
        f = p % 64
        if f < 16:
            t[p, 0] = np.float32(inv[f % 8])
    return t


def emit_mod_cols(P, nc, adaw_d, adabT_d, cT_d, ncols, shp, psum_buf):
    sb = P.sbuf
    cT = sb("cT", [128, 8], F32)
    cond = sb("cond", [128, 8], F32)
    modp = sb("modp", [128, ncols // 128], F32)
    abT = sb("abT", [128, ncols // 128], F32)
    P.dma("sp", lambda e: e.dma_start(out=cT.t[:], in_=cT_d), writes=[cT])
    P.dma("sp", lambda e: e.dma_start(out=abT.t[:], in_=adabT_d), writes=[abT])
    P.op("act", lambda e: e.activation(cond.t[:], cT.t[:], AF.Silu), reads=[cT], writes=[cond])
    P.push()
    awb = [sb("awb%d" % i, [128, 8, 512], F32) for i in range(2)]
    for blk in range(ncols // 512):
        wb = awb[blk % 2]
        P.dma(["sp", "act"][blk % 2], lambda e, blk=blk, wb=wb: e.dma_start(
            out=wb.t[:], in_=adaw_d[:, blk * 512:(blk + 1) * 512].rearrange("(k p) n -> p k n", p=128)), writes=[wb])
        for j in range(4):
            for kk in range(8):
                P.op("pe", lambda e, j=j, kk=kk, wb=wb, blk=blk: e.matmul(
                    psum_buf.t[:, blk * 4 + j:blk * 4 + j + 1], wb.t[:, kk, j * 128:(j + 1) * 128], cond.t[:, kk:kk + 1],
                    start=(kk == 0), stop=(kk == 7)), reads=[wb, cond], writes=[psum_buf])
    P.op("dve", lambda e: e.tensor_tensor(modp.t[:], psum_buf.t[:, 0:ncols // 128], abT.t[:], ALU.add),
         reads=[psum_buf, abT], writes=[modp])
    P.pop()
    return modp


def emit_rope_tables(P, posB, posI, invp, negpi, Ct, St, tmp, pos_ap, n):
    C1 = 6.28125
    C2 = TWO_PI - C1
    P.dma("sp", lambda e: e.dma_start(out=posI.t[:, 0:n], in_=pos_ap.to_broadcast([128, n])), writes=[posI])
    P.op("dve", lambda e: e.tensor_copy(posB.t[:, 0:n], posI.t[:, 0:n]), reads=[posI], writes=[posB])
    for off, dst in ((0.0, St), (0.5 * PI, Ct)):
        P.op("dve", lambda e, off=off: e.tensor_scalar(tmp.t[:, 0:n], posB.t[:, 0:n], invp.t[:, 0:1], off, ALU.mult, ALU.add),
             reads=[posB, invp], writes=[tmp])
        P.op("dve", lambda e: e.tensor_scalar(dst.t[:, 0:n], tmp.t[:, 0:n], 1.0 / TWO_PI, None, ALU.mult), reads=[tmp], writes=[dst])
        P.op("dve", lambda e: e.tensor_copy(posI.t[:, 0:n], dst.t[:, 0:n]), reads=[dst], writes=[posI])
        P.op("dve", lambda e, dst=dst: e.tensor_copy(dst.t[:, 0:n], posI.t[:, 0:n]), reads=[posI], writes=[dst])
        P.op("dve", lambda e, dst=dst: e.scalar_tensor_tensor(tmp.t[:, 0:n], dst.t[:, 0:n], -C1, tmp.t[:, 0:n], ALU.mult, ALU.add),
             reads=[dst, tmp], writes=[tmp])
        P.op("dve", lambda e, dst=dst: e.scalar_tensor_tensor(tmp.t[:, 0:n], dst.t[:, 0:n], -C2, tmp.t[:, 0:n], ALU.mult, ALU.add),
             reads=[dst, tmp], writes=[tmp])
        P.op("dve", lambda e, dst=dst: e.tensor_scalar(dst.t[:, 0:n], tmp.t[:, 0:n], PI, -TWO_PI, ALU.is_gt, ALU.mult), reads=[tmp], writes=[dst])
        P.op("dve", lambda e, dst=dst: e.tensor_tensor(tmp.t[:, 0:n], tmp.t[:, 0:n], dst.t[:, 0:n], ALU.add), reads=[tmp, dst], writes=[tmp])
        P.op("dve", lambda e, dst=dst: e.tensor_scalar(dst.t[:, 0:n], tmp.t[:, 0:n], -PI, TWO_PI, ALU.is_lt, ALU.mult), reads=[tmp], writes=[dst])
        P.op("dve", lambda e, dst=dst: e.tensor_tensor(tmp.t[:, 0:n], tmp.t[:, 0:n], dst.t[:, 0:n], ALU.add), reads=[tmp, dst], writes=[tmp])
        P.op("dve", lambda e: e.tensor_scalar(tmp.t[:, 0:n], tmp.t[:, 0:n], PI, -PI, ALU.min, ALU.max), reads=[tmp], writes=[tmp])
        P.op("act", lambda e, dst=dst: e.activation(dst.t[:, 0:n], tmp.t[:, 0:n], AF.Sin), reads=[tmp], writes=[dst])


def emit_rot_weights(P, w, wr, nheads):
    P.op("pool", lambda e: e.memset(wr.t[:], 0.0), writes=[wr])
    for k in range(8):
        wv = w.t[:, k, :].rearrange("p (h e) -> p h e", e=64)
        rv = wr.t[:, k, :].rearrange("p (h e) -> p h e", e=64)
        P.op("dve", lambda e, wv=wv, rv=rv: e.tensor_scalar(rv[:, :, 0:8], wv[:, :, 8:16], -1.0, None, ALU.mult), reads=[w], writes=[wr])
        P.op("dve", lambda e, wv=wv, rv=rv: e.tensor_copy(rv[:, :, 8:16], wv[:, :, 0:8]), reads=[w], writes=[wr])


def emit_hmodT_tile(P, x_d, tok0, xt, psX, idn, modp, hT, col0, sc_off, q, width=1024):
    P.dma(q, lambda e: e.dma_start(out=xt.t[:], in_=x_d[tok0:tok0 + 128, :]), writes=[xt])
    nb_ = width // 128
    for k0 in range(0, 8, nb_):
        for kk in range(nb_):
            k = k0 + kk
            P.op("pe", lambda e, k=k, kk=kk: e.transpose(psX.t[:, kk * 128:(kk + 1) * 128], xt.t[:, k * 128:(k + 1) * 128], idn.t[:]),
                 reads=[xt, idn], writes=[psX])
        for kk in range(nb_):
            k = k0 + kk
            P.op("act", lambda e, k=k, kk=kk: e.activation(hT.t[:, k, col0:col0 + 128], psX.t[:, kk * 128:(kk + 1) * 128], AF.Identity,
                                                         bias=modp.t[:, k:k + 1], scale=modp.t[:, sc_off + k:sc_off + k + 1]),
                 reads=[psX, modp], writes=[hT])


def emit_proj_rope(P, hT, t0, n, w, wr, wc0, ps_a, ps_b, Ct, St, tcol0, tmpa, tmpb, out_ap_fn, out_buf):
    for k in range(8):
        P.op("pe", lambda e, k=k: e.matmul(ps_a.t[:, 0:n], w.t[:, k, wc0:wc0 + 128], hT.t[:, k, t0:t0 + n], start=(k == 0), stop=(k == 7)),
             reads=[w, hT], writes=[ps_a])
    for k in range(8):
        P.op("pe", lambda e, k=k: e.matmul(ps_b.t[:, 0:n], wr.t[:, k, wc0:wc0 + 128], hT.t[:, k, t0:t0 + n], start=(k == 0), stop=(k == 7)),
             reads=[wr, hT], writes=[ps_b])
    P.op("dve", lambda e: e.tensor_tensor(tmpa.t[:, 0:n], ps_a.t[:, 0:n], Ct.t[:, tcol0:tcol0 + n], ALU.mult), reads=[ps_a, Ct], writes=[tmpa])
    P.op("dve", lambda e: e.tensor_tensor(tmpb.t[:, 0:n], ps_b.t[:, 0:n], St.t[:, tcol0:tcol0 + n], ALU.mult), reads=[ps_b, St], writes=[tmpb])
    P.op("pool", lambda e: e.tensor_tensor(out_ap_fn(), tmpa.t[:, 0:n], tmpb.t[:, 0:n], ALU.add), reads=[tmpa, tmpb], writes=[out_buf])


def tri_masks():
    p = np.arange(128)[:, None]
    f = np.arange(128)[None, :]
    m0 = np.where(f <= p, 0.0, NEG).astype(np.float32)
    m1 = np.where(f >= p, 0.0, NEG).astype(np.float32)
    return np.stack([np.tile(m0, (1, 4)), np.tile(m1, (1, 4))], axis=1)


DIL = (1, 4, 16)


def build_dil():
    nc = bass.Bass("TRN2", target_bir_lowering=False)
    dt_in = lambda name, shape, dt=F32: nc.dram_tensor(name, list(shape), dt, kind="ExternalInput").ap()
    x_d = dt_in("x", [S_LEN, D])
    cT_d = dt_in("cT", [128, 8])
    adaw_d = dt_in("adaw", [D, 2 * D])
    adabT_d = dt_in("adabT", [128, 16])
    wq_d = dt_in("wq", [D, 256])
    wk_d = dt_in("wk", [3, D, 64])
    wv_d = dt_in("wv", [3, D, 64])
    pos_d = dt_in("pos", [1, S_LEN], I32)
    inv_d = dt_in("inv", [128, 1])
    idn_d = dt_in("idn", [128, 128])
    msk_d = dt_in("msk", [128, 2, 512])
    o_d = nc.dram_tensor("o", [S_LEN, 256], F32, kind="ExternalOutput").ap()
    OP = [nc.dram_tensor("OP%d" % p, [S_LEN, 260], F32).ap() for p in range(3)]

    with ExitStack() as st:
        P = Prog(nc, st)
        sb = P.sbuf
        idn = sb("idn", [128, 128], F32)
        idnb = sb("idnb", [128, 128], BF16)
        msk = sb("msk", [128, 2, 512], BF16)
        invp = sb("invp", [128, 1], F32)
        negpi = sb("negpi", [128, 1], F32)
        wq = sb("wq", [128, 8, 256], BF16)
        wqr = sb("wqr", [128, 8, 256], BF16)
        wk = [sb("wk%d" % p, [128, 8, 128], BF16) for p in range(3)]
        wkr = [sb("wkr%d" % p, [128, 8, 128], BF16) for p in range(3)]
        wv = [sb("wv%d" % p, [128, 8, 64], BF16) for p in range(3)]
        ps = [P.psum("pb%d" % i, [128, 512], F32) for i in range(6)]
        psX = P.psum("psX", [128, 1024], F32)

        P.dma("sp", lambda e: e.dma_start(out=idn.t[:], in_=idn_d), writes=[idn])
        P.dma("pool", lambda e: e.dma_start(out=idnb.t[:], in_=idn_d), writes=[idnb])
        P.dma("pool", lambda e: e.dma_start(out=msk.t[:], in_=msk_d), writes=[msk])
        P.dma("sp", lambda e: e.dma_start(out=invp.t[:], in_=inv_d), writes=[invp])
        P.op("pool", lambda e: e.memset(negpi.t[:], -PI), writes=[negpi])
        P.dma("pool", lambda e: e.dma_start(out=wq.t[:], in_=wq_d.rearrange("(k p) n -> p k n", p=128)), writes=[wq])
        for p in range(3):
            for h in range(2):
                P.dma("pool", lambda e, p=p, h=h: e.dma_start(out=wk[p].t[:, :, h * 64:(h + 1) * 64],
                                                              in_=wk_d[p].rearrange("(k p) n -> p k n", p=128)), writes=[wk[p]])
            P.dma("pool", lambda e, p=p: e.dma_start(out=wv[p].t[:], in_=wv_d[p].rearrange("(k p) n -> p k n", p=128)), writes=[wv[p]])
        emit_rot_weights(P, wq, wqr, 4)
        for p in range(3):
            emit_rot_weights(P, wk[p], wkr[p], 2)
        modp = emit_mod_cols(P, nc, adaw_d, adabT_d, cT_d, 2 * D, None, ps[0])
        P.op("dve", lambda e: e.tensor_scalar(modp.t[:, 8:16], modp.t[:, 8:16], 1.0, None, ALU.add), reads=[modp], writes=[modp])

        NSP = S_LEN // SPAN
        hT = sb("hT", [128, 8, SPAN], BF16)
        xts = [sb("xts%d" % i, [128, D], F32) for i in range(2)]
        qT = [sb("qT%d" % i, [128, 2, SPAN], BF16) for i in range(2)]
        kT = [[sb("kT%d_%d" % (p, s_), [128, SPAN], BF16) for s_ in range(2)] for p in range(3)]
        V = [[sb("V%d_%d" % (p, s_), [128, 16, 80], BF16) for s_ in range(2)] for p in range(3)]
        posI = sb("posI", [128, SPAN], I32)
        posB = sb("posB", [128, SPAN], F32)
        Ct = sb("Ct", [128, SPAN], F32)
        St = sb("St", [128, SPAN], F32)
        tmpT = sb("tmpT", [128, SPAN], F32)
        tmpa = sb("tmpa", [128, 512], F32)
        tmpb = sb("tmpb", [128, 512], F32)
        PT = [sb("PT%d" % i, [128, 512], BF16) for i in range(6)]
        accs = [sb("accs%d" % i, [128, 260], F32) for i in range(3)]
        for p in range(3):
            for s_ in range(2):
                P.op("pool", lambda e, p=p, s_=s_: e.memset(V[p][s_].t[:, :, 64:65], 1.0), writes=[V[p][s_]])
        OPb = [Buf("OP%d" % p) for p in range(3)]
        import os
        STG = int(os.environ.get("DILSTAGE", "9"))
        ATT = int(os.environ.get("DILATT", "9"))
        NSP = int(os.environ.get("DILNSP", str(NSP)))
        cntd = {"PT": 0, "acc": 0}
        for s in range(NSP):
            sl = s % 2
            tok0 = s * SPAN
            for ti in range(16):
                emit_hmodT_tile(P, x_d, tok0 + ti * 128, xts[ti % 2], psX, idn, modp, hT, ti * 128, 8, ["sp", "act"][ti % 2])
            if STG < 2:
                continue
            emit_rope_tables(P, posB, posI, invp, negpi, Ct, St, tmpT, pos_d[0:1, tok0:tok0 + SPAN], SPAN)
            if STG < 3:
                continue
            for qc in range(2):
                for tg in range(4):
                    emit_proj_rope(P, hT, tg * 512, 512, wq, wqr, qc * 128, ps[0], ps[1], Ct, St, tg * 512, tmpa, tmpb,
                                   lambda qc=qc, tg=tg, sl=sl: qT[sl].t[:, qc, tg * 512:(tg + 1) * 512], qT[sl])
            for p in range(3):
                for tg in range(4):
                    emit_proj_rope(P, hT, tg * 512, 512, wk[p], wkr[p], 0, ps[0], ps[1], Ct, St, tg * 512, tmpa, tmpb,
                                   lambda p=p, tg=tg, sl=sl: kT[p][sl].t[:, tg * 512:(tg + 1) * 512], kT[p][sl])
            if STG < 4:
                continue
            for p, d in enumerate(DIL):
                ncb = 16 // d
                for r in range(d):
                    for c in range(ncb):
                        idx = r * ncb + c
                        a0 = r + d * 128 * c
                        pv = ps[2 + idx % 2]
                        for k in range(8):
                            P.op("pe", lambda e, k=k, a0=a0, d=d, p=p, pv=pv: e.matmul(
                                pv.t[:, 0:64], hT.t[:, k, ss(a0, 128, d)], wv[p].t[:, k, :],
                                start=(k == 0), stop=(k == 7)), reads=[hT, wv[p]], writes=[pv])
                        P.op("act", lambda e, p=p, sl=sl, idx=idx, pv=pv: e.copy(V[p][sl].t[:, idx, 0:64], pv.t[:, 0:64]),
                             reads=[pv], writes=[V[p][sl]])
            if STG < 5:
                continue
            tiles = []
            for p, d in enumerate(DIL):
                ncb = 16 // d
                for r in range(d):
                    for j in range(ncb):
                        chunks = [(j - 1, 0), (j, 1)]
                        chunks = [(kb, mc) for kb, mc in chunks if not (s == 0 and kb < 0)]
                        tiles.append((p, d, ncb, r, j, chunks))

            def stage_a(ti):
                p, d, ncb, r, j, chunks = tiles[ti]
                pts = []
                for ci, (kb, mc) in enumerate(chunks):
                    ksl = sl if kb >= 0 else 1 - sl
                    kbb = kb if kb >= 0 else ncb - 1
                    ka0 = r + d * 128 * kbb
                    qa0 = r + d * 128 * j
                    Sp = ps[(ti % 2) * 2 + ci]
                    for h in range(4):
                        qc, hf = h // 2, h % 2
                        rows = slice(hf * 64, (hf + 1) * 64)
                        P.op("pe", lambda e, h=h, qc=qc, rows=rows, p=p, ksl=ksl, ka0=ka0, qa0=qa0, d=d, Sp=Sp, sl=sl: e.matmul(
                            Sp.t[:, h * 128:(h + 1) * 128],
                            kT[p][ksl].t[rows, ss(ka0, 128, d)],
                            qT[sl].t[rows, qc, ss(qa0, 128, d)],
                            start=True, stop=False), reads=[kT[p][ksl], qT[sl]], writes=[Sp])
                        P.op("pe", lambda e, h=h, mc=mc, Sp=Sp: e.matmul(Sp.t[:, h * 128:(h + 1) * 128], idnb.t[:], msk.t[:, mc, 0:128],
                                                                      start=False, stop=True), reads=[idnb, msk], writes=[Sp])
                    pt = PT[cntd["PT"] % 6]
                    cntd["PT"] += 1
                    P.op("act", lambda e, pt=pt, Sp=Sp: e.activation(pt.t[:], Sp.t[:, 0:512], AF.Exp, scale=0.125), reads=[Sp], writes=[pt])
                    pts.append((pt, ksl, r * ncb + kbb))
                return pts

            def stage_b(ti, pts):
                p, d, ncb, r, j, chunks = tiles[ti]
                acc = ps[4 + ti % 2]
                for h in range(4):
                    for ci, (pt, ksl, vidx) in enumerate(pts):
                        P.op("pe", lambda e, h=h, pt=pt, p=p, ksl=ksl, vidx=vidx, acc=acc, ci=ci, nch=len(pts): e.matmul(
                            acc.t[:, h * 80:h * 80 + 65], pt.t[:, h * 128:(h + 1) * 128], V[p][ksl].t[:, vidx, 0:65],
                            start=(ci == 0), stop=(ci == nch - 1)), reads=[pt, V[p][ksl]], writes=[acc])
                ab = accs[cntd["acc"] % 3]
                cntd["acc"] += 1
                P.op("dve", lambda e, ab=ab, acc=acc: e.tensor_copy(ab.t[:].rearrange("p (h e) -> p h e", e=65), acc.t[:, 0:320].rearrange("p (h e) -> p h e", e=80)[:, :, 0:65]), reads=[acc], writes=[ab])
                g0 = tok0 + r + d * 128 * j
                P.dma("sp", lambda e, ab=ab, p=p, g0=g0, d=d: e.dma_start(out=OP[p][ss(g0, 128, d), :], in_=ab.t[:]), reads=[ab], writes=[OPb[p]])

            nxt = stage_a(0)
            for ti in range(len(tiles)):
                cur = nxt
                if ti + 1 < len(tiles):
                    nxt = stage_a(ti + 1)
                stage_b(ti, cur)
        P.barrier()
        if STG < 6:
            P.emit()
            return nc
        ld = [[sb("ld%d_%d" % (p, i), [128, 260], F32) for i in range(2)] for p in range(3)]
        rl = sb("rl", [128, 4], F32)
        ot = [sb("otl%d" % i, [128, 256], F32) for i in range(2)]
        Ob = Buf("o")
        for T in range(S_LEN // 128):
            b = T % 2
            for p in range(3):
                P.dma(["sp", "act", "sp"][p], lambda e, p=p, b=b, T=T: e.dma_start(out=ld[p][b].t[:], in_=OP[p][T * 128:(T + 1) * 128, :]),
                      reads=[OPb[p]], writes=[ld[p][b]])
            P.op("dve", lambda e, b=b: e.tensor_tensor(ld[0][b].t[:], ld[0][b].t[:], ld[1][b].t[:], ALU.add), reads=[ld[0][b], ld[1][b]], writes=[ld[0][b]])
            P.op("dve", lambda e, b=b: e.tensor_tensor(ld[0][b].t[:], ld[0][b].t[:], ld[2][b].t[:], ALU.add), reads=[ld[0][b], ld[2][b]], writes=[ld[0][b]])
            a3 = ld[0][b].t[:].rearrange("p (h e) -> p h e", e=65)
            P.op("dve", lambda e, a3=a3: e.reciprocal(rl.t[:], a3[:, :, 64]), reads=[ld[0][b]], writes=[rl])
            P.op("dve", lambda e, a3=a3, b=b: e.tensor_tensor(ot[b].t[:].rearrange("p (h e) -> p h e", e=64), a3[:, :, 0:64],
                                                             rl.t[:].unsqueeze(2).to_broadcast([128, 4, 64]), ALU.mult),
                 reads=[ld[0][b], rl], writes=[ot[b]])
            P.dma("sp", lambda e, b=b, T=T: e.dma_start(out=o_d[T * 128:(T + 1) * 128, :], in_=ot[b].t[:]), reads=[ot[b]], writes=[Ob])
        P.wait_all("sp", [Ob])
        P.emit()
    return nc


def run_dil(x, c, positions, ada_w_s, ada_b_s, w_in):
    if "dil" not in _CACHE:
        _CACHE["dil"] = build_dil()
    nc = _CACHE["dil"]
    idn = np.eye(128, dtype=np.float32)
    inv = rope_inv_table()
    msk = tri_masks()
    in_maps = []
    for core in range(8):
        b, g = core // 4, core % 4
        wk = np.stack([w_in[:, D + p * 512 + g * 64: D + p * 512 + g * 64 + 64] for p in range(3)])
        wv = np.stack([w_in[:, D + p * 512 + 256 + g * 64: D + p * 512 + 256 + g * 64 + 64] for p in range(3)])
        in_maps.append({
            "x": x[b], "cT": np.ascontiguousarray(c[b].reshape(8, 128).T),
            "adaw": np.ascontiguousarray(ada_w_s[:, 0:2 * D]), "adabT": np.ascontiguousarray(ada_b_s[0:2 * D].reshape(16, 128).T),
            "wq": np.ascontiguousarray(w_in[:, g * 256:(g + 1) * 256]), "wk": np.ascontiguousarray(wk), "wv": np.ascontiguousarray(wv),
            "pos": np.ascontiguousarray(positions[b:b + 1]), "inv": inv, "idn": idn, "msk": msk,
        })
    res = run_bass_kernel_spmd(nc, in_maps, core_ids=list(range(8)))
    B = x.shape[0]
    o = np.zeros((B, S_LEN, D), np.float32)
    for core in range(8):
        b, g = core // 4, core % 4
        o[b, :, g * 256:(g + 1) * 256] = res.results[core]["o"]
    return o


QG = 512
NG = S_LEN // QG
NCMP = 1023


def nsa_masks():
    p = np.arange(128)[:, None]
    f = np.arange(512)[None, :]
    ms = []
    for jj in range(4):
        ms.append(np.where(f - p - 128 * jj >= 0, 0.0, NEG))
    for c in range(4):
        ms.append(np.where(f - p < 128 * c, 0.0, NEG))
    for m in range(5):
        ms.append(np.where(f - 16 * p + 512 * m - 31 >= 0, 0.0, NEG))
    return np.stack(ms, axis=1).astype(np.float32)


def nsa_indc():
    t = np.zeros((128, 64, 128), np.float32)
    for jm in range(64):
        t[2 * jm, jm, 0:64] = 1.0
        t[2 * jm + 1, jm, 64:128] = 1.0
    return t


def nsa_vc_const():
    t = np.zeros((1024, 257), np.float32)
    t[:, 0] = 1.0
    for s_ in range(256):
        for n in range(max(0, 4 * s_ - 1), min(NCMP, 4 * s_ + 4)):
            t[n, 1 + s_] = 1.0
    return t


def nsa_forced():
    t = np.zeros((128, 3), np.float32)
    for p in range(128):
        c = p // 64
        for x in (-1, 0, 1):
            if x == c or x == c - 1:
                t[p, x + 1] = 1.0e4
    return t


def build_nsa():
    nc = bass.Bass("TRN2", target_bir_lowering=False)
    dt_in = lambda name, shape, dt=F32: nc.dram_tensor(name, list(shape), dt, kind="ExternalInput").ap()
    x_d = dt_in("x", [S_LEN, D])
    cT_d = dt_in("cT", [128, 8])
    adaw_d = dt_in("adaw", [D, 2 * D])
    adabT_d = dt_in("adabT", [128, 16])
    wq_d = dt_in("wq", [D, 256])
    wkv_d = dt_in("wkv", [6, D, 64])
    wgt_d = dt_in("wgt", [D, 12])
    pos_d = dt_in("pos", [1, S_LEN], I32)
    posc_d = dt_in("posc", [1, 1024], I32)
    inv_d = dt_in("inv", [128, 1])
    idn_d = dt_in("idn", [128, 128])
    msk_d = dt_in("msk", [128, 13, 512])
    gp_d = dt_in("gp", [64, S_LEN])
    vcc_d = dt_in("vcc", [1024, 257])
    frc_d = dt_in("frc", [128, 3])
    w1_d = dt_in("w1", [2, 2048, 256])
    w2_d = dt_in("w2", [2, 256, 64])
    cpos_d = dt_in("cposT", [128, 32])
    o_d = nc.dram_tensor("o", [S_LEN, 256], F32, kind="ExternalOutput").ap()

    with ExitStack() as st:
        P = Prog(nc, st)
        sb = P.sbuf
        idn = sb("idn", [128, 128], F32)
        idnb = sb("idnb", [128, 128], BF16)
        msk = sb("msk", [128, 13, 512], BF16)
        frc = sb("frc", [128, 3], F32)
        invp = sb("invp", [128, 1], F32)
        wqh = [sb("wqh%d" % h_, [128, 8, 128], BF16) for h_ in range(4)]
        wqhr = [sb("wqhr%d" % h_, [128, 8, 128], BF16) for h_ in range(4)]
        wks = sb("wks", [128, 8, 128], BF16)
        wksr = sb("wksr", [128, 8, 128], BF16)
        wkw = sb("wkw", [128, 8, 128], BF16)
        wkwr = sb("wkwr", [128, 8, 128], BF16)
        wkvc = sb("wkvc", [128, 8, 128], BF16)
        wvs = sb("wvs", [128, 8, 64], BF16)
        wvw = sb("wvw", [128, 8, 64], BF16)
        wgt = sb("wgt", [128, 8, 12], BF16)
        kselT = sb("kselT", [128, S_LEN], BF16)
        Vsel = sb("Vsel", [128, 128, 80], BF16)
        kcT = sb("kcT", [128, 1024], BF16)
        VC = sb("VC", [128, 8, 336], BF16)
        ps = [P.psum("pb%d" % i, [128, 512], F32) for i in range(6)]
        psXb = P.psum("psX", [128, 512], F32)
        psS3 = P.psum("psS3", [128, 512], F32)
        spr = [ps[0], ps[1], psS3]

        def ld(q, dst, src, ap=None):
            P.dma(q, lambda e: e.dma_start(out=dst.t[:] if ap is None else ap, in_=src), writes=[dst])
        ld("sp", idn, idn_d)
        ld("pool", idnb, idn_d)
        ld("pool", msk, msk_d)
        ld("sp", frc, frc_d)
        ld("sp", invp, inv_d)
        kp = lambda a: a.rearrange("(k p) n -> p k n", p=128)
        for h_ in range(4):
            P.op("pool", lambda e, h_=h_: e.memset(wqh[h_].t[:], 0.0), writes=[wqh[h_]])
            ld("pool", wqh[h_], kp(wq_d[:, h_ * 64:(h_ + 1) * 64]), wqh[h_].t[:, :, 0:64])
        for h in range(2):
            ld("pool", wks, kp(wkv_d[2]), wks.t[:, :, h * 64:(h + 1) * 64])
            ld("pool", wkw, kp(wkv_d[4]), wkw.t[:, :, h * 64:(h + 1) * 64])
        ld("pool", wkvc, kp(wkv_d[0]), wkvc.t[:, :, 0:64])
        ld("pool", wkvc, kp(wkv_d[1]), wkvc.t[:, :, 64:128])
        ld("pool", wvs, kp(wkv_d[3]))
        ld("pool", wvw, kp(wkv_d[5]))
        ld("pool", wgt, kp(wgt_d))
        for h_ in range(4):
            emit_rot_weights(P, wqh[h_], wqhr[h_], 2)
        emit_rot_weights(P, wks, wksr, 2)
        emit_rot_weights(P, wkw, wkwr, 2)
        P.op("pool", lambda e: e.memset(Vsel.t[:, :, 64:65], 1.0), writes=[Vsel])
        for c in range(8):
            P.dma("pool", lambda e, c=c: e.dma_start(out=VC.t[:, c, 64:321], in_=vcc_d[c * 128:(c + 1) * 128, :]), writes=[VC])
        modp = emit_mod_cols(P, nc, adaw_d, adabT_d, cT_d, 2 * D, None, ps[0])
        P.op("dve", lambda e: e.tensor_scalar(modp.t[:, 8:16], modp.t[:, 8:16], 1.0, None, ALU.add), reads=[modp], writes=[modp])

        hT = sb("hT", [128, 8, QG], BF16)
        xts = [sb("xts%d" % i, [128, D], F32) for i in range(2)]
        posI = sb("posI", [128, 512], I32)
        posB = sb("posB", [128, 512], F32)
        Ct = sb("Ct", [128, 512], F32)
        St = sb("St", [128, 512], F32)
        tmpT = sb("tmpT", [128, 512], F32)
        tmpa = sb("tmpa", [128, 512], F32)
        tmpb = sb("tmpb", [128, 512], F32)

        import os
        NST = int(os.environ.get("NSA_STAGE", "9"))
        if NST < 2:
            P.barrier(); P.emit(); return nc
        P.push()
        kvcT = sb("kvcT", [128, S_LEN], BF16)
        for G in range(NG):
            t0 = G * QG
            for ti in range(4):
                emit_hmodT_tile(P, x_d, t0 + ti * 128, xts[ti % 2], psXb, idn, modp, hT, ti * 128, 8, ["sp", "act"][ti % 2], width=512)
            emit_rope_tables(P, posB, posI, invp, None, Ct, St, tmpT, pos_d[0:1, t0:t0 + QG], QG)
            emit_proj_rope(P, hT, 0, QG, wks, wksr, 0, ps[0], ps[1], Ct, St, 0, tmpa, tmpb,
                           lambda t0=t0: kselT.t[:, t0:t0 + QG], kselT)
            for k in range(8):
                P.op("pe", lambda e, k=k: e.matmul(ps[2].t[:, 0:QG], wkvc.t[:, k, :], hT.t[:, k, :], start=(k == 0), stop=(k == 7)),
                     reads=[wkvc, hT], writes=[ps[2]])
            P.op("act", lambda e, t0=t0: e.copy(kvcT.t[:, t0:t0 + QG], ps[2].t[:, 0:QG]), reads=[ps[2]], writes=[kvcT])
            for ti in range(4):
                pv = ps[3 + ti % 2]
                for k in range(8):
                    P.op("pe", lambda e, k=k, ti=ti, pv=pv: e.matmul(pv.t[:, 0:64], hT.t[:, k, ti * 128:(ti + 1) * 128], wvs.t[:, k, :],
                                                                     start=(k == 0), stop=(k == 7)), reads=[hT, wvs], writes=[pv])
                P.op("act", lambda e, ti=ti, G=G, pv=pv: e.copy(Vsel.t[:, G * 4 + ti, 0:64], pv.t[:, 0:64]), reads=[pv], writes=[Vsel])

        P.dma("pool", lambda e: e.dma_start(out=kselT.t[64:128, :], in_=gp_d), writes=[kselT])
        if NST < 3:
            P.barrier(); P.emit(); return nc
        w1 = sb("w1", [128, 32, 256], BF16)
        w2k = sb("w2k", [128, 2, 128], BF16)
        w2kr = sb("w2kr", [128, 2, 128], BF16)
        w2v = sb("w2v", [128, 2, 64], BF16)
        cposT = sb("cposT", [128, 32], BF16)
        cbias = sb("cbias", [128, 4], F32)
        hid = [[sb("hid%d_%d" % (kv, hc), [128, 1024], BF16) for hc in range(2)] for kv in range(2)]
        xg = sb("xg", [128, 512], F32)
        ug = sb("ug", [128, 512], F32)
        for kv in range(2):
            P.dma("pool", lambda e, kv=kv: e.dma_start(out=w1.t[kv * 64:(kv + 1) * 64, :, :], in_=w1_d[kv].rearrange("(l e) h -> e l h", e=64)), writes=[w1])
        for h in range(2):
            P.dma("pool", lambda e, h=h: e.dma_start(out=w2k.t[:, :, h * 64:(h + 1) * 64], in_=w2_d[0].rearrange("(k p) n -> p k n", p=128)), writes=[w2k])
        P.dma("pool", lambda e: e.dma_start(out=w2v.t[:], in_=w2_d[1].rearrange("(k p) n -> p k n", p=128)), writes=[w2v])
        P.dma("pool", lambda e: e.dma_start(out=cposT.t[:], in_=cpos_d), writes=[cposT])
        P.op("pool", lambda e: e.memset(w2kr.t[:], 0.0), writes=[w2kr])
        for k in range(2):
            wv_ = w2k.t[:, k, :].rearrange("p (h e) -> p h e", e=64)
            rv_ = w2kr.t[:, k, :].rearrange("p (h e) -> p h e", e=64)
            P.op("dve", lambda e, wv_=wv_, rv_=rv_: e.tensor_scalar(rv_[:, :, 0:8], wv_[:, :, 8:16], -1.0, None, ALU.mult), reads=[w2k], writes=[w2kr])
            P.op("dve", lambda e, wv_=wv_, rv_=rv_: e.tensor_copy(rv_[:, :, 8:16], wv_[:, :, 0:8]), reads=[w2k], writes=[w2kr])
        for kv in range(2):
            for hc in range(2):
                P.op("pool", lambda e, kv=kv, hc=hc: e.memset(hid[kv][hc].t[:], 0.0), writes=[hid[kv][hc]])
        for kv in range(2):
            rows = slice(kv * 64, (kv + 1) * 64)
            for hc in range(2):
                col = kv * 2 + hc
                for l in range(32):
                    P.op("pe", lambda e, rows=rows, hc=hc, l=l, col=col: e.matmul(
                        ps[0].t[:, col:col + 1], w1.t[rows, l, hc * 128:(hc + 1) * 128], cposT.t[rows, l:l + 1],
                        start=(l == 0), stop=(l == 31)), reads=[w1, cposT], writes=[ps[0]])
        P.op("dve", lambda e: e.tensor_copy(cbias.t[:], ps[0].t[:, 0:4]), reads=[ps[0]], writes=[cbias])
        for kv in range(2):
            rows = slice(kv * 64, (kv + 1) * 64)
            for hc in range(2):
                col = kv * 2 + hc
                for gi in range(2):
                    n0 = gi * 512
                    nn = 512 if gi == 0 else 511
                    pz = ps[1 + (hc * 2 + gi) % 2]
                    for l in range(32):
                        P.op("pe", lambda e, rows=rows, hc=hc, l=l, n0=n0, nn=nn, pz=pz: e.matmul(
                            pz.t[:, 0:nn], w1.t[rows, l, hc * 128:(hc + 1) * 128], kvcT.t[rows, ss(16 * n0 + l, nn, 16)],
                            start=(l == 0), stop=(l == 31)), reads=[w1, kvcT], writes=[pz])
                    P.op("act", lambda e, pz=pz, nn=nn, col=col: e.activation(xg.t[:, 0:nn], pz.t[:, 0:nn], AF.Identity, bias=cbias.t[:, col:col + 1]),
                         reads=[pz, cbias], writes=[xg])
                    P.op("dve", lambda e, nn=nn: e.tensor_tensor(ug.t[:, 0:nn], xg.t[:, 0:nn], xg.t[:, 0:nn], ALU.mult), reads=[xg], writes=[ug])
                    P.op("dve", lambda e, nn=nn: e.tensor_scalar(ug.t[:, 0:nn], ug.t[:, 0:nn], 0.044715, 1.0, ALU.mult, ALU.add), reads=[ug], writes=[ug])
                    P.op("dve", lambda e, nn=nn: e.tensor_tensor(ug.t[:, 0:nn], ug.t[:, 0:nn], xg.t[:, 0:nn], ALU.mult), reads=[ug, xg], writes=[ug])
                    P.op("act", lambda e, nn=nn: e.activation(ug.t[:, 0:nn], ug.t[:, 0:nn], AF.Sigmoid, scale=1.5957691216057308), reads=[ug], writes=[ug])
                    P.op("dve", lambda e, nn=nn, kv=kv, hc=hc, n0=n0: e.tensor_tensor(hid[kv][hc].t[:, n0:n0 + nn], ug.t[:, 0:nn], xg.t[:, 0:nn], ALU.mult),
                         reads=[ug, xg], writes=[hid[kv][hc]])
        for gi in range(2):
            n0 = gi * 512
            emit_rope_tables(P, posB, posI, invp, None, Ct, St, tmpT, posc_d[0:1, n0:n0 + 512], 512)
            for hc in range(2):
                P.op("pe", lambda e, hc=hc, n0=n0: e.matmul(ps[0].t[:, 0:512], w2k.t[:, hc, :], hid[0][hc].t[:, n0:n0 + 512], start=(hc == 0), stop=(hc == 1)),
                     reads=[w2k, hid[0][hc]], writes=[ps[0]])
            for hc in range(2):
                P.op("pe", lambda e, hc=hc, n0=n0: e.matmul(ps[1].t[:, 0:512], w2kr.t[:, hc, :], hid[0][hc].t[:, n0:n0 + 512], start=(hc == 0), stop=(hc == 1)),
                     reads=[w2kr, hid[0][hc]], writes=[ps[1]])
            P.op("dve", lambda e, n0=n0: e.tensor_tensor(tmpa.t[:], ps[0].t[:, 0:512], Ct.t[:, 0:512], ALU.mult), reads=[ps[0], Ct], writes=[tmpa])
            P.op("dve", lambda e, n0=n0: e.tensor_tensor(tmpb.t[:], ps[1].t[:, 0:512], St.t[:, 0:512], ALU.mult), reads=[ps[1], St], writes=[tmpb])
            P.op("pool", lambda e, n0=n0: e.tensor_tensor(kcT.t[:, n0:n0 + 512], tmpa.t[:], tmpb.t[:], ALU.add), reads=[tmpa, tmpb], writes=[kcT])
        for c in range(8):
            pv = ps[2 + c % 2]
            for hc in range(2):
                P.op("pe", lambda e, hc=hc, c=c, pv=pv: e.matmul(pv.t[:, 0:64], hid[1][hc].t[:, c * 128:(c + 1) * 128], w2v.t[:, hc, :],
                                                                 start=(hc == 0), stop=(hc == 1)), reads=[hid[1][hc], w2v], writes=[pv])
            P.op("act", lambda e, c=c, pv=pv: e.copy(VC.t[:, c, 0:64], pv.t[:, 0:64]), reads=[pv], writes=[VC])
        P.pop()
        if NST < 4:
            P.barrier(); P.emit(); return nc

        R = [[sb("R%d_%d" % (h_, w_), [128, QG], BF16) for w_ in range(4)] for h_ in range(4)]
        nbTw = [sb("nbTw%d" % w_, [128, QG], BF16) for w_ in range(4)]
        nbp = sb("nbp", [128, 320], F32)
        P.op("pool", lambda e: e.memset(nbp.t[:], 0.0), writes=[nbp])
        kwT = [sb("kwT%d" % i, [128, QG], BF16) for i in range(2)]
        Vw = sb("Vw", [128, 8, 80], BF16)
        gts = sb("gts", [128, 4, 12], F32)
        PT = [sb("PT%d" % i, [128, 512], BF16) for i in range(4)]
        impw = sb("impw", [128, 256], F32)
        m8a = sb("m8a", [128, 8], F32)
        m8b = sb("m8b", [128, 8], F32)
        P.op("pool", lambda e: e.memset(Vw.t[:, :, 64:65], 1.0), writes=[Vw])
        Ob = Buf("o")
        acc = [ps[2], ps[3], ps[4], ps[5]]
        psW = acc
        cnt = {"S": 0, "PT": 0, "first": [True] * 4}

        def attend(h, chunks, ncols, G):
            qc, hf = h // 2, h % 2
            rows = slice(hf * 64, (hf + 1) * 64)
            first = {s_: True for s_ in range(4)}
            last_idx = {}
            for ci, ch in enumerate(chunks):
                for s_ in range(ch[3], ch[4] + 1):
                    last_idx[s_] = ci
            def qk(ci):
                kfn, masks, vfn, slo, shi = chunks[ci]
                Sp = spr[cnt["S"] % 3]
                cnt["S"] += 1
                P.op("pe", lambda e, kfn=kfn, Sp=Sp, nm0=len(masks): e.matmul(Sp.t[:, 0:QG], kfn(rows)[0], kfn(rows)[1], start=True, stop=(nm0 == 0)),
                     reads=[kselT, kcT, kwT[0], kwT[1]] + R[h], writes=[Sp])
                for mi, (lf, rf) in enumerate(masks):
                    P.op("pe", lambda e, lf=lf, rf=rf, Sp=Sp, mi=mi, nm=len(masks): e.matmul(Sp.t[:, 0:QG], lf(), rf(), start=False, stop=(mi == nm - 1)),
                         reads=[idnb, msk], writes=[Sp])
                return Sp

            def ex(ci, Sp):
                pt = PT[cnt["PT"] % 4]
                cnt["PT"] += 1
                P.op("act", lambda e, pt=pt, Sp=Sp: e.activation(pt.t[:], Sp.t[:, 0:QG], AF.Exp, scale=0.125), reads=[Sp], writes=[pt])
                return pt

            def pv(ci, pt):
                kfn, masks, vfn, slo, shi = chunks[ci]
                for s_ in range(slo, shi + 1):
                    P.op("pe", lambda e, s_=s_, pt=pt, vfn=vfn, st_=first[s_], sp_=(last_idx[s_] == ci): e.matmul(
                        acc[s_].t[:, 0:ncols], pt.t[:, s_ * 128:(s_ + 1) * 128], vfn(), start=st_, stop=sp_),
                        reads=[pt, Vsel, VC, Vw], writes=[acc[s_]])
                    first[s_] = False

            sps = {0: qk(0)}
            if len(chunks) > 1:
                sps[1] = qk(1)
            for ci in range(len(chunks)):
                pt = ex(ci, sps.pop(ci))
                if ci + 2 < len(chunks):
                    sps[ci + 2] = qk(ci + 2)
                pv(ci, pt)

        stg = [sb("stg%d" % br, [128, 4, 4, 65], F32) for br in range(3)]
        stgB = [[Buf("stgB%d_%d" % (br, s_), stg[br].t) for s_ in range(4)] for br in range(3)]
        impS = sb("impS", [128, 4, 4, 256], F32)
        impSB = [Buf("impSB%d" % s_, impS.t) for s_ in range(4)]
        osb4 = sb("osb4", [128, 4, 4, 64], F32)
        otmp = sb("otmp", [128, 4, 4, 64], F32)
        rlb = sb("rlb", [128, 4, 4], F32)
        wgb = sb("wgb", [128, 4, 4], F32)
        impacc4 = sb("impacc4", [128, 4, 256], F32)

        def evac(h, s_, br):
            P.op("dve", lambda e: e.tensor_copy(stg[br].t[:, s_, h, :], acc[s_].t[:, 0:65]), reads=[acc[s_]], writes=[stgB[br][s_]])
            if br == 0:
                P.op("dve", lambda e: e.tensor_copy(impS.t[:, s_, h, :], acc[s_].t[:, 65:321]), reads=[acc[s_]], writes=[impSB[s_]])

        def finish_branch(br, first_branch):
            P.op("dve", lambda e: e.tensor_scalar(rlb.t[:], stg[br].t[:, :, :, 64], 1e-30, None, ALU.max), reads=stgB[br], writes=[rlb])
            P.op("dve", lambda e: e.reciprocal(rlb.t[:], rlb.t[:]), reads=[rlb], writes=[rlb])
            P.op("dve", lambda e: e.tensor_tensor(wgb.t[:], rlb.t[:], gts.t[:, :, ss(br, 4, 3)], ALU.mult), reads=[rlb, gts], writes=[wgb])
            dst = osb4 if first_branch else otmp
            P.op("dve", lambda e: e.tensor_tensor(dst.t[:], stg[br].t[:, :, :, 0:64], wgb.t[:].unsqueeze(3).to_broadcast([128, 4, 4, 64]), ALU.mult),
                 reads=stgB[br] + [wgb], writes=[dst])
            if not first_branch:
                P.op("pool", lambda e: e.tensor_tensor(osb4.t[:], osb4.t[:], otmp.t[:], ALU.add), reads=[otmp, osb4], writes=[osb4])
            if br == 0:
                P.op("dve", lambda e: e.tensor_tensor(impS.t[:], impS.t[:], rlb.t[:].unsqueeze(3).to_broadcast([128, 4, 4, 256]), ALU.mult),
                     reads=impSB + [rlb], writes=impSB)
                P.op("pool", lambda e: e.tensor_tensor(impacc4.t[:], impS.t[:, :, 0, :], impS.t[:, :, 1, :], ALU.add), reads=impSB, writes=[impacc4])
                P.op("pool", lambda e: e.tensor_tensor(impacc4.t[:], impacc4.t[:], impS.t[:, :, 2, :], ALU.add), reads=impSB + [impacc4], writes=[impacc4])
                P.op("pool", lambda e: e.tensor_tensor(impacc4.t[:], impacc4.t[:], impS.t[:, :, 3, :], ALU.add), reads=impSB + [impacc4], writes=[impacc4])

        import os
        NG_RUN = int(os.environ.get("NSA_NG", str(NG)))
        for G in range(NG_RUN):
            t0 = G * QG
            sl = G % 2
            for ti in range(4):
                emit_hmodT_tile(P, x_d, t0 + ti * 128, xts[ti % 2], psXb, idn, modp, hT, ti * 128, 8, ["sp", "act"][ti % 2], width=512)
            emit_rope_tables(P, posB, posI, invp, None, Ct, St, tmpT, pos_d[0:1, t0:t0 + QG], QG)
            for h_ in range(4):
                emit_proj_rope(P, hT, 0, QG, wqh[h_], wqhr[h_], 0, ps[0], ps[1], Ct, St, 0, tmpa, tmpb,
                               lambda h_=h_: R[h_][0].t[:, :], R[h_][0])
            for w_ in range(1, (4 * G + 3) // 32 + 1):
                for h_ in range(4):
                    P.op("pool", lambda e, w_=w_, h_=h_: e.tensor_copy(R[h_][w_].t[0:64, :], R[h_][0].t[0:64, :]), reads=[R[h_][0]], writes=[R[h_][w_]])
            emit_proj_rope(P, hT, 0, QG, wkw, wkwr, 0, ps[0], ps[1], Ct, St, 0, tmpa, tmpb, lambda sl=sl: kwT[sl].t[:, :], kwT[sl])
            for ti in range(4):
                pv = ps[2 + ti % 2]
                for k in range(8):
                    P.op("pe", lambda e, k=k, ti=ti, pv=pv: e.matmul(pv.t[:, 0:64], hT.t[:, k, ti * 128:(ti + 1) * 128], wvw.t[:, k, :],
                                                                     start=(k == 0), stop=(k == 7)), reads=[hT, wvw], writes=[pv])
                P.op("act", lambda e, ti=ti, sl=sl, pv=pv: e.copy(Vw.t[:, sl * 4 + ti, 0:64], pv.t[:, 0:64]), reads=[pv], writes=[Vw])
                pg = ps[4 + ti % 2]
                for k in range(8):
                    P.op("pe", lambda e, k=k, ti=ti, pg=pg: e.matmul(pg.t[:, 0:12], hT.t[:, k, ti * 128:(ti + 1) * 128], wgt.t[:, k, :],
                                                                     start=(k == 0), stop=(k == 7)), reads=[hT, wgt], writes=[pg])
                P.op("act", lambda e, ti=ti, pg=pg: e.activation(gts.t[:, ti, :], pg.t[:, 0:12], AF.Sigmoid), reads=[pg], writes=[gts])

            if NST < 5:
                continue
            cmax = min(7, G // 4)
            for h in range(4):
                chunks = []
                for c in range(cmax + 1):
                    m = G - 4 * c
                    masks = []
                    if m <= 4:
                        masks = [(lambda: idnb.t[:], lambda m=m: msk.t[:, 8 + m, :])]
                    chunks.append((lambda rows, c=c, h=h: (kcT.t[0:64, c * 128:(c + 1) * 128], R[h][0].t[0:64, :]), masks, lambda c=c: VC.t[:, c, 0:321], 0, 3))
                attend(h, chunks, 321, G)
                for s_ in range(4):
                    evac(h, s_, 0)
            finish_branch(0, True)
            if NST < 6:
                continue
            for s_ in range(4):
                T = G * 4 + s_
                ia = Buf("iav", impacc4.t[:, s_, :])
                lo = max(0, 2 * T - 1)
                hi = min(256, 2 * T + 2)
                P.op("dve", lambda e, ia=ia, lo=lo, hi=hi, T=T: e.tensor_tensor(ia.t[:, lo:hi], ia.t[:, lo:hi], frc.t[:, lo - (2 * T - 1):hi - (2 * T - 1)], ALU.add),
                     reads=[impacc4, frc], writes=[impacc4])
                P.op("dve", lambda e, ia=ia: e.tensor_scalar(ia.t[:, 0:1], ia.t[:, 0:1], 1.0e4, None, ALU.add), reads=[impacc4], writes=[impacc4])
                P.op("dve", lambda e, ia=ia: e.max(out=m8a.t[:], in_=ia.t[:]), reads=[impacc4], writes=[m8a])
                P.op("dve", lambda e, ia=ia: e.match_replace(out=impw.t[:], in_to_replace=m8a.t[:], in_values=ia.t[:], imm_value=-1.0e30),
                     reads=[impacc4, m8a], writes=[impw])
                P.op("dve", lambda e: e.max(out=m8b.t[:], in_=impw.t[:]), reads=[impw], writes=[m8b])
                P.op("dve", lambda e, ia=ia: e.tensor_scalar(nbp.t[:, 64:320], ia.t[:], m8b.t[:, 7:8], NEG, ALU.is_lt, ALU.mult), reads=[impacc4, m8b], writes=[nbp])
                nW = (4 * G + 3) // 32 + 1
                for w_ in range(nW):
                    P.op("pe", lambda e, w_=w_, s_=s_: e.transpose(psW[w_].t[:, s_ * 128:(s_ + 1) * 128], nbp.t[:, 64 * w_:64 * w_ + 128], idn.t[:]),
                         reads=[nbp, idn], writes=[psW[w_]])
            nW = (4 * G + 3) // 32 + 1
            for w_ in range(nW):
                P.op("act", lambda e, w_=w_: e.copy(nbTw[w_].t[:], psW[w_].t[:, 0:QG]), reads=[psW[w_]], writes=[nbTw[w_]])
                for h_ in range(4):
                    P.op(["pool", "dve"][h_ % 2], lambda e, w_=w_, h_=h_: e.tensor_copy(R[h_][w_].t[64:128, :], nbTw[w_].t[64:128, :]), reads=[nbTw[w_]], writes=[R[h_][w_]])
            if NST < 7:
                continue
            for h in range(4):
                chunks = []
                for j in range(4 * G + 4):
                    w_ = j // 32
                    masks = []
                    jj = j - 4 * G
                    if jj >= 0:
                        masks.append((lambda: idnb.t[:], lambda jj=jj: msk.t[:, jj, :]))
                    chunks.append((lambda rows, j=j, w_=w_, h=h: (kselT.t[:, j * 128:(j + 1) * 128], R[h][w_].t[:, :]), masks, lambda j=j: Vsel.t[:, j, 0:65],
                                   max(0, jj), 3))
                attend(h, chunks, 65, G)
                for s_ in range(4):
                    evac(h, s_, 1)
            finish_branch(1, False)
            if NST < 8:
                continue
            for h in range(4):
                chunks = []
                for c in range(8):
                    if t0 - 512 + 128 * c < 0:
                        continue
                    if c < 4:
                        kfn = lambda rows, c=c, sl=sl, h=h: (kwT[1 - sl].t[0:64, c * 128:(c + 1) * 128], R[h][0].t[0:64, :])
                        vfn = lambda c=c, sl=sl: Vw.t[:, (1 - sl) * 4 + c, 0:65]
                        mk = 4 + c
                    else:
                        kfn = lambda rows, c=c, sl=sl, h=h: (kwT[sl].t[0:64, (c - 4) * 128:(c - 3) * 128], R[h][0].t[0:64, :])
                        vfn = lambda c=c, sl=sl: Vw.t[:, sl * 4 + (c - 4), 0:65]
                        mk = c - 4
                    masks = [(lambda: idnb.t[:], lambda mk=mk: msk.t[:, mk, :])]
                    chunks.append((kfn, masks, vfn, max(0, c - 4), min(3, c)))
                attend(h, chunks, 65, G)
                for s_ in range(4):
                    evac(h, s_, 2)
            finish_branch(2, False)
            for s_ in range(4):
                r0 = t0 + s_ * 128
                P.dma("sp", lambda e, s_=s_, r0=r0: e.dma_start(out=o_d[r0:r0 + 128, :], in_=osb4.t[:, s_, :, :].rearrange("p h e -> p (h e)")), reads=[osb4], writes=[Ob])
        P.barrier()
        P.wait_all("sp", [Ob])
        P.emit()
    return nc


def nsa_in_maps(x, c, positions, ada_w_s, ada_b_s, w_in, pos_k, w1_k, w2_k, pos_v, w1_v, w2_v, cores=range(8)):
    idn = np.eye(128, dtype=np.float32)
    inv = rope_inv_table()
    msk = nsa_masks()
    gp = np.zeros((64, S_LEN), np.float32)
    kk = np.arange(S_LEN)
    gp[(kk // 64) % 64, kk] = 1.0
    vcc = nsa_vc_const()
    frc = nsa_forced()
    cposT = np.ascontiguousarray(np.concatenate([pos_k.T, pos_v.T], axis=0))
    w1 = np.ascontiguousarray(np.stack([w1_k, w1_v]))
    w2 = np.ascontiguousarray(np.stack([w2_k, w2_v]))
    in_maps = []
    for core in cores:
        b, g = core // 4, core % 4
        wkv = np.stack([w_in[:, D + br * 512 + kv * 256 + g * 64: D + br * 512 + kv * 256 + g * 64 + 64] for br in range(3) for kv in range(2)])
        posc = np.zeros((1, 1024), np.int32)
        posc[0, :NCMP] = positions[b, 31::16][:NCMP]
        in_maps.append({
            "x": x[b], "cT": np.ascontiguousarray(c[b].reshape(8, 128).T),
            "adaw": np.ascontiguousarray(ada_w_s[:, 0:2 * D]), "adabT": np.ascontiguousarray(ada_b_s[0:2 * D].reshape(16, 128).T),
            "wq": np.ascontiguousarray(w_in[:, g * 256:(g + 1) * 256]), "wkv": np.ascontiguousarray(wkv),
            "wgt": np.ascontiguousarray(w_in[:, D + 1536 + 12 * g: D + 1536 + 12 * g + 12]),
            "pos": np.ascontiguousarray(positions[b:b + 1]), "posc": posc, "inv": inv, "idn": idn, "msk": msk,
            "gp": gp, "vcc": vcc, "frc": frc, "w1": w1, "w2": w2, "cposT": cposT,
        })
    return in_maps


def run_nsa(x, c, positions, ada_w_s, ada_b_s, w_in, pos_k, w1_k, w2_k, pos_v, w1_v, w2_v):
    if "nsa" not in _CACHE:
        _CACHE["nsa"] = build_nsa()
    nc = _CACHE["nsa"]
    in_maps = nsa_in_maps(x, c, positions, ada_w_s, ada_b_s, w_in, pos_k, w1_k, w2_k, pos_v, w1_v, w2_v)
    res = run_bass_kernel_spmd(nc, in_maps, core_ids=list(range(8)))
    B = x.shape[0]
    o = np.zeros((B, S_LEN, D), np.float32)
    for core in range(8):
        b, g = core // 4, core % 4
        o[b, :, g * 256:(g + 1) * 256] = res.results[core]["o"]
    return o


def kernel(x, c, positions, ada_w, ada_b, ln_g, ln_b,
           nsa_w_in, nsa_cmp_pos_k, nsa_cmp_w1_k, nsa_cmp_w2_k,
           nsa_cmp_pos_v, nsa_cmp_w1_v, nsa_cmp_w2_v, nsa_w_o,
           dil_w_in, dil_w_o, router_w, router_b, moe_w_gate, moe_w_up, moe_w_down):
    f = lambda a: np.ascontiguousarray(np.asarray(a))
    x, c, positions = f(x), f(c), f(positions)
    ada_w, ada_b, ln_g, ln_b = f(ada_w), f(ada_b), f(ln_g), f(ln_b)
    nsa_w_in, nsa_w_o, dil_w_in, dil_w_o = f(nsa_w_in), f(nsa_w_o), f(dil_w_in), f(dil_w_o)
    router_w, router_b = f(router_w), f(router_b)
    moe_w_gate, moe_w_up, moe_w_down = f(moe_w_gate), f(moe_w_up), f(moe_w_down)
    o0 = run_nsa(x, c, positions, ada_w[0, 0], ada_b[0, 0], nsa_w_in[0],
                 f(nsa_cmp_pos_k)[0], f(nsa_cmp_w1_k)[0], f(nsa_cmp_w2_k)[0],
                 f(nsa_cmp_pos_v)[0], f(nsa_cmp_w1_v)[0], f(nsa_cmp_w2_v)[0])
    x1 = run_ffn(x, o0, c, ada_w[0], ada_b[0], ln_g[0], ln_b[0], nsa_w_o[0], router_w, router_b,
                 moe_w_gate[0], moe_w_up[0], moe_w_down[0])
    o1 = run_dil(x1, c, positions, ada_w[1, 0], ada_b[1, 0], dil_w_in[0])
    out = run_ffn(x1, o1, c, ada_w[1], ada_b[1], ln_g[1], ln_b[1], dil_w_o[0], router_w, router_b,
                  moe_w_gate[1], moe_w_up[1], moe_w_down[1])
    return out.astype(np.float32)
```

```python
import numpy as np
from contextlib import ExitStack
import concourse.bass as bass
import concourse.mybir as mybir
from concourse.bass_utils import run_bass_kernel_spmd

F32 = mybir.dt.float32
BF16 = mybir.dt.bfloat16
I32 = mybir.dt.int32
AF = mybir.ActivationFunctionType
ALU = mybir.AluOpType
AX = mybir.AxisListType

ENGS = ("pe", "act", "dve", "pool", "sp")

D = 1024
ALPHA = (2.0 * 2) ** 0.25
LN_EPS = 1e-5
NEG = -30000.0
import os as _os
SAME_ENGINE_SYNC = _os.environ.get("SES", "1") == "1"


_UID = [0]


class Buf:
    __slots__ = ("name", "t", "writer", "readers", "dma_sem", "dma_cnt", "uid")

    def __init__(self, name, t=None):
        _UID[0] += 1
        self.uid = _UID[0]
        self.name = name
        self.t = t
        self.writer = None
        self.readers = []
        self.dma_sem = None
        self.dma_cnt = 0


class Prog:
    def __init__(self, nc, stack):
        self.nc = nc
        self.stack = stack
        self.ops = {e: [] for e in ENGS}
        self.cnt = {e: 0 for e in ENGS}
        self.waited = {}
        self.sem = {}
        for e in ENGS:
            self.sem[e] = stack.enter_context(nc.semaphore("s_" + e))
        self.n_dma_sems = 0
        self.dma_bufs = []
        self.scopes = []

    def sbuf(self, name, shape, dt):
        st = self.scopes[-1] if self.scopes else self.stack
        t = st.enter_context(self.nc.sbuf_tensor("sb_" + name, list(shape), dt))
        return Buf(name, t)

    def push(self):
        self.scopes.append(ExitStack())

    def pop(self):
        self.barrier()
        self.scopes.pop().close()

    def barrier(self):
        for eng in ENGS:
            waits = []
            for e2 in ENGS:
                if self.cnt[e2] > 0:
                    self._need(eng, ("eng", e2, self.cnt[e2]), waits)
            if eng == "pe" and self.cnt["pe"] > 0:
                pass
            for b in self.dma_bufs:
                self._need(eng, ("dma", b, b.dma_cnt), waits)
            self.ops[eng].append((waits, None, None))

    def psum(self, name, shape, dt=F32):
        t = self.stack.enter_context(self.nc.psum_tensor("ps_" + name, list(shape), dt))
        return Buf(name, t)

    def _need(self, eng, dep, waits):
        if dep[0] == "eng":
            _, e, idx = dep
            if e == eng and (e == "pe" or not SAME_ENGINE_SYNC):
                return
            key = (eng, "E", e)
            val = idx
            sem = self.sem[e]
        else:
            _, b, c = dep
            key = (eng, "D", b.uid)
            val = 16 * c
            sem = b.dma_sem
        if self.waited.get(key, 0) >= val:
            return
        self.waited[key] = val
        waits.append((sem, val))

    def _deps(self, eng, reads, writes):
        waits = []
        for b in reads:
            if b.writer is not None:
                self._need(eng, b.writer, waits)
        for b in writes:
            if b.writer is not None:
                self._need(eng, b.writer, waits)
            for r in b.readers:
                self._need(eng, r, waits)
        return waits

    def op(self, eng, fn, reads=(), writes=()):
        waits = self._deps(eng, reads, writes)
        self.cnt[eng] += 1
        me = ("eng", eng, self.cnt[eng])
        for b in reads:
            b.readers.append(me)
        for b in writes:
            b.writer = me
            b.readers = []
        self.ops[eng].append((waits, fn, (self.sem[eng], 1)))

    def dma(self, eng, fn, reads=(), writes=()):
        waits = self._deps(eng, reads, writes)
        dst = writes[0]
        if dst.dma_sem is None:
            dst.dma_sem = self.stack.enter_context(self.nc.semaphore("d%d" % self.n_dma_sems))
            self.n_dma_sems += 1
            self.dma_bufs.append(dst)
        dst.dma_cnt += 1
        me = ("dma", dst, dst.dma_cnt)
        for b in reads:
            b.readers.append(me)
        for b in writes:
            b.writer = me
            b.readers = []
        self.ops[eng].append((waits, fn, (dst.dma_sem, 16)))

    def wait_all(self, eng, bufs):
        waits = []
        for b in bufs:
            if b.writer is not None:
                self._need(eng, b.writer, waits)
        self.ops[eng].append((waits, None, None))

    def emit(self):
        nc = self.nc
        ops = self.ops
        with nc.Block() as block:
            def run(e, lst):
                for waits, fn, inc in lst:
                    for sem, val in waits:
                        e.wait_ge(sem, val)
                    if fn is not None:
                        fn(e).then_inc(inc[0], inc[1])

            @block.tensor
            def _(e):
                run(e, ops["pe"])

            @block.scalar
            def _(e):
                run(e, ops["act"])

            @block.vector
            def _(e):
                run(e, ops["dve"])

            @block.gpsimd
            def _(e):
                run(e, ops["pool"])

            @block.sync
            def _(e):
                run(e, ops["sp"])


def ss(a0, n, d):
    return slice(a0, a0 + (n - 1) * d + 1, d) if d > 1 else slice(a0, a0 + n)


def bcast_row(ap_row, n):
    return ap_row.to_broadcast([128, n])


def emit_layernorm(P, z, stats, mv, rstd, xn):
    for h in range(2):
        P.op("dve", lambda e, h=h: e.bn_stats(stats.t[:, h, :], z.t[:, h * 512:(h + 1) * 512]), reads=[z], writes=[stats])
    P.op("dve", lambda e: e.bn_aggr(mv.t[:], stats.t[:]), reads=[stats], writes=[mv])
    P.op("dve", lambda e: e.tensor_scalar(rstd.t[:], mv.t[:, 1:2], LN_EPS, None, ALU.add), reads=[mv], writes=[rstd])
    P.op("act", lambda e: e.sqrt(rstd.t[:], rstd.t[:]), reads=[rstd], writes=[rstd])
    P.op("dve", lambda e: e.reciprocal(rstd.t[:], rstd.t[:]), reads=[rstd], writes=[rstd])
    P.op("dve", lambda e: e.tensor_scalar(xn.t[:], z.t[:], mv.t[:, 0:1], rstd.t[:, 0:1], ALU.subtract, ALU.mult),
         reads=[z, mv, rstd], writes=[xn])


NTOK = 4096
CAP = 1024
NEXP = 32


def build_ffn(dbg=False):
    nc = bass.Bass("TRN2", target_bir_lowering=False)
    NT = NTOK // 128
    dt_in = lambda name, shape, dt=F32: nc.dram_tensor(name, list(shape), dt, kind="ExternalInput").ap()
    x_d = dt_in("x", [NTOK, D])
    o_d = dt_in("o", [NTOK, D])
    cT_d = dt_in("cT", [128, 8])
    adaw_d = dt_in("adaw", [2, D, 3 * D])
    adab_d = dt_in("adab", [2, 3 * D])
    lng_d = dt_in("lng", [2, D])
    lnb_d = dt_in("lnb", [2, D])
    wo_d = dt_in("wo", [D, D])
    rw_d = dt_in("rw", [D, NEXP])
    rb_d = dt_in("rb", [1, NEXP])
    NE_ = (1 if dbg == 1 else 2) if dbg in (1, 2, 3) else NEXP
    if dbg == 4:
        dbg_a = nc.dram_tensor("dbg_a", [128, 4 * D], F32, kind="ExternalOutput").ap()
    if dbg in (3, 4):
        dbg_i = nc.dram_tensor("dbg_i", [128, NTOK // 128 * 2], I32, kind="ExternalOutput").ap()
    wg_d = dt_in("wg", [NE_, D, 512])
    wu_d = dt_in("wu", [NE_, D, 512])
    wd_d = dt_in("wd", [NE_, 512, D])
    idn_d = dt_in("idn", [128, 128])
    tri_d = dt_in("tri", [128, 128])
    offs_d = dt_in("offs", [1, NEXP])
    y_d = nc.dram_tensor("y", [NTOK, D], F32, kind="ExternalOutput").ap()
    XS = nc.dram_tensor("XS", [NEXP * CAP, D], BF16).ap()
    YS = nc.dram_tensor("YS", [NEXP * CAP, D], F32).ap()
    X1 = nc.dram_tensor("X1", [NTOK, D], F32, kind="ExternalOutput" if dbg else "Internal").ap()
    if dbg == 2:
        dbg_xs = nc.dram_tensor("dbg_xs", [2 * CAP, D], BF16, kind="ExternalOutput").ap()
        dbg_ys = nc.dram_tensor("dbg_ys", [2 * CAP, D], F32, kind="ExternalOutput").ap()
    if dbg in (1, 2):
        dbg_i = nc.dram_tensor("dbg_i", [128, NTOK // 128 * 2], I32, kind="ExternalOutput").ap()
        dbg_w = nc.dram_tensor("dbg_w", [128, NTOK // 128 * 2], F32, kind="ExternalOutput").ap()
        dbg_m = nc.dram_tensor("dbg_m", [128, 4 * D], F32, kind="ExternalOutput").ap()

    with ExitStack() as st:
        P = Prog(nc, st)
        sb = P.sbuf
        idn = sb("idn", [128, 128], F32)
        idnb = sb("idnb", [128, 128], BF16)
        tri = sb("tri", [128, 128], BF16)
        ones = sb("ones", [128, 128], BF16)
        offsB = sb("offsB", [128, NEXP], F32)
        rbB = sb("rbB", [128, NEXP], F32)
        rw = sb("rw", [128, 8, NEXP], F32)
        wo = sb("wo", [128, 8, D], BF16)
        g1B = sb("g1B", [128, D], F32)
        sh2B = sb("sh2B", [128, D], F32)
        sc2B = sb("sc2B", [128, D], F32)
        g2B = sb("g2B", [128, D], F32)
        lgB = [sb("lgB%d" % i, [128, D], F32) for i in range(2)]
        lbB = [sb("lbB%d" % i, [128, D], F32) for i in range(2)]
        cT = sb("cT", [128, 8], F32)
        cond = sb("cond", [128, 8], F32)
        condB = sb("condB", [128, 8, 128], F32)
        base = sb("base", [128, NEXP], F32)
        desti = sb("desti", [128, NT, 2], I32)
        wts = sb("wts", [128, NT, 2], F32)
        psA = P.psum("psA", [128, 1024], BF16)
        psY = P.psum("psY", [128, 1024], F32)
        psT = P.psum("psT", [128, 1024], F32)
        psU = P.psum("psU", [128, 1024], F32)
        psS = P.psum("psS", [128, 512], F32)

        dmaq = ["sp", "act"]
        P.dma("sp", lambda e: e.dma_start(out=idn.t[:], in_=idn_d), writes=[idn])
        P.dma("pool", lambda e: e.dma_start(out=idnb.t[:], in_=idn_d), writes=[idnb])
        P.dma("pool", lambda e: e.dma_start(out=tri.t[:], in_=tri_d), writes=[tri])
        P.dma("sp", lambda e: e.dma_start(out=offsB.t[:], in_=bcast_row(offs_d, NEXP)), writes=[offsB])
        P.dma("sp", lambda e: e.dma_start(out=rbB.t[:], in_=bcast_row(rb_d, NEXP)), writes=[rbB])
        P.dma("sp", lambda e: e.dma_start(out=rw.t[:], in_=rw_d.rearrange("(k p) n -> p k n", p=128)), writes=[rw])
        P.dma("pool", lambda e: e.dma_start(out=wo.t[:], in_=wo_d.rearrange("(k p) n -> p k n", p=128)), writes=[wo])
        for i in range(2):
            P.dma("sp", lambda e, i=i: e.dma_start(out=lgB[i].t[:], in_=bcast_row(lng_d[i:i + 1, :], D)), writes=[lgB[i]])
            P.dma("sp", lambda e, i=i: e.dma_start(out=lbB[i].t[:], in_=bcast_row(lnb_d[i:i + 1, :], D)), writes=[lbB[i]])
        P.dma("sp", lambda e: e.dma_start(out=cT.t[:], in_=cT_d), writes=[cT])
        P.op("pool", lambda e: e.memset(ones.t[:], 1.0), writes=[ones])
        P.op("pool", lambda e: e.memset(base.t[:], 0.0), writes=[base])
        P.op("act", lambda e: e.activation(cond.t[:], cT.t[:], AF.Silu), reads=[cT], writes=[cond])
        for k in range(8):
            P.op("dve", lambda e, k=k: e.tensor_copy(condB.t[:, k, :], cond.t[:, k:k + 1].to_broadcast([128, 128])),
                 reads=[cond], writes=[condB])

        P.push()
        awb = [sb("awb%d" % i, [128, 8, 512], F32) for i in range(2)]
        abb = [sb("abb%d" % i, [128, 512], F32) for i in range(2)]
        jobs = []
        for h in range(2):
            jobs.append((0, 2 * D + h * 512, g1B, h * 512, False))
        for h in range(2):
            jobs.append((1, 0 * D + h * 512, sh2B, h * 512, False))
        for h in range(2):
            jobs.append((1, 1 * D + h * 512, sc2B, h * 512, True))
        for h in range(2):
            jobs.append((1, 2 * D + h * 512, g2B, h * 512, False))
        psM = [Buf("psM0", psS.t), Buf("psM1", psU.t)]
        for j, (s, c0, dst, d0, plus1) in enumerate(jobs):
            wb = awb[j % 2]
            bb = abb[j % 2]
            pm = psM[j % 2]
            P.dma(dmaq[j % 2], lambda e, s=s, c0=c0, wb=wb: e.dma_start(
                out=wb.t[:], in_=adaw_d[s, :, c0:c0 + 512].rearrange("(k p) n -> p k n", p=128)), writes=[wb])
            P.dma("sp", lambda e, s=s, c0=c0, bb=bb: e.dma_start(
                out=bb.t[:], in_=bcast_row(adab_d[s:s + 1, c0:c0 + 512], 512)), writes=[bb])
            for k in range(8):
                P.op("pe", lambda e, k=k, wb=wb, pm=pm: e.matmul(pm.t[:, 0:512], condB.t[:, k, :], wb.t[:, k, :],
                                                                 start=(k == 0), stop=(k == 7)),
                     reads=[condB, wb], writes=[pm])
            P.op("dve", lambda e, pm=pm, bb=bb, dst=dst, d0=d0: e.tensor_tensor(
                dst.t[:, d0:d0 + 512], pm.t[:, 0:512], bb.t[:], ALU.add), reads=[pm, bb], writes=[dst])
            if plus1:
                P.op("dve", lambda e, dst=dst, d0=d0: e.tensor_scalar(
                    dst.t[:, d0:d0 + 512], dst.t[:, d0:d0 + 512], 1.0, None, ALU.add), reads=[dst], writes=[dst])

        P.pop()
        P.push()
        NB = 2
        xt = [sb("xt%d" % i, [128, D], F32) for i in range(NB)]
        ot = [sb("ot%d" % i, [128, D], F32) for i in range(NB)]
        ob = [sb("ob%d" % i, [128, D], BF16) for i in range(NB)]
        oT = [sb("oT%d" % i, [128, 8, 128], BF16) for i in range(NB)]
        t1 = [sb("t1%d" % i_, [128, D], F32) for i_ in range(2)]
        z = [sb("z%d" % i_, [128, D], F32) for i_ in range(2)]
        xn = [sb("xn%d" % i_, [128, D], F32) for i_ in range(2)]
        x1 = [sb("x1_%d" % i, [128, D], F32) for i in range(NB)]
        h2 = [sb("h2_%d" % i, [128, D], F32) for i in range(NB)]
        hb = [sb("hb%d" % i, [128, D], BF16) for i in range(NB)]
        h2T = [sb("h2T%d" % i, [128, 8, 128], F32) for i in range(NB)]
        stats = [sb("stats%d" % i_, [128, 2, 6], F32) for i_ in range(2)]
        mv = [sb("mv%d" % i_, [128, 2], F32) for i_ in range(2)]
        rstd = [sb("rstd%d" % i_, [128, 1], F32) for i_ in range(2)]
        sc = [sb("sc%d" % i_, [128, NEXP], F32) for i_ in range(2)]
        grp = [sb("grp%d" % i_, [128, NEXP], F32) for i_ in range(2)]
        m8 = [sb("m8%d" % i_, [128, 4, 8], F32) for i_ in range(2)]
        gs = [sb("gs%d" % i_, [128, 4], F32) for i_ in range(2)]
        gmax = [sb("gmax%d" % i_, [128, 1], F32) for i_ in range(2)]
        oh = [sb("oh%d" % i_, [128, 4], F32) for i_ in range(2)]
        tmp4 = [sb("tmp4%d" % i_, [128, 4], F32) for i_ in range(2)]
        thr = [sb("thr%d" % i_, [128, 1], F32) for i_ in range(2)]
        ge = [sb("ge%d" % i_, [128, NEXP], F32) for i_ in range(2)]
        sel = [sb("sel%d" % i_, [128, NEXP], F32) for i_ in range(2)]
        selb = [sb("selb%d" % i_, [128, NEXP], BF16) for i_ in range(2)]
        ws = [sb("ws%d" % i_, [128, NEXP], F32) for i_ in range(2)]
        wsum = [sb("wsum%d" % i_, [128, 1], F32) for i_ in range(2)]
        wt = [sb("wt%d" % i_, [128, NEXP], F32) for i_ in range(2)]
        dall = [sb("dall%d" % i_, [128, NEXP], F32) for i_ in range(2)]
        dhi = [sb("dhi%d" % i_, [128, 1], F32) for i_ in range(2)]
        dsum = [sb("dsum%d" % i_, [128, 1], F32) for i_ in range(2)]
        dpair = [sb("dpair%d" % i_, [128, 2], F32) for i_ in range(2)]
        eq = [sb("eq%d" % i_, [128, NEXP], F32) for i_ in range(2)]
        XSb = Buf("XS")
        X1b = Buf("X1")
        psLog = Buf("psLog", psS.t)

        def chain(t):
            b = t % NB
            yield
            r0 = t * 128
            yield
            P.dma("sp", lambda e, b=b, r0=r0: e.dma_start(out=xt[b].t[:], in_=x_d[r0:r0 + 128, :]), writes=[xt[b]])
            yield
            P.dma("act", lambda e, b=b, r0=r0: e.dma_start(out=ot[b].t[:], in_=o_d[r0:r0 + 128, :]), writes=[ot[b]])
            yield
            P.op("dve", lambda e, b=b: e.tensor_copy(ob[b].t[:], ot[b].t[:]), reads=[ot[b]], writes=[ob[b]])
            yield
            for k in range(8):
                P.op("pe", lambda e, b=b, k=k: e.transpose(psA.t[:, k * 128:(k + 1) * 128], ob[b].t[:, k * 128:(k + 1) * 128], idnb.t[:]),
                     reads=[ob[b], idnb], writes=[psA])
            P.op("act", lambda e, b=b: e.copy(oT[b].t[:].rearrange("p k m -> p (k m)"), psA.t[:]), reads=[psA], writes=[oT[b]])
            yield
            for nh in range(2):
                for k in range(8):
                    P.op("pe", lambda e, b=b, k=k, nh=nh: e.matmul(psY.t[:, nh * 512:(nh + 1) * 512], oT[b].t[:, k, :],
                                                                   wo.t[:, k, nh * 512:(nh + 1) * 512], start=(k == 0), stop=(k == 7)),
                         reads=[oT[b], wo], writes=[psY])
            for nh in range(2):
                sl = slice(nh * 512, (nh + 1) * 512)
                P.op("dve", lambda e, sl=sl: e.tensor_tensor(t1[b].t[:, sl], psY.t[:, sl], g1B.t[:, sl], ALU.mult),
                     reads=[psY, g1B], writes=[t1[b]])
            P.op("dve", lambda e, b=b: e.scalar_tensor_tensor(z[b].t[:], xt[b].t[:], ALPHA, t1[b].t[:], ALU.mult, ALU.add),
                 reads=[xt[b], t1[b]], writes=[z[b]])
            emit_layernorm(P, z[b], stats[b], mv[b], rstd[b], xn[b])
            yield
            P.op("dve", lambda e, b=b: e.tensor_tensor(x1[b].t[:], xn[b].t[:], lgB[0].t[:], ALU.mult), reads=[xn[b], lgB[0]], writes=[x1[b]])
            yield
            P.op("dve", lambda e, b=b: e.tensor_tensor(x1[b].t[:], x1[b].t[:], lbB[0].t[:], ALU.add), reads=[x1[b], lbB[0]], writes=[x1[b]])
            yield
            P.dma("sp", lambda e, b=b, r0=r0: e.dma_start(out=X1[r0:r0 + 128, :], in_=x1[b].t[:]), reads=[x1[b]], writes=[X1b])
            yield
            P.op("dve", lambda e, b=b: e.tensor_tensor(h2[b].t[:], x1[b].t[:], sc2B.t[:], ALU.mult), reads=[x1[b], sc2B], writes=[h2[b]])
            yield
            P.op("dve", lambda e, b=b: e.tensor_tensor(h2[b].t[:], h2[b].t[:], sh2B.t[:], ALU.add), reads=[h2[b], sh2B], writes=[h2[b]])
            yield
            P.op("act", lambda e, b=b: e.copy(hb[b].t[:], h2[b].t[:]), reads=[h2[b]], writes=[hb[b]])
            yield
            for k in range(8):
                P.op("pe", lambda e, b=b, k=k: e.transpose(psT.t[:, k * 128:(k + 1) * 128], h2[b].t[:, k * 128:(k + 1) * 128], idn.t[:]),
                     reads=[h2[b], idn], writes=[psT])
            P.op("act", lambda e, b=b: e.copy(h2T[b].t[:].rearrange("p k m -> p (k m)"), psT.t[:]), reads=[psT], writes=[h2T[b]])
            yield
            for k in range(8):
                P.op("pe", lambda e, b=b, k=k: e.matmul(psS.t[:, 0:NEXP], h2T[b].t[:, k, :], rw.t[:, k, :], start=(k == 0), stop=(k == 7)),
                     reads=[h2T[b], rw], writes=[psLog])
            P.op("act", lambda e: e.activation(sc[b].t[:], psS.t[:, 0:NEXP], AF.Sigmoid), reads=[psLog], writes=[sc[b]])
            yield
            P.op("dve", lambda e: e.tensor_tensor(grp[b].t[:], sc[b].t[:], rbB.t[:], ALU.add), reads=[sc[b], rbB], writes=[grp[b]])
            yield
            for g in range(4):
                P.op("dve", lambda e, g=g: e.max(out=m8[b].t[:, g, :], in_=grp[b].t[:, g * 8:(g + 1) * 8]), reads=[grp[b]], writes=[m8[b]])
            P.op("dve", lambda e: e.tensor_tensor(gs[b].t[:], m8[b].t[:, :, 0], m8[b].t[:, :, 1], ALU.add), reads=[m8[b]], writes=[gs[b]])
            yield
            P.op("dve", lambda e: e.reduce_max(gmax[b].t[:], gs[b].t[:], AX.X), reads=[gs[b]], writes=[gmax[b]])
            yield
            P.op("dve", lambda e: e.tensor_scalar(oh[b].t[:], gs[b].t[:], gmax[b].t[:, 0:1], None, ALU.is_equal), reads=[gs[b], gmax[b]], writes=[oh[b]])
            yield
            P.op("dve", lambda e: e.tensor_tensor(tmp4[b].t[:], oh[b].t[:], m8[b].t[:, :, 1], ALU.mult), reads=[oh[b], m8[b]], writes=[tmp4[b]])
            yield
            P.op("dve", lambda e: e.reduce_sum(thr[b].t[:], tmp4[b].t[:], AX.X), reads=[tmp4[b]], writes=[thr[b]])
            yield
            P.op("dve", lambda e: e.tensor_scalar(ge[b].t[:], grp[b].t[:], thr[b].t[:, 0:1], None, ALU.is_ge), reads=[grp[b], thr[b]], writes=[ge[b]])
            yield
            P.op("dve", lambda e: e.tensor_tensor(sel[b].t[:].rearrange("p (g j) -> p g j", j=8), ge[b].t[:].rearrange("p (g j) -> p g j", j=8),
                                                  oh[b].t[:].unsqueeze(2).to_broadcast([128, 4, 8]), ALU.mult), reads=[ge[b], oh[b]], writes=[sel[b]])
            P.op("dve", lambda e: e.tensor_tensor(ws[b].t[:], sc[b].t[:], sel[b].t[:], ALU.mult), reads=[sc[b], sel[b]], writes=[ws[b]])
            yield
            P.op("dve", lambda e: e.reduce_sum(wsum[b].t[:], ws[b].t[:], AX.X), reads=[ws[b]], writes=[wsum[b]])
            yield
            P.op("dve", lambda e: e.reciprocal(wsum[b].t[:], wsum[b].t[:]), reads=[wsum[b]], writes=[wsum[b]])
            yield
            P.op("dve", lambda e: e.tensor_scalar(wt[b].t[:], ws[b].t[:], wsum[b].t[:, 0:1], None, ALU.mult), reads=[ws[b], wsum[b]], writes=[wt[b]])
            yield
            P.op("dve", lambda e: e.tensor_copy(selb[b].t[:], sel[b].t[:]), reads=[sel[b]], writes=[selb[b]])
            yield
            P.op("pe", lambda e: e.matmul(psS.t[:, 64:64 + NEXP], tri.t[:], selb[b].t[:], start=True, stop=True), reads=[tri, selb[b]], writes=[psLog])
            yield
            P.op("pe", lambda e: e.matmul(psS.t[:, 128:128 + NEXP], ones.t[:], selb[b].t[:], start=True, stop=True), reads=[ones, selb[b]], writes=[psLog])
            yield
            P.op("dve", lambda e: e.tensor_tensor(dall[b].t[:], psS.t[:, 64:64 + NEXP], base.t[:], ALU.add), reads=[psLog, base], writes=[dall[b]])
            yield
            P.op("dve", lambda e: e.tensor_tensor(dall[b].t[:], dall[b].t[:], offsB.t[:], ALU.add), reads=[dall[b], offsB], writes=[dall[b]])
            yield
            P.op("dve", lambda e: e.tensor_tensor(dall[b].t[:], dall[b].t[:], sel[b].t[:], ALU.mult), reads=[dall[b], sel[b]], writes=[dall[b]])
            yield
            P.op("dve", lambda e: e.tensor_tensor(base.t[:], base.t[:], psS.t[:, 128:128 + NEXP], ALU.add), reads=[psLog, base], writes=[base])
            yield
            P.op("dve", lambda e: e.reduce_max(dhi[b].t[:], dall[b].t[:], AX.X), reads=[dall[b]], writes=[dhi[b]])
            yield
            P.op("dve", lambda e: e.reduce_sum(dsum[b].t[:], dall[b].t[:], AX.X), reads=[dall[b]], writes=[dsum[b]])
            yield
            P.op("dve", lambda e: e.tensor_scalar(dpair[b].t[:, 0:1], dhi[b].t[:], -1.0, None, ALU.add), reads=[dhi[b]], writes=[dpair[b]])
            yield
            P.op("dve", lambda e: e.scalar_tensor_tensor(dpair[b].t[:, 1:2], dsum[b].t[:], -1.0, dhi[b].t[:], ALU.add, ALU.subtract),
                 reads=[dsum[b], dhi[b]], writes=[dpair[b]])
            P.op("dve", lambda e, t=t: e.tensor_copy(desti.t[:, t, :], dpair[b].t[:]), reads=[dpair[b]], writes=[desti])
            yield
            P.op("dve", lambda e: e.tensor_scalar(eq[b].t[:], dall[b].t[:], dhi[b].t[:, 0:1], None, ALU.is_equal), reads=[dall[b], dhi[b]], writes=[eq[b]])
            yield
            P.op("dve", lambda e: e.tensor_tensor(eq[b].t[:], eq[b].t[:], wt[b].t[:], ALU.mult), reads=[eq[b], wt[b]], writes=[eq[b]])
            yield
            P.op("dve", lambda e, t=t: e.reduce_sum(wts.t[:, t, 0:1], eq[b].t[:], AX.X), reads=[eq[b]], writes=[wts])
            yield
            P.op("dve", lambda e, t=t: e.tensor_scalar(wts.t[:, t, 1:2], wts.t[:, t, 0:1], -1.0, 1.0, ALU.mult, ALU.add), reads=[wts], writes=[wts])
            yield
            for j in range(2):
                P.dma("pool", lambda e, b=b, t=t, j=j: e.indirect_dma_start(
                    out=XS[:, :], out_offset=bass.IndirectOffsetOnAxis(ap=desti.t[:, t, j:j + 1], axis=0),
                    in_=hb[b].t[:, :], in_offset=None), reads=[hb[b], desti], writes=[XSb])


        LAG = 10
        for t0_ in range(0, NT, 2):
            ga, gb = chain(t0_), chain(t0_ + 1)
            la = lb = True
            na = 0
            while la or lb:
                if la:
                    try:
                        next(ga)
                        na += 1
                    except StopIteration:
                        la = False
                if lb and (na >= LAG or not la):
                    try:
                        next(gb)
                    except StopIteration:
                        lb = False
        if dbg == 1:
            Db = Buf("dbg")
            P.dma("sp", lambda e: e.dma_start(out=dbg_i, in_=desti.t[:].rearrange("p t j -> p (t j)")), reads=[desti], writes=[Db])
            P.dma("sp", lambda e: e.dma_start(out=dbg_w, in_=wts.t[:].rearrange("p t j -> p (t j)")), reads=[wts], writes=[Db])
            for i_, tl in enumerate([g1B, sh2B, sc2B, g2B]):
                P.dma("sp", lambda e, i_=i_, tl=tl: e.dma_start(out=dbg_m[:, i_ * D:(i_ + 1) * D], in_=tl.t[:]), reads=[tl], writes=[Db])
            P.wait_all("sp", [Db, X1b])
            P.pop()
            P.emit()
            return nc
        P.pop()
        P.push()
        wgs = [sb("wgs%d" % i, [128, 8, 512], BF16) for i in range(2)]
        wus = [sb("wus%d" % i, [128, 8, 512], BF16) for i in range(2)]
        wds = [sb("wds%d" % i, [128, 4, D], BF16) for i in range(2)]
        xs_tok = [sb("xs_tok%d" % i, [128, D], BF16) for i in range(2)]
        xsT = [sb("xsT%d" % i, [128, 8, CAP], BF16) for i in range(2)]
        sg = [sb("sg%d" % i, [128, 512], F32) for i in range(2)]
        hT = [sb("hT%d" % i, [128, 4, CAP], BF16) for i in range(2)]
        ysb = [sb("ysb%d" % i, [128, D], F32) for i in range(2)]
        psG = [Buf("psG0", psT.t), Buf("psG1", psU.t)]
        YSb = Buf("YS")
        RB = CAP // 128
        nx = 0
        ny = 0
        for ex in range(NE_):
            wb = ex % 2
            P.dma("pool", lambda e, ex=ex, wb=wb: e.dma_start(out=wgs[wb].t[:], in_=wg_d[ex].rearrange("(k p) n -> p k n", p=128)), writes=[wgs[wb]])
            P.dma("pool", lambda e, ex=ex, wb=wb: e.dma_start(out=wus[wb].t[:], in_=wu_d[ex].rearrange("(k p) n -> p k n", p=128)), writes=[wus[wb]])
            P.dma("pool", lambda e, ex=ex, wb=wb: e.dma_start(out=wds[wb].t[:], in_=wd_d[ex].rearrange("(k p) n -> p k n", p=128)), writes=[wds[wb]])
            xT = xsT[wb]
            for rb in range(RB):
                xb = xs_tok[nx % 2]
                nx += 1
                r0 = ex * CAP + rb * 128
                P.dma("sp", lambda e, xb=xb, r0=r0: e.dma_start(out=xb.t[:], in_=XS[r0:r0 + 128, :]), reads=[XSb], writes=[xb])
                for k in range(8):
                    P.op("pe", lambda e, xb=xb, k=k: e.transpose(psA.t[:, k * 128:(k + 1) * 128], xb.t[:, k * 128:(k + 1) * 128], idnb.t[:]),
                         reads=[xb, idnb], writes=[psA])
                P.op("act", lambda e, xT=xT, rb=rb: e.copy(xT.t[:, :, rb * 128:(rb + 1) * 128], psA.t[:].rearrange("p (k m) -> p k m", m=128)),
                     reads=[psA], writes=[xT])
            hh = hT[wb]
            for fc in range(4):
              for hf in range(CAP // 512):
                pg = psG[(fc * (CAP // 512) + hf) % 2]
                s0 = hf * 512
                for k in range(8):
                    P.op("pe", lambda e, k=k, fc=fc, pg=pg, wb=wb, xT=xT, s0=s0: e.matmul(
                        pg.t[:, 0:512], wgs[wb].t[:, k, fc * 128:(fc + 1) * 128], xT.t[:, k, s0:s0 + 512], start=(k == 0), stop=(k == 7)),
                        reads=[wgs[wb], xT], writes=[pg])
                for k in range(8):
                    P.op("pe", lambda e, k=k, fc=fc, pg=pg, wb=wb, xT=xT, s0=s0: e.matmul(
                        pg.t[:, 512:1024], wus[wb].t[:, k, fc * 128:(fc + 1) * 128], xT.t[:, k, s0:s0 + 512], start=(k == 0), stop=(k == 7)),
                        reads=[wus[wb], xT], writes=[pg])
                s_ = sg[(fc * (CAP // 512) + hf) % 2]
                P.op("act", lambda e, pg=pg, s_=s_: e.activation(s_.t[:], pg.t[:, 0:512], AF.Silu), reads=[pg], writes=[s_])
                P.op("dve", lambda e, pg=pg, s_=s_, hh=hh, fc=fc, s0=s0: e.tensor_tensor(hh.t[:, fc, s0:s0 + 512], s_.t[:], pg.t[:, 512:1024], ALU.mult),
                     reads=[pg, s_], writes=[hh])
            for rb in range(RB):
                yb = ysb[ny % 2]
                ny += 1
                for nh in range(2):
                    for fc in range(4):
                        P.op("pe", lambda e, rb=rb, nh=nh, fc=fc, hh=hh, wb=wb: e.matmul(
                            psY.t[:, nh * 512:(nh + 1) * 512], hh.t[:, fc, rb * 128:(rb + 1) * 128],
                            wds[wb].t[:, fc, nh * 512:(nh + 1) * 512], start=(fc == 0), stop=(fc == 3)),
                            reads=[hh, wds[wb]], writes=[psY])
                P.op("act", lambda e, yb=yb: e.copy(yb.t[:], psY.t[:]), reads=[psY], writes=[yb])
                r0 = ex * CAP + rb * 128
                P.dma("sp", lambda e, yb=yb, r0=r0: e.dma_start(out=YS[r0:r0 + 128, :], in_=yb.t[:]), reads=[yb], writes=[YSb])

        if dbg == 2:
            Db = Buf("dbg")
            P.dma("sp", lambda e: e.dma_start(out=dbg_xs, in_=XS[0:2 * CAP, :]), reads=[XSb], writes=[Db])
            P.dma("sp", lambda e: e.dma_start(out=dbg_ys, in_=YS[0:2 * CAP, :]), reads=[YSb], writes=[Db])
            P.dma("sp", lambda e: e.dma_start(out=dbg_i, in_=desti.t[:].rearrange("p t j -> p (t j)")), reads=[desti], writes=[Db])
            P.wait_all("sp", [Db])
            P.pop()
            P.emit()
            return nc
        P.pop()
        P.push()
        cxt = [sb("cxt%d" % i, [128, D], F32) for i in range(2)]
        ct1 = sb("ct1", [128, D], F32)
        cz = sb("cz", [128, D], F32)
        cxn = sb("cxn", [128, D], F32)
        cstats = sb("cstats", [128, 2, 6], F32)
        cmv = sb("cmv", [128, 2], F32)
        crstd = sb("crstd", [128, 1], F32)
        yh = [sb("yh%d" % i, [128, D], F32) for i in range(2)]
        yl = [sb("yl%d" % i, [128, D], F32) for i in range(2)]
        outt = [sb("outt%d" % i, [128, D], F32) for i in range(2)]
        Yb = Buf("y")
        import os
        COMB = os.environ.get("COMB", "full")
        for t in range(NT):
            b = t % 2
            r0 = t * 128
            if COMB == "a":
                P.dma("sp", lambda e, b=b, r0=r0: e.dma_start(out=cxt[b].t[:], in_=X1[r0:r0 + 128, :]), reads=[X1b], writes=[cxt[b]])
                P.dma("sp", lambda e, b=b, r0=r0: e.dma_start(out=y_d[r0:r0 + 128, :], in_=cxt[b].t[:]), reads=[cxt[b]], writes=[Yb])
                continue
            if COMB == "none":
                continue
            P.dma("pool", lambda e, b=b, t=t: e.indirect_dma_start(
                out=yh[b].t[:, :], out_offset=None, in_=YS[:, :],
                in_offset=bass.IndirectOffsetOnAxis(ap=desti.t[:, t, 0:1], axis=0)), reads=[YSb, desti], writes=[yh[b]])
            P.dma("pool", lambda e, b=b, t=t: e.indirect_dma_start(
                out=yl[b].t[:, :], out_offset=None, in_=YS[:, :],
                in_offset=bass.IndirectOffsetOnAxis(ap=desti.t[:, t, 1:2], axis=0)), reads=[YSb, desti], writes=[yl[b]])
            P.dma("sp", lambda e, b=b, r0=r0: e.dma_start(out=cxt[b].t[:], in_=X1[r0:r0 + 128, :]), reads=[X1b], writes=[cxt[b]])
            if dbg == 4 and t == 0:
                Dbb = Buf("dbb")
                P.dma("sp", lambda e: e.dma_start(out=dbg_a[:, 0:D], in_=yh[0].t[:]), reads=[yh[0]], writes=[Dbb])
                P.dma("sp", lambda e: e.dma_start(out=dbg_a[:, D:2 * D], in_=yl[0].t[:]), reads=[yl[0]], writes=[Dbb])
                P.dma("sp", lambda e: e.dma_start(out=dbg_a[:, 2 * D:3 * D], in_=cxt[0].t[:]), reads=[cxt[0]], writes=[Dbb])
            P.op("dve", lambda e, b=b, t=t: e.tensor_scalar(yh[b].t[:], yh[b].t[:], wts.t[:, t, 0:1], None, ALU.mult), reads=[yh[b], wts], writes=[yh[b]])
            P.op("dve", lambda e, b=b, t=t: e.scalar_tensor_tensor(yh[b].t[:], yl[b].t[:], wts.t[:, t, 1:2], yh[b].t[:], ALU.mult, ALU.add),
                 reads=[yl[b], yh[b], wts], writes=[yh[b]])
            P.op("dve", lambda e, b=b: e.tensor_tensor(ct1.t[:], yh[b].t[:], g2B.t[:], ALU.mult), reads=[yh[b], g2B], writes=[ct1])
            P.op("dve", lambda e, b=b: e.scalar_tensor_tensor(cz.t[:], cxt[b].t[:], ALPHA, ct1.t[:], ALU.mult, ALU.add), reads=[cxt[b], ct1], writes=[cz])
            if dbg == 4 and t == 0:
                P.dma("sp", lambda e: e.dma_start(out=dbg_a[:, 3 * D:4 * D], in_=cz.t[:]), reads=[cz], writes=[Dbb])
            emit_layernorm(P, cz, cstats, cmv, crstd, cxn)
            P.op("dve", lambda e, b=b: e.tensor_tensor(outt[b].t[:], cxn.t[:], lgB[1].t[:], ALU.mult), reads=[cxn, lgB[1]], writes=[outt[b]])
            P.op("dve", lambda e, b=b: e.tensor_tensor(outt[b].t[:], outt[b].t[:], lbB[1].t[:], ALU.add), reads=[outt[b], lbB[1]], writes=[outt[b]])
            P.dma("sp", lambda e, b=b, r0=r0: e.dma_start(out=y_d[r0:r0 + 128, :], in_=outt[b].t[:]), reads=[outt[b]], writes=[Yb])
        if dbg in (3, 4):
            P.dma("sp", lambda e: e.dma_start(out=dbg_i, in_=desti.t[:].rearrange("p t j -> p (t j)")), reads=[desti], writes=[Yb])
        P.wait_all("sp", [Yb])
        P.pop()
        P.emit()
    return nc


_CACHE = {}


def _consts():
    idn = np.eye(128, dtype=np.float32)
    tri = np.triu(np.ones((128, 128), np.float32), 1)
    offs = (np.arange(NEXP, dtype=np.float32) * CAP + 1.0)[None, :]
    return idn, tri, offs


def run_ffn(x, o, c, ada_w_l, ada_b_l, ln_g_l, ln_b_l, w_o, router_w, router_b, wg, wu, wd):
    if "ffn" not in _CACHE:
        _CACHE["ffn"] = build_ffn()
    nc = _CACHE["ffn"]
    idn, tri, offs = _consts()
    B, S, _ = x.shape
    xf = x.reshape(B * S, D)
    of = o.reshape(B * S, D)
    in_maps = []
    for core in range(8):
        r0 = core * NTOK
        b = r0 // S
        in_maps.append({
            "x": np.ascontiguousarray(xf[r0:r0 + NTOK]), "o": np.ascontiguousarray(of[r0:r0 + NTOK]),
            "cT": np.ascontiguousarray(c[b].reshape(8, 128).T),
            "adaw": ada_w_l, "adab": ada_b_l, "lng": ln_g_l, "lnb": ln_b_l, "wo": w_o,
            "rw": router_w, "rb": router_b.reshape(1, NEXP), "wg": wg, "wu": wu, "wd": wd,
            "idn": idn, "tri": tri, "offs": offs,
        })
    res = run_bass_kernel_spmd(nc, in_maps, core_ids=list(range(8)))
    return np.concatenate([r["y"] for r in res.results], axis=0).reshape(B, S, D)


S_LEN = 16384
SPAN = 2048
TWO_PI = 6.283185307179586
PI = 3.141592653589793


def rope_inv_table():
    inv = 500000.0 ** (-np.arange(8, dtype=np.float64) * (2.0 / 16))
    t = np.zeros((128, 1), np.float32)
    for p in range(128):
        f = p % 64
        if f < 16:
            t[p, 0] = np.float32(inv[f % 8])
    return t


def emit_mod_cols(P, nc, adaw_d, adabT_d, cT_d, ncols, shp, psum_buf):
    sb = P.sbuf
    cT = sb("cT", [128, 8], F32)
    cond = sb("cond", [128, 8], F32)
    modp = sb("modp", [128, ncols // 128], F32)
    abT = sb("abT", [128, ncols // 128], F32)
    P.dma("sp", lambda e: e.dma_start(out=cT.t[:], in_=cT_d), writes=[cT])
    P.dma("sp", lambda e: e.dma_start(out=abT.t[:], in_=adabT_d), writes=[abT])
    P.op("act", lambda e: e.activation(cond.t[:], cT.t[:], AF.Silu), reads=[cT], writes=[cond])
    P.push()
    awb = [sb("awb%d" % i, [128, 8, 512], F32) for i in range(2)]
    for blk in range(ncols // 512):
        wb = awb[blk % 2]
        P.dma(["sp", "act"][blk % 2], lambda e, blk=blk, wb=wb: e.dma_start(
            out=wb.t[:], in_=adaw_d[:, blk * 512:(blk + 1) * 512].rearrange("(k p) n -> p k n", p=128)), writes=[wb])
        for j in range(4):
            for kk in range(8):
                P.op("pe", lambda e, j=j, kk=kk, wb=wb, blk=blk: e.matmul(
                    psum_buf.t[:, blk * 4 + j:blk * 4 + j + 1], wb.t[:, kk, j * 128:(j + 1) * 128], cond.t[:, kk:kk + 1],
                    start=(kk == 0), stop=(kk == 7)), reads=[wb, cond], writes=[psum_buf])
    P.op("dve", lambda e: e.tensor_tensor(modp.t[:], psum_buf.t[:, 0:ncols // 128], abT.t[:], ALU.add),
         reads=[psum_buf, abT], writes=[modp])
    P.pop()
    return modp


def emit_rope_tables(P, posB, posI, invp, negpi, Ct, St, tmp, pos_ap, n):
    C1 = 6.28125
    C2 = TWO_PI - C1
    P.dma("sp", lambda e: e.dma_start(out=posI.t[:, 0:n], in_=pos_ap.to_broadcast([128, n])), writes=[posI])
    P.op("dve", lambda e: e.tensor_copy(posB.t[:, 0:n], posI.t[:, 0:n]), reads=[posI], writes=[posB])
    for off, dst in ((0.0, St), (0.5 * PI, Ct)):
        P.op("dve", lambda e, off=off: e.tensor_scalar(tmp.t[:, 0:n], posB.t[:, 0:n], invp.t[:, 0:1], off, ALU.mult, ALU.add),
             reads=[posB, invp], writes=[tmp])
        P.op("dve", lambda e: e.tensor_scalar(dst.t[:, 0:n], tmp.t[:, 0:n], 1.0 / TWO_PI, None, ALU.mult), reads=[tmp], writes=[dst])
        P.op("dve", lambda e: e.tensor_copy(posI.t[:, 0:n], dst.t[:, 0:n]), reads=[dst], writes=[posI])
        P.op("dve", lambda e, dst=dst: e.tensor_copy(dst.t[:, 0:n], posI.t[:, 0:n]), reads=[posI], writes=[dst])
        P.op("dve", lambda e, dst=dst: e.scalar_tensor_tensor(tmp.t[:, 0:n], dst.t[:, 0:n], -C1, tmp.t[:, 0:n], ALU.mult, ALU.add),
             reads=[dst, tmp], writes=[tmp])
        P.op("dve", lambda e, dst=dst: e.scalar_tensor_tensor(tmp.t[:, 0:n], dst.t[:, 0:n], -C2, tmp.t[:, 0:n], ALU.mult, ALU.add),
             reads=[dst, tmp], writes=[tmp])
        P.op("dve", lambda e, dst=dst: e.tensor_scalar(dst.t[:, 0:n], tmp.t[:, 0:n], PI, -TWO_PI, ALU.is_gt, ALU.mult), reads=[tmp], writes=[dst])
        P.op("dve", lambda e, dst=dst: e.tensor_tensor(tmp.t[:, 0:n], tmp.t[:, 0:n], dst.t[:, 0:n], ALU.add), reads=[tmp, dst], writes=[tmp])
        P.op("dve", lambda e, dst=dst: e.tensor_scalar(dst.t[:, 0:n], tmp.t[:, 0:n], -PI, TWO_PI, ALU.is_lt, ALU.mult), reads=[tmp], writes=[dst])
        P.op("dve", lambda e, dst=dst: e.tensor_tensor(tmp.t[:, 0:n], tmp.t[:, 0:n], dst.t[:, 0:n], ALU.add), reads=[tmp, dst], writes=[tmp])
        P.op("dve", lambda e: e.tensor_scalar(tmp.t[:, 0:n], tmp.t[:, 0:n], PI, -PI, ALU.min, ALU.max), reads=[tmp], writes=[tmp])
        P.op("act", lambda e, dst=dst: e.activation(dst.t[:, 0:n], tmp.t[:, 0:n], AF.Sin), reads=[tmp], writes=[dst])


def emit_rot_weights(P, w, wr, nheads):
    P.op("pool", lambda e: e.memset(wr.t[:], 0.0), writes=[wr])
    for k in range(8):
        wv = w.t[:, k, :].rearrange("p (h e) -> p h e", e=64)
        rv = wr.t[:, k, :].rearrange("p (h e) -> p h e", e=64)
        P.op("dve", lambda e, wv=wv, rv=rv: e.tensor_scalar(rv[:, :, 0:8], wv[:, :, 8:16], -1.0, None, ALU.mult), reads=[w], writes=[wr])
        P.op("dve", lambda e, wv=wv, rv=rv: e.tensor_copy(rv[:, :, 8:16], wv[:, :, 0:8]), reads=[w], writes=[wr])


def emit_hmodT_tile(P, x_d, tok0, xt, psX, idn, modp, hT, col0, sc_off, q, width=1024):
    P.dma(q, lambda e: e.dma_start(out=xt.t[:], in_=x_d[tok0:tok0 + 128, :]), writes=[xt])
    nb_ = width // 128
    for k0 in range(0, 8, nb_):
        for kk in range(nb_):
            k = k0 + kk
            P.op("pe", lambda e, k=k, kk=kk: e.transpose(psX.t[:, kk * 128:(kk + 1) * 128], xt.t[:, k * 128:(k + 1) * 128], idn.t[:]),
                 reads=[xt, idn], writes=[psX])
        for kk in range(nb_):
            k = k0 + kk
            P.op("act", lambda e, k=k, kk=kk: e.activation(hT.t[:, k, col0:col0 + 128], psX.t[:, kk * 128:(kk + 1) * 128], AF.Identity,
                                                         bias=modp.t[:, k:k + 1], scale=modp.t[:, sc_off + k:sc_off + k + 1]),
                 reads=[psX, modp], writes=[hT])


def emit_proj_rope(P, hT, t0, n, w, wr, wc0, ps_a, ps_b, Ct, St, tcol0, tmpa, tmpb, out_ap_fn, out_buf):
    for k in range(8):
        P.op("pe", lambda e, k=k: e.matmul(ps_a.t[:, 0:n], w.t[:, k, wc0:wc0 + 128], hT.t[:, k, t0:t0 + n], start=(k == 0), stop=(k == 7)),
             reads=[w, hT], writes=[ps_a])
    for k in range(8):
        P.op("pe", lambda e, k=k: e.matmul(ps_b.t[:, 0:n], wr.t[:, k, wc0:wc0 + 128], hT.t[:, k, t0:t0 + n], start=(k == 0), stop=(k == 7)),
             reads=[wr, hT], writes=[ps_b])
    P.op("dve", lambda e: e.tensor_tensor(tmpa.t[:, 0:n], ps_a.t[:, 0:n], Ct.t[:, tcol0:tcol0 + n], ALU.mult), reads=[ps_a, Ct], writes=[tmpa])
    P.op("dve", lambda e: e.tensor_tensor(tmpb.t[:, 0:n], ps_b.t[:, 0:n], St.t[:, tcol0:tcol0 + n], ALU.mult), reads=[ps_b, St], writes=[tmpb])
    P.op("dve", lambda e: e.tensor_tensor(out_ap_fn(), tmpa.t[:, 0:n], tmpb.t[:, 0:n], ALU.add), reads=[tmpa, tmpb], writes=[out_buf])


def tri_masks():
    p = np.arange(128)[:, None]
    f = np.arange(128)[None, :]
    m0 = np.where(f <= p, 0.0, NEG).astype(np.float32)
    m1 = np.where(f >= p, 0.0, NEG).astype(np.float32)
    return np.stack([np.tile(m0, (1, 4)), np.tile(m1, (1, 4))], axis=1)


DIL = (1, 4, 16)


def build_dil():
    nc = bass.Bass("TRN2", target_bir_lowering=False)
    dt_in = lambda name, shape, dt=F32: nc.dram_tensor(name, list(shape), dt, kind="ExternalInput").ap()
    x_d = dt_in("x", [S_LEN, D])
    cT_d = dt_in("cT", [128, 8])
    adaw_d = dt_in("adaw", [D, 2 * D])
    adabT_d = dt_in("adabT", [128, 16])
    wq_d = dt_in("wq", [D, 256])
    wk_d = dt_in("wk", [3, D, 64])
    wv_d = dt_in("wv", [3, D, 64])
    pos_d = dt_in("pos", [1, S_LEN], I32)
    inv_d = dt_in("inv", [128, 1])
    idn_d = dt_in("idn", [128, 128])
    msk_d = dt_in("msk", [128, 2, 512])
    o_d = nc.dram_tensor("o", [S_LEN, 256], F32, kind="ExternalOutput").ap()
    OP = [nc.dram_tensor("OP%d" % p, [S_LEN, 260], F32).ap() for p in range(3)]

    with ExitStack() as st:
        P = Prog(nc, st)
        sb = P.sbuf
        idn = sb("idn", [128, 128], F32)
        idnb = sb("idnb", [128, 128], BF16)
        msk = sb("msk", [128, 2, 512], BF16)
        invp = sb("invp", [128, 1], F32)
        negpi = sb("negpi", [128, 1], F32)
        wq = sb("wq", [128, 8, 256], BF16)
        wqr = sb("wqr", [128, 8, 256], BF16)
        wk = [sb("wk%d" % p, [128, 8, 128], BF16) for p in range(3)]
        wkr = [sb("wkr%d" % p, [128, 8, 128], BF16) for p in range(3)]
        wv = [sb("wv%d" % p, [128, 8, 64], BF16) for p in range(3)]
        ps = [P.psum("pb%d" % i, [128, 512], F32) for i in range(6)]
        psX = P.psum("psX", [128, 1024], F32)

        P.dma("sp", lambda e: e.dma_start(out=idn.t[:], in_=idn_d), writes=[idn])
        P.dma("pool", lambda e: e.dma_start(out=idnb.t[:], in_=idn_d), writes=[idnb])
        P.dma("pool", lambda e: e.dma_start(out=msk.t[:], in_=msk_d), writes=[msk])
        P.dma("sp", lambda e: e.dma_start(out=invp.t[:], in_=inv_d), writes=[invp])
        P.op("pool", lambda e: e.memset(negpi.t[:], -PI), writes=[negpi])
        P.dma("pool", lambda e: e.dma_start(out=wq.t[:], in_=wq_d.rearrange("(k p) n -> p k n", p=128)), writes=[wq])
        for p in range(3):
            for h in range(2):
                P.dma("pool", lambda e, p=p, h=h: e.dma_start(out=wk[p].t[:, :, h * 64:(h + 1) * 64],
                                                              in_=wk_d[p].rearrange("(k p) n -> p k n", p=128)), writes=[wk[p]])
            P.dma("pool", lambda e, p=p: e.dma_start(out=wv[p].t[:], in_=wv_d[p].rearrange("(k p) n -> p k n", p=128)), writes=[wv[p]])
        emit_rot_weights(P, wq, wqr, 4)
        for p in range(3):
            emit_rot_weights(P, wk[p], wkr[p], 2)
        modp = emit_mod_cols(P, nc, adaw_d, adabT_d, cT_d, 2 * D, None, ps[0])
        P.op("dve", lambda e: e.tensor_scalar(modp.t[:, 8:16], modp.t[:, 8:16], 1.0, None, ALU.add), reads=[modp], writes=[modp])

        NSP = S_LEN // SPAN
        hT = sb("hT", [128, 8, SPAN], BF16)
        xts = [sb("xts%d" % i, [128, D], F32) for i in range(2)]
        qT = [sb("qT%d" % i, [128, 2, SPAN], BF16) for i in range(2)]
        kT = [[sb("kT%d_%d" % (p, s_), [128, SPAN], BF16) for s_ in range(2)] for p in range(3)]
        V = [[sb("V%d_%d" % (p, s_), [128, 16, 80], BF16) for s_ in range(2)] for p in range(3)]
        posI = sb("posI", [128, SPAN], I32)
        posB = sb("posB", [128, SPAN], F32)
        Ct = sb("Ct", [128, SPAN], F32)
        St = sb("St", [128, SPAN], F32)
        tmpT = sb("tmpT", [128, SPAN], F32)
        tmpa = sb("tmpa", [128, 512], F32)
        tmpb = sb("tmpb", [128, 512], F32)
        PT = [sb("PT%d" % i, [128, 512], BF16) for i in range(6)]
        accs = [sb("accs%d" % i, [128, 260], F32) for i in range(3)]
        for p in range(3):
            for s_ in range(2):
                P.op("pool", lambda e, p=p, s_=s_: e.memset(V[p][s_].t[:, :, 64:65], 1.0), writes=[V[p][s_]])
        OPb = [Buf("OP%d" % p) for p in range(3)]
        import os
        STG = int(os.environ.get("DILSTAGE", "9"))
        ATT = int(os.environ.get("DILATT", "9"))
        NSP = int(os.environ.get("DILNSP", str(NSP)))
        cntd = {"PT": 0, "acc": 0}
        for s in range(NSP):
            sl = s % 2
            tok0 = s * SPAN
            for ti in range(16):
                emit_hmodT_tile(P, x_d, tok0 + ti * 128, xts[ti % 2], psX, idn, modp, hT, ti * 128, 8, ["sp", "act"][ti % 2])
            if STG < 2:
                continue
            emit_rope_tables(P, posB, posI, invp, negpi, Ct, St, tmpT, pos_d[0:1, tok0:tok0 + SPAN], SPAN)
            if STG < 3:
                continue
            for qc in range(2):
                for tg in range(4):
                    emit_proj_rope(P, hT, tg * 512, 512, wq, wqr, qc * 128, ps[0], ps[1], Ct, St, tg * 512, tmpa, tmpb,
                                   lambda qc=qc, tg=tg, sl=sl: qT[sl].t[:, qc, tg * 512:(tg + 1) * 512], qT[sl])
            for p in range(3):
                for tg in range(4):
                    emit_proj_rope(P, hT, tg * 512, 512, wk[p], wkr[p], 0, ps[0], ps[1], Ct, St, tg * 512, tmpa, tmpb,
                                   lambda p=p, tg=tg, sl=sl: kT[p][sl].t[:, tg * 512:(tg + 1) * 512], kT[p][sl])
            if STG < 4:
                continue
            for p, d in enumerate(DIL):
                ncb = 16 // d
                for r in range(d):
                    for c in range(ncb):
                        idx = r * ncb + c
                        a0 = r + d * 128 * c
                        pv = ps[2 + idx % 2]
                        for k in range(8):
                            P.op("pe", lambda e, k=k, a0=a0, d=d, p=p, pv=pv: e.matmul(
                                pv.t[:, 0:64], hT.t[:, k, ss(a0, 128, d)], wv[p].t[:, k, :],
                                start=(k == 0), stop=(k == 7)), reads=[hT, wv[p]], writes=[pv])
                        P.op("act", lambda e, p=p, sl=sl, idx=idx, pv=pv: e.copy(V[p][sl].t[:, idx, 0:64], pv.t[:, 0:64]),
                             reads=[pv], writes=[V[p][sl]])
            if STG < 5:
                continue
            tiles = []
            for p, d in enumerate(DIL):
                ncb = 16 // d
                for r in range(d):
                    for j in range(ncb):
                        chunks = [(j - 1, 0), (j, 1)]
                        chunks = [(kb, mc) for kb, mc in chunks if not (s == 0 and kb < 0)]
                        tiles.append((p, d, ncb, r, j, chunks))

            def stage_a(ti):
                p, d, ncb, r, j, chunks = tiles[ti]
                pts = []
                for ci, (kb, mc) in enumerate(chunks):
                    ksl = sl if kb >= 0 else 1 - sl
                    kbb = kb if kb >= 0 else ncb - 1
                    ka0 = r + d * 128 * kbb
                    qa0 = r + d * 128 * j
                    Sp = ps[(ti % 2) * 2 + ci]
                    for h in range(4):
                        qc, hf = h // 2, h % 2
                        rows = slice(hf * 64, (hf + 1) * 64)
                        P.op("pe", lambda e, h=h, qc=qc, rows=rows, p=p, ksl=ksl, ka0=ka0, qa0=qa0, d=d, Sp=Sp, sl=sl: e.matmul(
                            Sp.t[:, h * 128:(h + 1) * 128],
                            kT[p][ksl].t[rows, ss(ka0, 128, d)],
                            qT[sl].t[rows, qc, ss(qa0, 128, d)],
                            start=True, stop=False), reads=[kT[p][ksl], qT[sl]], writes=[Sp])
                        P.op("pe", lambda e, h=h, mc=mc, Sp=Sp: e.matmul(Sp.t[:, h * 128:(h + 1) * 128], idnb.t[:], msk.t[:, mc, 0:128],
                                                                      start=False, stop=True), reads=[idnb, msk], writes=[Sp])
                    pt = PT[cntd["PT"] % 6]
                    cntd["PT"] += 1
                    P.op("act", lambda e, pt=pt, Sp=Sp: e.activation(pt.t[:], Sp.t[:, 0:512], AF.Exp, scale=0.125), reads=[Sp], writes=[pt])
                    pts.append((pt, ksl, r * ncb + kbb))
                return pts

            def stage_b(ti, pts):
                p, d, ncb, r, j, chunks = tiles[ti]
                acc = ps[4 + ti % 2]
                for h in range(4):
                    for ci, (pt, ksl, vidx) in enumerate(pts):
                        P.op("pe", lambda e, h=h, pt=pt, p=p, ksl=ksl, vidx=vidx, acc=acc, ci=ci, nch=len(pts): e.matmul(
                            acc.t[:, h * 80:h * 80 + 65], pt.t[:, h * 128:(h + 1) * 128], V[p][ksl].t[:, vidx, 0:65],
                            start=(ci == 0), stop=(ci == nch - 1)), reads=[pt, V[p][ksl]], writes=[acc])
                ab = accs[cntd["acc"] % 3]
                cntd["acc"] += 1
                P.op("dve", lambda e, ab=ab, acc=acc: e.tensor_copy(ab.t[:].rearrange("p (h e) -> p h e", e=65), acc.t[:, 0:320].rearrange("p (h e) -> p h e", e=80)[:, :, 0:65]), reads=[acc], writes=[ab])
                g0 = tok0 + r + d * 128 * j
                P.dma("sp", lambda e, ab=ab, p=p, g0=g0, d=d: e.dma_start(out=OP[p][ss(g0, 128, d), :], in_=ab.t[:]), reads=[ab], writes=[OPb[p]])

            nxt = stage_a(0)
            for ti in range(len(tiles)):
                cur = nxt
                if ti + 1 < len(tiles):
                    nxt = stage_a(ti + 1)
                stage_b(ti, cur)
        P.barrier()
        if STG < 6:
            P.emit()
            return nc
        ld = [[sb("ld%d_%d" % (p, i), [128, 260], F32) for i in range(2)] for p in range(3)]
        rl = sb("rl", [128, 4], F32)
        ot = [sb("otl%d" % i, [128, 256], F32) for i in range(2)]
        Ob = Buf("o")
        for T in range(S_LEN // 128):
            b = T % 2
            for p in range(3):
                P.dma(["sp", "act", "sp"][p], lambda e, p=p, b=b, T=T: e.dma_start(out=ld[p][b].t[:], in_=OP[p][T * 128:(T + 1) * 128, :]),
                      reads=[OPb[p]], writes=[ld[p][b]])
            P.op("dve", lambda e, b=b: e.tensor_tensor(ld[0][b].t[:], ld[0][b].t[:], ld[1][b].t[:], ALU.add), reads=[ld[0][b], ld[1][b]], writes=[ld[0][b]])
            P.op("dve", lambda e, b=b: e.tensor_tensor(ld[0][b].t[:], ld[0][b].t[:], ld[2][b].t[:], ALU.add), reads=[ld[0][b], ld[2][b]], writes=[ld[0][b]])
            a3 = ld[0][b].t[:].rearrange("p (h e) -> p h e", e=65)
            P.op("dve", lambda e, a3=a3: e.reciprocal(rl.t[:], a3[:, :, 64]), reads=[ld[0][b]], writes=[rl])
            P.op("dve", lambda e, a3=a3, b=b: e.tensor_tensor(ot[b].t[:].rearrange("p (h e) -> p h e", e=64), a3[:, :, 0:64],
                                                             rl.t[:].unsqueeze(2).to_broadcast([128, 4, 64]), ALU.mult),
                 reads=[ld[0][b], rl], writes=[ot[b]])
            P.dma("sp", lambda e, b=b, T=T: e.dma_start(out=o_d[T * 128:(T + 1) * 128, :], in_=ot[b].t[:]), reads=[ot[b]], writes=[Ob])
        P.wait_all("sp", [Ob])
        P.emit()
    return nc


def run_dil(x, c, positions, ada_w_s, ada_b_s, w_in):
    if "dil" not in _CACHE:
        _CACHE["dil"] = build_dil()
    nc = _CACHE["dil"]
    idn = np.eye(128, dtype=np.float32)
    inv = rope_inv_table()
    msk = tri_masks()
    in_maps = []
    for core in range(8):
        b, g = core // 4, core % 4
        wk = np.stack([w_in[:, D + p * 512 + g * 64: D + p * 512 + g * 64 + 64] for p in range(3)])
        wv = np.stack([w_in[:, D + p * 512 + 256 + g * 64: D + p * 512 + 256 + g * 64 + 64] for p in range(3)])
        in_maps.append({
            "x": x[b], "cT": np.ascontiguousarray(c[b].reshape(8, 128).T),
            "adaw": np.ascontiguousarray(ada_w_s[:, 0:2 * D]), "adabT": np.ascontiguousarray(ada_b_s[0:2 * D].reshape(16, 128).T),
            "wq": np.ascontiguousarray(w_in[:, g * 256:(g + 1) * 256]), "wk": np.ascontiguousarray(wk), "wv": np.ascontiguousarray(wv),
            "pos": np.ascontiguousarray(positions[b:b + 1]), "inv": inv, "idn": idn, "msk": msk,
        })
    res = run_bass_kernel_spmd(nc, in_maps, core_ids=list(range(8)))
    B = x.shape[0]
    o = np.zeros((B, S_LEN, D), np.float32)
    for core in range(8):
        b, g = core // 4, core % 4
        o[b, :, g * 256:(g + 1) * 256] = res.results[core]["o"]
    return o


QG = 512
NG = S_LEN // QG
NCMP = 1023


def nsa_masks():
    p = np.arange(128)[:, None]
    f = np.arange(512)[None, :]
    ms = []
    for jj in range(4):
        ms.append(np.where(f - p - 128 * jj >= 0, 0.0, NEG))
    for c in range(4):
        ms.append(np.where(f - p < 128 * c, 0.0, NEG))
    for m in range(5):
        ms.append(np.where(f - 16 * p + 512 * m - 31 >= 0, 0.0, NEG))
    return np.stack(ms, axis=1).astype(np.float32)


def nsa_indc():
    t = np.zeros((128, 64, 128), np.float32)
    for jm in range(64):
        t[2 * jm, jm, 0:64] = 1.0
        t[2 * jm + 1, jm, 64:128] = 1.0
    return t


def nsa_vc_const():
    t = np.zeros((1024, 257), np.float32)
    t[:, 0] = 1.0
    for s_ in range(256):
        for n in range(max(0, 4 * s_ - 1), min(NCMP, 4 * s_ + 4)):
            t[n, 1 + s_] = 1.0
    return t


def nsa_forced():
    t = np.zeros((128, 3), np.float32)
    for p in range(128):
        c = p // 64
        for x in (-1, 0, 1):
            if x == c or x == c - 1:
                t[p, x + 1] = 1.0e4
    return t


def build_nsa():
    nc = bass.Bass("TRN2", target_bir_lowering=False)
    dt_in = lambda name, shape, dt=F32: nc.dram_tensor(name, list(shape), dt, kind="ExternalInput").ap()
    x_d = dt_in("x", [S_LEN, D])
    cT_d = dt_in("cT", [128, 8])
    adaw_d = dt_in("adaw", [D, 2 * D])
    adabT_d = dt_in("adabT", [128, 16])
    wq_d = dt_in("wq", [D, 256])
    wkv_d = dt_in("wkv", [6, D, 64])
    wgt_d = dt_in("wgt", [D, 12])
    pos_d = dt_in("pos", [1, S_LEN], I32)
    posc_d = dt_in("posc", [1, 1024], I32)
    inv_d = dt_in("inv", [128, 1])
    idn_d = dt_in("idn", [128, 128])
    msk_d = dt_in("msk", [128, 13, 512])
    gp_d = dt_in("gp", [64, S_LEN])
    vcc_d = dt_in("vcc", [1024, 257])
    frc_d = dt_in("frc", [128, 3])
    w1_d = dt_in("w1", [2, 2048, 256])
    w2_d = dt_in("w2", [2, 256, 64])
    cpos_d = dt_in("cposT", [128, 32])
    o_d = nc.dram_tensor("o", [S_LEN, 256], F32, kind="ExternalOutput").ap()

    with ExitStack() as st:
        P = Prog(nc, st)
        sb = P.sbuf
        idn = sb("idn", [128, 128], F32)
        idnb = sb("idnb", [128, 128], BF16)
        msk = sb("msk", [128, 13, 512], BF16)
        frc = sb("frc", [128, 3], F32)
        invp = sb("invp", [128, 1], F32)
        wqh = [sb("wqh%d" % h_, [128, 8, 128], BF16) for h_ in range(4)]
        wqhr = [sb("wqhr%d" % h_, [128, 8, 128], BF16) for h_ in range(4)]
        wks = sb("wks", [128, 8, 128], BF16)
        wksr = sb("wksr", [128, 8, 128], BF16)
        wkw = sb("wkw", [128, 8, 128], BF16)
        wkwr = sb("wkwr", [128, 8, 128], BF16)
        wkvc = sb("wkvc", [128, 8, 128], BF16)
        wvs = sb("wvs", [128, 8, 64], BF16)
        wvw = sb("wvw", [128, 8, 64], BF16)
        wgt = sb("wgt", [128, 8, 12], BF16)
        kselT = sb("kselT", [128, S_LEN], BF16)
        Vsel = sb("Vsel", [128, 128, 80], BF16)
        kcT = sb("kcT", [128, 1024], BF16)
        VC = sb("VC", [128, 8, 336], BF16)
        ps = [P.psum("pb%d" % i, [128, 512], F32) for i in range(6)]
        psXb = P.psum("psX", [128, 512], F32)
        psS3 = P.psum("psS3", [128, 512], F32)
        spr = [ps[0], ps[1], psS3]

        def ld(q, dst, src, ap=None):
            P.dma(q, lambda e: e.dma_start(out=dst.t[:] if ap is None else ap, in_=src), writes=[dst])
        ld("sp", idn, idn_d)
        ld("pool", idnb, idn_d)
        ld("pool", msk, msk_d)
        ld("sp", frc, frc_d)
        ld("sp", invp, inv_d)
        kp = lambda a: a.rearrange("(k p) n -> p k n", p=128)
        for h_ in range(4):
            P.op("pool", lambda e, h_=h_: e.memset(wqh[h_].t[:], 0.0), writes=[wqh[h_]])
            ld("pool", wqh[h_], kp(wq_d[:, h_ * 64:(h_ + 1) * 64]), wqh[h_].t[:, :, 0:64])
        for h in range(2):
            ld("pool", wks, kp(wkv_d[2]), wks.t[:, :, h * 64:(h + 1) * 64])
            ld("pool", wkw, kp(wkv_d[4]), wkw.t[:, :, h * 64:(h + 1) * 64])
        ld("pool", wkvc, kp(wkv_d[0]), wkvc.t[:, :, 0:64])
        ld("pool", wkvc, kp(wkv_d[1]), wkvc.t[:, :, 64:128])
        ld("pool", wvs, kp(wkv_d[3]))
        ld("pool", wvw, kp(wkv_d[5]))
        ld("pool", wgt, kp(wgt_d))
        for h_ in range(4):
            emit_rot_weights(P, wqh[h_], wqhr[h_], 2)
        emit_rot_weights(P, wks, wksr, 2)
        emit_rot_weights(P, wkw, wkwr, 2)
        P.op("pool", lambda e: e.memset(Vsel.t[:, :, 64:65], 1.0), writes=[Vsel])
        for c in range(8):
            P.dma("pool", lambda e, c=c: e.dma_start(out=VC.t[:, c, 64:321], in_=vcc_d[c * 128:(c + 1) * 128, :]), writes=[VC])
        modp = emit_mod_cols(P, nc, adaw_d, adabT_d, cT_d, 2 * D, None, ps[0])
        P.op("dve", lambda e: e.tensor_scalar(modp.t[:, 8:16], modp.t[:, 8:16], 1.0, None, ALU.add), reads=[modp], writes=[modp])

        hT = sb("hT", [128, 8, QG], BF16)
        xts = [sb("xts%d" % i, [128, D], F32) for i in range(2)]
        posI = sb("posI", [128, 512], I32)
        posB = sb("posB", [128, 512], F32)
        Ct = sb("Ct", [128, 512], F32)
        St = sb("St", [128, 512], F32)
        tmpT = sb("tmpT", [128, 512], F32)
        tmpa = sb("tmpa", [128, 512], F32)
        tmpb = sb("tmpb", [128, 512], F32)

        import os
        NST = int(os.environ.get("NSA_STAGE", "9"))
        if NST < 2:
            P.barrier(); P.emit(); return nc
        P.push()
        kvcT = sb("kvcT", [128, S_LEN], BF16)
        for G in range(NG):
            t0 = G * QG
            for ti in range(4):
                emit_hmodT_tile(P, x_d, t0 + ti * 128, xts[ti % 2], psXb, idn, modp, hT, ti * 128, 8, ["sp", "act"][ti % 2], width=512)
            emit_rope_tables(P, posB, posI, invp, None, Ct, St, tmpT, pos_d[0:1, t0:t0 + QG], QG)
            emit_proj_rope(P, hT, 0, QG, wks, wksr, 0, ps[0], ps[1], Ct, St, 0, tmpa, tmpb,
                           lambda t0=t0: kselT.t[:, t0:t0 + QG], kselT)
            for k in range(8):
                P.op("pe", lambda e, k=k: e.matmul(ps[2].t[:, 0:QG], wkvc.t[:, k, :], hT.t[:, k, :], start=(k == 0), stop=(k == 7)),
                     reads=[wkvc, hT], writes=[ps[2]])
            P.op("act", lambda e, t0=t0: e.copy(kvcT.t[:, t0:t0 + QG], ps[2].t[:, 0:QG]), reads=[ps[2]], writes=[kvcT])
            for ti in range(4):
                pv = ps[3 + ti % 2]
                for k in range(8):
                    P.op("pe", lambda e, k=k, ti=ti, pv=pv: e.matmul(pv.t[:, 0:64], hT.t[:, k, ti * 128:(ti + 1) * 128], wvs.t[:, k, :],
                                                                     start=(k == 0), stop=(k == 7)), reads=[hT, wvs], writes=[pv])
                P.op("act", lambda e, ti=ti, G=G, pv=pv: e.copy(Vsel.t[:, G * 4 + ti, 0:64], pv.t[:, 0:64]), reads=[pv], writes=[Vsel])

        P.dma("pool", lambda e: e.dma_start(out=kselT.t[64:128, :], in_=gp_d), writes=[kselT])
        if NST < 3:
            P.barrier(); P.emit(); return nc
        w1 = sb("w1", [128, 32, 256], BF16)
        w2k = sb("w2k", [128, 2, 128], BF16)
        w2kr = sb("w2kr", [128, 2, 128], BF16)
        w2v = sb("w2v", [128, 2, 64], BF16)
        cposT = sb("cposT", [128, 32], BF16)
        cbias = sb("cbias", [128, 4], F32)
        hid = [[sb("hid%d_%d" % (kv, hc), [128, 1024], BF16) for hc in range(2)] for kv in range(2)]
        xg = sb("xg", [128, 512], F32)
        ug = sb("ug", [128, 512], F32)
        for kv in range(2):
            P.dma("pool", lambda e, kv=kv: e.dma_start(out=w1.t[kv * 64:(kv + 1) * 64, :, :], in_=w1_d[kv].rearrange("(l e) h -> e l h", e=64)), writes=[w1])
        for h in range(2):
            P.dma("pool", lambda e, h=h: e.dma_start(out=w2k.t[:, :, h * 64:(h + 1) * 64], in_=w2_d[0].rearrange("(k p) n -> p k n", p=128)), writes=[w2k])
        P.dma("pool", lambda e: e.dma_start(out=w2v.t[:], in_=w2_d[1].rearrange("(k p) n -> p k n", p=128)), writes=[w2v])
        P.dma("pool", lambda e: e.dma_start(out=cposT.t[:], in_=cpos_d), writes=[cposT])
        P.op("pool", lambda e: e.memset(w2kr.t[:], 0.0), writes=[w2kr])
        for k in range(2):
            wv_ = w2k.t[:, k, :].rearrange("p (h e) -> p h e", e=64)
            rv_ = w2kr.t[:, k, :].rearrange("p (h e) -> p h e", e=64)
            P.op("dve", lambda e, wv_=wv_, rv_=rv_: e.tensor_scalar(rv_[:, :, 0:8], wv_[:, :, 8:16], -1.0, None, ALU.mult), reads=[w2k], writes=[w2kr])
            P.op("dve", lambda e, wv_=wv_, rv_=rv_: e.tensor_copy(rv_[:, :, 8:16], wv_[:, :, 0:8]), reads=[w2k], writes=[w2kr])
        for kv in range(2):
            for hc in range(2):
                P.op("pool", lambda e, kv=kv, hc=hc: e.memset(hid[kv][hc].t[:], 0.0), writes=[hid[kv][hc]])
        for kv in range(2):
            rows = slice(kv * 64, (kv + 1) * 64)
            for hc in range(2):
                col = kv * 2 + hc
                for l in range(32):
                    P.op("pe", lambda e, rows=rows, hc=hc, l=l, col=col: e.matmul(
                        ps[0].t[:, col:col + 1], w1.t[rows, l, hc * 128:(hc + 1) * 128], cposT.t[rows, l:l + 1],
                        start=(l == 0), stop=(l == 31)), reads=[w1, cposT], writes=[ps[0]])
        P.op("dve", lambda e: e.tensor_copy(cbias.t[:], ps[0].t[:, 0:4]), reads=[ps[0]], writes=[cbias])
        for kv in range(2):
            rows = slice(kv * 64, (kv + 1) * 64)
            for hc in range(2):
                col = kv * 2 + hc
                for gi in range(2):
                    n0 = gi * 512
                    nn = 512 if gi == 0 else 511
                    pz = ps[1 + (hc * 2 + gi) % 2]
                    for l in range(32):
                        P.op("pe", lambda e, rows=rows, hc=hc, l=l, n0=n0, nn=nn, pz=pz: e.matmul(
                            pz.t[:, 0:nn], w1.t[rows, l, hc * 128:(hc + 1) * 128], kvcT.t[rows, ss(16 * n0 + l, nn, 16)],
                            start=(l == 0), stop=(l == 31)), reads=[w1, kvcT], writes=[pz])
                    P.op("act", lambda e, pz=pz, nn=nn, col=col: e.activation(xg.t[:, 0:nn], pz.t[:, 0:nn], AF.Identity, bias=cbias.t[:, col:col + 1]),
                         reads=[pz, cbias], writes=[xg])
                    P.op("dve", lambda e, nn=nn: e.tensor_tensor(ug.t[:, 0:nn], xg.t[:, 0:nn], xg.t[:, 0:nn], ALU.mult), reads=[xg], writes=[ug])
                    P.op("dve", lambda e, nn=nn: e.tensor_scalar(ug.t[:, 0:nn], ug.t[:, 0:nn], 0.044715, 1.0, ALU.mult, ALU.add), reads=[ug], writes=[ug])
                    P.op("dve", lambda e, nn=nn: e.tensor_tensor(ug.t[:, 0:nn], ug.t[:, 0:nn], xg.t[:, 0:nn], ALU.mult), reads=[ug, xg], writes=[ug])
                    P.op("act", lambda e, nn=nn: e.activation(ug.t[:, 0:nn], ug.t[:, 0:nn], AF.Sigmoid, scale=1.5957691216057308), reads=[ug], writes=[ug])
                    P.op("dve", lambda e, nn=nn, kv=kv, hc=hc, n0=n0: e.tensor_tensor(hid[kv][hc].t[:, n0:n0 + nn], ug.t[:, 0:nn], xg.t[:, 0:nn], ALU.mult),
                         reads=[ug, xg], writes=[hid[kv][hc]])
        for gi in range(2):
            n0 = gi * 512
            emit_rope_tables(P, posB, posI, invp, None, Ct, St, tmpT, posc_d[0:1, n0:n0 + 512], 512)
            for hc in range(2):
                P.op("pe", lambda e, hc=hc, n0=n0: e.matmul(ps[0].t[:, 0:512], w2k.t[:, hc, :], hid[0][hc].t[:, n0:n0 + 512], start=(hc == 0), stop=(hc == 1)),
                     reads=[w2k, hid[0][hc]], writes=[ps[0]])
            for hc in range(2):
                P.op("pe", lambda e, hc=hc, n0=n0: e.matmul(ps[1].t[:, 0:512], w2kr.t[:, hc, :], hid[0][hc].t[:, n0:n0 + 512], start=(hc == 0), stop=(hc == 1)),
                     reads=[w2kr, hid[0][hc]], writes=[ps[1]])
            P.op("dve", lambda e, n0=n0: e.tensor_tensor(tmpa.t[:], ps[0].t[:, 0:512], Ct.t[:, 0:512], ALU.mult), reads=[ps[0], Ct], writes=[tmpa])
            P.op("dve", lambda e, n0=n0: e.tensor_tensor(tmpb.t[:], ps[1].t[:, 0:512], St.t[:, 0:512], ALU.mult), reads=[ps[1], St], writes=[tmpb])
            P.op("pool", lambda e, n0=n0: e.tensor_tensor(kcT.t[:, n0:n0 + 512], tmpa.t[:], tmpb.t[:], ALU.add), reads=[tmpa, tmpb], writes=[kcT])
        for c in range(8):
            pv = ps[2 + c % 2]
            for hc in range(2):
                P.op("pe", lambda e, hc=hc, c=c, pv=pv: e.matmul(pv.t[:, 0:64], hid[1][hc].t[:, c * 128:(c + 1) * 128], w2v.t[:, hc, :],
                                                                 start=(hc == 0), stop=(hc == 1)), reads=[hid[1][hc], w2v], writes=[pv])
            P.op("act", lambda e, c=c, pv=pv: e.copy(VC.t[:, c, 0:64], pv.t[:, 0:64]), reads=[pv], writes=[VC])
        P.pop()
        if NST < 4:
            P.barrier(); P.emit(); return nc

        R = [[sb("R%d_%d" % (h_, w_), [128, QG], BF16) for w_ in range(4)] for h_ in range(4)]
        nbTw = [sb("nbTw%d" % w_, [128, QG], BF16) for w_ in range(4)]
        nbp = sb("nbp", [128, 320], F32)
        P.op("pool", lambda e: e.memset(nbp.t[:], 0.0), writes=[nbp])
        kwT = [sb("kwT%d" % i, [128, QG], BF16) for i in range(2)]
        Vw = sb("Vw", [128, 8, 80], BF16)
        gts = sb("gts", [128, 4, 12], F32)
        PT = [sb("PT%d" % i, [128, 512], BF16) for i in range(4)]
        impw = sb("impw", [128, 256], F32)
        m8a = sb("m8a", [128, 8], F32)
        m8b = sb("m8b", [128, 8], F32)
        P.op("pool", lambda e: e.memset(Vw.t[:, :, 64:65], 1.0), writes=[Vw])
        Ob = Buf("o")
        acc = [ps[2], ps[3], ps[4], ps[5]]
        psW = acc
        cnt = {"S": 0, "PT": 0, "first": [True] * 4}

        def attend(h, chunks, ncols, G):
            qc, hf = h // 2, h % 2
            rows = slice(hf * 64, (hf + 1) * 64)
            first = {s_: True for s_ in range(4)}
            last_idx = {}
            for ci, ch in enumerate(chunks):
                for s_ in range(ch[3], ch[4] + 1):
                    last_idx[s_] = ci
            def qk(ci):
                kfn, masks, vfn, slo, shi = chunks[ci]
                Sp = spr[cnt["S"] % 3]
                cnt["S"] += 1
                P.op("pe", lambda e, kfn=kfn, Sp=Sp, nm0=len(masks): e.matmul(Sp.t[:, 0:QG], kfn(rows)[0], kfn(rows)[1], start=True, stop=(nm0 == 0)),
                     reads=[kselT, kcT, kwT[0], kwT[1]] + R[h], writes=[Sp])
                for mi, (lf, rf) in enumerate(masks):
                    P.op("pe", lambda e, lf=lf, rf=rf, Sp=Sp, mi=mi, nm=len(masks): e.matmul(Sp.t[:, 0:QG], lf(), rf(), start=False, stop=(mi == nm - 1)),
                         reads=[idnb, msk], writes=[Sp])
                return Sp

            def ex(ci, Sp):
                pt = PT[cnt["PT"] % 4]
                cnt["PT"] += 1
                P.op("act", lambda e, pt=pt, Sp=Sp: e.activation(pt.t[:], Sp.t[:, 0:QG], AF.Exp, scale=0.125), reads=[Sp], writes=[pt])
                return pt

            def pv(ci, pt):
                kfn, masks, vfn, slo, shi = chunks[ci]
                for s_ in range(slo, shi + 1):
                    P.op("pe", lambda e, s_=s_, pt=pt, vfn=vfn, st_=first[s_], sp_=(last_idx[s_] == ci): e.matmul(
                        acc[s_].t[:, 0:ncols], pt.t[:, s_ * 128:(s_ + 1) * 128], vfn(), start=st_, stop=sp_),
                        reads=[pt, Vsel, VC, Vw], writes=[acc[s_]])
                    first[s_] = False

            sps = {0: qk(0)}
            if len(chunks) > 1:
                sps[1] = qk(1)
            for ci in range(len(chunks)):
                pt = ex(ci, sps.pop(ci))
                if ci + 2 < len(chunks):
                    sps[ci + 2] = qk(ci + 2)
                pv(ci, pt)

        stg = [sb("stg%d" % br, [128, 4, 4, 65], F32) for br in range(3)]
        stgB = [[Buf("stgB%d_%d" % (br, s_), stg[br].t) for s_ in range(4)] for br in range(3)]
        impS = sb("impS", [128, 4, 4, 256], F32)
        impSB = [Buf("impSB%d" % s_, impS.t) for s_ in range(4)]
        osb4 = sb("osb4", [128, 4, 4, 64], F32)
        otmp = sb("otmp", [128, 4, 4, 64], F32)
        rlb = sb("rlb", [128, 4, 4], F32)
        wgb = sb("wgb", [128, 4, 4], F32)
        impacc4 = sb("impacc4", [128, 4, 256], F32)

        def evac(h, s_, br):
            P.op("dve", lambda e: e.tensor_copy(stg[br].t[:, s_, h, :], acc[s_].t[:, 0:65]), reads=[acc[s_]], writes=[stgB[br][s_]])
            if br == 0:
                P.op("dve", lambda e: e.tensor_copy(impS.t[:, s_, h, :], acc[s_].t[:, 65:321]), reads=[acc[s_]], writes=[impSB[s_]])

        def finish_branch(br, first_branch):
            P.op("dve", lambda e: e.tensor_scalar(rlb.t[:], stg[br].t[:, :, :, 64], 1e-30, None, ALU.max), reads=stgB[br], writes=[rlb])
            P.op("dve", lambda e: e.reciprocal(rlb.t[:], rlb.t[:]), reads=[rlb], writes=[rlb])
            P.op("dve", lambda e: e.tensor_tensor(wgb.t[:], rlb.t[:], gts.t[:, :, ss(br, 4, 3)], ALU.mult), reads=[rlb, gts], writes=[wgb])
            dst = osb4 if first_branch else otmp
            P.op("dve", lambda e: e.tensor_tensor(dst.t[:], stg[br].t[:, :, :, 0:64], wgb.t[:].unsqueeze(3).to_broadcast([128, 4, 4, 64]), ALU.mult),
                 reads=stgB[br] + [wgb], writes=[dst])
            if not first_branch:
                P.op("pool", lambda e: e.tensor_tensor(osb4.t[:], osb4.t[:], otmp.t[:], ALU.add), reads=[otmp, osb4], writes=[osb4])
            if br == 0:
                P.op("dve", lambda e: e.tensor_tensor(impS.t[:], impS.t[:], rlb.t[:].unsqueeze(3).to_broadcast([128, 4, 4, 256]), ALU.mult),
                     reads=impSB + [rlb], writes=impSB)
                P.op("dve", lambda e: e.tensor_tensor(impacc4.t[:], impS.t[:, :, 0, :], impS.t[:, :, 1, :], ALU.add), reads=impSB, writes=[impacc4])
                P.op("dve", lambda e: e.tensor_tensor(impacc4.t[:], impacc4.t[:], impS.t[:, :, 2, :], ALU.add), reads=impSB + [impacc4], writes=[impacc4])
                P.op("dve", lambda e: e.tensor_tensor(impacc4.t[:], impacc4.t[:], impS.t[:, :, 3, :], ALU.add), reads=impSB + [impacc4], writes=[impacc4])

        import os
        NG_RUN = int(os.environ.get("NSA_NG", str(NG)))
        for G in range(NG_RUN):
            t0 = G * QG
            sl = G % 2
            for ti in range(4):
                emit_hmodT_tile(P, x_d, t0 + ti * 128, xts[ti % 2], psXb, idn, modp, hT, ti * 128, 8, ["sp", "act"][ti % 2], width=512)
            emit_rope_tables(P, posB, posI, invp, None, Ct, St, tmpT, pos_d[0:1, t0:t0 + QG], QG)
            for h_ in range(4):
                emit_proj_rope(P, hT, 0, QG, wqh[h_], wqhr[h_], 0, ps[0], ps[1], Ct, St, 0, tmpa, tmpb,
                               lambda h_=h_: R[h_][0].t[:, :], R[h_][0])
            for w_ in range(1, (4 * G + 3) // 32 + 1):
                for h_ in range(4):
                    P.op("pool", lambda e, w_=w_, h_=h_: e.tensor_copy(R[h_][w_].t[0:64, :], R[h_][0].t[0:64, :]), reads=[R[h_][0]], writes=[R[h_][w_]])
            emit_proj_rope(P, hT, 0, QG, wkw, wkwr, 0, ps[0], ps[1], Ct, St, 0, tmpa, tmpb, lambda sl=sl: kwT[sl].t[:, :], kwT[sl])
            for ti in range(4):
                pv = ps[2 + ti % 2]
                for k in range(8):
                    P.op("pe", lambda e, k=k, ti=ti, pv=pv: e.matmul(pv.t[:, 0:64], hT.t[:, k, ti * 128:(ti + 1) * 128], wvw.t[:, k, :],
                                                                     start=(k == 0), stop=(k == 7)), reads=[hT, wvw], writes=[pv])
                P.op("act", lambda e, ti=ti, sl=sl, pv=pv: e.copy(Vw.t[:, sl * 4 + ti, 0:64], pv.t[:, 0:64]), reads=[pv], writes=[Vw])
                pg = ps[4 + ti % 2]
                for k in range(8):
                    P.op("pe", lambda e, k=k, ti=ti, pg=pg: e.matmul(pg.t[:, 0:12], hT.t[:, k, ti * 128:(ti + 1) * 128], wgt.t[:, k, :],
                                                                     start=(k == 0), stop=(k == 7)), reads=[hT, wgt], writes=[pg])
                P.op("act", lambda e, ti=ti, pg=pg: e.activation(gts.t[:, ti, :], pg.t[:, 0:12], AF.Sigmoid), reads=[pg], writes=[gts])

            if NST < 5:
                continue
            cmax = min(7, G // 4)
            for h in range(4):
                chunks = []
                for c in range(cmax + 1):
                    m = G - 4 * c
                    masks = []
                    if m <= 4:
                        masks = [(lambda: idnb.t[:], lambda m=m: msk.t[:, 8 + m, :])]
                    chunks.append((lambda rows, c=c, h=h: (kcT.t[0:64, c * 128:(c + 1) * 128], R[h][0].t[0:64, :]), masks, lambda c=c: VC.t[:, c, 0:321], 0, 3))
                attend(h, chunks, 321, G)
                for s_ in range(4):
                    evac(h, s_, 0)
            finish_branch(0, True)
            if NST < 6:
                continue
            for s_ in range(4):
                T = G * 4 + s_
                ia = Buf("iav", impacc4.t[:, s_, :])
                lo = max(0, 2 * T - 1)
                hi = min(256, 2 * T + 2)
                P.op("dve", lambda e, ia=ia, lo=lo, hi=hi, T=T: e.tensor_tensor(ia.t[:, lo:hi], ia.t[:, lo:hi], frc.t[:, lo - (2 * T - 1):hi - (2 * T - 1)], ALU.add),
                     reads=[impacc4, frc], writes=[impacc4])
                P.op("dve", lambda e, ia=ia: e.tensor_scalar(ia.t[:, 0:1], ia.t[:, 0:1], 1.0e4, None, ALU.add), reads=[impacc4], writes=[impacc4])
                P.op("dve", lambda e, ia=ia: e.max(out=m8a.t[:], in_=ia.t[:]), reads=[impacc4], writes=[m8a])
                P.op("dve", lambda e, ia=ia: e.match_replace(out=impw.t[:], in_to_replace=m8a.t[:], in_values=ia.t[:], imm_value=-1.0e30),
                     reads=[impacc4, m8a], writes=[impw])
                P.op("dve", lambda e: e.max(out=m8b.t[:], in_=impw.t[:]), reads=[impw], writes=[m8b])
                P.op("dve", lambda e, ia=ia: e.tensor_scalar(nbp.t[:, 64:320], ia.t[:], m8b.t[:, 7:8], NEG, ALU.is_lt, ALU.mult), reads=[impacc4, m8b], writes=[nbp])
                nW = (4 * G + 3) // 32 + 1
                for w_ in range(nW):
                    P.op("pe", lambda e, w_=w_, s_=s_: e.transpose(psW[w_].t[:, s_ * 128:(s_ + 1) * 128], nbp.t[:, 64 * w_:64 * w_ + 128], idn.t[:]),
                         reads=[nbp, idn], writes=[psW[w_]])
            nW = (4 * G + 3) // 32 + 1
            for w_ in range(nW):
                P.op("act", lambda e, w_=w_: e.copy(nbTw[w_].t[:], psW[w_].t[:, 0:QG]), reads=[psW[w_]], writes=[nbTw[w_]])
                for h_ in range(4):
                    P.op(["pool", "dve"][h_ % 2], lambda e, w_=w_, h_=h_: e.tensor_copy(R[h_][w_].t[64:128, :], nbTw[w_].t[64:128, :]), reads=[nbTw[w_]], writes=[R[h_][w_]])
            if NST < 7:
                continue
            for h in range(4):
                chunks = []
                for j in range(4 * G + 4):
                    w_ = j // 32
                    masks = []
                    jj = j - 4 * G
                    if jj >= 0:
                        masks.append((lambda: idnb.t[:], lambda jj=jj: msk.t[:, jj, :]))
                    chunks.append((lambda rows, j=j, w_=w_, h=h: (kselT.t[:, j * 128:(j + 1) * 128], R[h][w_].t[:, :]), masks, lambda j=j: Vsel.t[:, j, 0:65],
                                   max(0, jj), 3))
                attend(h, chunks, 65, G)
                for s_ in range(4):
                    evac(h, s_, 1)
            finish_branch(1, False)
            if NST < 8:
                continue
            for h in range(4):
                chunks = []
                for c in range(8):
                    if t0 - 512 + 128 * c < 0:
                        continue
                    if c < 4:
                        kfn = lambda rows, c=c, sl=sl, h=h: (kwT[1 - sl].t[0:64, c * 128:(c + 1) * 128], R[h][0].t[0:64, :])
                        vfn = lambda c=c, sl=sl: Vw.t[:, (1 - sl) * 4 + c, 0:65]
                        mk = 4 + c
                    else:
                        kfn = lambda rows, c=c, sl=sl, h=h: (kwT[sl].t[0:64, (c - 4) * 128:(c - 3) * 128], R[h][0].t[0:64, :])
                        vfn = lambda c=c, sl=sl: Vw.t[:, sl * 4 + (c - 4), 0:65]
                        mk = c - 4
                    masks = [(lambda: idnb.t[:], lambda mk=mk: msk.t[:, mk, :])]
                    chunks.append((kfn, masks, vfn, max(0, c - 4), min(3, c)))
                attend(h, chunks, 65, G)
                for s_ in range(4):
                    evac(h, s_, 2)
            finish_branch(2, False)
            for s_ in range(4):
                r0 = t0 + s_ * 128
                P.dma("sp", lambda e, s_=s_, r0=r0: e.dma_start(out=o_d[r0:r0 + 128, :], in_=osb4.t[:, s_, :, :].rearrange("p h e -> p (h e)")), reads=[osb4], writes=[Ob])
        P.barrier()
        P.wait_all("sp", [Ob])
        P.emit()
    return nc


def nsa_in_maps(x, c, positions, ada_w_s, ada_b_s, w_in, pos_k, w1_k, w2_k, pos_v, w1_v, w2_v, cores=range(8)):
    idn = np.eye(128, dtype=np.float32)
    inv = rope_inv_table()
    msk = nsa_masks()
    gp = np.zeros((64, S_LEN), np.float32)
    kk = np.arange(S_LEN)
    gp[(kk // 64) % 64, kk] = 1.0
    vcc = nsa_vc_const()
    frc = nsa_forced()
    cposT = np.ascontiguousarray(np.concatenate([pos_k.T, pos_v.T], axis=0))
    w1 = np.ascontiguousarray(np.stack([w1_k, w1_v]))
    w2 = np.ascontiguousarray(np.stack([w2_k, w2_v]))
    in_maps = []
    for core in cores:
        b, g = core // 4, core % 4
        wkv = np.stack([w_in[:, D + br * 512 + kv * 256 + g * 64: D + br * 512 + kv * 256 + g * 64 + 64] for br in range(3) for kv in range(2)])
        posc = np.zeros((1, 1024), np.int32)
        posc[0, :NCMP] = positions[b, 31::16][:NCMP]
        in_maps.append({
            "x": x[b], "cT": np.ascontiguousarray(c[b].reshape(8, 128).T),
            "adaw": np.ascontiguousarray(ada_w_s[:, 0:2 * D]), "adabT": np.ascontiguousarray(ada_b_s[0:2 * D].reshape(16, 128).T),
            "wq": np.ascontiguousarray(w_in[:, g * 256:(g + 1) * 256]), "wkv": np.ascontiguousarray(wkv),
            "wgt": np.ascontiguousarray(w_in[:, D + 1536 + 12 * g: D + 1536 + 12 * g + 12]),
            "pos": np.ascontiguousarray(positions[b:b + 1]), "posc": posc, "inv": inv, "idn": idn, "msk": msk,
            "gp": gp, "vcc": vcc, "frc": frc, "w1": w1, "w2": w2, "cposT": cposT,
        })
    return in_maps


def run_nsa(x, c, positions, ada_w_s, ada_b_s, w_in, pos_k, w1_k, w2_k, pos_v, w1_v, w2_v):
    if "nsa" not in _CACHE:
        _CACHE["nsa"] = build_nsa()
    nc = _CACHE["nsa"]
    in_maps = nsa_in_maps(x, c, positions, ada_w_s, ada_b_s, w_in, pos_k, w1_k, w2_k, pos_v, w1_v, w2_v)
    res = run_bass_kernel_spmd(nc, in_maps, core_ids=list(range(8)))
    B = x.shape[0]
    o = np.zeros((B, S_LEN, D), np.float32)
    for core in range(8):
        b, g = core // 4, core % 4
        o[b, :, g * 256:(g + 1) * 256] = res.results[core]["o"]
    return o


def kernel(x, c, positions, ada_w, ada_b, ln_g, ln_b,
           nsa_w_in, nsa_cmp_pos_k, nsa_cmp_w1_k, nsa_cmp_w2_k,
           nsa_cmp_pos_v, nsa_cmp_w1_v, nsa_cmp_w2_v, nsa_w_o,
           dil_w_in, dil_w_o, router_w, router_b, moe_w_gate, moe_w_up, moe_w_down):
    f = lambda a: np.ascontiguousarray(np.asarray(a))
    x, c, positions = f(x), f(c), f(positions)
    ada_w, ada_b, ln_g, ln_b = f(ada_w), f(ada_b), f(ln_g), f(ln_b)
    nsa_w_in, nsa_w_o, dil_w_in, dil_w_o = f(nsa_w_in), f(nsa_w_o), f(dil_w_in), f(dil_w_o)
    router_w, router_b = f(router_w), f(router_b)
    moe_w_gate, moe_w_up, moe_w_down = f(moe_w_gate), f(moe_w_up), f(moe_w_down)
    o0 = run_nsa(x, c, positions, ada_w[0, 0], ada_b[0, 0], nsa_w_in[0],
                 f(nsa_cmp_pos_k)[0], f(nsa_cmp_w1_k)[0], f(nsa_cmp_w2_k)[0],
                 f(nsa_cmp_pos_v)[0], f(nsa_cmp_w1_v)[0], f(nsa_cmp_w2_v)[0])
    x1 = run_ffn(x, o0, c, ada_w[0], ada_b[0], ln_g[0], ln_b[0], nsa_w_o[0], router_w, router_b,
                 moe_w_gate[0], moe_w_up[0], moe_w_down[0])
    o1 = run_dil(x1, c, positions, ada_w[1, 0], ada_b[1, 0], dil_w_in[0])
    out = run_ffn(x1, o1, c, ada_w[1], ada_b[1], ln_g[1], ln_b[1], dil_w_o[0], router_w, router_b,
                  moe_w_gate[1], moe_w_up[1], moe_w_down[1])
    return out.astype(np.float32)
```
